# Optimizing a Trainium2 kernel written in Bass

```python
import math
import jax, jax.numpy as jnp
from jax import lax
import numpy as np

D_MODEL = 1024
BATCH = 8
SEQ = 4096
DEPTH = 1

N_HEADS_A = 8
HEAD_DIM_A = 128
WIDTH_A = N_HEADS_A * HEAD_DIM_A
SHORT_CONV = 5
CHUNK = 64
WIDTH_B = D_MODEL
DW_CONV = 31
N_EXPERTS = 32
TOP_K = 4
D_FF_EXPERT = D_MODEL
SWIGLU_ALPHA = 1.702
SWIGLU_LIMIT = 7.0
EXPERT_BLOCK = 512
DN_ALPHA = (2.0 * DEPTH) ** 0.25
DN_BETA = (8.0 * DEPTH) ** -0.25
LN_EPS = 1e-5
RMS_EPS = 1e-6
L2_EPS = 1e-6
COL_SIZES = (WIDTH_A, WIDTH_A, WIDTH_A, WIDTH_A,
             N_HEADS_A, N_HEADS_A, N_HEADS_A, N_HEADS_A,
             2 * WIDTH_B, 2 * D_MODEL)
N_IN_COLS = sum(COL_SIZES)

kernel_name = 'hybrid_deltanet_conformer_moe_encoder'


def layer_norm(x, g, b):
    xf = x.astype(jnp.float32)
    mu = jnp.mean(xf, axis=-1, keepdims=True)
    var = jnp.mean(jnp.square(xf - mu), axis=-1, keepdims=True)
    y = (xf - mu) * lax.rsqrt(var + LN_EPS) * g.astype(jnp.float32) + b.astype(jnp.float32)
    return y.astype(x.dtype)


def l2norm(t):
    return t * lax.rsqrt(jnp.sum(jnp.square(t), axis=-1, keepdims=True) + L2_EPS)


def depthwise_conv(x, w):
    width, ch = w.shape
    pad = width // 2
    return lax.conv_general_dilated(
        x, w.astype(x.dtype)[:, None, :], window_strides=(1,), padding=[(pad, pad)],
        dimension_numbers=('NWC', 'WIO', 'NWC'), feature_group_count=ch)


def split_cols(u):
    offsets = np.cumsum(COL_SIZES)[:-1].tolist()
    return jnp.split(u, offsets, axis=-1)


def gated_delta_rule(q, k, v, log_a, beta):
    b, h, s, dk = k.shape
    dv = v.shape[-1]
    n = s // CHUNK
    q = q.reshape(b, h, n, CHUNK, dk)
    k = k.reshape(b, h, n, CHUNK, dk)
    v = v.reshape(b, h, n, CHUNK, dv)
    beta = beta.reshape(b, h, n, CHUNK)
    g = jnp.cumsum(log_a.reshape(b, h, n, CHUNK), axis=-1)
    lower = jnp.tril(jnp.ones((CHUNK, CHUNK), dtype=bool))
    strict = jnp.tril(jnp.ones((CHUNK, CHUNK), dtype=bool), -1)
    decay = jnp.exp(jnp.where(lower, g[..., :, None] - g[..., None, :], -jnp.inf))
    kk = jnp.einsum('bhnid,bhnjd->bhnij', k * beta[..., None], k)
    eye = jnp.eye(CHUNK, dtype=k.dtype)
    a_mat = jnp.where(strict, kk * decay, 0.0) + eye
    t_inv = lax.linalg.triangular_solve(
        a_mat, jnp.broadcast_to(eye, a_mat.shape), left_side=True, lower=True, unit_diagonal=True)
    u = jnp.einsum('bhnij,bhnjd->bhnid', t_inv, v * beta[..., None])
    w = jnp.einsum('bhnij,bhnjd->bhnid', t_inv, k * (beta * jnp.exp(g))[..., None])
    qk = jnp.einsum('bhnid,bhnjd->bhnij', q, k) * decay
    q_dec = q * jnp.exp(g)[..., None]
    k_tail = k * jnp.exp(g[..., -1:] - g)[..., None]
    chunk_dec = jnp.exp(g[..., -1])

    def step(state, inp):
        qk_c, qd_c, w_c, u_c, kt_c, cd_c = inp
        v_new = u_c - jnp.einsum('bhik,bhkv->bhiv', w_c, state)
        o_c = (jnp.einsum('bhik,bhkv->bhiv', qd_c, state)
               + jnp.einsum('bhij,bhjv->bhiv', qk_c, v_new))
        state = state * cd_c[..., None, None] + jnp.einsum('bhik,bhiv->bhkv', kt_c, v_new)
        return state, o_c

    xs = (jnp.moveaxis(qk, 2, 0), jnp.moveaxis(q_dec, 2, 0), jnp.moveaxis(w, 2, 0),
          jnp.moveaxis(u, 2, 0), jnp.moveaxis(k_tail, 2, 0), jnp.moveaxis(chunk_dec, 2, 0))
    state0 = jnp.zeros((b, h, dk, dv), jnp.float32)
    _, o = lax.scan(step, state0, xs)
    return jnp.moveaxis(o, 0, 2).reshape(b, h, s, dv)


def deltanet_branch(uq, uk, uv, uz, ub_f, ub_b, ua_f, ua_b, conv_qkv, a_log, dt_bias, norm_g, w_o):
    bsz, s, _ = uq.shape
    qkv = jax.nn.silu(depthwise_conv(jnp.concatenate([uq, uk, uv], axis=-1), conv_qkv))
    q, k, v = jnp.split(qkv.astype(jnp.float32), 3, axis=-1)

    def heads(t):
        return t.reshape(bsz, s, N_HEADS_A, HEAD_DIM_A).transpose(0, 2, 1, 3)

    def per_head(t):
        return t.astype(jnp.float32).transpose(0, 2, 1)

    q = l2norm(heads(q)) * (HEAD_DIM_A ** -0.5)
    k = l2norm(heads(k))
    v = heads(v)
    a_log = a_log.astype(jnp.float32)
    dt_bias = dt_bias.astype(jnp.float32)
    beta_f = jax.nn.sigmoid(per_head(ub_f))
    beta_b = jax.nn.sigmoid(per_head(ub_b))
    la_f = -jnp.exp(a_log[0])[:, None] * jax.nn.softplus(per_head(ua_f) + dt_bias[0][:, None])
    la_b = -jnp.exp(a_log[1])[:, None] * jax.nn.softplus(per_head(ua_b) + dt_bias[1][:, None])
    o_f = gated_delta_rule(q, k, v, la_f, beta_f)
    o_b = jnp.flip(gated_delta_rule(jnp.flip(q, 2), jnp.flip(k, 2), jnp.flip(v, 2),
                                    jnp.flip(la_b, 2), jnp.flip(beta_b, 2)), 2)
    o = (o_f + o_b).transpose(0, 2, 1, 3)
    z = uz.astype(jnp.float32).reshape(bsz, s, N_HEADS_A, HEAD_DIM_A)
    o = (o * lax.rsqrt(jnp.mean(jnp.square(o), axis=-1, keepdims=True) + RMS_EPS)
         * norm_g.astype(jnp.float32) * jax.nn.silu(z))
    return o.reshape(bsz, s, WIDTH_A).astype(uq.dtype) @ w_o


def conformer_branch(uglu, b_glu, conv_dw, b_dw, ln_g, ln_b, w_o, b_o):
    hg = uglu + b_glu
    y = hg[..., :WIDTH_B] * jax.nn.sigmoid(hg[..., WIDTH_B:])
    y = depthwise_conv(y, conv_dw) + b_dw
    y = jax.nn.silu(layer_norm(y, ln_g, ln_b))
    return y @ w_o + b_o


def clamped_swiglu(hgu):
    glu = jnp.minimum(hgu[..., :D_FF_EXPERT], SWIGLU_LIMIT)
    lin = jnp.clip(hgu[..., D_FF_EXPERT:], -SWIGLU_LIMIT, SWIGLU_LIMIT)
    return glu * jax.nn.sigmoid(SWIGLU_ALPHA * glu) * (lin + 1.0)


def moe_ffn(h, w_router, b_router, w_gu, b_gu, w_down, b_down):
    bsz, s, d = h.shape
    x = h.reshape(-1, d)
    t = x.shape[0]
    logits = (x @ w_router + b_router).astype(jnp.float32)
    top_val, top_idx = lax.top_k(logits, TOP_K)
    gate = jax.nn.softmax(top_val, axis=-1)
    n_assign = t * TOP_K
    e_flat = top_idx.reshape(-1).astype(jnp.int32)
    tok_flat = jnp.arange(n_assign, dtype=jnp.int32) // TOP_K
    gate_flat = gate.reshape(-1)
    order = jnp.argsort(e_flat)
    e_sorted = e_flat[order]
    counts = jnp.zeros((N_EXPERTS,), jnp.int32).at[e_flat].add(1)
    start = jnp.cumsum(counts) - counts
    padded = (counts + EXPERT_BLOCK - 1) // EXPERT_BLOCK * EXPERT_BLOCK
    pad_end = jnp.cumsum(padded)
    pad_start = pad_end - padded
    dest = pad_start[e_sorted] + jnp.arange(n_assign, dtype=jnp.int32) - start[e_sorted]
    n_blocks = -(-(n_assign + N_EXPERTS * (EXPERT_BLOCK - 1)) // EXPERT_BLOCK)
    n_slots = n_blocks * EXPERT_BLOCK
    slot_tok = jnp.zeros((n_slots,), jnp.int32).at[dest].set(tok_flat[order])
    slot_gate = jnp.zeros((n_slots,), jnp.float32).at[dest].set(gate_flat[order])
    block_start = jnp.arange(n_blocks, dtype=jnp.int32) * EXPERT_BLOCK
    block_exp = jnp.minimum(jnp.searchsorted(pad_end, block_start, side='right'),
                            N_EXPERTS - 1).astype(jnp.int32)

    def expert_block(acc, inp):
        tok, gt, e = inp
        xb = x[tok]
        hgu = xb @ w_gu[e] + b_gu[e]
        y = clamped_swiglu(hgu) @ w_down[e] + b_down[e]
        return acc.at[tok].add((y * gt[:, None]).astype(jnp.float32)), None

    acc0 = jnp.zeros((t, d), jnp.float32)
    out, _ = lax.scan(expert_block, acc0,
                      (slot_tok.reshape(n_blocks, EXPERT_BLOCK),
                       slot_gate.reshape(n_blocks, EXPERT_BLOCK), block_exp))
    return out.astype(h.dtype).reshape(bsz, s, d)


def setup_inputs(seed: int = 0) -> dict:
    key = jax.random.key(seed)
    ks = jax.random.split(key, 32)
    L = DEPTH

    def nrm(k, shape, scale):
        return jax.random.normal(k, shape, jnp.float32) * scale

    x = jax.random.normal(ks[0], (BATCH, SEQ, D_MODEL), jnp.float32)
    emb_ln_g = 1.0 + nrm(ks[1], (D_MODEL,), 0.02)
    emb_ln_b = nrm(ks[2], (D_MODEL,), 0.02)
    w_in = nrm(ks[3], (L, D_MODEL, N_IN_COLS), D_MODEL ** -0.5)
    conv_qkv = nrm(ks[4], (L, SHORT_CONV, 3 * WIDTH_A), SHORT_CONV ** -0.5)
    a_log = jnp.log(jax.random.uniform(ks[5], (L, 2, N_HEADS_A), jnp.float32, 1.0, 16.0))
    dt = jnp.exp(jax.random.uniform(ks[6], (L, 2, N_HEADS_A), jnp.float32,
                                    math.log(1e-3), math.log(1e-1)))
    dt_bias = dt + jnp.log(-jnp.expm1(-dt))
    dn_norm_g = 1.0 + nrm(ks[7], (L, HEAD_DIM_A), 0.02)
    w_a_o = nrm(ks[8], (L, WIDTH_A, D_MODEL), WIDTH_A ** -0.5 * DN_BETA)
    b_glu = nrm(ks[9], (L, 2 * WIDTH_B), 0.01)
    conv_dw = nrm(ks[10], (L, DW_CONV, WIDTH_B), DW_CONV ** -0.5)
    b_dw = nrm(ks[11], (L, WIDTH_B), 0.01)
    conv_ln_g = 1.0 + nrm(ks[12], (L, WIDTH_B), 0.02)
    conv_ln_b = nrm(ks[13], (L, WIDTH_B), 0.02)
    w_b_o = nrm(ks[14], (L, WIDTH_B, D_MODEL), WIDTH_B ** -0.5 * DN_BETA)
    b_b_o = nrm(ks[15], (L, D_MODEL), 0.01)
    b_gate = nrm(ks[16], (L, 2 * D_MODEL), 0.1)
    w_out = nrm(ks[17], (L, D_MODEL, D_MODEL), D_MODEL ** -0.5 * DN_BETA)
    ln1_g = 1.0 + nrm(ks[18], (L, D_MODEL), 0.02)
    ln1_b = nrm(ks[19], (L, D_MODEL), 0.02)
    w_router = nrm(ks[20], (L, D_MODEL, N_EXPERTS), D_MODEL ** -0.5)
    b_router = nrm(ks[21], (L, N_EXPERTS), 0.01)
    w_gu = nrm(ks[22], (L, N_EXPERTS, D_MODEL, 2 * D_FF_EXPERT), D_MODEL ** -0.5)
    b_gu = nrm(ks[23], (L, N_EXPERTS, 2 * D_FF_EXPERT), 0.01)
    w_down = nrm(ks[24], (L, N_EXPERTS, D_FF_EXPERT, D_MODEL), D_FF_EXPERT ** -0.5 * DN_BETA)
    b_down = nrm(ks[25], (L, N_EXPERTS, D_MODEL), 0.01)
    ln2_g = 1.0 + nrm(ks[26], (L, D_MODEL), 0.02)
    ln2_b = nrm(ks[27], (L, D_MODEL), 0.02)
    return {'x': x, 'emb_ln_g': emb_ln_g, 'emb_ln_b': emb_ln_b, 'w_in': w_in,
            'conv_qkv': conv_qkv, 'a_log': a_log, 'dt_bias': dt_bias, 'dn_norm_g': dn_norm_g,
            'w_a_o': w_a_o, 'b_glu': b_glu, 'conv_dw': conv_dw, 'b_dw': b_dw,
            'conv_ln_g': conv_ln_g, 'conv_ln_b': conv_ln_b, 'w_b_o': w_b_o, 'b_b_o': b_b_o,
            'b_gate': b_gate, 'w_out': w_out, 'ln1_g': ln1_g, 'ln1_b': ln1_b,
            'w_router': w_router, 'b_router': b_router, 'w_gu': w_gu, 'b_gu': b_gu,
            'w_down': w_down, 'b_down': b_down, 'ln2_g': ln2_g, 'ln2_b': ln2_b}


def reference(x, emb_ln_g, emb_ln_b, w_in, conv_qkv, a_log, dt_bias, dn_norm_g, w_a_o,
              b_glu, conv_dw, b_dw, conv_ln_g, conv_ln_b, w_b_o, b_b_o, b_gate, w_out,
              ln1_g, ln1_b, w_router, b_router, w_gu, b_gu, w_down, b_down, ln2_g, ln2_b):
    h = layer_norm(x, emb_ln_g, emb_ln_b)
    for l in range(DEPTH):
        u = h @ w_in[l]
        uq, uk, uv, uz, ub_f, ub_b, ua_f, ua_b, uglu, ugate = split_cols(u)
        y_a = deltanet_branch(uq, uk, uv, uz, ub_f, ub_b, ua_f, ua_b,
                              conv_qkv[l], a_log[l], dt_bias[l], dn_norm_g[l], w_a_o[l])
        y_b = conformer_branch(uglu, b_glu[l], conv_dw[l], b_dw[l],
                               conv_ln_g[l], conv_ln_b[l], w_b_o[l], b_b_o[l])
        g_a, g_b = jnp.split(jax.nn.sigmoid(ugate + b_gate[l]), 2, axis=-1)
        mix = (g_a * y_a + g_b * y_b) @ w_out[l]
        h = layer_norm(DN_ALPHA * h + mix, ln1_g[l], ln1_b[l])
        f = moe_ffn(h, w_router[l], b_router[l], w_gu[l], b_gu[l], w_down[l], b_down[l])
        h = layer_norm(DN_ALPHA * h + f, ln2_g[l], ln2_b[l])
    return h
```

```python
import os
import numpy as np
import concourse.bass as bass
import concourse.mybir as mybir
from concourse.bass_utils import run_bass_kernel_spmd

F32 = mybir.dt.float32
BF16 = mybir.dt.bfloat16
AF = mybir.ActivationFunctionType
ALU = mybir.AluOpType
AX = mybir.AxisListType

S = 4096
D = 1024
NCOL = 8224
NCH = 64
DN_ALPHA = 2.0 ** 0.25
LN_EPS = 1e-5
RMS_EPS = 1e-6
L2_EPS = 1e-6
NEG_BIG = -1.0e30
COL_Q, COL_K, COL_V, COL_Z, COL_SM, COL_GLU, COL_GATE = 0, 1024, 2048, 3072, 4096, 4128, 6176


class Prog:
    def __init__(self, nc, n_dma_sems=12, self_sync=True):
        self.nc = nc
        self.eng = {'pe': nc.tensor, 'act': nc.scalar, 'dve': nc.vector, 'pool': nc.gpsimd, 'sp': nc.sync}
        self.streams = {e: [] for e in self.eng}
        self.cnt = {e: 0 for e in self.eng}
        self.sem = {e: nc.alloc_semaphore(name=f"s_{e}") for e in self.eng}
        self.seen = {e: {} for e in self.eng}
        self.lastw = {}
        self.reads = {}
        self.semobj = {('e', e): self.sem[e] for e in self.eng}
        self.dma_sems = {}
        self.n_dma_sems = n_dma_sems
        self.self_sync = self_sync
        self.n_inst = 0

    def _dma_sem(self, q):
        if q not in self.dma_sems:
            lst = []
            for i in range(self.n_dma_sems):
                s = self.nc.alloc_semaphore(name=f"d_{q}_{i}")
                key = ('d', q, i)
                self.semobj[key] = s
                lst.append([key, 0])
            self.dma_sems[q] = [lst, 0]
        lst, idx = self.dma_sems[q]
        self.dma_sems[q][1] = (idx + 1) % len(lst)
        return lst[idx]

    def _deps(self, e, reads, writes):
        deps = {}

        def add(k, v):
            if deps.get(k, 0) < v:
                deps[k] = v
        for r in reads:
            ev = self.lastw.get(r)
            if ev is not None:
                add(*ev)
        for w in writes:
            ev = self.lastw.get(w)
            if ev is not None:
                add(*ev)
            for k, v in self.reads.get(w, {}).items():
                add(k, v)
        waits = []
        for k, v in deps.items():
            if k == ('e', e):
                if not self.self_sync or e in ('pe', 'sp'):
                    continue
            if self.seen[e].get(k, 0) >= v:
                continue
            self.seen[e][k] = v
            waits.append((self.semobj[k], v))
        return waits

    def _commit(self, ev, reads, writes):
        k, v = ev
        for w in writes:
            self.lastw[w] = ev
            self.reads[w] = {}
        for r in reads:
            d = self.reads.setdefault(r, {})
            if d.get(k, 0) < v:
                d[k] = v

    @staticmethod
    def _names(aps):
        out = []
        for a in aps:
            if a is None or isinstance(a, (int, float)):
                continue
            if isinstance(a, (str, tuple)):
                out.append(a)
            else:
                out.append(a.name)
        return out

    def op(self, e, fn, reads, writes):
        reads = self._names(reads)
        writes = self._names(writes)
        pr = [n for n in reads if isinstance(n, str) and n.startswith('bank')]
        if pr:
            reads = [n for n in reads if n not in pr]
            writes = writes + [n for n in pr if n not in writes]
        waits = self._deps(e, reads, writes)
        self.cnt[e] += 1
        seq = self.cnt[e]
        sem = self.sem[e]

        def emit(engobj, waits=waits, fn=fn, sem=sem):
            for s, v in waits:
                engobj.wait_ge(s, v)
            fn(engobj).then_inc(sem, 1)
        self.streams[e].append(emit)
        self._commit((('e', e), seq), reads, writes)
        self.n_inst += 1

    def dma(self, q, out, in_, r=None, w=None, **kw):
        reads = self._names(r if r is not None else [in_])
        writes = self._names(w if w is not None else [out])
        slot = self._dma_sem(q)
        key, prev = slot
        waits = self._deps(q, reads, writes)
        if prev > 0 and self.seen[q].get(key, 0) < prev:
            self.seen[q][key] = prev
            waits.append((self.semobj[key], prev))
        slot[1] = prev + 16
        tgt = slot[1]
        s = self.semobj[key]

        def emit(engobj, waits=waits, s=s, out=out, in_=in_, kw=kw):
            for ss, v in waits:
                engobj.wait_ge(ss, v)
            engobj.dma_start(out=out, in_=in_, **kw).then_inc(s, 16)
        self.streams[q].append(emit)
        self._commit((key, tgt), reads, writes)
        self.n_inst += 1

    def idma(self, out, in_, out_off=None, in_off=None, r=None, w=None):
        q = 'pool'
        reads = self._names(r if r is not None else [in_, out_off, in_off])
        writes = self._names(w if w is not None else [out])
        slot = self._dma_sem(q)
        key, prev = slot
        waits = self._deps(q, reads, writes)
        if prev > 0 and self.seen[q].get(key, 0) < prev:
            self.seen[q][key] = prev
            waits.append((self.semobj[key], prev))
        slot[1] = prev + 16
        tgt = slot[1]
        s = self.semobj[key]

        def emit(engobj, waits=waits, s=s):
            for ss, v in waits:
                engobj.wait_ge(ss, v)
            oo = bass.IndirectOffsetOnAxis(ap=out_off, axis=0) if out_off is not None else None
            io = bass.IndirectOffsetOnAxis(ap=in_off, axis=0) if in_off is not None else None
            engobj.indirect_dma_start(out=out, out_offset=oo, in_=in_, in_offset=io).then_inc(s, 16)
        self.streams[q].append(emit)
        self._commit((key, tgt), reads, writes)
        self.n_inst += 1

    def barrier(self):
        cur = {}
        for e in self.eng:
            if self.cnt[e] > 0:
                cur[('e', e)] = self.cnt[e]
        for q, (lst, _) in self.dma_sems.items():
            for key, val in lst:
                if val > 0:
                    cur[key] = val
        for e in self.eng:
            waits = []
            for k, v in cur.items():
                if k == ('e', e):
                    continue
                if self.seen[e].get(k, 0) >= v:
                    continue
                self.seen[e][k] = v
                waits.append((self.semobj[k], v))

            def emit(engobj, waits=waits):
                for s, v in waits:
                    engobj.wait_ge(s, v)
            self.streams[e].append(emit)

    def finish(self):
        self.barrier()
        nc = self.nc
        with nc.Block() as block:
            @block.tensor
            def _(e):
                for f in self.streams['pe']:
                    f(e)

            @block.scalar
            def _(e):
                for f in self.streams['act']:
                    f(e)

            @block.vector
            def _(e):
                for f in self.streams['dve']:
                    f(e)

            @block.gpsimd
            def _(e):
                for f in self.streams['pool']:
                    f(e)

            @block.sync
            def _(e):
                for f in self.streams['sp']:
                    f(e)

    def mm(self, out, lhsT, rhs, start=True, stop=True, r=None, w=None):
        self.op('pe', lambda e: e.matmul(out, lhsT, rhs, start=start, stop=stop),
                r if r is not None else [lhsT, rhs], w if w is not None else [out])

    def tr(self, out, in_, ident, r=None, w=None):
        self.op('pe', lambda e: e.transpose(out=out, in_=in_, identity=ident),
                r if r is not None else [in_, ident], w if w is not None else [out])

    def act(self, out, in_, func, bias=None, scale=None, accum=None, r=None, w=None):
        kw = {}
        if bias is not None:
            kw['bias'] = bias
        if scale is not None:
            kw['scale'] = scale
        if accum is not None:
            kw['accum_out'] = accum
        self.op('act', lambda e: e.activation(out=out, in_=in_, func=func, **kw),
                r if r is not None else [in_, bias, scale], w if w is not None else [out, accum])

    def tt(self, eng, out, in0, in1, op, r=None, w=None):
        self.op(eng, lambda e: e.tensor_tensor(out=out, in0=in0, in1=in1, op=op),
                r if r is not None else [in0, in1], w if w is not None else [out])

    def ts(self, eng, out, in0, s1, s2, op0, op1=None, r=None, w=None):
        if op1 is None:
            fn = lambda e: e.tensor_scalar(out=out, in0=in0, scalar1=s1, scalar2=None, op0=op0)
        else:
            fn = lambda e: e.tensor_scalar(out=out, in0=in0, scalar1=s1, scalar2=s2, op0=op0, op1=op1)
        self.op(eng, fn, r if r is not None else [in0, s1, s2], w if w is not None else [out])

    def stt(self, out, in0, scalar, in1, op0, op1, r=None, w=None):
        self.op('dve', lambda e: e.scalar_tensor_tensor(out=out, in0=in0, scalar=scalar, in1=in1, op0=op0, op1=op1),
                r if r is not None else [in0, scalar, in1], w if w is not None else [out])

    def copy(self, eng, out, in_, r=None, w=None):
        if eng == 'act':
            fn = lambda e: e.copy(out=out, in_=in_)
        else:
            fn = lambda e: e.tensor_copy(out=out, in_=in_)
        self.op(eng, fn, r if r is not None else [in_], w if w is not None else [out])

    def memset(self, eng, ap, val):
        self.op(eng, lambda e: e.memset(ap, val), [], [ap])


def bc(ap, shape, axis):
    return ap.unsqueeze(axis).to_broadcast(shape)


class Arena:
    def __init__(self, nc, P, base=16512, top=229344):
        self.nc = nc; self.P = P; self.ptr = base; self.top = top; self.n = 0; self.peak = base

    def alloc(self, name, shape, dt=F32):
        size = 2 if dt == BF16 else 4
        n = 1
        for d in shape[1:]:
            n *= d
        nbytes = (n * size + 63) // 64 * 64
        self.n += 1
        t = self.nc.alloc_sbuf_tensor_at(f"{name}_{self.n}", list(shape), dt, offset=self.ptr)
        self.ptr += nbytes
        self.peak = max(self.peak, self.ptr)
        assert self.ptr <= self.top, f"SBUF arena overflow at {name}: {self.ptr} > {self.top}"
        return t

    def mark(self):
        return self.ptr

    def release(self, m):
        self.P.barrier()
        self.ptr = m


def build_program(stage=99, debug=False):
    nc = bass.Bass("TRN2", target_bir_lowering=False)
    P = Prog(nc)
    A = Arena(nc, P)
    dbg = {}

    def dram_in(name, shape):
        return nc.dram_tensor(name, shape, F32, kind="ExternalInput").ap()

    def scratch(name, shape, dt, force_internal=False):
        kind = "ExternalOutput" if (debug and not force_internal) else "Internal"
        return nc.dram_tensor(name, shape, dt, kind=kind).ap()

    def dbg_out(name, src_ap, shape, dt, r=None):
        if not debug:
            return
        t = nc.dram_tensor(name, shape, dt, kind="ExternalOutput").ap()
        P.dma('sp', t, src_ap, r=r)

    sb = A.alloc

    x = dram_in("x", [S, D])
    emb_ln_g = dram_in("emb_ln_g", [D]); emb_ln_b = dram_in("emb_ln_b", [D])
    w_in = dram_in("w_in", [D, NCOL])
    conv_qkv = dram_in("conv_qkv", [5, 3072])
    a_log = dram_in("a_log", [16]); dt_bias = dram_in("dt_bias", [16])
    dn_norm_g = dram_in("dn_norm_g", [128])
    w_a_o = dram_in("w_a_o", [D, D])
    b_glu = dram_in("b_glu", [2048]); conv_dw = dram_in("conv_dw", [31, D]); b_dw = dram_in("b_dw", [D])
    conv_ln_g = dram_in("conv_ln_g", [D]); conv_ln_b = dram_in("conv_ln_b", [D])
    w_b_o = dram_in("w_b_o", [D, D]); b_b_o = dram_in("b_b_o", [D]); b_gate = dram_in("b_gate", [2048])
    w_out = dram_in("w_out", [D, D])
    ln1_g = dram_in("ln1_g", [D]); ln1_b = dram_in("ln1_b", [D])
    w_router = dram_in("w_router", [D, 32]); b_router = dram_in("b_router", [32])
    w_gu = dram_in("w_gu", [32, D, 2048]); b_gu = dram_in("b_gu", [32, 2048])
    w_down = dram_in("w_down", [32, D, D]); b_down = dram_in("b_down", [32, D])
    ln2_g = dram_in("ln2_g", [D]); ln2_b = dram_in("ln2_b", [D])
    out = nc.dram_tensor("out", [S, D], F32, kind="ExternalOutput").ap()

    h0_d = scratch("h0_d", [S, D], F32)
    hT_d = scratch("hT_d", [D, S], BF16)
    ogT_d = scratch("ogT_d", [D, S], BF16)
    ybT_d = scratch("ybT_d", [D, S], BF16)
    gate_d = scratch("gate_d", [2048, S], BF16)
    h1_d = scratch("h1_d", [S, D], F32)
    st_d = scratch("st_d", [2, S], F32)
    h1b_d = scratch("h1b_d", [S, D], BF16)
    xs_d = scratch("xs_d", [32768, D], BF16, force_internal=True)
    ys_d = scratch("ys_d", [32768, D], F32, force_internal=True)

    hT_dv = hT_d.rearrange("(kc p) t -> p kc t", p=128)
    w_in_v = w_in.rearrange("(kc p) c -> p kc c", p=128)

    identf = sb("identf", [128, 128]); identb = sb("identb", [128, 128], BF16)
    onesf = sb("onesf", [128, 128]); onesb = sb("onesb", [128, 128], BF16)
    zerof = sb("zerof", [128, 512])
    P.memset('pool', onesf[:], 1.0)
    P.memset('pool', zerof[:], 0.0)
    P.memset('pool', identf[:], 1.0)
    P.op('pool', lambda e: e.affine_select(out=identf[:], in_=identf[:], pattern=[[-1, 128]], compare_op=ALU.is_equal,
                                           fill=0.0, base=0, channel_multiplier=1), [identf], [identf])
    P.copy('dve', identb[:], identf[:])
    P.copy('dve', onesb[:], onesf[:])
    eps_ln = sb("eps_ln", [128, 1])
    P.memset('pool', eps_ln[:], LN_EPS)
    bank = [nc.alloc_psum_tensor(f"bank{i}", [128, 512], F32) for i in range(8)]

    def bank_bf(i):
        return bank[i][:].bitcast(BF16)

    ln_st = sb("ln_st", [128, 12]); ln_mv = sb("ln_mv", [128, 2]); ln_rs = sb("ln_rs", [128, 1])
    lnp = {}

    def layer_norm_rows(t, o, np_=128):
        P.op('dve', lambda e: e.bn_stats(out=ln_st[:np_, 0:6], in_=t[:np_, 0:512]), [t], [ln_st])
        P.op('dve', lambda e: e.bn_stats(out=ln_st[:np_, 6:12], in_=t[:np_, 512:1024]), [t], [ln_st])
        P.op('dve', lambda e: e.bn_aggr(out=ln_mv[:np_, :], in_=ln_st[:np_, :]), [ln_st], [ln_mv])
        P.act(ln_rs[:np_, :], ln_mv[:np_, 1:2], AF.Sqrt, bias=eps_ln[:np_, 0:1])
        P.op('dve', lambda e: e.reciprocal(out=ln_rs[:np_, :], in_=ln_rs[:np_, :]), [ln_rs], [ln_rs])
        P.ts('dve', t[:np_, :], t[:np_, :], ln_mv[:np_, 0:1], ln_rs[:np_, 0:1], ALU.subtract, ALU.mult)
        P.tt('pool', t[:np_, :], t[:np_, :], lnp['g'][:np_, :], ALU.mult)
        P.tt('dve', o[:np_, :], t[:np_, :], lnp['b'][:np_, :], ALU.add)

    vp = sb("vp", [128, 64])
    base_mark = A.mark()
    lnp['g'] = sb("gB0", [128, D]); lnp['b'] = sb("bB0", [128, D])
    P.dma('sp', lnp['g'][:], emb_ln_g.partition_broadcast(128))
    P.dma('sp', lnp['b'][:], emb_ln_b.partition_broadcast(128))
    xt = [sb(f"xt{i}", [128, D]) for i in range(2)]
    ht = [sb(f"ht{i}", [128, D]) for i in range(2)]
    hTs = [sb(f"hTs{i}", [128, 8, 512], BF16) for i in range(2)]
    for i in range(32):
        xb = xt[i % 2]; hb = ht[i % 2]; hs = hTs[(i // 4) % 2]
        P.dma('sp', xb[:], x[i * 128:(i + 1) * 128, :])
        layer_norm_rows(xb, hb)
        P.dma('sp', h0_d[i * 128:(i + 1) * 128, :], hb[:], w=[('h0_d', i)])
        for half in range(2):
            pb = bank[half]
            for q4 in range(4):
                kc = half * 4 + q4
                P.tr(pb[:, q4 * 128:(q4 + 1) * 128], hb[:, kc * 128:(kc + 1) * 128], identf[:])
            P.copy('act', hs[:, half * 4:(half + 1) * 4, (i % 4) * 128:(i % 4 + 1) * 128],
                   pb[:].rearrange("p (k t) -> p k t", t=128))
        if i % 4 == 3:
            tt_ = i // 4
            P.dma('sp', hT_dv[:, :, tt_ * 512:(tt_ + 1) * 512], hs[:], w=[('hT_d', tt_)])
    A.release(base_mark)
    if stage <= 1:
        P.finish()
        return nc, dbg

    tri = {}; ntri = {}; negT = {}; nstrT = {}
    for d_, (cm, st_) in (('f', (-1, 1)), ('b', (1, -1))):
        t_ = sb(f"tri_{d_}", [64, 64]); nt_ = sb(f"ntri_{d_}", [64, 64])
        P.op('pool', lambda e, t_=t_, cm=cm, st_=st_: e.affine_select(out=t_[:], in_=onesf[0:64, 0:64], pattern=[[st_, 64]],
             compare_op=ALU.is_ge, fill=0.0, base=0, channel_multiplier=cm), [onesf], [t_])
        P.ts('dve', nt_[:], t_[:], -1.0, None, ALU.mult)
        tri[d_] = t_; ntri[d_] = nt_
        ng_ = sb(f"negT_{d_}", [64, 8, 64])
        P.op('pool', lambda e, ng_=ng_, cm=cm, st_=st_: e.affine_select(out=ng_[:], in_=zerof[0:64, 0:512].rearrange("p (c i) -> p c i", i=64),
             pattern=[[0, 8], [st_, 64]], compare_op=ALU.is_ge, fill=NEG_BIG, base=0, channel_multiplier=cm), [zerof], [ng_])
        negT[d_] = ng_
        ns_ = sb(f"nstrT_{d_}", [64, 64])
        P.op('pool', lambda e, ns_=ns_, cm=cm, st_=st_: e.affine_select(out=ns_[:], in_=onesf[0:64, 0:64], pattern=[[st_, 64]],
             compare_op=ALU.is_gt, fill=0.0, base=0, channel_multiplier=cm), [onesf], [ns_])
        P.ts('dve', ns_[:], ns_[:], -1.0, None, ALU.mult)
        nstrT[d_] = ns_
    BETA = {d_: sb(f"BETA_{d_}", [64, 8, 64]) for d_ in 'fb'}
    LA = {d_: sb(f"LA_{d_}", [64, 8, 64]) for d_ in 'fb'}
    hTt = [sb(f"hTt{i}", [128, 8, 512], BF16) for i in range(2)]
    hcnt = [0]

    def load_hT(tt_):
        hb = hTt[hcnt[0] % 2]; hcnt[0] += 1
        P.dma('sp', hb[:], hT_dv[:, :, tt_ * 512:(tt_ + 1) * 512], r=[('hT_d', tt_)])
        return hb

    s2_mark = A.mark()
    SMraw = sb("SMraw", [64, 64, 32])
    Wsm = sb("Wsm", [128, 8, 32], BF16)
    alB = sb("alB", [64, 16]); dtB = sb("dtB", [64, 16]); nea = sb("nea", [64, 16])
    X_ = sb("spX", [64, 8, 64]); A_ = sb("spA", [64, 8, 64])
    P.dma('pool', Wsm[:], w_in_v[:, :, COL_SM:COL_SM + 32])
    P.dma('sp', alB[:], a_log.partition_broadcast(64))
    P.dma('sp', dtB[:], dt_bias.partition_broadcast(64))
    P.act(nea[:], alB[:], AF.Exp)
    P.ts('dve', nea[:], nea[:], -1.0, None, ALU.mult)
    for tt_ in range(8):
        hb = load_hT(tt_)
        pb = bank[2 + tt_ % 2]
        for cc in range(8):
            for kc in range(8):
                P.mm(pb[0:64, cc * 32:(cc + 1) * 32], hb[:, kc, cc * 64:(cc + 1) * 64], Wsm[:, kc, :], start=(kc == 0), stop=(kc == 7))
        P.copy('dve', SMraw[:, tt_ * 8:(tt_ + 1) * 8, :], pb[0:64, 0:256].rearrange("p (c k) -> p c k", k=32))
    for di, d_ in enumerate('fb'):
        ub = SMraw[:, :, di * 8:(di + 1) * 8].rearrange("p c h -> p h c")
        ua = SMraw[:, :, 16 + di * 8:24 + di * 8].rearrange("p c h -> p h c")
        P.act(BETA[d_][:], ub, AF.Sigmoid)
        P.tt('dve', X_[:], ua, bc(dtB[:, di * 8:(di + 1) * 8], [64, 8, 64], 2), ALU.add)
        P.stt(A_[:], X_[:], -1.0, X_[:], ALU.mult, ALU.max)
        P.act(A_[:], A_[:], AF.Exp, scale=-1.0)
        P.act(A_[:], A_[:], AF.Ln, bias=1.0)
        P.stt(A_[:], X_[:], 0.0, A_[:], ALU.max, ALU.add)
        P.tt('dve', LA[d_][:], A_[:], bc(nea[:, di * 8:(di + 1) * 8], [64, 8, 64], 2), ALU.mult)
    if debug:
        for d_ in 'fb':
            dbg_out(f"dbg_beta_{d_}", BETA[d_][:], [64, 8, 64], F32)
            dbg_out(f"dbg_la_{d_}", LA[d_][:], [64, 8, 64], F32)
    A.release(s2_mark)
    if stage <= 2:
        P.finish()
        return nc, dbg


    nheads = int(os.environ.get("MK_NHEADS", "8")) if debug else 8
    head_mark = A.mark()
    cst = sb("cst", [120, 128]); cwq = sb("cwq", [128, 120])
    P.dma('sp', cst[:], conv_qkv.rearrange("t (c p) -> (t c) p", p=128))
    P.tr(bank[0][:, 0:120], cst[:, :], identf[0:120, 0:120])
    P.copy('dve', cwq[:], bank[0][:, 0:120])
    eps_l2 = sb("eps_l2", [128, 1]); P.memset('pool', eps_l2[:], L2_EPS)
    eps_rms = sb("eps_rms", [128, 1]); P.memset('pool', eps_rms[:], RMS_EPS)
    ngB = sb("ngB", [64, 128]); P.dma('sp', ngB[:], dn_norm_g.partition_broadcast(64))
    Wg = [sb(f"Wg{i}", [128, 8, 128], BF16) for i in range(2)]
    U = sb("U", [128, 4100], BF16)
    P.memset('pool', U[:, 0:2], 0.0); P.memset('pool', U[:, 4098:4100], 0.0)
    C = sb("C", [128, 4096], BF16)
    Dg = sb("Dg", [128, 5, 128], BF16)
    sqb = sb("sqb", [128, 512], BF16); lnb = sb("lnb", [128, 512])
    qT = sb("qT", [128, 4096], BF16); kT = sb("kT", [128, 4096], BF16)
    Ktok = sb("Ktok", [64, 64, 128], BF16); Vtok = sb("Vtok", [64, 64, 128], BF16)
    O = sb("O", [64, 64, 128])
    qkT = {d_: sb(f"qkT_{d_}", [64, 64, 64], BF16) for d_ in 'fb'}
    TT = {d_: sb(f"TT_{d_}", [64, 64, 64], BF16) for d_ in 'fb'}
    Gd = {d_: sb(f"Gd_{d_}", [64, 64]) for d_ in 'fb'}
    EG = {d_: sb(f"EG_{d_}", [64, 64]) for d_ in 'fb'}
    NEG = {d_: sb(f"NEG_{d_}", [64, 64]) for d_ in 'fb'}
    TAIL = {d_: sb(f"TAIL_{d_}", [64, 64]) for d_ in 'fb'}
    CD = {d_: sb(f"CD_{d_}", [128, 64]) for d_ in 'fb'}
    LAm = sb("LAm", [64, 8, 64]); LAb = sb("LAb", [64, 8, 64])
    decT = {d_: sb(f"decT_{d_}", [64, 8, 64]) for d_ in 'fb'}; tY = {d_: sb(f"tY_{d_}", [64, 8, 64]) for d_ in 'fb'}
    Yb = {d_: [sb(f"Yb{d_}{i}", [64, 8, 64], BF16) for i in range(2)] for d_ in 'fb'}
    Zb = {d_: [sb(f"Zb{d_}{i}", [64, 8, 64], BF16) for i in range(2)] for d_ in 'fb'}
    Gm = {d_: [sb(f"Gm{d_}{i}", [64, 8, 64], BF16) for i in range(2)] for d_ in 'fb'}
    Sst = {d_: sb(f"S_{d_}", [128, 128]) for d_ in 'fb'}
    Sbf = {d_: sb(f"Sb_{d_}", [128, 128], BF16) for d_ in 'fb'}
    Rt = {d_: sb(f"R_{d_}", [64, 128], BF16) for d_ in 'fb'}
    vnew = {d_: sb(f"vnew_{d_}", [64, 128], BF16) for d_ in 'fb'}
    tq = {d_: sb(f"tq_{d_}", [64, 128]) for d_ in 'fb'}
    ktl = {d_: sb(f"ktl_{d_}", [64, 128], BF16) for d_ in 'fb'}
    otmp = sb("otmp", [64, 128])
    ssq = sb("ssq", [64, 64])
    wcnt = [0]; pcnt = [0]

    def nextbank():
        b_ = bank[pcnt[0] % 2]; pcnt[0] += 1
        return b_

    def inproj_chunk(col0, evac):
        wb = Wg[wcnt[0] % 2]; wcnt[0] += 1
        P.dma('pool', wb[:], w_in_v[:, :, col0:col0 + 128])
        for tt_ in range(8):
            hb = load_hT(tt_)
            pb = nextbank()
            for kc in range(8):
                P.mm(pb[:, :], wb[:, kc, :], hb[:, kc, :], start=(kc == 0), stop=(kc == 7))
            evac(tt_, pb)

    def evac_U(tt_, pb):
        P.copy('act', U[:, 2 + tt_ * 512:2 + (tt_ + 1) * 512], pb[:, :])

    def conv_silu(ci):
        for t in range(5):
            P.ts('dve', Dg[:, t, :], identb[:], cwq[:, t * 24 + ci:t * 24 + ci + 1], None, ALU.mult)
        for tt_ in range(8):
            pb = nextbank()
            for t in range(5):
                P.mm(pb[:, :], Dg[:, t, :], U[:, tt_ * 512 + t:tt_ * 512 + t + 512], start=(t == 0), stop=(t == 4))
            P.act(C[:, tt_ * 512:(tt_ + 1) * 512], pb[:, :], AF.Silu)

    def l2norm_to(dst, scale):
        for tt_ in range(8):
            sl = slice(tt_ * 512, (tt_ + 1) * 512)
            P.act(sqb[:], C[:, sl], AF.Square)
            pb = nextbank()
            P.mm(pb[:, :], onesb[:], sqb[:])
            P.act(lnb[:], pb[:, :], AF.Ln, bias=eps_l2[:, 0:1])
            P.act(lnb[:], lnb[:], AF.Exp, scale=-0.5)
            P.stt(dst[:, sl], C[:, sl], scale, lnb[:], ALU.mult, ALU.mult)

    def to_tokmajor(src, dst):
        for g in range(8):
            bi = 2 + g % 2
            pbf = bank_bf(bi)
            for cc in range(8):
                c = g * 8 + cc
                P.tr(pbf[0:64, cc * 128:(cc + 1) * 128], src[:, c * 64:(c + 1) * 64], identb[:])
            P.copy('dve' if g % 2 == 0 else 'act', dst[:, g * 8:(g + 1) * 8, :], pbf[0:64, :].rearrange("p (c d) -> p c d", d=128))

    def flat(t3):
        return t3[:].rearrange("p c i -> p (c i)")

    for h in range(nheads):
        inproj_chunk(COL_Q + h * 128, evac_U); conv_silu(h); l2norm_to(qT, 128.0 ** -0.5)
        inproj_chunk(COL_K + h * 128, evac_U); conv_silu(8 + h); l2norm_to(kT, 1.0)
        to_tokmajor(kT, Ktok)
        inproj_chunk(COL_V + h * 128, evac_U); conv_silu(16 + h)
        to_tokmajor(C, Vtok)
        if debug and h == 0:
            dbg_out("dbg_qT", qT[:], [128, 4096], BF16)
            dbg_out("dbg_kT", kT[:], [128, 4096], BF16)
            dbg_out("dbg_Ktok", Ktok[:], [64, 64, 128], BF16)
            dbg_out("dbg_Vtok", Vtok[:], [64, 64, 128], BF16)
        if stage <= 3:
            continue
        for d_ in 'fb':
            la_h = LA[d_][:, h, :]
            P.mm(bank[4][0:64, 0:64], tri[d_][:], la_h)
            P.copy('dve', Gd[d_][:], bank[4][0:64, 0:64])
            P.mm(bank[5][:, 0:64], onesf[0:64, :], la_h)
            P.act(CD[d_][:], bank[5][:, 0:64], AF.Exp)
            P.tt('dve', TAIL[d_][:], bank[5][0:64, 0:64], Gd[d_][:], ALU.subtract)
            P.act(TAIL[d_][:], TAIL[d_][:], AF.Exp)
            P.act(EG[d_][:], Gd[d_][:], AF.Exp)
            P.ts('dve', NEG[d_][:], EG[d_][:], -1.0, None, ALU.mult)
        for g in range(8):
            c0 = g * 8
            for cc in range(8):
                ch = slice((c0 + cc) * 64, (c0 + cc + 1) * 64)
                P.mm(bank[4][0:64, cc * 64:(cc + 1) * 64], kT[:, ch], kT[:, ch])
            for cc in range(8):
                ch = slice((c0 + cc) * 64, (c0 + cc + 1) * 64)
                P.mm(bank[5][0:64, cc * 64:(cc + 1) * 64], kT[:, ch], qT[:, ch])
            bk = {'f': (2, 3, 7), 'b': (0, 1, 6)}
            for d_ in 'fb':
                la_g = LA[d_][:, h, c0:c0 + 8]
                P.tt('pool', LAm[:], bc(la_g, [64, 8, 64], 2), bc(tri[d_][:], [64, 8, 64], 1), ALU.mult)
                P.copy('pool', LAb[:], bc(la_g, [64, 8, 64], 2))
                P.mm(bank[6][0:64, :], onesf[0:64, 0:64], flat(LAm), start=True, stop=False)
                P.mm(bank[6][0:64, :], ntri[d_][:], flat(LAb), start=False, stop=False)
                P.mm(bank[6][0:64, :], identf[0:64, 0:64], flat(negT[d_]), start=False, stop=True)
                P.act(flat(decT[d_]), bank[6][0:64, :], AF.Exp)
            for d_ in 'fb':
                P.tt('dve', flat(qkT[d_])[:, c0 * 64:(c0 + 8) * 64], bank[5][0:64, :], flat(decT[d_]), ALU.mult)
                P.tt('dve', flat(tY[d_]), bank[4][0:64, :], flat(decT[d_]), ALU.mult)
                P.tt('pool', tY[d_][:], tY[d_][:], bc(BETA[d_][:, h, c0:c0 + 8], [64, 8, 64], 2), ALU.mult)
                P.tt('dve', tY[d_][:], tY[d_][:], bc(nstrT[d_][:], [64, 8, 64], 1), ALU.mult)
                P.copy('act', Yb[d_][0][:], tY[d_][:])
                P.tt('pool', Gm[d_][0][:], tY[d_][:], bc(identf[0:64, 0:64], [64, 8, 64], 1), ALU.add)
            for d_ in 'fb':
                z0 = bank_bf(bk[d_][2])
                for cc in range(8):
                    P.tr(z0[0:64, cc * 64:(cc + 1) * 64], Yb[d_][0][:, cc, :], identb[0:64, 0:64])
                P.copy('dve' if d_ == 'f' else 'act', flat(Zb[d_][0]), z0[0:64, 0:512])
            for k in range(5):
                cur = k % 2; nxt = 1 - cur
                for d_ in 'fb':
                    by_, bz_, bg_ = bk[d_]
                    if k < 4:
                        for cc in range(8):
                            P.mm(bank[by_][0:64, cc * 64:(cc + 1) * 64], Zb[d_][cur][:, cc, :], Yb[d_][cur][:, cc, :])
                    for cc in range(8):
                        P.mm(bank[bz_][0:64, cc * 64:(cc + 1) * 64], Yb[d_][cur][:, cc, :], Zb[d_][cur][:, cc, :])
                for d_ in 'fb':
                    by_, bz_, bg_ = bk[d_]
                    if k < 4:
                        P.copy('act', flat(Yb[d_][nxt]), bank[by_][0:64, :])
                    P.copy('dve', flat(Zb[d_][nxt]), bank[bz_][0:64, :])
                for d_ in 'fb':
                    by_, bz_, bg_ = bk[d_]
                    for cc in range(8):
                        P.mm(bank[bg_][0:64, cc * 64:(cc + 1) * 64], Zb[d_][nxt][:, cc, :], Gm[d_][cur][:, cc, :])
                for d_ in 'fb':
                    by_, bz_, bg_ = bk[d_]
                    dstG = flat(Gm[d_][nxt]) if k < 4 else flat(TT[d_])[:, c0 * 64:(c0 + 8) * 64]
                    P.tt('dve' if d_ == 'f' else 'pool', dstG, bank[bg_][0:64, :], flat(Gm[d_][cur]), ALU.add) if d_ == 'f' else P.tt('dve', dstG, bank[bg_][0:64, :], flat(Gm[d_][cur]), ALU.add)
        if debug and h == 0:
            for d_ in 'fb':
                dbg_out(f"dbg_TT_{d_}", TT[d_][:], [64, 64, 64], BF16)
                dbg_out(f"dbg_qkT_{d_}", qkT[d_][:], [64, 64, 64], BF16)
        if stage <= 4:
            continue
        for d_ in 'fb':
            P.memset('pool', Sst[d_][:], 0.0)
            P.memset('pool', Sbf[d_][:], 0.0)
        nsteps = int(os.environ.get('MK_STEPS', '64')) if debug else 64
        for s_ in range(nsteps):
            cs = {'f': s_, 'b': 63 - s_}
            pv = {}
            for di, d_ in enumerate('fb'):
                bA = bank[di * 3]; bB_ = bank[di * 3 + 1]; bC = bank[di * 3 + 2]
                pv[d_] = (bA[0:64, 0:128], bB_[0:64, 0:128], bA[0:64, 128:256], bB_[0:64, 128:256], bC[:, 0:128])
            for d_ in 'fb':
                c = cs[d_]; ch = slice(c * 64, (c + 1) * 64); ps1, ps2, ps3, ps4, ps5 = pv[d_]
                P.mm(ps1, kT[:, ch], Sbf[d_][:])
                P.mm(ps2, qT[:, ch], Sbf[d_][:])
            for d_ in 'fb':
                c = cs[d_]; ps1, ps2, ps3, ps4, ps5 = pv[d_]
                P.stt(Rt[d_][:], ps1, NEG[d_][:, c:c + 1], Vtok[:, c, :], ALU.mult, ALU.add)
                P.act(ktl[d_][:], Ktok[:, c, :], AF.Identity, scale=TAIL[d_][:, c:c + 1])
                P.act(tq[d_][:], ps2, AF.Identity, scale=EG[d_][:, c:c + 1])
            for d_ in 'fb':
                c = cs[d_]; ps1, ps2, ps3, ps4, ps5 = pv[d_]
                P.mm(ps3, TT[d_][:, c, :], Rt[d_][:])
            for d_ in 'fb':
                c = cs[d_]; ps1, ps2, ps3, ps4, ps5 = pv[d_]
                P.act(vnew[d_][:], ps3, AF.Identity, scale=BETA[d_][:, h, c:c + 1])
            for d_ in 'fb':
                c = cs[d_]; ps1, ps2, ps3, ps4, ps5 = pv[d_]
                P.mm(ps5, ktl[d_][:], vnew[d_][:])
                P.mm(ps4, qkT[d_][:, c, :], vnew[d_][:])
            for d_ in 'fb':
                c = cs[d_]; ps1, ps2, ps3, ps4, ps5 = pv[d_]
                P.stt(Sbf[d_][:], Sst[d_][:], CD[d_][:, c:c + 1], ps5, ALU.mult, ALU.add)
            for d_ in 'fb':
                c = cs[d_]; ps1, ps2, ps3, ps4, ps5 = pv[d_]
                P.stt(Sst[d_][:], Sst[d_][:], CD[d_][:, c:c + 1], ps5, ALU.mult, ALU.add)
                first = (d_ == 'f' and c < 32) or (d_ == 'b' and c >= 32)
                if first:
                    P.tt('dve', O[:, c, :], tq[d_][:], ps4, ALU.add)
                else:
                    P.tt('dve', otmp[:], tq[d_][:], ps4, ALU.add)
                    P.tt('pool', O[:, c, :], O[:, c, :], otmp[:], ALU.add)
        if debug and h == 0:
            dbg_out("dbg_O", O[:], [64, 64, 128], F32)
            dbg_out("dbg_S_f", Sst['f'][:], [128, 128], F32)
            dbg_out("dbg_vnew_f", vnew['f'][:], [64, 128], BF16)
            dbg_out("dbg_R_f", Rt['f'][:], [64, 128], BF16)
            dbg_out("dbg_tq_f", tq['f'][:], [64, 128], F32)
            dbg_out("dbg_ktl_f", ktl['f'][:], [64, 128], BF16)
            dbg_out("dbg_CD_f", CD['f'][:], [128, 64], F32)
            dbg_out("dbg_TAIL_f", TAIL['f'][:], [64, 64], F32)
            dbg_out("dbg_EG_f", EG['f'][:], [64, 64], F32)
        Zs = Ktok
        wz = Wg[wcnt[0] % 2]; wcnt[0] += 1
        P.dma('pool', wz[:], w_in_v[:, :, COL_Z + h * 128:COL_Z + (h + 1) * 128])
        for tt_ in range(8):
            hb = load_hT(tt_)
            for hf in range(2):
                pb = nextbank()
                for c4 in range(4):
                    cc = hf * 4 + c4
                    for kc in range(8):
                        P.mm(pb[0:64, c4 * 128:(c4 + 1) * 128], hb[:, kc, cc * 64:(cc + 1) * 64], wz[:, kc, :], start=(kc == 0), stop=(kc == 7))
                c_lo = tt_ * 8 + hf * 4
                P.act(Zs[:, c_lo:c_lo + 4, :], pb[0:64, :].rearrange("p (c d) -> p c d", d=128), AF.Silu)
        SQ = Vtok
        P.tt('dve', SQ[:], O[:], O[:], ALU.mult)
        P.op('dve', lambda e: e.tensor_reduce(out=ssq[:], in_=SQ[:], axis=AX.X, op=ALU.add), [SQ], [ssq])
        P.act(ssq[:], ssq[:], AF.Ln, bias=eps_rms[0:64, 0:1], scale=1.0 / 128.0)
        P.act(ssq[:], ssq[:], AF.Exp, scale=-0.5)
        P.tt('dve', O[:], O[:], bc(ssq[:], [64, 64, 128], 2), ALU.mult)
        P.tt('pool', O[:], O[:], bc(ngB[:], [64, 64, 128], 1), ALU.mult)
        OGb = Vtok
        P.tt('dve', OGb[:], O[:], Zs[:], ALU.mult)
        for g in range(8):
            bi = 2 + g % 2
            pbf = bank_bf(bi)
            for cc in range(8):
                P.tr(pbf[:, cc * 64:(cc + 1) * 64], OGb[:, g * 8 + cc, :], identb[0:64, 0:64])
            P.copy('act', C[:, g * 512:(g + 1) * 512], pbf[:, 0:512])
        P.dma('sp', ogT_d[h * 128:(h + 1) * 128, :], C[:], w=[('ogT_d', h)])
    if stage <= 5:
        P.finish()
        return nc, dbg

    A.release(head_mark)
    vst = sb("vst", [64, 128])
    for r0, src, n in ((0, b_glu, 16), (16, b_gate, 16), (32, b_dw, 8), (40, conv_ln_g, 8), (48, conv_ln_b, 8), (56, b_b_o, 8)):
        P.dma('sp', vst[r0:r0 + n, :], src.rearrange("(c p) -> c p", p=128))
    P.tr(bank[0][:, 0:64], vst[:, :], identf[0:64, 0:64])
    P.copy('dve', vp[:], bank[0][:, 0:64])
    VB_GLU, VB_GATE, VB_DW, VB_LNG, VB_LNB, VB_BBO = 0, 16, 32, 40, 48, 56
    cdst1 = sb("cdst1", [128, 128]); cdst2 = sb("cdst2", [120, 128]); cwd = sb("cwd", [128, 248])
    cdv = conv_dw.rearrange("t (c p) -> (t c) p", p=128)
    P.dma('sp', cdst1[:], cdv[0:128, :])
    P.dma('sp', cdst2[:], cdv[128:248, :])
    P.tr(bank[1][:, 0:128], cdst1[:, :], identf[:, :])
    P.copy('dve', cwd[:, 0:128], bank[1][:, 0:128])
    P.tr(bank[1][:, 128:248], cdst2[:, :], identf[0:120, 0:120])
    P.copy('dve', cwd[:, 128:248], bank[1][:, 128:248])
    Wg4 = [sb(f"Wg4_{i}", [128, 8, 512], BF16) for i in range(2)]
    GA = [sb(f"GA{i}", [128, 4096], BF16) for i in range(2)]
    Ug = [sb(f"Ug{i}", [128, 4096 + 30], BF16) for i in range(2)]
    for u_ in Ug:
        P.memset('pool', u_[:, 0:15], 0.0); P.memset('pool', u_[:, 4096 + 15:4096 + 30], 0.0)
    Ycb = [sb(f"Ycb{i}", [128, 4096], BF16) for i in range(4)]
    Dg31 = sb("Dg31", [128, 31, 128], BF16)
    sgt = sb("sgt", [128, 512]); sq2 = sb("sq2", [128, 512], BF16)
    SUM = sb("SUM", [1, 4096]); SSQ = sb("SSQ", [1, 4096])
    w4cnt = [0]

    def inproj_group(cols, evacs):
        wb = Wg4[w4cnt[0] % 2]; w4cnt[0] += 1
        for jj, c0_ in enumerate(cols):
            P.dma('pool', wb[:, :, jj * 128:(jj + 1) * 128], w_in_v[:, :, c0_:c0_ + 128], w=[f"{wb.name}_{jj}"])
        for tt_ in range(8):
            hb = load_hT(tt_)
            for jj in range(len(cols)):
                pb = nextbank()
                for kc in range(8):
                    P.mm(pb[:, :], wb[:, kc, jj * 128:(jj + 1) * 128], hb[:, kc, :], start=(kc == 0), stop=(kc == 7),
                         r=[f"{wb.name}_{jj}", hb])
                evacs[jj](tt_, pb)

    for g4 in range(4):
        evs = []
        for jj in range(4):
            cg = g4 * 4 + jj

            def evac_gate(tt_, pb, cg=cg, jj=jj):
                P.act(Ycb[jj][:, tt_ * 512:(tt_ + 1) * 512], pb[:, :], AF.Sigmoid, bias=vp[:, VB_GATE + cg:VB_GATE + cg + 1])
            evs.append(evac_gate)
        inproj_group([COL_GATE + (g4 * 4 + jj) * 128 for jj in range(4)], evs)
        for jj in range(4):
            cg = g4 * 4 + jj
            P.dma('sp', gate_d[cg * 128:(cg + 1) * 128, :], Ycb[jj][:], w=[('gate_d', cg)])
    for j2 in range(4):
        evs = []; cols = []
        for u in range(2):
            j = j2 * 2 + u

            def evac_a(tt_, pb, j=j, u=u):
                P.act(GA[u][:, tt_ * 512:(tt_ + 1) * 512], pb[:, :], AF.Identity, bias=vp[:, VB_GLU + j:VB_GLU + j + 1])

            def evac_g(tt_, pb, j=j, u=u):
                P.act(sgt[:], pb[:, :], AF.Sigmoid, bias=vp[:, VB_GLU + 8 + j:VB_GLU + 8 + j + 1])
                P.tt('dve', Ug[u][:, 15 + tt_ * 512:15 + (tt_ + 1) * 512], GA[u][:, tt_ * 512:(tt_ + 1) * 512], sgt[:], ALU.mult)
            cols += [COL_GLU + j * 128, COL_GLU + 1024 + j * 128]
            evs += [evac_a, evac_g]
        inproj_group(cols, evs)
        for u in range(2):
            j = j2 * 2 + u
            yc = Ycb[u]
            for t in range(31):
                P.ts('dve' if t % 2 == 0 else 'pool', Dg31[:, t, :], identb[:], cwd[:, t * 8 + j:t * 8 + j + 1], None, ALU.mult)
            for tt_ in range(8):
                pb = nextbank()
                for t in range(31):
                    P.mm(pb[:, :], Dg31[:, t, :], Ug[u][:, tt_ * 512 + t:tt_ * 512 + t + 512], start=(t == 0), stop=(t == 30))
                sl = slice(tt_ * 512, (tt_ + 1) * 512)
                P.act(yc[:, sl], pb[:, :], AF.Identity, bias=vp[:, VB_DW + j:VB_DW + j + 1])
                P.act(sq2[:], pb[:, :], AF.Square, bias=vp[:, VB_DW + j:VB_DW + j + 1])
                P.mm(bank[2][0:1, :], onesb[:, 0:1], yc[:, sl])
                P.mm(bank[3][0:1, :], onesb[:, 0:1], sq2[:])
                if j == 0:
                    P.copy('dve', SUM[:, sl], bank[2][0:1, :])
                    P.copy('dve', SSQ[:, sl], bank[3][0:1, :])
                else:
                    P.tt('dve', SUM[:, sl], SUM[:, sl], bank[2][0:1, :], ALU.add)
                    P.tt('dve', SSQ[:, sl], SSQ[:, sl], bank[3][0:1, :], ALU.add)
            P.dma('sp', ybT_d[j * 128:(j + 1) * 128, :], yc[:], w=[('ybT_d', j)])
    stt_ = sb("stt_", [1, 512])
    for tt_ in range(8):
        sl = slice(tt_ * 512, (tt_ + 1) * 512)
        P.ts('dve', SUM[:, sl], SUM[:, sl], 1.0 / 1024.0, None, ALU.mult)
        P.tt('dve', stt_[:], SUM[:, sl], SUM[:, sl], ALU.mult)
        P.stt(SSQ[:, sl], SSQ[:, sl], 1.0 / 1024.0, stt_[:], ALU.mult, ALU.subtract)
        P.act(SSQ[:, sl], SSQ[:, sl], AF.Ln, bias=eps_ln[0:1, 0:1])
        P.act(SSQ[:, sl], SSQ[:, sl], AF.Exp, scale=-0.5)
        P.stt(SUM[:, sl], SUM[:, sl], -1.0, SSQ[:, sl], ALU.mult, ALU.mult)
    RSTD = SSQ; NMR = SUM
    P.dma('sp', st_d[0:1, :], RSTD[:])
    P.dma('sp', st_d[1:2, :], NMR[:])
    s5_mark = A.mark()
    if stage <= 6:
        P.finish()
        return nc, dbg

    A.release(base_mark)
    LG = sb("LG", [128, 32, 32]); M8 = sb("M8", [128, 32, 8]); MSK = sb("MSK", [128, 32, 32], BF16); G4 = sb("G4", [128, 32, 4])
    moe_mark = A.mark()
    Wao = sb("Wao", [128, 8, 1024], BF16); Wbo = sb("Wbo", [128, 8, 1024], BF16); Wout = sb("Wout", [128, 8, 1024], BF16)
    P.dma('pool', Wao[:], w_a_o.rearrange("(kc p) c -> p kc c", p=128))
    P.dma('pool', Wbo[:], w_b_o.rearrange("(kc p) c -> p kc c", p=128))
    P.dma('pool', Wout[:], w_out.rearrange("(kc p) c -> p kc c", p=128))
    Wr = sb("Wr", [128, 8, 32]); brB = sb("brB", [128, 32])
    P.dma('sp', Wr[:], w_router.rearrange("(kc p) c -> p kc c", p=128))
    P.dma('sp', brB[:], b_router.partition_broadcast(128))
    lnp['g'] = sb("gB1", [128, D]); lnp['b'] = sb("bB1", [128, D])
    P.dma('sp', lnp['g'][:], ln1_g.partition_broadcast(128))
    P.dma('sp', lnp['b'][:], ln1_b.partition_broadcast(128))
    ogt = sb("ogt", [128, 8, 512], BF16); ybt = sb("ybt", [128, 8, 512], BF16); gtt = sb("gtt", [128, 16, 512], BF16)
    ybaT = sb("ybaT", [128, 8, 512], BF16); mixT = sb("mixT", [128, 8, 512], BF16)
    t1 = sb("t1", [128, 512]); t2 = sb("t2", [128, 512])
    rs_t = sb("rs_t", [1, 512]); nm_t = sb("nm_t", [1, 512])
    h0t = sb("h0t", [128, D]); rt = sb("rt", [128, D]); h1t = sb("h1t", [128, D]); h1Tf = sb("h1Tf", [128, 8, 128])
    nmx = sb("nmx", [128, 1]); esum = sb("esum", [128, 1]); h1tb = sb("h1tb", [128, D], BF16)
    ogv = ogT_d.rearrange("(j p) t -> p j t", p=128)
    ybv = ybT_d.rearrange("(j p) t -> p j t", p=128)
    gtv = gate_d.rearrange("(j p) t -> p j t", p=128)
    for tt_ in range(8):
        sl = slice(tt_ * 512, (tt_ + 1) * 512)
        P.dma('sp', ogt[:], ogv[:, :, sl], r=[('ogT_d', j_) for j_ in range(8)])
        P.dma('sp', ybt[:], ybv[:, :, sl], r=[('ybT_d', j_) for j_ in range(8)])
        P.dma('sp', gtt[:], gtv[:, :, sl], r=[('gate_d', j_) for j_ in range(16)])
        P.dma('sp', rs_t[:], st_d[0:1, sl])
        P.dma('sp', nm_t[:], st_d[1:2, sl])
        P.mm(bank[6][:, :], onesf[0:1, :], rs_t[0:1, :])
        P.mm(bank[7][:, :], onesf[0:1, :], nm_t[0:1, :])
        for j in range(8):
            P.tt('dve', t1[:], ybt[:, j, :], bank[6][:, :], ALU.mult)
            P.tt('dve', t1[:], t1[:], bank[7][:, :], ALU.add)
            P.act(ybaT[:, j, :], t1[:], AF.Silu, bias=vp[:, VB_LNB + j:VB_LNB + j + 1], scale=vp[:, VB_LNG + j:VB_LNG + j + 1])
        if debug and tt_ == 0:
            dbg_out("dbg_ybaT", ybaT[:], [128, 8, 512], BF16)
        for m in range(8):
            pa = bank[0]; pb_ = bank[1]
            for kc in range(8):
                P.mm(pa[:, :], Wao[:, kc, m * 128:(m + 1) * 128], ogt[:, kc, :], start=(kc == 0), stop=(kc == 7))
            for kc in range(8):
                P.mm(pb_[:, :], Wbo[:, kc, m * 128:(m + 1) * 128], ybaT[:, kc, :], start=(kc == 0), stop=(kc == 7))
            P.tt('dve', t1[:], pa[:, :], gtt[:, m, :], ALU.mult)
            P.stt(t2[:], pb_[:, :], vp[:, VB_BBO + m:VB_BBO + m + 1], gtt[:, 8 + m, :], ALU.add, ALU.mult)
            P.tt('pool', mixT[:, m, :], t1[:], t2[:], ALU.add)
        for sub in range(4):
            i = tt_ * 4 + sub
            P.dma('sp', h0t[:], h0_d[i * 128:(i + 1) * 128, :], r=[('h0_d', i)])
            for hf in range(2):
                pm = bank[2 + hf]
                for m in range(8):
                    P.mm(pm[:, :], mixT[:, m, sub * 128:(sub + 1) * 128], Wout[:, m, hf * 512:(hf + 1) * 512], start=(m == 0), stop=(m == 7))
                P.stt(rt[:, hf * 512:(hf + 1) * 512], h0t[:, hf * 512:(hf + 1) * 512], DN_ALPHA, pm[:, :], ALU.mult, ALU.add)
            layer_norm_rows(rt, h1t)
            P.dma('sp', h1_d[i * 128:(i + 1) * 128, :], h1t[:], w=[('h1_d', i)])
            for hf in range(2):
                pt = bank[4 + hf]
                for q4 in range(4):
                    kc = hf * 4 + q4
                    P.tr(pt[:, q4 * 128:(q4 + 1) * 128], h1t[:, kc * 128:(kc + 1) * 128], identf[:])
                P.copy('dve', h1Tf[:, hf * 4:(hf + 1) * 4, :], pt[:].rearrange("p (k t) -> p k t", t=128))
            pr_ = bank[6 + (sub % 2)]
            for kc in range(8):
                P.mm(pr_[:, 0:32], h1Tf[:, kc, :], Wr[:, kc, :], start=(kc == 0), stop=(kc == 7))
            P.tt('dve', LG[:, i, :], pr_[:, 0:32], brB[:], ALU.add)
            P.op('dve', lambda e, i=i: e.max(out=M8[:, i, :], in_=LG[:, i, :]), [LG], [M8])
            P.ts('dve', MSK[:, i, :], LG[:, i, :], M8[:, i, 3:4], None, ALU.is_ge)
            P.ts('dve', nmx[:], M8[:, i, 0:1], -1.0, None, ALU.mult)
            P.act(G4[:, i, :], M8[:, i, 0:4], AF.Exp, bias=nmx[:, 0:1])
            P.op('dve', lambda e, i=i: e.tensor_reduce(out=esum[:], in_=G4[:, i, :], axis=AX.X, op=ALU.add), [G4], [esum])
            P.op('dve', lambda e: e.reciprocal(out=esum[:], in_=esum[:]), [esum], [esum])
            P.ts('dve', G4[:, i, :], G4[:, i, :], esum[:, 0:1], None, ALU.mult)
            P.copy('act', h1tb[:], h1t[:])
            P.dma('sp', h1b_d[i * 128:(i + 1) * 128, :], h1tb[:], w=[('h1b_d', i)])
    if debug:
        dbg_out("dbg_G4", G4[:], [128, 32, 4], F32)
        dbg_out("dbg_MSK", MSK[:], [128, 32, 32], BF16)
    if stage <= 7:
        P.finish()
        return nc, dbg

    A.release(moe_mark)
    I32 = mybir.dt.int32
    NBLK = 64
    lnp['g'] = sb("gB2", [128, D]); lnp['b'] = sb("bB2", [128, D])
    P.dma('sp', lnp['g'][:], ln2_g.partition_broadcast(128))
    P.dma('sp', lnp['b'][:], ln2_b.partition_broadcast(128))
    stri = sb("stri", [128, 128], BF16)
    strf = sb("strf", [128, 128])
    P.op('pool', lambda e: e.affine_select(out=strf[:], in_=onesf[:], pattern=[[1, 128]], compare_op=ALU.is_gt, fill=0.0,
                                           base=0, channel_multiplier=-1), [onesf], [strf])
    P.copy('dve', stri[:], strf[:])
    POS = sb("POS", [128, 32, 32]); run = sb("run", [128, 32], BF16)
    P.memset('pool', run[:], 0.0)
    for i in range(32):
        pp = bank[i % 2]
        P.mm(pp[:, 0:32], stri[:], MSK[:, i, :], start=True, stop=False)
        P.mm(pp[:, 0:32], onesb[:], run[:], start=False, stop=True)
        P.copy('act', POS[:, i, :], pp[:, 0:32])
        P.tt('dve', run[:], run[:], MSK[:, i, :], ALU.add)
    cnt = sb("cnt", [128, 32]); padded = sb("padded", [128, 32]); pend = sb("pend", [128, 32]); pstart = sb("pstart", [128, 32])
    tmp32 = sb("tmp32", [128, 32]); ones32 = sb("ones32", [128, 32])
    P.memset('pool', ones32[:], 1.0)
    P.mm(bank[2][:, 0:32], onesb[:], run[:])
    P.copy('dve', cnt[:], bank[2][:, 0:32])
    P.memset('pool', padded[:], 0.0)
    for j in range(8):
        P.ts('dve', tmp32[:], cnt[:], float(512 * j), 512.0, ALU.is_gt, ALU.mult)
        P.tt('dve', padded[:], padded[:], tmp32[:], ALU.add)
    P.op('dve', lambda e: e.tensor_tensor_scan(out=pend[:], data0=ones32[:], data1=padded[:], initial=0.0, op0=ALU.mult, op1=ALU.add),
         [ones32, padded], [pend])
    P.tt('dve', pstart[:], pend[:], padded[:], ALU.subtract)
    bthr = sb("bthr", [128, NBLK, 32]); BE = sb("BE", [128, NBLK])
    P.op('pool', lambda e: e.iota(out=bthr[:], pattern=[[512, NBLK], [0, 32]], base=0, channel_multiplier=0,
                                  allow_small_or_imprecise_dtypes=True), [], [bthr])
    P.tt('dve', bthr[:], bc(pend[:], [128, NBLK, 32], 1), bthr[:], ALU.is_le)
    P.op('dve', lambda e: e.tensor_reduce(out=BE[:], in_=bthr[:], axis=AX.X, op=ALU.add), [bthr], [BE])
    P.ts('dve', BE[:], BE[:], 31.0, None, ALU.min)
    basef = sb("basef", [128, 8]); IDXF = sb("IDXF", [128, NBLK, 8]); IDXW = sb("IDXW", [128, NBLK, 8], I32)
    P.op('pool', lambda e: e.iota(out=basef[:], pattern=[[128, 8]], base=0, channel_multiplier=1,
                                  allow_small_or_imprecise_dtypes=True), [], [basef])
    P.stt(IDXF[:], bc(BE[:], [128, NBLK, 8], 2), 1024.0, bc(basef[:], [128, NBLK, 8], 1), ALU.mult, ALU.add)
    P.copy('dve', IDXW[:], IDXF[:])
    pidx = sb("pidx", [32, 1]); OH = sb("OH", [32, NBLK], BF16)
    P.op('pool', lambda e: e.iota(out=pidx[:], pattern=[[0, 1]], base=0, channel_multiplier=1,
                                  allow_small_or_imprecise_dtypes=True), [], [pidx])
    P.ts('dve', OH[:], BE[0:32, :], pidx[0:32, 0:1], None, ALU.is_equal)
    bgu_all = sb("bgu_all", [32, 2048], BF16); bd_all = sb("bd_all", [32, D], BF16)
    P.dma('pool', bgu_all[:], b_gu)
    P.dma('pool', bd_all[:], b_down)
    SLOTF = sb("SLOTF", [128, 32, 4]); SLOT = sb("SLOT", [128, 32, 4], I32)
    valt = sb("valt", [128, 32]); mk = sb("mk", [128, 4, 32])
    for i in range(32):
        P.tt('dve', valt[:], POS[:, i, :], pstart[:], ALU.add)
        P.tt('dve', mk[:], bc(LG[:, i, :], [128, 4, 32], 1), bc(M8[:, i, 0:4], [128, 4, 32], 2), ALU.is_equal)
        P.tt('dve', mk[:], mk[:], bc(valt[:], [128, 4, 32], 1), ALU.mult)
        P.op('dve', lambda e, i=i: e.tensor_reduce(out=SLOTF[:, i, :], in_=mk[:], axis=AX.X, op=ALU.add), [mk], [SLOTF])
    P.copy('dve', SLOT[:], SLOTF[:])
    if debug:
        dbg_out("dbg_SLOT", SLOT[:], [128, 32, 4], I32)
        dbg_out("dbg_BE", BE[:], [128, NBLK], F32)
        dbg_out("dbg_cnt", cnt[:], [128, 32], F32)
    disp_mark = A.mark()
    hb_t = [sb(f"hb_t{i}", [128, D], BF16) for i in range(2)]
    for i in range(32):
        hb_ = hb_t[i % 2]
        P.dma('sp', hb_[:], h1b_d[i * 128:(i + 1) * 128, :], r=[('h1b_d', i)])
        for k in range(4):
            P.idma(xs_d, hb_[:], out_off=SLOT[:, i, k:k + 1], w=[('xs', i, k)])
    A.release(disp_mark)
    if debug and os.environ.get("MK_MOE_STOP") == "1":
        P.finish()
        return nc, dbg
    wguB = [sb(f"wguB{i}", [128, 8, 2048], BF16) for i in range(2)]
    wdB = [sb(f"wdB{i}", [128, 8, D], BF16) for i in range(2)]
    xtm = [sb(f"xtm{i}", [128, 4, D], BF16) for i in range(2)]; xT = sb("xT", [128, 8, 512], BF16)
    actT = sb("actT", [128, 8, 512], BF16)
    OHs = [sb(f"OHs{i}", [32, 512], BF16) for i in range(2)]
    NROT = 3
    gl = [sb(f"gl{i}", [128, 512]) for i in range(NROT)]; sg = [sb(f"sg{i}", [128, 512]) for i in range(NROT)]
    li = [sb(f"li{i}", [128, 512]) for i in range(NROT)]; p1 = [sb(f"p1{i}", [128, 512]) for i in range(NROT)]
    ysb = [sb(f"ysb{i}", [128, D]) for i in range(2)]
    wgu_rows = w_gu.rearrange("e k c -> (e k) c")
    wd_rows = w_down.rearrange("e k c -> (e k) c")
    nblk_run = int(os.environ.get("MK_NBLK", str(NBLK))) if debug else NBLK
    SIG_MAX = float(1.0 / (1.0 + np.exp(-1.702 * 7.0)))

    def load_block_weights(b):
        for kc in range(8):
            P.idma(wguB[b % 2][:, kc, :], wgu_rows, in_off=IDXW[:, b, kc:kc + 1], r=[IDXW], w=[f"wguB{b % 2}_{kc}"])
        for kc in range(8):
            P.idma(wdB[b % 2][:, kc, :], wd_rows, in_off=IDXW[:, b, kc:kc + 1], r=[IDXW], w=[f"wdB{b % 2}_{kc}"])

    def load_block_x(b):
        P.dma('sp', xtm[b % 2][:], xs_d[b * 512:(b + 1) * 512, :].rearrange("(s p) d -> p s d", p=128), r=[])
        P.copy('act', OHs[b % 2][:], OH[:, b:b + 1].to_broadcast([32, 512]))

    l1b = [0]; l2b = [0]; ycnt = [0]; rot = [0]
    load_block_weights(0)
    load_block_x(0)
    for b in range(nblk_run):
        if b + 1 < nblk_run:
            load_block_weights(b + 1)
            load_block_x(b + 1)
        wg_ = wguB[b % 2]; wd_ = wdB[b % 2]; oh_ = OHs[b % 2]; xt_ = xtm[b % 2]
        for k2 in range(4):
            pbf = bank_bf(4 + k2 % 2)
            for kk in range(2):
                kc = k2 * 2 + kk
                for sub in range(4):
                    P.tr(pbf[:, kk * 512 + sub * 128:kk * 512 + (sub + 1) * 128], xt_[:, sub, kc * 128:(kc + 1) * 128], identb[:])
            P.copy('act' if k2 % 2 == 0 else 'dve', xT[:, k2 * 2:k2 * 2 + 2, :], pbf[:, :].rearrange("p (k t) -> p k t", t=512))
        for m in range(8):
            pbg = bank[l1b[0] % 4]; pbl = bank[(l1b[0] + 1) % 4]; l1b[0] += 2
            ri = rot[0] % NROT; rot[0] += 1
            for kc in range(8):
                P.mm(pbg[:, :], wg_[:, kc, m * 128:(m + 1) * 128], xT[:, kc, :], start=(kc == 0), stop=False, r=[f"wguB{b % 2}_{kc}", xT])
            P.mm(pbg[:, :], bgu_all[:, m * 128:(m + 1) * 128], oh_[:], start=False, stop=True)
            for kc in range(8):
                P.mm(pbl[:, :], wg_[:, kc, 1024 + m * 128:1024 + (m + 1) * 128], xT[:, kc, :], start=(kc == 0), stop=False, r=[f"wguB{b % 2}_{kc}", xT])
            P.mm(pbl[:, :], bgu_all[:, 1024 + m * 128:1024 + (m + 1) * 128], oh_[:], start=False, stop=True)
            P.act(sg[ri][:], pbg[:, :], AF.Sigmoid, scale=1.702)
            P.ts('dve', gl[ri][:], pbg[:, :], 7.0, None, ALU.min)
            P.ts('dve', li[ri][:], pbl[:, :], 7.0, -7.0, ALU.min, ALU.max)
            P.stt(p1[ri][:], sg[ri][:], SIG_MAX, gl[ri][:], ALU.min, ALU.mult)
            P.stt(actT[:, m, :], li[ri][:], 1.0, p1[ri][:], ALU.add, ALU.mult)
        for sub in range(4):
            yb_ = ysb[ycnt[0] % 2]; ycnt[0] += 1
            for hf in range(2):
                pb = bank[4 + l2b[0] % 4]; l2b[0] += 1
                for m in range(8):
                    P.mm(pb[:, :], actT[:, m, sub * 128:(sub + 1) * 128], wd_[:, m, hf * 512:(hf + 1) * 512], start=(m == 0), stop=False, r=[f"wdB{b % 2}_{m}", actT])
                P.mm(pb[:, :], oh_[:, 0:128], bd_all[:, hf * 512:(hf + 1) * 512], start=False, stop=True)
                P.copy('act' if hf == 0 else 'pool_never', yb_[:, hf * 512:(hf + 1) * 512], pb[:, :]) if hf == 0 else P.copy('dve', yb_[:, hf * 512:(hf + 1) * 512], pb[:, :])
            if not (debug and os.environ.get('MK_NOY')):
                P.dma('sp', ys_d[b * 512 + sub * 128:b * 512 + (sub + 1) * 128, :], yb_[:], w=[('ys', b, sub)])
    A.release(disp_mark)
    if debug and os.environ.get("MK_MOE_STOP") == "2":
        P.finish()
        return nc, dbg
    yk = [[sb(f"yk{j}_{k}", [128, D]) for k in range(4)] for j in range(2)]
    rt2 = [sb(f"rt2_{j}", [128, D]) for j in range(2)]; ot = [sb(f"ot{j}", [128, D]) for j in range(2)]
    for i in range(32):
        j = i % 2
        P.dma('sp', rt2[j][:], h1_d[i * 128:(i + 1) * 128, :], r=[('h1_d', i)])
        for k in range(4):
            P.idma(yk[j][k][:], ys_d, in_off=SLOT[:, i, k:k + 1], r=[SLOT])
        P.ts('dve', rt2[j][:], rt2[j][:], DN_ALPHA, None, ALU.mult)
        for k in range(4):
            P.stt(rt2[j][:], yk[j][k][:], G4[:, i, k:k + 1], rt2[j][:], ALU.mult, ALU.add)
        layer_norm_rows(rt2[j], ot[j])
        P.dma('sp', out[i * 128:(i + 1) * 128, :], ot[j][:])
    P.finish()
    return nc, dbg


_IN_NAMES = ['emb_ln_g', 'emb_ln_b', 'w_in', 'conv_qkv', 'a_log', 'dt_bias', 'dn_norm_g', 'w_a_o', 'b_glu', 'conv_dw',
             'b_dw', 'conv_ln_g', 'conv_ln_b', 'w_b_o', 'b_b_o', 'b_gate', 'w_out', 'ln1_g', 'ln1_b', 'w_router',
             'b_router', 'w_gu', 'b_gu', 'w_down', 'b_down', 'ln2_g', 'ln2_b']


def make_in_maps(inputs, n_cores=8):
    shared = {}
    for k in _IN_NAMES:
        a = np.ascontiguousarray(np.asarray(inputs[k], dtype=np.float32))
        a = a[0] if k not in ('emb_ln_g', 'emb_ln_b') else a
        if k in ('a_log', 'dt_bias'):
            a = a.reshape(16)
        shared[k] = np.ascontiguousarray(a)
    xs = np.asarray(inputs['x'], dtype=np.float32)
    maps = []
    for c in range(n_cores):
        m = dict(shared)
        m['x'] = np.ascontiguousarray(xs[c])
        maps.append(m)
    return maps


def kernel(**inputs):
    nc, _ = build_program()
    maps = make_in_maps(inputs, 8)
    res = run_bass_kernel_spmd(nc, maps, core_ids=list(range(8)))
    return np.stack([np.asarray(res.results[c]["out"]) for c in range(8)], axis=0).astype(np.float32)
```

```python
import os
import numpy as np
import concourse.bass as bass
import concourse.mybir as mybir
from concourse.bass_utils import run_bass_kernel_spmd

F32 = mybir.dt.float32
BF16 = mybir.dt.bfloat16
AF = mybir.ActivationFunctionType
ALU = mybir.AluOpType
AX = mybir.AxisListType

S = 4096
D = 1024
NCOL = 8224
NCH = 64
DN_ALPHA = 2.0 ** 0.25
LN_EPS = 1e-5
RMS_EPS = 1e-6
L2_EPS = 1e-6
NEG_BIG = -1.0e30
COL_Q, COL_K, COL_V, COL_Z, COL_SM, COL_GLU, COL_GATE = 0, 1024, 2048, 3072, 4096, 4128, 6176


class Prog:
    def __init__(self, nc, n_dma_sems=12, self_sync=True):
        self.nc = nc
        self.eng = {'pe': nc.tensor, 'act': nc.scalar, 'dve': nc.vector, 'pool': nc.gpsimd, 'sp': nc.sync}
        self.streams = {e: [] for e in self.eng}
        self.cnt = {e: 0 for e in self.eng}
        self.sem = {e: nc.alloc_semaphore(name=f"s_{e}") for e in self.eng}
        self.seen = {e: {} for e in self.eng}
        self.lastw = {}
        self.reads = {}
        self.semobj = {('e', e): self.sem[e] for e in self.eng}
        self.dma_sems = {}
        self.n_dma_sems = n_dma_sems
        self.self_sync = self_sync
        self.n_inst = 0

    def _dma_sem(self, q):
        if q not in self.dma_sems:
            lst = []
            for i in range(self.n_dma_sems):
                s = self.nc.alloc_semaphore(name=f"d_{q}_{i}")
                key = ('d', q, i)
                self.semobj[key] = s
                lst.append([key, 0])
            self.dma_sems[q] = [lst, 0]
        lst, idx = self.dma_sems[q]
        self.dma_sems[q][1] = (idx + 1) % len(lst)
        return lst[idx]

    def _deps(self, e, reads, writes):
        deps = {}

        def add(k, v):
            if deps.get(k, 0) < v:
                deps[k] = v
        for r in reads:
            ev = self.lastw.get(r)
            if ev is not None:
                add(*ev)
        for w in writes:
            ev = self.lastw.get(w)
            if ev is not None:
                add(*ev)
            for k, v in self.reads.get(w, {}).items():
                add(k, v)
        waits = []
        for k, v in deps.items():
            if k == ('e', e):
                if not self.self_sync or e in ('pe', 'sp'):
                    continue
            if self.seen[e].get(k, 0) >= v:
                continue
            self.seen[e][k] = v
            waits.append((self.semobj[k], v))
        return waits

    def _commit(self, ev, reads, writes):
        k, v = ev
        for w in writes:
            self.lastw[w] = ev
            self.reads[w] = {}
        for r in reads:
            d = self.reads.setdefault(r, {})
            if d.get(k, 0) < v:
                d[k] = v

    @staticmethod
    def _names(aps):
        out = []
        for a in aps:
            if a is None or isinstance(a, (int, float)):
                continue
            if isinstance(a, (str, tuple)):
                out.append(a)
            else:
                out.append(a.name)
        return out

    def op(self, e, fn, reads, writes):
        reads = self._names(reads)
        writes = self._names(writes)
        pr = [n for n in reads if isinstance(n, str) and n.startswith('bank')]
        if pr:
            reads = [n for n in reads if n not in pr]
            writes = writes + [n for n in pr if n not in writes]
        waits = self._deps(e, reads, writes)
        self.cnt[e] += 1
        seq = self.cnt[e]
        sem = self.sem[e]

        def emit(engobj, waits=waits, fn=fn, sem=sem):
            for s, v in waits:
                engobj.wait_ge(s, v)
            fn(engobj).then_inc(sem, 1)
        self.streams[e].append(emit)
        self._commit((('e', e), seq), reads, writes)
        self.n_inst += 1

    def dma(self, q, out, in_, r=None, w=None, **kw):
        reads = self._names(r if r is not None else [in_])
        writes = self._names(w if w is not None else [out])
        slot = self._dma_sem(q)
        key, prev = slot
        waits = self._deps(q, reads, writes)
        if prev > 0 and self.seen[q].get(key, 0) < prev:
            self.seen[q][key] = prev
            waits.append((self.semobj[key], prev))
        slot[1] = prev + 16
        tgt = slot[1]
        s = self.semobj[key]

        def emit(engobj, waits=waits, s=s, out=out, in_=in_, kw=kw):
            for ss, v in waits:
                engobj.wait_ge(ss, v)
            engobj.dma_start(out=out, in_=in_, **kw).then_inc(s, 16)
        self.streams[q].append(emit)
        self._commit((key, tgt), reads, writes)
        self.n_inst += 1

    def idma(self, out, in_, out_off=None, in_off=None, r=None, w=None):
        q = 'pool'
        reads = self._names(r if r is not None else [in_, out_off, in_off])
        writes = self._names(w if w is not None else [out])
        slot = self._dma_sem(q)
        key, prev = slot
        waits = self._deps(q, reads, writes)
        if prev > 0 and self.seen[q].get(key, 0) < prev:
            self.seen[q][key] = prev
            waits.append((self.semobj[key], prev))
        slot[1] = prev + 16
        tgt = slot[1]
        s = self.semobj[key]

        def emit(engobj, waits=waits, s=s):
            for ss, v in waits:
                engobj.wait_ge(ss, v)
            oo = bass.IndirectOffsetOnAxis(ap=out_off, axis=0) if out_off is not None else None
            io = bass.IndirectOffsetOnAxis(ap=in_off, axis=0) if in_off is not None else None
            engobj.indirect_dma_start(out=out, out_offset=oo, in_=in_, in_offset=io).then_inc(s, 16)
        self.streams[q].append(emit)
        self._commit((key, tgt), reads, writes)
        self.n_inst += 1

    def cond_begin(self, flag_ap):
        st = {'cnt0': dict(self.cnt), 'seen0': {e: dict(d) for e, d in self.seen.items()}, 'dma0': {}, 'ctx': {}, 'used': {}}
        for q, (lst, _) in self.dma_sems.items():
            for key, val in lst:
                st['dma0'][key] = val
        self._cond = st
        if not hasattr(self, '_cregs'):
            self._cregs = {}
        for e in self.eng:
            waits = self._deps(e, self._names([flag_ap]), [])

            def emit(engobj, waits=waits, e=e, st=st):
                if not st['used'].get(e):
                    return
                for s_, v in waits:
                    engobj.wait_ge(s_, v)
                if e not in self._cregs:
                    self._cregs[e] = engobj.alloc_register(f"cflag_{e}")
                r = self._cregs[e]
                engobj.reg_load(r, flag_ap)
                v = engobj.snap(r)
                ctx = engobj.If(v == 1)
                ctx.__enter__()
                st['ctx'][e] = (ctx, v)
            self.streams[e].append(emit)

    def cond_end(self):
        st = self._cond
        for e in self.eng:
            n_e = self.cnt[e] - st['cnt0'].get(e, 0)
            comp = []
            if e in self.dma_sems:
                for key, val in self.dma_sems[e][0]:
                    d = val - st['dma0'].get(key, 0)
                    if d:
                        comp.append((self.semobj[key], d, st['dma0'].get(key, 0)))
            st['used'][e] = bool(n_e or comp)

            def emit(engobj, e=e, n_e=n_e, comp=comp, st=st):
                if not st['used'].get(e):
                    return
                ctx, v = st['ctx'][e]
                ctx.__exit__(None, None, None)
                with engobj.Else():
                    engobj.drain()
                    for so, d, before in comp:
                        if before > 0:
                            engobj.wait_ge(so, before)
                        engobj.sem_inc(so, d)
                    if n_e:
                        engobj.sem_inc(self.sem[e], n_e)
                engobj.free_register(v.val)
            self.streams[e].append(emit)
        self.seen = st['seen0']
        self._cond = None

    def barrier(self):
        cur = {}
        for e in self.eng:
            if self.cnt[e] > 0:
                cur[('e', e)] = self.cnt[e]
        for q, (lst, _) in self.dma_sems.items():
            for key, val in lst:
                if val > 0:
                    cur[key] = val
        for e in self.eng:
            waits = []
            for k, v in cur.items():
                if k == ('e', e):
                    continue
                if self.seen[e].get(k, 0) >= v:
                    continue
                self.seen[e][k] = v
                waits.append((self.semobj[k], v))

            def emit(engobj, waits=waits):
                for s, v in waits:
                    engobj.wait_ge(s, v)
            self.streams[e].append(emit)

    def finish(self):
        self.barrier()
        nc = self.nc
        with nc.Block() as block:
            @block.tensor
            def _(e):
                for f in self.streams['pe']:
                    f(e)

            @block.scalar
            def _(e):
                for f in self.streams['act']:
                    f(e)

            @block.vector
            def _(e):
                for f in self.streams['dve']:
                    f(e)

            @block.gpsimd
            def _(e):
                for f in self.streams['pool']:
                    f(e)

            @block.sync
            def _(e):
                for f in self.streams['sp']:
                    f(e)

    def mm(self, out, lhsT, rhs, start=True, stop=True, r=None, w=None):
        self.op('pe', lambda e: e.matmul(out, lhsT, rhs, start=start, stop=stop),
                r if r is not None else [lhsT, rhs], w if w is not None else [out])

    def tr(self, out, in_, ident, r=None, w=None):
        self.op('pe', lambda e: e.transpose(out=out, in_=in_, identity=ident),
                r if r is not None else [in_, ident], w if w is not None else [out])

    def act(self, out, in_, func, bias=None, scale=None, accum=None, r=None, w=None):
        kw = {}
        if bias is not None:
            kw['bias'] = bias
        if scale is not None:
            kw['scale'] = scale
        if accum is not None:
            kw['accum_out'] = accum
        self.op('act', lambda e: e.activation(out=out, in_=in_, func=func, **kw),
                r if r is not None else [in_, bias, scale], w if w is not None else [out, accum])

    def tt(self, eng, out, in0, in1, op, r=None, w=None):
        self.op(eng, lambda e: e.tensor_tensor(out=out, in0=in0, in1=in1, op=op),
                r if r is not None else [in0, in1], w if w is not None else [out])

    def ts(self, eng, out, in0, s1, s2, op0, op1=None, r=None, w=None):
        if op1 is None:
            fn = lambda e: e.tensor_scalar(out=out, in0=in0, scalar1=s1, scalar2=None, op0=op0)
        else:
            fn = lambda e: e.tensor_scalar(out=out, in0=in0, scalar1=s1, scalar2=s2, op0=op0, op1=op1)
        self.op(eng, fn, r if r is not None else [in0, s1, s2], w if w is not None else [out])

    def stt(self, out, in0, scalar, in1, op0, op1, r=None, w=None):
        self.op('dve', lambda e: e.scalar_tensor_tensor(out=out, in0=in0, scalar=scalar, in1=in1, op0=op0, op1=op1),
                r if r is not None else [in0, scalar, in1], w if w is not None else [out])

    def copy(self, eng, out, in_, r=None, w=None):
        if eng == 'act':
            fn = lambda e: e.copy(out=out, in_=in_)
        else:
            fn = lambda e: e.tensor_copy(out=out, in_=in_)
        self.op(eng, fn, r if r is not None else [in_], w if w is not None else [out])

    def memset(self, eng, ap, val):
        self.op(eng, lambda e: e.memset(ap, val), [], [ap])


def bc(ap, shape, axis):
    return ap.unsqueeze(axis).to_broadcast(shape)


class Arena:
    def __init__(self, nc, P, base=16512, top=229344):
        self.nc = nc; self.P = P; self.ptr = base; self.top = top; self.n = 0; self.peak = base

    def alloc(self, name, shape, dt=F32):
        size = 2 if dt == BF16 else 4
        n = 1
        for d in shape[1:]:
            n *= d
        nbytes = (n * size + 63) // 64 * 64
        self.n += 1
        t = self.nc.alloc_sbuf_tensor_at(f"{name}_{self.n}", list(shape), dt, offset=self.ptr)
        self.ptr += nbytes
        self.peak = max(self.peak, self.ptr)
        assert self.ptr <= self.top, f"SBUF arena overflow at {name}: {self.ptr} > {self.top}"
        return t

    def mark(self):
        return self.ptr

    def release(self, m):
        self.P.barrier()
        self.ptr = m


def build_program(stage=99, debug=False):
    nc = bass.Bass("TRN2", target_bir_lowering=False)
    P = Prog(nc)
    A = Arena(nc, P)
    dbg = {}

    def dram_in(name, shape):
        return nc.dram_tensor(name, shape, F32, kind="ExternalInput").ap()

    def scratch(name, shape, dt, force_internal=False):
        kind = "ExternalOutput" if (debug and not force_internal) else "Internal"
        return nc.dram_tensor(name, shape, dt, kind=kind).ap()

    def dbg_out(name, src_ap, shape, dt, r=None):
        if not debug:
            return
        t = nc.dram_tensor(name, shape, dt, kind="ExternalOutput").ap()
        P.dma('sp', t, src_ap, r=r)

    sb = A.alloc

    x = dram_in("x", [S, D])
    emb_ln_g = dram_in("emb_ln_g", [D]); emb_ln_b = dram_in("emb_ln_b", [D])
    w_in = dram_in("w_in", [D, NCOL])
    conv_qkv = dram_in("conv_qkv", [5, 3072])
    a_log = dram_in("a_log", [16]); dt_bias = dram_in("dt_bias", [16])
    dn_norm_g = dram_in("dn_norm_g", [128])
    w_a_o = dram_in("w_a_o", [D, D])
    b_glu = dram_in("b_glu", [2048]); conv_dw = dram_in("conv_dw", [31, D]); b_dw = dram_in("b_dw", [D])
    conv_ln_g = dram_in("conv_ln_g", [D]); conv_ln_b = dram_in("conv_ln_b", [D])
    w_b_o = dram_in("w_b_o", [D, D]); b_b_o = dram_in("b_b_o", [D]); b_gate = dram_in("b_gate", [2048])
    w_out = dram_in("w_out", [D, D])
    ln1_g = dram_in("ln1_g", [D]); ln1_b = dram_in("ln1_b", [D])
    w_router = dram_in("w_router", [D, 32]); b_router = dram_in("b_router", [32])
    w_gu = dram_in("w_gu", [32, D, 2048]); b_gu = dram_in("b_gu", [32, 2048])
    w_down = dram_in("w_down", [32, D, D]); b_down = dram_in("b_down", [32, D])
    ln2_g = dram_in("ln2_g", [D]); ln2_b = dram_in("ln2_b", [D])
    out = nc.dram_tensor("out", [S, D], F32, kind="ExternalOutput").ap()

    h0_d = scratch("h0_d", [S, D], F32)
    hT_d = scratch("hT_d", [D, S], BF16)
    ogT_d = scratch("ogT_d", [D, S], BF16)
    ybT_d = scratch("ybT_d", [D, S], BF16)
    gate_d = scratch("gate_d", [2048, S], BF16)
    h1_d = scratch("h1_d", [S, D], F32)
    st_d = scratch("st_d", [2, S], F32)
    h1b_d = scratch("h1b_d", [S, D], BF16)
    xs_d = scratch("xs_d", [32768, D], BF16, force_internal=True)
    ys_d = scratch("ys_d", [32768, D], F32, force_internal=True)

    hT_dv = hT_d.rearrange("(kc p) t -> p kc t", p=128)
    w_in_v = w_in.rearrange("(kc p) c -> p kc c", p=128)

    identf = sb("identf", [128, 128]); identb = sb("identb", [128, 128], BF16)
    onesf = sb("onesf", [128, 128]); onesb = sb("onesb", [128, 128], BF16)
    zerof = sb("zerof", [128, 512])
    P.memset('pool', onesf[:], 1.0)
    P.memset('pool', zerof[:], 0.0)
    P.memset('pool', identf[:], 1.0)
    P.op('pool', lambda e: e.affine_select(out=identf[:], in_=identf[:], pattern=[[-1, 128]], compare_op=ALU.is_equal,
                                           fill=0.0, base=0, channel_multiplier=1), [identf], [identf])
    P.copy('dve', identb[:], identf[:])
    P.copy('dve', onesb[:], onesf[:])
    eps_ln = sb("eps_ln", [128, 1])
    P.memset('pool', eps_ln[:], LN_EPS)
    bank = [nc.alloc_psum_tensor(f"bank{i}", [128, 512], F32) for i in range(8)]

    def bank_bf(i):
        return bank[i][:].bitcast(BF16)

    ln_st = sb("ln_st", [128, 12]); ln_mv = sb("ln_mv", [128, 2]); ln_rs = sb("ln_rs", [128, 1])
    lnp = {}

    def layer_norm_rows(t, o, np_=128):
        P.op('dve', lambda e: e.bn_stats(out=ln_st[:np_, 0:6], in_=t[:np_, 0:512]), [t], [ln_st])
        P.op('dve', lambda e: e.bn_stats(out=ln_st[:np_, 6:12], in_=t[:np_, 512:1024]), [t], [ln_st])
        P.op('dve', lambda e: e.bn_aggr(out=ln_mv[:np_, :], in_=ln_st[:np_, :]), [ln_st], [ln_mv])
        P.act(ln_rs[:np_, :], ln_mv[:np_, 1:2], AF.Sqrt, bias=eps_ln[:np_, 0:1])
        P.op('dve', lambda e: e.reciprocal(out=ln_rs[:np_, :], in_=ln_rs[:np_, :]), [ln_rs], [ln_rs])
        P.ts('dve', t[:np_, :], t[:np_, :], ln_mv[:np_, 0:1], ln_rs[:np_, 0:1], ALU.subtract, ALU.mult)
        P.tt('pool', t[:np_, :], t[:np_, :], lnp['g'][:np_, :], ALU.mult)
        P.tt('dve', o[:np_, :], t[:np_, :], lnp['b'][:np_, :], ALU.add)

    vp = sb("vp", [128, 64])
    base_mark = A.mark()
    lnp['g'] = sb("gB0", [128, D]); lnp['b'] = sb("bB0", [128, D])
    P.dma('sp', lnp['g'][:], emb_ln_g.partition_broadcast(128))
    P.dma('sp', lnp['b'][:], emb_ln_b.partition_broadcast(128))
    xt = [sb(f"xt{i}", [128, D]) for i in range(2)]
    ht = [sb(f"ht{i}", [128, D]) for i in range(2)]
    hTs = [sb(f"hTs{i}", [128, 8, 512], BF16) for i in range(2)]
    for i in range(32):
        xb = xt[i % 2]; hb = ht[i % 2]; hs = hTs[(i // 4) % 2]
        P.dma('sp', xb[:], x[i * 128:(i + 1) * 128, :])
        layer_norm_rows(xb, hb)
        P.dma('sp', h0_d[i * 128:(i + 1) * 128, :], hb[:], w=[('h0_d', i)])
        for half in range(2):
            pb = bank[half]
            for q4 in range(4):
                kc = half * 4 + q4
                P.tr(pb[:, q4 * 128:(q4 + 1) * 128], hb[:, kc * 128:(kc + 1) * 128], identf[:])
            P.copy('act', hs[:, half * 4:(half + 1) * 4, (i % 4) * 128:(i % 4 + 1) * 128],
                   pb[:].rearrange("p (k t) -> p k t", t=128))
        if i % 4 == 3:
            tt_ = i // 4
            P.dma('sp', hT_dv[:, :, tt_ * 512:(tt_ + 1) * 512], hs[:], w=[('hT_d', tt_)])
    A.release(base_mark)
    if stage <= 1:
        P.finish()
        return nc, dbg

    tri = {}; ntri = {}; negT = {}; nstrT = {}
    for d_, (cm, st_) in (('f', (-1, 1)), ('b', (1, -1))):
        t_ = sb(f"tri_{d_}", [64, 64]); nt_ = sb(f"ntri_{d_}", [64, 64])
        P.op('pool', lambda e, t_=t_, cm=cm, st_=st_: e.affine_select(out=t_[:], in_=onesf[0:64, 0:64], pattern=[[st_, 64]],
             compare_op=ALU.is_ge, fill=0.0, base=0, channel_multiplier=cm), [onesf], [t_])
        P.ts('dve', nt_[:], t_[:], -1.0, None, ALU.mult)
        tri[d_] = t_; ntri[d_] = nt_
        ng_ = sb(f"negT_{d_}", [64, 8, 64])
        P.op('pool', lambda e, ng_=ng_, cm=cm, st_=st_: e.affine_select(out=ng_[:], in_=zerof[0:64, 0:512].rearrange("p (c i) -> p c i", i=64),
             pattern=[[0, 8], [st_, 64]], compare_op=ALU.is_ge, fill=NEG_BIG, base=0, channel_multiplier=cm), [zerof], [ng_])
        negT[d_] = ng_
        ns_ = sb(f"nstrT_{d_}", [64, 64])
        P.op('pool', lambda e, ns_=ns_, cm=cm, st_=st_: e.affine_select(out=ns_[:], in_=onesf[0:64, 0:64], pattern=[[st_, 64]],
             compare_op=ALU.is_gt, fill=0.0, base=0, channel_multiplier=cm), [onesf], [ns_])
        P.ts('dve', ns_[:], ns_[:], -1.0, None, ALU.mult)
        nstrT[d_] = ns_
    BETA = {d_: sb(f"BETA_{d_}", [64, 8, 64]) for d_ in 'fb'}
    LA = {d_: sb(f"LA_{d_}", [64, 8, 64]) for d_ in 'fb'}
    hTt = [sb(f"hTt{i}", [128, 8, 512], BF16) for i in range(2)]
    hcnt = [0]

    def load_hT(tt_):
        hb = hTt[hcnt[0] % 2]; hcnt[0] += 1
        P.dma('sp', hb[:], hT_dv[:, :, tt_ * 512:(tt_ + 1) * 512], r=[('hT_d', tt_)])
        return hb

    s2_mark = A.mark()
    SMraw = sb("SMraw", [64, 64, 32])
    Wsm = sb("Wsm", [128, 8, 32], BF16)
    alB = sb("alB", [64, 16]); dtB = sb("dtB", [64, 16]); nea = sb("nea", [64, 16])
    X_ = sb("spX", [64, 8, 64]); A_ = sb("spA", [64, 8, 64])
    P.dma('pool', Wsm[:], w_in_v[:, :, COL_SM:COL_SM + 32])
    P.dma('sp', alB[:], a_log.partition_broadcast(64))
    P.dma('sp', dtB[:], dt_bias.partition_broadcast(64))
    P.act(nea[:], alB[:], AF.Exp)
    P.ts('dve', nea[:], nea[:], -1.0, None, ALU.mult)
    for tt_ in range(8):
        hb = load_hT(tt_)
        pb = bank[2 + tt_ % 2]
        for cc in range(8):
            for kc in range(8):
                P.mm(pb[0:64, cc * 32:(cc + 1) * 32], hb[:, kc, cc * 64:(cc + 1) * 64], Wsm[:, kc, :], start=(kc == 0), stop=(kc == 7))
        P.copy('dve', SMraw[:, tt_ * 8:(tt_ + 1) * 8, :], pb[0:64, 0:256].rearrange("p (c k) -> p c k", k=32))
    for di, d_ in enumerate('fb'):
        ub = SMraw[:, :, di * 8:(di + 1) * 8].rearrange("p c h -> p h c")
        ua = SMraw[:, :, 16 + di * 8:24 + di * 8].rearrange("p c h -> p h c")
        P.act(BETA[d_][:], ub, AF.Sigmoid)
        P.tt('dve', X_[:], ua, bc(dtB[:, di * 8:(di + 1) * 8], [64, 8, 64], 2), ALU.add)
        P.stt(A_[:], X_[:], -1.0, X_[:], ALU.mult, ALU.max)
        P.act(A_[:], A_[:], AF.Exp, scale=-1.0)
        P.act(A_[:], A_[:], AF.Ln, bias=1.0)
        P.stt(A_[:], X_[:], 0.0, A_[:], ALU.max, ALU.add)
        P.tt('dve', LA[d_][:], A_[:], bc(nea[:, di * 8:(di + 1) * 8], [64, 8, 64], 2), ALU.mult)
    if debug:
        for d_ in 'fb':
            dbg_out(f"dbg_beta_{d_}", BETA[d_][:], [64, 8, 64], F32)
            dbg_out(f"dbg_la_{d_}", LA[d_][:], [64, 8, 64], F32)
    A.release(s2_mark)
    if stage <= 2:
        P.finish()
        return nc, dbg


    nheads = int(os.environ.get("MK_NHEADS", "8")) if debug else 8
    head_mark = A.mark()
    cst = sb("cst", [120, 128]); cwq = sb("cwq", [128, 120])
    P.dma('sp', cst[:], conv_qkv.rearrange("t (c p) -> (t c) p", p=128))
    P.tr(bank[0][:, 0:120], cst[:, :], identf[0:120, 0:120])
    P.copy('dve', cwq[:], bank[0][:, 0:120])
    eps_l2 = sb("eps_l2", [128, 1]); P.memset('pool', eps_l2[:], L2_EPS)
    eps_rms = sb("eps_rms", [128, 1]); P.memset('pool', eps_rms[:], RMS_EPS)
    ngB = sb("ngB", [64, 128]); P.dma('sp', ngB[:], dn_norm_g.partition_broadcast(64))
    Wg = [sb(f"Wg{i}", [128, 8, 128], BF16) for i in range(2)]
    U = sb("U", [128, 4100], BF16)
    P.memset('pool', U[:, 0:2], 0.0); P.memset('pool', U[:, 4098:4100], 0.0)
    C = sb("C", [128, 4096], BF16)
    Dg = sb("Dg", [128, 5, 128], BF16)
    sqb = sb("sqb", [128, 512], BF16); lnb = sb("lnb", [128, 512])
    qT = sb("qT", [128, 4096], BF16); kT = sb("kT", [128, 4096], BF16)
    Ktok = sb("Ktok", [64, 64, 128], BF16); Vtok = sb("Vtok", [64, 64, 128], BF16)
    O = sb("O", [64, 64, 128])
    qkT = {d_: sb(f"qkT_{d_}", [64, 64, 64], BF16) for d_ in 'fb'}
    TT = {d_: sb(f"TT_{d_}", [64, 64, 64], BF16) for d_ in 'fb'}
    Gd = {d_: sb(f"Gd_{d_}", [64, 64]) for d_ in 'fb'}
    EG = {d_: sb(f"EG_{d_}", [64, 64]) for d_ in 'fb'}
    NEG = {d_: sb(f"NEG_{d_}", [64, 64]) for d_ in 'fb'}
    TAIL = {d_: sb(f"TAIL_{d_}", [64, 64]) for d_ in 'fb'}
    CD = {d_: sb(f"CD_{d_}", [128, 64]) for d_ in 'fb'}
    LAm = sb("LAm", [64, 8, 64]); LAb = sb("LAb", [64, 8, 64])
    decT = {d_: sb(f"decT_{d_}", [64, 8, 64]) for d_ in 'fb'}; tY = {d_: sb(f"tY_{d_}", [64, 8, 64]) for d_ in 'fb'}
    Yb = {d_: [sb(f"Yb{d_}{i}", [64, 8, 64], BF16) for i in range(2)] for d_ in 'fb'}
    Zb = {d_: [sb(f"Zb{d_}{i}", [64, 8, 64], BF16) for i in range(2)] for d_ in 'fb'}
    Gm = {d_: [sb(f"Gm{d_}{i}", [64, 8, 64], BF16) for i in range(2)] for d_ in 'fb'}
    Sst = {d_: sb(f"S_{d_}", [128, 128]) for d_ in 'fb'}
    Sbf = {d_: sb(f"Sb_{d_}", [128, 128], BF16) for d_ in 'fb'}
    Rt = {d_: sb(f"R_{d_}", [64, 128], BF16) for d_ in 'fb'}
    vnew = {d_: sb(f"vnew_{d_}", [64, 128], BF16) for d_ in 'fb'}
    tq = {d_: sb(f"tq_{d_}", [64, 128]) for d_ in 'fb'}
    ktl = {d_: sb(f"ktl_{d_}", [64, 128], BF16) for d_ in 'fb'}
    otmp = sb("otmp", [64, 128])
    ssq = sb("ssq", [64, 64])
    wcnt = [0]; pcnt = [0]

    def nextbank():
        b_ = bank[pcnt[0] % 2]; pcnt[0] += 1
        return b_

    def inproj_chunk(col0, evac):
        wb = Wg[wcnt[0] % 2]; wcnt[0] += 1
        P.dma('pool', wb[:], w_in_v[:, :, col0:col0 + 128])
        for tt_ in range(8):
            hb = load_hT(tt_)
            pb = nextbank()
            for kc in range(8):
                P.mm(pb[:, :], wb[:, kc, :], hb[:, kc, :], start=(kc == 0), stop=(kc == 7))
            evac(tt_, pb)

    def evac_U(tt_, pb):
        P.copy('act', U[:, 2 + tt_ * 512:2 + (tt_ + 1) * 512], pb[:, :])

    def conv_silu(ci):
        for t in range(5):
            P.ts('dve', Dg[:, t, :], identb[:], cwq[:, t * 24 + ci:t * 24 + ci + 1], None, ALU.mult)
        for tt_ in range(8):
            pb = nextbank()
            for t in range(5):
                P.mm(pb[:, :], Dg[:, t, :], U[:, tt_ * 512 + t:tt_ * 512 + t + 512], start=(t == 0), stop=(t == 4))
            P.act(C[:, tt_ * 512:(tt_ + 1) * 512], pb[:, :], AF.Silu)

    def l2norm_to(dst, scale):
        for tt_ in range(8):
            sl = slice(tt_ * 512, (tt_ + 1) * 512)
            P.act(sqb[:], C[:, sl], AF.Square)
            pb = nextbank()
            P.mm(pb[:, :], onesb[:], sqb[:])
            P.act(lnb[:], pb[:, :], AF.Ln, bias=eps_l2[:, 0:1])
            P.act(lnb[:], lnb[:], AF.Exp, scale=-0.5)
            P.stt(dst[:, sl], C[:, sl], scale, lnb[:], ALU.mult, ALU.mult)

    def to_tokmajor(src, dst):
        for g in range(8):
            bi = 2 + g % 2
            pbf = bank_bf(bi)
            for cc in range(8):
                c = g * 8 + cc
                P.tr(pbf[0:64, cc * 128:(cc + 1) * 128], src[:, c * 64:(c + 1) * 64], identb[:])
            P.copy('dve' if g % 2 == 0 else 'act', dst[:, g * 8:(g + 1) * 8, :], pbf[0:64, :].rearrange("p (c d) -> p c d", d=128))

    def flat(t3):
        return t3[:].rearrange("p c i -> p (c i)")

    for h in range(nheads):
        inproj_chunk(COL_Q + h * 128, evac_U); conv_silu(h); l2norm_to(qT, 128.0 ** -0.5)
        inproj_chunk(COL_K + h * 128, evac_U); conv_silu(8 + h); l2norm_to(kT, 1.0)
        to_tokmajor(kT, Ktok)
        inproj_chunk(COL_V + h * 128, evac_U); conv_silu(16 + h)
        to_tokmajor(C, Vtok)
        if debug and h == 0:
            dbg_out("dbg_qT", qT[:], [128, 4096], BF16)
            dbg_out("dbg_kT", kT[:], [128, 4096], BF16)
            dbg_out("dbg_Ktok", Ktok[:], [64, 64, 128], BF16)
            dbg_out("dbg_Vtok", Vtok[:], [64, 64, 128], BF16)
        if stage <= 3:
            continue
        for d_ in 'fb':
            la_h = LA[d_][:, h, :]
            P.mm(bank[4][0:64, 0:64], tri[d_][:], la_h)
            P.copy('dve', Gd[d_][:], bank[4][0:64, 0:64])
            P.mm(bank[5][:, 0:64], onesf[0:64, :], la_h)
            P.act(CD[d_][:], bank[5][:, 0:64], AF.Exp)
            P.tt('dve', TAIL[d_][:], bank[5][0:64, 0:64], Gd[d_][:], ALU.subtract)
            P.act(TAIL[d_][:], TAIL[d_][:], AF.Exp)
            P.act(EG[d_][:], Gd[d_][:], AF.Exp)
            P.ts('dve', NEG[d_][:], EG[d_][:], -1.0, None, ALU.mult)
        for g in range(8):
            c0 = g * 8
            for cc in range(8):
                ch = slice((c0 + cc) * 64, (c0 + cc + 1) * 64)
                P.mm(bank[4][0:64, cc * 64:(cc + 1) * 64], kT[:, ch], kT[:, ch])
            for cc in range(8):
                ch = slice((c0 + cc) * 64, (c0 + cc + 1) * 64)
                P.mm(bank[5][0:64, cc * 64:(cc + 1) * 64], kT[:, ch], qT[:, ch])
            bk = {'f': (2, 3, 7), 'b': (0, 1, 6)}
            for d_ in 'fb':
                la_g = LA[d_][:, h, c0:c0 + 8]
                P.tt('pool', LAm[:], bc(la_g, [64, 8, 64], 2), bc(tri[d_][:], [64, 8, 64], 1), ALU.mult)
                P.copy('pool', LAb[:], bc(la_g, [64, 8, 64], 2))
                P.mm(bank[6][0:64, :], onesf[0:64, 0:64], flat(LAm), start=True, stop=False)
                P.mm(bank[6][0:64, :], ntri[d_][:], flat(LAb), start=False, stop=False)
                P.mm(bank[6][0:64, :], identf[0:64, 0:64], flat(negT[d_]), start=False, stop=True)
                P.act(flat(decT[d_]), bank[6][0:64, :], AF.Exp)
            for d_ in 'fb':
                P.tt('dve', flat(qkT[d_])[:, c0 * 64:(c0 + 8) * 64], bank[5][0:64, :], flat(decT[d_]), ALU.mult)
                P.tt('dve', flat(tY[d_]), bank[4][0:64, :], flat(decT[d_]), ALU.mult)
                P.tt('pool', tY[d_][:], tY[d_][:], bc(BETA[d_][:, h, c0:c0 + 8], [64, 8, 64], 2), ALU.mult)
                P.tt('dve', tY[d_][:], tY[d_][:], bc(nstrT[d_][:], [64, 8, 64], 1), ALU.mult)
                P.copy('act', Yb[d_][0][:], tY[d_][:])
                P.tt('pool', Gm[d_][0][:], tY[d_][:], bc(identf[0:64, 0:64], [64, 8, 64], 1), ALU.add)
            for d_ in 'fb':
                z0 = bank_bf(bk[d_][2])
                for cc in range(8):
                    P.tr(z0[0:64, cc * 64:(cc + 1) * 64], Yb[d_][0][:, cc, :], identb[0:64, 0:64])
                P.copy('dve' if d_ == 'f' else 'act', flat(Zb[d_][0]), z0[0:64, 0:512])
            for k in range(5):
                cur = k % 2; nxt = 1 - cur
                for d_ in 'fb':
                    by_, bz_, bg_ = bk[d_]
                    if k < 4:
                        for cc in range(8):
                            P.mm(bank[by_][0:64, cc * 64:(cc + 1) * 64], Zb[d_][cur][:, cc, :], Yb[d_][cur][:, cc, :])
                    for cc in range(8):
                        P.mm(bank[bz_][0:64, cc * 64:(cc + 1) * 64], Yb[d_][cur][:, cc, :], Zb[d_][cur][:, cc, :])
                for d_ in 'fb':
                    by_, bz_, bg_ = bk[d_]
                    if k < 4:
                        P.copy('act', flat(Yb[d_][nxt]), bank[by_][0:64, :])
                    P.copy('dve', flat(Zb[d_][nxt]), bank[bz_][0:64, :])
                for d_ in 'fb':
                    by_, bz_, bg_ = bk[d_]
                    for cc in range(8):
                        P.mm(bank[bg_][0:64, cc * 64:(cc + 1) * 64], Zb[d_][nxt][:, cc, :], Gm[d_][cur][:, cc, :])
                for d_ in 'fb':
                    by_, bz_, bg_ = bk[d_]
                    dstG = flat(Gm[d_][nxt]) if k < 4 else flat(TT[d_])[:, c0 * 64:(c0 + 8) * 64]
                    P.tt('dve' if d_ == 'f' else 'pool', dstG, bank[bg_][0:64, :], flat(Gm[d_][cur]), ALU.add) if d_ == 'f' else P.tt('dve', dstG, bank[bg_][0:64, :], flat(Gm[d_][cur]), ALU.add)
        if debug and h == 0:
            for d_ in 'fb':
                dbg_out(f"dbg_TT_{d_}", TT[d_][:], [64, 64, 64], BF16)
                dbg_out(f"dbg_qkT_{d_}", qkT[d_][:], [64, 64, 64], BF16)
        if stage <= 4:
            continue
        for d_ in 'fb':
            P.memset('pool', Sst[d_][:], 0.0)
            P.memset('pool', Sbf[d_][:], 0.0)
        nsteps = int(os.environ.get('MK_STEPS', '64')) if debug else 64
        for s_ in range(nsteps):
            cs = {'f': s_, 'b': 63 - s_}
            pv = {}
            for di, d_ in enumerate('fb'):
                bA = bank[di * 3]; bB_ = bank[di * 3 + 1]; bC = bank[di * 3 + 2]
                pv[d_] = (bA[0:64, 0:128], bB_[0:64, 0:128], bA[0:64, 128:256], bB_[0:64, 128:256], bC[:, 0:128])
            for d_ in 'fb':
                c = cs[d_]; ch = slice(c * 64, (c + 1) * 64); ps1, ps2, ps3, ps4, ps5 = pv[d_]
                P.mm(ps1, kT[:, ch], Sbf[d_][:])
                P.mm(ps2, qT[:, ch], Sbf[d_][:])
            for d_ in 'fb':
                c = cs[d_]; ps1, ps2, ps3, ps4, ps5 = pv[d_]
                P.stt(Rt[d_][:], ps1, NEG[d_][:, c:c + 1], Vtok[:, c, :], ALU.mult, ALU.add)
                P.act(ktl[d_][:], Ktok[:, c, :], AF.Identity, scale=TAIL[d_][:, c:c + 1])
                P.act(tq[d_][:], ps2, AF.Identity, scale=EG[d_][:, c:c + 1])
            for d_ in 'fb':
                c = cs[d_]; ps1, ps2, ps3, ps4, ps5 = pv[d_]
                P.mm(ps3, TT[d_][:, c, :], Rt[d_][:])
            for d_ in 'fb':
                c = cs[d_]; ps1, ps2, ps3, ps4, ps5 = pv[d_]
                P.act(vnew[d_][:], ps3, AF.Identity, scale=BETA[d_][:, h, c:c + 1])
            for d_ in 'fb':
                c = cs[d_]; ps1, ps2, ps3, ps4, ps5 = pv[d_]
                P.mm(ps5, ktl[d_][:], vnew[d_][:])
                P.mm(ps4, qkT[d_][:, c, :], vnew[d_][:])
            for d_ in 'fb':
                c = cs[d_]; ps1, ps2, ps3, ps4, ps5 = pv[d_]
                P.stt(Sbf[d_][:], Sst[d_][:], CD[d_][:, c:c + 1], ps5, ALU.mult, ALU.add)
            for d_ in 'fb':
                c = cs[d_]; ps1, ps2, ps3, ps4, ps5 = pv[d_]
                P.stt(Sst[d_][:], Sst[d_][:], CD[d_][:, c:c + 1], ps5, ALU.mult, ALU.add)
                first = (d_ == 'f' and c < 32) or (d_ == 'b' and c >= 32)
                if first:
                    P.tt('dve', O[:, c, :], tq[d_][:], ps4, ALU.add)
                else:
                    P.tt('dve', otmp[:], tq[d_][:], ps4, ALU.add)
                    P.tt('pool', O[:, c, :], O[:, c, :], otmp[:], ALU.add)
        if debug and h == 0:
            dbg_out("dbg_O", O[:], [64, 64, 128], F32)
            dbg_out("dbg_S_f", Sst['f'][:], [128, 128], F32)
            dbg_out("dbg_vnew_f", vnew['f'][:], [64, 128], BF16)
            dbg_out("dbg_R_f", Rt['f'][:], [64, 128], BF16)
            dbg_out("dbg_tq_f", tq['f'][:], [64, 128], F32)
            dbg_out("dbg_ktl_f", ktl['f'][:], [64, 128], BF16)
            dbg_out("dbg_CD_f", CD['f'][:], [128, 64], F32)
            dbg_out("dbg_TAIL_f", TAIL['f'][:], [64, 64], F32)
            dbg_out("dbg_EG_f", EG['f'][:], [64, 64], F32)
        Zs = Ktok
        wz = Wg[wcnt[0] % 2]; wcnt[0] += 1
        P.dma('pool', wz[:], w_in_v[:, :, COL_Z + h * 128:COL_Z + (h + 1) * 128])
        for tt_ in range(8):
            hb = load_hT(tt_)
            for hf in range(2):
                pb = nextbank()
                for c4 in range(4):
                    cc = hf * 4 + c4
                    for kc in range(8):
                        P.mm(pb[0:64, c4 * 128:(c4 + 1) * 128], hb[:, kc, cc * 64:(cc + 1) * 64], wz[:, kc, :], start=(kc == 0), stop=(kc == 7))
                c_lo = tt_ * 8 + hf * 4
                P.act(Zs[:, c_lo:c_lo + 4, :], pb[0:64, :].rearrange("p (c d) -> p c d", d=128), AF.Silu)
        SQ = Vtok
        P.tt('dve', SQ[:], O[:], O[:], ALU.mult)
        P.op('dve', lambda e: e.tensor_reduce(out=ssq[:], in_=SQ[:], axis=AX.X, op=ALU.add), [SQ], [ssq])
        P.act(ssq[:], ssq[:], AF.Ln, bias=eps_rms[0:64, 0:1], scale=1.0 / 128.0)
        P.act(ssq[:], ssq[:], AF.Exp, scale=-0.5)
        P.tt('dve', O[:], O[:], bc(ssq[:], [64, 64, 128], 2), ALU.mult)
        P.tt('pool', O[:], O[:], bc(ngB[:], [64, 64, 128], 1), ALU.mult)
        OGb = Vtok
        P.tt('dve', OGb[:], O[:], Zs[:], ALU.mult)
        for g in range(8):
            bi = 2 + g % 2
            pbf = bank_bf(bi)
            for cc in range(8):
                P.tr(pbf[:, cc * 64:(cc + 1) * 64], OGb[:, g * 8 + cc, :], identb[0:64, 0:64])
            P.copy('act', C[:, g * 512:(g + 1) * 512], pbf[:, 0:512])
        P.dma('sp', ogT_d[h * 128:(h + 1) * 128, :], C[:], w=[('ogT_d', h)])
    if stage <= 5:
        P.finish()
        return nc, dbg

    A.release(head_mark)
    vst = sb("vst", [64, 128])
    for r0, src, n in ((0, b_glu, 16), (16, b_gate, 16), (32, b_dw, 8), (40, conv_ln_g, 8), (48, conv_ln_b, 8), (56, b_b_o, 8)):
        P.dma('sp', vst[r0:r0 + n, :], src.rearrange("(c p) -> c p", p=128))
    P.tr(bank[0][:, 0:64], vst[:, :], identf[0:64, 0:64])
    P.copy('dve', vp[:], bank[0][:, 0:64])
    VB_GLU, VB_GATE, VB_DW, VB_LNG, VB_LNB, VB_BBO = 0, 16, 32, 40, 48, 56
    cdst1 = sb("cdst1", [128, 128]); cdst2 = sb("cdst2", [120, 128]); cwd = sb("cwd", [128, 248])
    cdv = conv_dw.rearrange("t (c p) -> (t c) p", p=128)
    P.dma('sp', cdst1[:], cdv[0:128, :])
    P.dma('sp', cdst2[:], cdv[128:248, :])
    P.tr(bank[1][:, 0:128], cdst1[:, :], identf[:, :])
    P.copy('dve', cwd[:, 0:128], bank[1][:, 0:128])
    P.tr(bank[1][:, 128:248], cdst2[:, :], identf[0:120, 0:120])
    P.copy('dve', cwd[:, 128:248], bank[1][:, 128:248])
    Wg4 = [sb(f"Wg4_{i}", [128, 8, 512], BF16) for i in range(2)]
    GA = [sb(f"GA{i}", [128, 4096], BF16) for i in range(2)]
    Ug = [sb(f"Ug{i}", [128, 4096 + 30], BF16) for i in range(2)]
    for u_ in Ug:
        P.memset('pool', u_[:, 0:15], 0.0); P.memset('pool', u_[:, 4096 + 15:4096 + 30], 0.0)
    Ycb = [sb(f"Ycb{i}", [128, 4096], BF16) for i in range(4)]
    Dg31 = sb("Dg31", [128, 31, 128], BF16)
    sgt = sb("sgt", [128, 512]); sq2 = sb("sq2", [128, 512], BF16)
    SUM = sb("SUM", [1, 4096]); SSQ = sb("SSQ", [1, 4096])
    w4cnt = [0]

    def inproj_group(cols, evacs):
        wb = Wg4[w4cnt[0] % 2]; w4cnt[0] += 1
        for jj, c0_ in enumerate(cols):
            P.dma('pool', wb[:, :, jj * 128:(jj + 1) * 128], w_in_v[:, :, c0_:c0_ + 128], w=[f"{wb.name}_{jj}"])
        for tt_ in range(8):
            hb = load_hT(tt_)
            for jj in range(len(cols)):
                pb = nextbank()
                for kc in range(8):
                    P.mm(pb[:, :], wb[:, kc, jj * 128:(jj + 1) * 128], hb[:, kc, :], start=(kc == 0), stop=(kc == 7),
                         r=[f"{wb.name}_{jj}", hb])
                evacs[jj](tt_, pb)

    for g4 in range(4):
        evs = []
        for jj in range(4):
            cg = g4 * 4 + jj

            def evac_gate(tt_, pb, cg=cg, jj=jj):
                P.act(Ycb[jj][:, tt_ * 512:(tt_ + 1) * 512], pb[:, :], AF.Sigmoid, bias=vp[:, VB_GATE + cg:VB_GATE + cg + 1])
            evs.append(evac_gate)
        inproj_group([COL_GATE + (g4 * 4 + jj) * 128 for jj in range(4)], evs)
        for jj in range(4):
            cg = g4 * 4 + jj
            P.dma('sp', gate_d[cg * 128:(cg + 1) * 128, :], Ycb[jj][:], w=[('gate_d', cg)])
    for j2 in range(4):
        evs = []; cols = []
        for u in range(2):
            j = j2 * 2 + u

            def evac_a(tt_, pb, j=j, u=u):
                P.act(GA[u][:, tt_ * 512:(tt_ + 1) * 512], pb[:, :], AF.Identity, bias=vp[:, VB_GLU + j:VB_GLU + j + 1])

            def evac_g(tt_, pb, j=j, u=u):
                P.act(sgt[:], pb[:, :], AF.Sigmoid, bias=vp[:, VB_GLU + 8 + j:VB_GLU + 8 + j + 1])
                P.tt('dve', Ug[u][:, 15 + tt_ * 512:15 + (tt_ + 1) * 512], GA[u][:, tt_ * 512:(tt_ + 1) * 512], sgt[:], ALU.mult)
            cols += [COL_GLU + j * 128, COL_GLU + 1024 + j * 128]
            evs += [evac_a, evac_g]
        inproj_group(cols, evs)
        for u in range(2):
            j = j2 * 2 + u
            yc = Ycb[u]
            for t in range(31):
                P.ts('dve' if t % 2 == 0 else 'pool', Dg31[:, t, :], identb[:], cwd[:, t * 8 + j:t * 8 + j + 1], None, ALU.mult)
            for tt_ in range(8):
                pb = nextbank()
                for t in range(31):
                    P.mm(pb[:, :], Dg31[:, t, :], Ug[u][:, tt_ * 512 + t:tt_ * 512 + t + 512], start=(t == 0), stop=(t == 30))
                sl = slice(tt_ * 512, (tt_ + 1) * 512)
                P.act(yc[:, sl], pb[:, :], AF.Identity, bias=vp[:, VB_DW + j:VB_DW + j + 1])
                P.act(sq2[:], pb[:, :], AF.Square, bias=vp[:, VB_DW + j:VB_DW + j + 1])
                P.mm(bank[2][0:1, :], onesb[:, 0:1], yc[:, sl])
                P.mm(bank[3][0:1, :], onesb[:, 0:1], sq2[:])
                if j == 0:
                    P.copy('dve', SUM[:, sl], bank[2][0:1, :])
                    P.copy('dve', SSQ[:, sl], bank[3][0:1, :])
                else:
                    P.tt('dve', SUM[:, sl], SUM[:, sl], bank[2][0:1, :], ALU.add)
                    P.tt('dve', SSQ[:, sl], SSQ[:, sl], bank[3][0:1, :], ALU.add)
            P.dma('sp', ybT_d[j * 128:(j + 1) * 128, :], yc[:], w=[('ybT_d', j)])
    stt_ = sb("stt_", [1, 512])
    for tt_ in range(8):
        sl = slice(tt_ * 512, (tt_ + 1) * 512)
        P.ts('dve', SUM[:, sl], SUM[:, sl], 1.0 / 1024.0, None, ALU.mult)
        P.tt('dve', stt_[:], SUM[:, sl], SUM[:, sl], ALU.mult)
        P.stt(SSQ[:, sl], SSQ[:, sl], 1.0 / 1024.0, stt_[:], ALU.mult, ALU.subtract)
        P.act(SSQ[:, sl], SSQ[:, sl], AF.Ln, bias=eps_ln[0:1, 0:1])
        P.act(SSQ[:, sl], SSQ[:, sl], AF.Exp, scale=-0.5)
        P.stt(SUM[:, sl], SUM[:, sl], -1.0, SSQ[:, sl], ALU.mult, ALU.mult)
    RSTD = SSQ; NMR = SUM
    P.dma('sp', st_d[0:1, :], RSTD[:])
    P.dma('sp', st_d[1:2, :], NMR[:])
    s5_mark = A.mark()
    if stage <= 6:
        P.finish()
        return nc, dbg

    A.release(base_mark)
    LG = sb("LG", [128, 32, 32]); M8 = sb("M8", [128, 32, 8]); MSK = sb("MSK", [128, 32, 32], BF16); G4 = sb("G4", [128, 32, 4])
    moe_mark = A.mark()
    Wao = sb("Wao", [128, 8, 1024], BF16); Wbo = sb("Wbo", [128, 8, 1024], BF16); Wout = sb("Wout", [128, 8, 1024], BF16)
    P.dma('pool', Wao[:], w_a_o.rearrange("(kc p) c -> p kc c", p=128))
    P.dma('pool', Wbo[:], w_b_o.rearrange("(kc p) c -> p kc c", p=128))
    P.dma('pool', Wout[:], w_out.rearrange("(kc p) c -> p kc c", p=128))
    Wr = sb("Wr", [128, 8, 32]); brB = sb("brB", [128, 32])
    P.dma('sp', Wr[:], w_router.rearrange("(kc p) c -> p kc c", p=128))
    P.dma('sp', brB[:], b_router.partition_broadcast(128))
    lnp['g'] = sb("gB1", [128, D]); lnp['b'] = sb("bB1", [128, D])
    P.dma('sp', lnp['g'][:], ln1_g.partition_broadcast(128))
    P.dma('sp', lnp['b'][:], ln1_b.partition_broadcast(128))
    ogt = sb("ogt", [128, 8, 512], BF16); ybt = sb("ybt", [128, 8, 512], BF16); gtt = sb("gtt", [128, 16, 512], BF16)
    ybaT = sb("ybaT", [128, 8, 512], BF16); mixT = sb("mixT", [128, 8, 512], BF16)
    t1 = sb("t1", [128, 512]); t2 = sb("t2", [128, 512])
    rs_t = sb("rs_t", [1, 512]); nm_t = sb("nm_t", [1, 512])
    h0t = sb("h0t", [128, D]); rt = sb("rt", [128, D]); h1t = sb("h1t", [128, D]); h1Tf = sb("h1Tf", [128, 8, 128])
    nmx = sb("nmx", [128, 1]); esum = sb("esum", [128, 1]); h1tb = sb("h1tb", [128, D], BF16)
    ogv = ogT_d.rearrange("(j p) t -> p j t", p=128)
    ybv = ybT_d.rearrange("(j p) t -> p j t", p=128)
    gtv = gate_d.rearrange("(j p) t -> p j t", p=128)
    for tt_ in range(8):
        sl = slice(tt_ * 512, (tt_ + 1) * 512)
        P.dma('sp', ogt[:], ogv[:, :, sl], r=[('ogT_d', j_) for j_ in range(8)])
        P.dma('sp', ybt[:], ybv[:, :, sl], r=[('ybT_d', j_) for j_ in range(8)])
        P.dma('sp', gtt[:], gtv[:, :, sl], r=[('gate_d', j_) for j_ in range(16)])
        P.dma('sp', rs_t[:], st_d[0:1, sl])
        P.dma('sp', nm_t[:], st_d[1:2, sl])
        P.mm(bank[6][:, :], onesf[0:1, :], rs_t[0:1, :])
        P.mm(bank[7][:, :], onesf[0:1, :], nm_t[0:1, :])
        for j in range(8):
            P.tt('dve', t1[:], ybt[:, j, :], bank[6][:, :], ALU.mult)
            P.tt('dve', t1[:], t1[:], bank[7][:, :], ALU.add)
            P.act(ybaT[:, j, :], t1[:], AF.Silu, bias=vp[:, VB_LNB + j:VB_LNB + j + 1], scale=vp[:, VB_LNG + j:VB_LNG + j + 1])
        if debug and tt_ == 0:
            dbg_out("dbg_ybaT", ybaT[:], [128, 8, 512], BF16)
        for m in range(8):
            pa = bank[0]; pb_ = bank[1]
            for kc in range(8):
                P.mm(pa[:, :], Wao[:, kc, m * 128:(m + 1) * 128], ogt[:, kc, :], start=(kc == 0), stop=(kc == 7))
            for kc in range(8):
                P.mm(pb_[:, :], Wbo[:, kc, m * 128:(m + 1) * 128], ybaT[:, kc, :], start=(kc == 0), stop=(kc == 7))
            P.tt('dve', t1[:], pa[:, :], gtt[:, m, :], ALU.mult)
            P.stt(t2[:], pb_[:, :], vp[:, VB_BBO + m:VB_BBO + m + 1], gtt[:, 8 + m, :], ALU.add, ALU.mult)
            P.tt('pool', mixT[:, m, :], t1[:], t2[:], ALU.add)
        for sub in range(4):
            i = tt_ * 4 + sub
            P.dma('sp', h0t[:], h0_d[i * 128:(i + 1) * 128, :], r=[('h0_d', i)])
            for hf in range(2):
                pm = bank[2 + hf]
                for m in range(8):
                    P.mm(pm[:, :], mixT[:, m, sub * 128:(sub + 1) * 128], Wout[:, m, hf * 512:(hf + 1) * 512], start=(m == 0), stop=(m == 7))
                P.stt(rt[:, hf * 512:(hf + 1) * 512], h0t[:, hf * 512:(hf + 1) * 512], DN_ALPHA, pm[:, :], ALU.mult, ALU.add)
            layer_norm_rows(rt, h1t)
            P.dma('sp', h1_d[i * 128:(i + 1) * 128, :], h1t[:], w=[('h1_d', i)])
            for hf in range(2):
                pt = bank[4 + hf]
                for q4 in range(4):
                    kc = hf * 4 + q4
                    P.tr(pt[:, q4 * 128:(q4 + 1) * 128], h1t[:, kc * 128:(kc + 1) * 128], identf[:])
                P.copy('dve', h1Tf[:, hf * 4:(hf + 1) * 4, :], pt[:].rearrange("p (k t) -> p k t", t=128))
            pr_ = bank[6 + (sub % 2)]
            for kc in range(8):
                P.mm(pr_[:, 0:32], h1Tf[:, kc, :], Wr[:, kc, :], start=(kc == 0), stop=(kc == 7))
            P.tt('dve', LG[:, i, :], pr_[:, 0:32], brB[:], ALU.add)
            P.op('dve', lambda e, i=i: e.max(out=M8[:, i, :], in_=LG[:, i, :]), [LG], [M8])
            P.ts('dve', MSK[:, i, :], LG[:, i, :], M8[:, i, 3:4], None, ALU.is_ge)
            P.ts('dve', nmx[:], M8[:, i, 0:1], -1.0, None, ALU.mult)
            P.act(G4[:, i, :], M8[:, i, 0:4], AF.Exp, bias=nmx[:, 0:1])
            P.op('dve', lambda e, i=i: e.tensor_reduce(out=esum[:], in_=G4[:, i, :], axis=AX.X, op=ALU.add), [G4], [esum])
            P.op('dve', lambda e: e.reciprocal(out=esum[:], in_=esum[:]), [esum], [esum])
            P.ts('dve', G4[:, i, :], G4[:, i, :], esum[:, 0:1], None, ALU.mult)
            P.copy('act', h1tb[:], h1t[:])
            P.dma('sp', h1b_d[i * 128:(i + 1) * 128, :], h1tb[:], w=[('h1b_d', i)])
    if debug:
        dbg_out("dbg_G4", G4[:], [128, 32, 4], F32)
        dbg_out("dbg_MSK", MSK[:], [128, 32, 32], BF16)
    if stage <= 7:
        P.finish()
        return nc, dbg

    A.release(moe_mark)
    I32 = mybir.dt.int32
    NBLK = 64
    lnp['g'] = sb("gB2", [128, D]); lnp['b'] = sb("bB2", [128, D])
    P.dma('sp', lnp['g'][:], ln2_g.partition_broadcast(128))
    P.dma('sp', lnp['b'][:], ln2_b.partition_broadcast(128))
    stri = sb("stri", [128, 128], BF16)
    strf = sb("strf", [128, 128])
    P.op('pool', lambda e: e.affine_select(out=strf[:], in_=onesf[:], pattern=[[1, 128]], compare_op=ALU.is_gt, fill=0.0,
                                           base=0, channel_multiplier=-1), [onesf], [strf])
    P.copy('dve', stri[:], strf[:])
    POS = sb("POS", [128, 32, 32]); run = sb("run", [128, 32], BF16)
    P.memset('pool', run[:], 0.0)
    for i in range(32):
        pp = bank[i % 2]
        P.mm(pp[:, 0:32], stri[:], MSK[:, i, :], start=True, stop=False)
        P.mm(pp[:, 0:32], onesb[:], run[:], start=False, stop=True)
        P.copy('act', POS[:, i, :], pp[:, 0:32])
        P.tt('dve', run[:], run[:], MSK[:, i, :], ALU.add)
    cnt = sb("cnt", [128, 32]); padded = sb("padded", [128, 32]); pend = sb("pend", [128, 32]); pstart = sb("pstart", [128, 32])
    tmp32 = sb("tmp32", [128, 32]); ones32 = sb("ones32", [128, 32])
    P.memset('pool', ones32[:], 1.0)
    P.mm(bank[2][:, 0:32], onesb[:], run[:])
    P.copy('dve', cnt[:], bank[2][:, 0:32])
    P.memset('pool', padded[:], 0.0)
    for j in range(8):
        P.ts('dve', tmp32[:], cnt[:], float(512 * j), 512.0, ALU.is_gt, ALU.mult)
        P.tt('dve', padded[:], padded[:], tmp32[:], ALU.add)
    P.op('dve', lambda e: e.tensor_tensor_scan(out=pend[:], data0=ones32[:], data1=padded[:], initial=0.0, op0=ALU.mult, op1=ALU.add),
         [ones32, padded], [pend])
    P.tt('dve', pstart[:], pend[:], padded[:], ALU.subtract)
    bthr = sb("bthr", [128, NBLK, 32]); BE = sb("BE", [128, NBLK])
    P.op('pool', lambda e: e.iota(out=bthr[:], pattern=[[512, NBLK], [0, 32]], base=0, channel_multiplier=0,
                                  allow_small_or_imprecise_dtypes=True), [], [bthr])
    P.tt('dve', bthr[:], bc(pend[:], [128, NBLK, 32], 1), bthr[:], ALU.is_le)
    P.op('dve', lambda e: e.tensor_reduce(out=BE[:], in_=bthr[:], axis=AX.X, op=ALU.add), [bthr], [BE])
    P.ts('dve', BE[:], BE[:], 31.0, None, ALU.min)
    basef = sb("basef", [128, 8]); IDXF = sb("IDXF", [128, NBLK, 8]); IDXW = sb("IDXW", [128, NBLK, 8], I32)
    P.op('pool', lambda e: e.iota(out=basef[:], pattern=[[128, 8]], base=0, channel_multiplier=1,
                                  allow_small_or_imprecise_dtypes=True), [], [basef])
    P.stt(IDXF[:], bc(BE[:], [128, NBLK, 8], 2), 1024.0, bc(basef[:], [128, NBLK, 8], 1), ALU.mult, ALU.add)
    P.copy('dve', IDXW[:], IDXF[:])
    pidx = sb("pidx", [32, 1]); OH = sb("OH", [32, NBLK], BF16)
    P.op('pool', lambda e: e.iota(out=pidx[:], pattern=[[0, 1]], base=0, channel_multiplier=1,
                                  allow_small_or_imprecise_dtypes=True), [], [pidx])
    P.ts('dve', OH[:], BE[0:32, :], pidx[0:32, 0:1], None, ALU.is_equal)
    thrb = sb("thrb", [1, NBLK]); ACTF = sb("ACTF", [1, NBLK], I32)
    P.op('pool', lambda e: e.iota(out=thrb[:], pattern=[[512, NBLK]], base=0, channel_multiplier=0,
                                  allow_small_or_imprecise_dtypes=True), [], [thrb])
    P.ts('dve', thrb[:], thrb[:], pend[0:1, 31:32], None, ALU.is_lt)
    P.copy('dve', ACTF[:], thrb[:])
    bgu_all = sb("bgu_all", [32, 2048], BF16); bd_all = sb("bd_all", [32, D], BF16)
    P.dma('pool', bgu_all[:], b_gu)
    P.dma('pool', bd_all[:], b_down)
    SLOTF = sb("SLOTF", [128, 32, 4]); SLOT = sb("SLOT", [128, 32, 4], I32)
    valt = sb("valt", [128, 32]); mk = sb("mk", [128, 4, 32])
    for i in range(32):
        P.tt('dve', valt[:], POS[:, i, :], pstart[:], ALU.add)
        P.tt('dve', mk[:], bc(LG[:, i, :], [128, 4, 32], 1), bc(M8[:, i, 0:4], [128, 4, 32], 2), ALU.is_equal)
        P.tt('dve', mk[:], mk[:], bc(valt[:], [128, 4, 32], 1), ALU.mult)
        P.op('dve', lambda e, i=i: e.tensor_reduce(out=SLOTF[:, i, :], in_=mk[:], axis=AX.X, op=ALU.add), [mk], [SLOTF])
    P.copy('dve', SLOT[:], SLOTF[:])
    if debug:
        dbg_out("dbg_SLOT", SLOT[:], [128, 32, 4], I32)
        dbg_out("dbg_BE", BE[:], [128, NBLK], F32)
        dbg_out("dbg_cnt", cnt[:], [128, 32], F32)
    disp_mark = A.mark()
    hb_t = [sb(f"hb_t{i}", [128, D], BF16) for i in range(2)]
    for i in range(32):
        hb_ = hb_t[i % 2]
        P.dma('sp', hb_[:], h1b_d[i * 128:(i + 1) * 128, :], r=[('h1b_d', i)])
        for k in range(4):
            P.idma(xs_d, hb_[:], out_off=SLOT[:, i, k:k + 1], w=[('xs', i, k)])
    A.release(disp_mark)
    if debug and os.environ.get("MK_MOE_STOP") == "1":
        P.finish()
        return nc, dbg
    wguB = [sb(f"wguB{i}", [128, 8, 2048], BF16) for i in range(2)]
    wdB = [sb(f"wdB{i}", [128, 8, D], BF16) for i in range(2)]
    xtm = [sb(f"xtm{i}", [128, 4, D], BF16) for i in range(2)]; xT = sb("xT", [128, 8, 512], BF16)
    actT = sb("actT", [128, 8, 512], BF16)
    OHs = [sb(f"OHs{i}", [32, 512], BF16) for i in range(2)]
    NROT = 3
    gl = [sb(f"gl{i}", [128, 512]) for i in range(NROT)]; sg = [sb(f"sg{i}", [128, 512]) for i in range(NROT)]
    li = [sb(f"li{i}", [128, 512]) for i in range(NROT)]; p1 = [sb(f"p1{i}", [128, 512]) for i in range(NROT)]
    ysb = [sb(f"ysb{i}", [128, D]) for i in range(2)]
    wgu_rows = w_gu.rearrange("e k c -> (e k) c")
    wd_rows = w_down.rearrange("e k c -> (e k) c")
    nblk_run = int(os.environ.get("MK_NBLK", str(NBLK))) if debug else NBLK
    SIG_MAX = float(1.0 / (1.0 + np.exp(-1.702 * 7.0)))

    def load_block_weights(b):
        for kc in range(8):
            P.idma(wguB[b % 2][:, kc, :], wgu_rows, in_off=IDXW[:, b, kc:kc + 1], r=[IDXW], w=[f"wguB{b % 2}_{kc}"])
        for kc in range(8):
            P.idma(wdB[b % 2][:, kc, :], wd_rows, in_off=IDXW[:, b, kc:kc + 1], r=[IDXW], w=[f"wdB{b % 2}_{kc}"])

    def load_block_x(b):
        P.dma('sp', xtm[b % 2][:], xs_d[b * 512:(b + 1) * 512, :].rearrange("(s p) d -> p s d", p=128), r=[])
        P.copy('act', OHs[b % 2][:], OH[:, b:b + 1].to_broadcast([32, 512]))

    l1b = [0]; l2b = [0]; ycnt = [0]; rot = [0]
    load_block_weights(0)
    load_block_x(0)
    use_skip = os.environ.get("MK_NOSKIP") is None
    for b in range(nblk_run):
        if use_skip:
            P.cond_begin(ACTF[0:1, b:b + 1])
        if b + 1 < nblk_run:
            load_block_weights(b + 1)
            load_block_x(b + 1)
        wg_ = wguB[b % 2]; wd_ = wdB[b % 2]; oh_ = OHs[b % 2]; xt_ = xtm[b % 2]
        for k2 in range(4):
            pbf = bank_bf(4 + k2 % 2)
            for kk in range(2):
                kc = k2 * 2 + kk
                for sub in range(4):
                    P.tr(pbf[:, kk * 512 + sub * 128:kk * 512 + (sub + 1) * 128], xt_[:, sub, kc * 128:(kc + 1) * 128], identb[:])
            P.copy('act' if k2 % 2 == 0 else 'dve', xT[:, k2 * 2:k2 * 2 + 2, :], pbf[:, :].rearrange("p (k t) -> p k t", t=512))
        for m in range(8):
            pbg = bank[l1b[0] % 4]; pbl = bank[(l1b[0] + 1) % 4]; l1b[0] += 2
            ri = rot[0] % NROT; rot[0] += 1
            for kc in range(8):
                P.mm(pbg[:, :], wg_[:, kc, m * 128:(m + 1) * 128], xT[:, kc, :], start=(kc == 0), stop=False, r=[f"wguB{b % 2}_{kc}", xT])
            P.mm(pbg[:, :], bgu_all[:, m * 128:(m + 1) * 128], oh_[:], start=False, stop=True)
            for kc in range(8):
                P.mm(pbl[:, :], wg_[:, kc, 1024 + m * 128:1024 + (m + 1) * 128], xT[:, kc, :], start=(kc == 0), stop=False, r=[f"wguB{b % 2}_{kc}", xT])
            P.mm(pbl[:, :], bgu_all[:, 1024 + m * 128:1024 + (m + 1) * 128], oh_[:], start=False, stop=True)
            P.act(sg[ri][:], pbg[:, :], AF.Sigmoid, scale=1.702)
            P.ts('dve', gl[ri][:], pbg[:, :], 7.0, None, ALU.min)
            P.ts('dve', li[ri][:], pbl[:, :], 7.0, -7.0, ALU.min, ALU.max)
            P.stt(p1[ri][:], sg[ri][:], SIG_MAX, gl[ri][:], ALU.min, ALU.mult)
            P.stt(actT[:, m, :], li[ri][:], 1.0, p1[ri][:], ALU.add, ALU.mult)
        for sub in range(4):
            yb_ = ysb[ycnt[0] % 2]; ycnt[0] += 1
            for hf in range(2):
                pb = bank[4 + l2b[0] % 4]; l2b[0] += 1
                for m in range(8):
                    P.mm(pb[:, :], actT[:, m, sub * 128:(sub + 1) * 128], wd_[:, m, hf * 512:(hf + 1) * 512], start=(m == 0), stop=False, r=[f"wdB{b % 2}_{m}", actT])
                P.mm(pb[:, :], oh_[:, 0:128], bd_all[:, hf * 512:(hf + 1) * 512], start=False, stop=True)
                P.copy('act' if hf == 0 else 'pool_never', yb_[:, hf * 512:(hf + 1) * 512], pb[:, :]) if hf == 0 else P.copy('dve', yb_[:, hf * 512:(hf + 1) * 512], pb[:, :])
            P.dma('sp', ys_d[b * 512 + sub * 128:b * 512 + (sub + 1) * 128, :], yb_[:], w=[('ys', b, sub)])
        if use_skip:
            P.cond_end()
    A.release(disp_mark)
    if debug and os.environ.get("MK_MOE_STOP") == "2":
        P.finish()
        return nc, dbg
    yk = [[sb(f"yk{j}_{k}", [128, D]) for k in range(4)] for j in range(2)]
    rt2 = [sb(f"rt2_{j}", [128, D]) for j in range(2)]; ot = [sb(f"ot{j}", [128, D]) for j in range(2)]
    for i in range(32):
        j = i % 2
        P.dma('sp', rt2[j][:], h1_d[i * 128:(i + 1) * 128, :], r=[('h1_d', i)])
        for k in range(4):
            P.idma(yk[j][k][:], ys_d, in_off=SLOT[:, i, k:k + 1], r=[SLOT])
        P.ts('dve', rt2[j][:], rt2[j][:], DN_ALPHA, None, ALU.mult)
        for k in range(4):
            P.stt(rt2[j][:], yk[j][k][:], G4[:, i, k:k + 1], rt2[j][:], ALU.mult, ALU.add)
        layer_norm_rows(rt2[j], ot[j])
        P.dma('sp', out[i * 128:(i + 1) * 128, :], ot[j][:])
    P.finish()
    return nc, dbg


_IN_NAMES = ['emb_ln_g', 'emb_ln_b', 'w_in', 'conv_qkv', 'a_log', 'dt_bias', 'dn_norm_g', 'w_a_o', 'b_glu', 'conv_dw',
             'b_dw', 'conv_ln_g', 'conv_ln_b', 'w_b_o', 'b_b_o', 'b_gate', 'w_out', 'ln1_g', 'ln1_b', 'w_router',
             'b_router', 'w_gu', 'b_gu', 'w_down', 'b_down', 'ln2_g', 'ln2_b']


def make_in_maps(inputs, n_cores=8):
    shared = {}
    for k in _IN_NAMES:
        a = np.ascontiguousarray(np.asarray(inputs[k], dtype=np.float32))
        a = a[0] if k not in ('emb_ln_g', 'emb_ln_b') else a
        if k in ('a_log', 'dt_bias'):
            a = a.reshape(16)
        shared[k] = np.ascontiguousarray(a)
    xs = np.asarray(inputs['x'], dtype=np.float32)
    maps = []
    for c in range(n_cores):
        m = dict(shared)
        m['x'] = np.ascontiguousarray(xs[c])
        maps.append(m)
    return maps


def kernel(**inputs):
    nc, _ = build_program()
    maps = make_in_maps(inputs, 8)
    res = run_bass_kernel_spmd(nc, maps, core_ids=list(range(8)))
    return np.stack([np.asarray(res.results[c]["out"]) for c in range(8)], axis=0).astype(np.float32)
```

```python
import os
import numpy as np
import concourse.bass as bass
import concourse.mybir as mybir
from concourse.bass_utils import run_bass_kernel_spmd

F32 = mybir.dt.float32
BF16 = mybir.dt.bfloat16
AF = mybir.ActivationFunctionType
ALU = mybir.AluOpType
AX = mybir.AxisListType

S = 4096
D = 1024
NCOL = 8224
NCH = 64
DN_ALPHA = 2.0 ** 0.25
LN_EPS = 1e-5
RMS_EPS = 1e-6
L2_EPS = 1e-6
NEG_BIG = -1.0e30
COL_Q, COL_K, COL_V, COL_Z, COL_SM, COL_GLU, COL_GATE = 0, 1024, 2048, 3072, 4096, 4128, 6176


class Prog:
    def __init__(self, nc, n_dma_sems=12, self_sync=True):
        self.nc = nc
        self.eng = {'pe': nc.tensor, 'act': nc.scalar, 'dve': nc.vector, 'pool': nc.gpsimd, 'sp': nc.sync}
        self.streams = {e: [] for e in self.eng}
        self.cnt = {e: 0 for e in self.eng}
        self.sem = {e: nc.alloc_semaphore(name=f"s_{e}") for e in self.eng}
        self.seen = {e: {} for e in self.eng}
        self.lastw = {}
        self.reads = {}
        self.semobj = {('e', e): self.sem[e] for e in self.eng}
        self.dma_sems = {}
        self.n_dma_sems = n_dma_sems
        self.self_sync = self_sync
        self.n_inst = 0

    def _dma_sem(self, q):
        if q not in self.dma_sems:
            lst = []
            for i in range(self.n_dma_sems):
                s = self.nc.alloc_semaphore(name=f"d_{q}_{i}")
                key = ('d', q, i)
                self.semobj[key] = s
                lst.append([key, 0])
            self.dma_sems[q] = [lst, 0]
        lst, idx = self.dma_sems[q]
        self.dma_sems[q][1] = (idx + 1) % len(lst)
        return lst[idx]

    def _deps(self, e, reads, writes):
        deps = {}

        def add(k, v):
            if deps.get(k, 0) < v:
                deps[k] = v
        for r in reads:
            ev = self.lastw.get(r)
            if ev is not None:
                add(*ev)
        for w in writes:
            ev = self.lastw.get(w)
            if ev is not None:
                add(*ev)
            for k, v in self.reads.get(w, {}).items():
                add(k, v)
        waits = []
        for k, v in deps.items():
            if k == ('e', e):
                if not self.self_sync or e in ('pe', 'sp'):
                    continue
            if self.seen[e].get(k, 0) >= v:
                continue
            self.seen[e][k] = v
            waits.append((self.semobj[k], v))
        return waits

    def _commit(self, ev, reads, writes):
        k, v = ev
        for w in writes:
            self.lastw[w] = ev
            self.reads[w] = {}
        for r in reads:
            d = self.reads.setdefault(r, {})
            if d.get(k, 0) < v:
                d[k] = v

    @staticmethod
    def _names(aps):
        out = []
        for a in aps:
            if a is None or isinstance(a, (int, float)):
                continue
            if isinstance(a, (str, tuple)):
                out.append(a)
            else:
                out.append(a.name)
        return out

    def op(self, e, fn, reads, writes):
        reads = self._names(reads)
        writes = self._names(writes)
        pr = [n for n in reads if isinstance(n, str) and n.startswith('bank')]
        if pr:
            reads = [n for n in reads if n not in pr]
            writes = writes + [n for n in pr if n not in writes]
        waits = self._deps(e, reads, writes)
        self.cnt[e] += 1
        seq = self.cnt[e]
        sem = self.sem[e]

        def emit(engobj, waits=waits, fn=fn, sem=sem):
            for s, v in waits:
                engobj.wait_ge(s, v)
            fn(engobj).then_inc(sem, 1)
        self.streams[e].append(emit)
        self._commit((('e', e), seq), reads, writes)
        self.n_inst += 1

    def dma(self, q, out, in_, r=None, w=None, **kw):
        reads = self._names(r if r is not None else [in_])
        writes = self._names(w if w is not None else [out])
        slot = self._dma_sem(q)
        key, prev = slot
        waits = self._deps(q, reads, writes)
        if prev > 0 and self.seen[q].get(key, 0) < prev:
            self.seen[q][key] = prev
            waits.append((self.semobj[key], prev))
        slot[1] = prev + 16
        tgt = slot[1]
        s = self.semobj[key]

        def emit(engobj, waits=waits, s=s, out=out, in_=in_, kw=kw):
            for ss, v in waits:
                engobj.wait_ge(ss, v)
            engobj.dma_start(out=out, in_=in_, **kw).then_inc(s, 16)
        self.streams[q].append(emit)
        self._commit((key, tgt), reads, writes)
        self.n_inst += 1

    def idma(self, out, in_, out_off=None, in_off=None, r=None, w=None):
        q = 'pool'
        reads = self._names(r if r is not None else [in_, out_off, in_off])
        writes = self._names(w if w is not None else [out])
        slot = self._dma_sem(q)
        key, prev = slot
        waits = self._deps(q, reads, writes)
        if prev > 0 and self.seen[q].get(key, 0) < prev:
            self.seen[q][key] = prev
            waits.append((self.semobj[key], prev))
        slot[1] = prev + 16
        tgt = slot[1]
        s = self.semobj[key]

        def emit(engobj, waits=waits, s=s):
            for ss, v in waits:
                engobj.wait_ge(ss, v)
            oo = bass.IndirectOffsetOnAxis(ap=out_off, axis=0) if out_off is not None else None
            io = bass.IndirectOffsetOnAxis(ap=in_off, axis=0) if in_off is not None else None
            engobj.indirect_dma_start(out=out, out_offset=oo, in_=in_, in_offset=io).then_inc(s, 16)
        self.streams[q].append(emit)
        self._commit((key, tgt), reads, writes)
        self.n_inst += 1

    def cond_begin(self, flag_ap):
        st = {'cnt0': dict(self.cnt), 'seen0': {e: dict(d) for e, d in self.seen.items()}, 'dma0': {}, 'ctx': {}, 'used': {}}
        for q, (lst, _) in self.dma_sems.items():
            for key, val in lst:
                st['dma0'][key] = val
        self._cond = st
        if not hasattr(self, '_cregs'):
            self._cregs = {}
        for e in self.eng:
            waits = self._deps(e, self._names([flag_ap]), [])

            def emit(engobj, waits=waits, e=e, st=st):
                if not st['used'].get(e):
                    return
                for s_, v in waits:
                    engobj.wait_ge(s_, v)
                if e not in self._cregs:
                    self._cregs[e] = engobj.alloc_register(f"cflag_{e}")
                r = self._cregs[e]
                engobj.reg_load(r, flag_ap)
                v = engobj.snap(r)
                ctx = engobj.If(v == 1)
                ctx.__enter__()
                st['ctx'][e] = (ctx, v)
            self.streams[e].append(emit)

    def cond_end(self):
        st = self._cond
        for e in self.eng:
            n_e = self.cnt[e] - st['cnt0'].get(e, 0)
            comp = []
            if e in self.dma_sems:
                for key, val in self.dma_sems[e][0]:
                    d = val - st['dma0'].get(key, 0)
                    if d:
                        comp.append((self.semobj[key], d, st['dma0'].get(key, 0)))
            st['used'][e] = bool(n_e or comp)

            def emit(engobj, e=e, n_e=n_e, comp=comp, st=st):
                if not st['used'].get(e):
                    return
                ctx, v = st['ctx'][e]
                ctx.__exit__(None, None, None)
                with engobj.Else():
                    engobj.drain()
                    for so, d, before in comp:
                        if before > 0:
                            engobj.wait_ge(so, before)
                        engobj.sem_inc(so, d)
                    if n_e:
                        engobj.sem_inc(self.sem[e], n_e)
                engobj.free_register(v.val)
            self.streams[e].append(emit)
        self.seen = st['seen0']
        self._cond = None

    def barrier(self):
        cur = {}
        for e in self.eng:
            if self.cnt[e] > 0:
                cur[('e', e)] = self.cnt[e]
        for q, (lst, _) in self.dma_sems.items():
            for key, val in lst:
                if val > 0:
                    cur[key] = val
        for e in self.eng:
            waits = []
            for k, v in cur.items():
                if k == ('e', e):
                    continue
                if self.seen[e].get(k, 0) >= v:
                    continue
                self.seen[e][k] = v
                waits.append((self.semobj[k], v))

            def emit(engobj, waits=waits):
                for s, v in waits:
                    engobj.wait_ge(s, v)
            self.streams[e].append(emit)

    def finish(self):
        self.barrier()
        nc = self.nc
        with nc.Block() as block:
            @block.tensor
            def _(e):
                for f in self.streams['pe']:
                    f(e)

            @block.scalar
            def _(e):
                for f in self.streams['act']:
                    f(e)

            @block.vector
            def _(e):
                for f in self.streams['dve']:
                    f(e)

            @block.gpsimd
            def _(e):
                for f in self.streams['pool']:
                    f(e)

            @block.sync
            def _(e):
                for f in self.streams['sp']:
                    f(e)

    def mm(self, out, lhsT, rhs, start=True, stop=True, r=None, w=None):
        self.op('pe', lambda e: e.matmul(out, lhsT, rhs, start=start, stop=stop),
                r if r is not None else [lhsT, rhs], w if w is not None else [out])

    def tr(self, out, in_, ident, r=None, w=None):
        self.op('pe', lambda e: e.transpose(out=out, in_=in_, identity=ident),
                r if r is not None else [in_, ident], w if w is not None else [out])

    def act(self, out, in_, func, bias=None, scale=None, accum=None, r=None, w=None):
        kw = {}
        if bias is not None:
            kw['bias'] = bias
        if scale is not None:
            kw['scale'] = scale
        if accum is not None:
            kw['accum_out'] = accum
        self.op('act', lambda e: e.activation(out=out, in_=in_, func=func, **kw),
                r if r is not None else [in_, bias, scale], w if w is not None else [out, accum])

    def tt(self, eng, out, in0, in1, op, r=None, w=None):
        self.op(eng, lambda e: e.tensor_tensor(out=out, in0=in0, in1=in1, op=op),
                r if r is not None else [in0, in1], w if w is not None else [out])

    def ts(self, eng, out, in0, s1, s2, op0, op1=None, r=None, w=None):
        if op1 is None:
            fn = lambda e: e.tensor_scalar(out=out, in0=in0, scalar1=s1, scalar2=None, op0=op0)
        else:
            fn = lambda e: e.tensor_scalar(out=out, in0=in0, scalar1=s1, scalar2=s2, op0=op0, op1=op1)
        self.op(eng, fn, r if r is not None else [in0, s1, s2], w if w is not None else [out])

    def stt(self, out, in0, scalar, in1, op0, op1, r=None, w=None):
        self.op('dve', lambda e: e.scalar_tensor_tensor(out=out, in0=in0, scalar=scalar, in1=in1, op0=op0, op1=op1),
                r if r is not None else [in0, scalar, in1], w if w is not None else [out])

    def copy(self, eng, out, in_, r=None, w=None):
        if eng == 'act':
            fn = lambda e: e.copy(out=out, in_=in_)
        else:
            fn = lambda e: e.tensor_copy(out=out, in_=in_)
        self.op(eng, fn, r if r is not None else [in_], w if w is not None else [out])

    def memset(self, eng, ap, val):
        self.op(eng, lambda e: e.memset(ap, val), [], [ap])


def bc(ap, shape, axis):
    return ap.unsqueeze(axis).to_broadcast(shape)


class Arena:
    def __init__(self, nc, P, base=16512, top=229344):
        self.nc = nc; self.P = P; self.ptr = base; self.top = top; self.n = 0; self.peak = base

    def alloc(self, name, shape, dt=F32):
        size = 2 if dt == BF16 else 4
        n = 1
        for d in shape[1:]:
            n *= d
        nbytes = (n * size + 63) // 64 * 64
        self.n += 1
        t = self.nc.alloc_sbuf_tensor_at(f"{name}_{self.n}", list(shape), dt, offset=self.ptr)
        self.ptr += nbytes
        self.peak = max(self.peak, self.ptr)
        assert self.ptr <= self.top, f"SBUF arena overflow at {name}: {self.ptr} > {self.top}"
        return t

    def mark(self):
        return self.ptr

    def release(self, m):
        self.P.barrier()
        self.ptr = m


def build_program(stage=99, debug=False):
    nc = bass.Bass("TRN2", target_bir_lowering=False)
    P = Prog(nc)
    A = Arena(nc, P)
    dbg = {}

    def dram_in(name, shape):
        return nc.dram_tensor(name, shape, F32, kind="ExternalInput").ap()

    def scratch(name, shape, dt, force_internal=False):
        kind = "ExternalOutput" if (debug and not force_internal) else "Internal"
        return nc.dram_tensor(name, shape, dt, kind=kind).ap()

    def dbg_out(name, src_ap, shape, dt, r=None):
        if not debug:
            return
        t = nc.dram_tensor(name, shape, dt, kind="ExternalOutput").ap()
        P.dma('sp', t, src_ap, r=r)

    sb = A.alloc

    x = dram_in("x", [S, D])
    emb_ln_g = dram_in("emb_ln_g", [D]); emb_ln_b = dram_in("emb_ln_b", [D])
    w_in = dram_in("w_in", [D, NCOL])
    conv_qkv = dram_in("conv_qkv", [5, 3072])
    a_log = dram_in("a_log", [16]); dt_bias = dram_in("dt_bias", [16])
    dn_norm_g = dram_in("dn_norm_g", [128])
    w_a_o = dram_in("w_a_o", [D, D])
    b_glu = dram_in("b_glu", [2048]); conv_dw = dram_in("conv_dw", [31, D]); b_dw = dram_in("b_dw", [D])
    conv_ln_g = dram_in("conv_ln_g", [D]); conv_ln_b = dram_in("conv_ln_b", [D])
    w_b_o = dram_in("w_b_o", [D, D]); b_b_o = dram_in("b_b_o", [D]); b_gate = dram_in("b_gate", [2048])
    w_out = dram_in("w_out", [D, D])
    ln1_g = dram_in("ln1_g", [D]); ln1_b = dram_in("ln1_b", [D])
    w_router = dram_in("w_router", [D, 32]); b_router = dram_in("b_router", [32])
    w_gu = dram_in("w_gu", [32, D, 2048]); b_gu = dram_in("b_gu", [32, 2048])
    w_down = dram_in("w_down", [32, D, D]); b_down = dram_in("b_down", [32, D])
    ln2_g = dram_in("ln2_g", [D]); ln2_b = dram_in("ln2_b", [D])
    out = nc.dram_tensor("out", [S, D], F32, kind="ExternalOutput").ap()

    h0_d = scratch("h0_d", [S, D], F32)
    hT_d = scratch("hT_d", [D, S], BF16)
    ogT_d = scratch("ogT_d", [D, S], BF16)
    ybT_d = scratch("ybT_d", [D, S], BF16)
    gate_d = scratch("gate_d", [2048, S], BF16)
    h1_d = scratch("h1_d", [S, D], F32)
    st_d = scratch("st_d", [2, S], F32)
    h1b_d = scratch("h1b_d", [S, D], BF16)
    xs_d = scratch("xs_d", [32768, D], BF16, force_internal=True)
    ys_d = scratch("ys_d", [32768, D], F32, force_internal=True)

    hT_dv = hT_d.rearrange("(kc p) t -> p kc t", p=128)
    w_in_v = w_in.rearrange("(kc p) c -> p kc c", p=128)

    identf = sb("identf", [128, 128]); identb = sb("identb", [128, 128], BF16)
    onesf = sb("onesf", [128, 128]); onesb = sb("onesb", [128, 128], BF16)
    zerof = sb("zerof", [128, 512])
    P.memset('pool', onesf[:], 1.0)
    P.memset('pool', zerof[:], 0.0)
    P.memset('pool', identf[:], 1.0)
    P.op('pool', lambda e: e.affine_select(out=identf[:], in_=identf[:], pattern=[[-1, 128]], compare_op=ALU.is_equal,
                                           fill=0.0, base=0, channel_multiplier=1), [identf], [identf])
    P.copy('dve', identb[:], identf[:])
    P.copy('dve', onesb[:], onesf[:])
    eps_ln = sb("eps_ln", [128, 1])
    P.memset('pool', eps_ln[:], LN_EPS)
    bank = [nc.alloc_psum_tensor(f"bank{i}", [128, 512], F32) for i in range(8)]

    def bank_bf(i):
        return bank[i][:].bitcast(BF16)

    ln_sets = [(sb(f"ln_st{i}", [128, 12]), sb(f"ln_mv{i}", [128, 2]), sb(f"ln_rs{i}", [128, 1])) for i in range(3)]
    ln_cnt = [0]
    lnp = {}

    def layer_norm_rows(t, o, np_=128):
        ln_st, ln_mv, ln_rs = ln_sets[ln_cnt[0] % 3]; ln_cnt[0] += 1
        P.op('dve', lambda e: e.bn_stats(out=ln_st[:np_, 0:6], in_=t[:np_, 0:512]), [t], [ln_st])
        P.op('dve', lambda e: e.bn_stats(out=ln_st[:np_, 6:12], in_=t[:np_, 512:1024]), [t], [ln_st])
        P.op('dve', lambda e: e.bn_aggr(out=ln_mv[:np_, :], in_=ln_st[:np_, :]), [ln_st], [ln_mv])
        P.act(ln_rs[:np_, :], ln_mv[:np_, 1:2], AF.Sqrt, bias=eps_ln[:np_, 0:1])
        P.op('dve', lambda e: e.reciprocal(out=ln_rs[:np_, :], in_=ln_rs[:np_, :]), [ln_rs], [ln_rs])
        P.ts('dve', t[:np_, :], t[:np_, :], ln_mv[:np_, 0:1], ln_rs[:np_, 0:1], ALU.subtract, ALU.mult)
        P.tt('pool', t[:np_, :], t[:np_, :], lnp['g'][:np_, :], ALU.mult)
        P.tt('dve', o[:np_, :], t[:np_, :], lnp['b'][:np_, :], ALU.add)

    vp = sb("vp", [128, 64])
    base_mark = A.mark()
    lnp['g'] = sb("gB0", [128, D]); lnp['b'] = sb("bB0", [128, D])
    P.dma('sp', lnp['g'][:], emb_ln_g.partition_broadcast(128))
    P.dma('sp', lnp['b'][:], emb_ln_b.partition_broadcast(128))
    xt = [sb(f"xt{i}", [128, D]) for i in range(2)]
    ht = [sb(f"ht{i}", [128, D]) for i in range(2)]
    hTs = [sb(f"hTs{i}", [128, 8, 512], BF16) for i in range(2)]
    for i in range(32):
        xb = xt[i % 2]; hb = ht[i % 2]; hs = hTs[(i // 4) % 2]
        P.dma('sp', xb[:], x[i * 128:(i + 1) * 128, :])
        layer_norm_rows(xb, hb)
        P.dma('sp', h0_d[i * 128:(i + 1) * 128, :], hb[:], w=[('h0_d', i)])
        for half in range(2):
            pb = bank[half]
            for q4 in range(4):
                kc = half * 4 + q4
                P.tr(pb[:, q4 * 128:(q4 + 1) * 128], hb[:, kc * 128:(kc + 1) * 128], identf[:])
            P.copy('act', hs[:, half * 4:(half + 1) * 4, (i % 4) * 128:(i % 4 + 1) * 128],
                   pb[:].rearrange("p (k t) -> p k t", t=128))
        if i % 4 == 3:
            tt_ = i // 4
            P.dma('sp', hT_dv[:, :, tt_ * 512:(tt_ + 1) * 512], hs[:], w=[('hT_d', tt_)])
    A.release(base_mark)
    if stage <= 1:
        P.finish()
        return nc, dbg

    tri = {}; ntri = {}; negT = {}; nstrT = {}
    for d_, (cm, st_) in (('f', (-1, 1)), ('b', (1, -1))):
        t_ = sb(f"tri_{d_}", [64, 64]); nt_ = sb(f"ntri_{d_}", [64, 64])
        P.op('pool', lambda e, t_=t_, cm=cm, st_=st_: e.affine_select(out=t_[:], in_=onesf[0:64, 0:64], pattern=[[st_, 64]],
             compare_op=ALU.is_ge, fill=0.0, base=0, channel_multiplier=cm), [onesf], [t_])
        P.ts('dve', nt_[:], t_[:], -1.0, None, ALU.mult)
        tri[d_] = t_; ntri[d_] = nt_
        ng_ = sb(f"negT_{d_}", [64, 8, 64])
        P.op('pool', lambda e, ng_=ng_, cm=cm, st_=st_: e.affine_select(out=ng_[:], in_=zerof[0:64, 0:512].rearrange("p (c i) -> p c i", i=64),
             pattern=[[0, 8], [st_, 64]], compare_op=ALU.is_ge, fill=NEG_BIG, base=0, channel_multiplier=cm), [zerof], [ng_])
        negT[d_] = ng_
        ns_ = sb(f"nstrT_{d_}", [64, 64])
        P.op('pool', lambda e, ns_=ns_, cm=cm, st_=st_: e.affine_select(out=ns_[:], in_=onesf[0:64, 0:64], pattern=[[st_, 64]],
             compare_op=ALU.is_gt, fill=0.0, base=0, channel_multiplier=cm), [onesf], [ns_])
        P.ts('dve', ns_[:], ns_[:], -1.0, None, ALU.mult)
        nstrT[d_] = ns_
    BETA = {d_: sb(f"BETA_{d_}", [64, 8, 64]) for d_ in 'fb'}
    LA = {d_: sb(f"LA_{d_}", [64, 8, 64]) for d_ in 'fb'}
    hTt = [sb(f"hTt{i}", [128, 8, 512], BF16) for i in range(2)]
    hcnt = [0]

    def load_hT(tt_):
        hb = hTt[hcnt[0] % 2]; hcnt[0] += 1
        P.dma('sp', hb[:], hT_dv[:, :, tt_ * 512:(tt_ + 1) * 512], r=[('hT_d', tt_)])
        return hb

    s2_mark = A.mark()
    SMraw = sb("SMraw", [64, 64, 32])
    Wsm = sb("Wsm", [128, 8, 32], BF16)
    alB = sb("alB", [64, 16]); dtB = sb("dtB", [64, 16]); nea = sb("nea", [64, 16])
    X_ = sb("spX", [64, 8, 64]); A_ = sb("spA", [64, 8, 64])
    P.dma('pool', Wsm[:], w_in_v[:, :, COL_SM:COL_SM + 32])
    P.dma('sp', alB[:], a_log.partition_broadcast(64))
    P.dma('sp', dtB[:], dt_bias.partition_broadcast(64))
    P.act(nea[:], alB[:], AF.Exp)
    P.ts('dve', nea[:], nea[:], -1.0, None, ALU.mult)
    for tt_ in range(8):
        hb = load_hT(tt_)
        pb = bank[2 + tt_ % 2]
        for cc in range(8):
            for kc in range(8):
                P.mm(pb[0:64, cc * 32:(cc + 1) * 32], hb[:, kc, cc * 64:(cc + 1) * 64], Wsm[:, kc, :], start=(kc == 0), stop=(kc == 7))
        P.copy('dve', SMraw[:, tt_ * 8:(tt_ + 1) * 8, :], pb[0:64, 0:256].rearrange("p (c k) -> p c k", k=32))
    for di, d_ in enumerate('fb'):
        ub = SMraw[:, :, di * 8:(di + 1) * 8].rearrange("p c h -> p h c")
        ua = SMraw[:, :, 16 + di * 8:24 + di * 8].rearrange("p c h -> p h c")
        P.act(BETA[d_][:], ub, AF.Sigmoid)
        P.tt('dve', X_[:], ua, bc(dtB[:, di * 8:(di + 1) * 8], [64, 8, 64], 2), ALU.add)
        P.stt(A_[:], X_[:], -1.0, X_[:], ALU.mult, ALU.max)
        P.act(A_[:], A_[:], AF.Exp, scale=-1.0)
        P.act(A_[:], A_[:], AF.Ln, bias=1.0)
        P.stt(A_[:], X_[:], 0.0, A_[:], ALU.max, ALU.add)
        P.tt('dve', LA[d_][:], A_[:], bc(nea[:, di * 8:(di + 1) * 8], [64, 8, 64], 2), ALU.mult)
    if debug:
        for d_ in 'fb':
            dbg_out(f"dbg_beta_{d_}", BETA[d_][:], [64, 8, 64], F32)
            dbg_out(f"dbg_la_{d_}", LA[d_][:], [64, 8, 64], F32)
    A.release(s2_mark)
    if stage <= 2:
        P.finish()
        return nc, dbg


    nheads = int(os.environ.get("MK_NHEADS", "8")) if debug else 8
    head_mark = A.mark()
    cst = sb("cst", [120, 128]); cwq = sb("cwq", [128, 120])
    P.dma('sp', cst[:], conv_qkv.rearrange("t (c p) -> (t c) p", p=128))
    P.tr(bank[0][:, 0:120], cst[:, :], identf[0:120, 0:120])
    P.copy('dve', cwq[:], bank[0][:, 0:120])
    eps_l2 = sb("eps_l2", [128, 1]); P.memset('pool', eps_l2[:], L2_EPS)
    eps_rms = sb("eps_rms", [128, 1]); P.memset('pool', eps_rms[:], RMS_EPS)
    ngB = sb("ngB", [64, 128]); P.dma('sp', ngB[:], dn_norm_g.partition_broadcast(64))
    Wg = [sb(f"Wg{i}", [128, 8, 128], BF16) for i in range(2)]
    U = sb("U", [128, 4100], BF16)
    P.memset('pool', U[:, 0:2], 0.0); P.memset('pool', U[:, 4098:4100], 0.0)
    C = sb("C", [128, 4096], BF16)
    Dg = sb("Dg", [128, 5, 128], BF16)
    sqb = sb("sqb", [128, 512], BF16); lnb = sb("lnb", [128, 512])
    qT = sb("qT", [128, 4096], BF16); kT = sb("kT", [128, 4096], BF16)
    Ktok = sb("Ktok", [64, 64, 128], BF16); Vtok = sb("Vtok", [64, 64, 128], BF16)
    O = sb("O", [64, 64, 128])
    qkT = {d_: sb(f"qkT_{d_}", [64, 64, 64], BF16) for d_ in 'fb'}
    TT = {d_: sb(f"TT_{d_}", [64, 64, 64], BF16) for d_ in 'fb'}
    Gd = {d_: sb(f"Gd_{d_}", [64, 64]) for d_ in 'fb'}
    EG = {d_: sb(f"EG_{d_}", [64, 64]) for d_ in 'fb'}
    NEG = {d_: sb(f"NEG_{d_}", [64, 64]) for d_ in 'fb'}
    TAIL = {d_: sb(f"TAIL_{d_}", [64, 64]) for d_ in 'fb'}
    CD = {d_: sb(f"CD_{d_}", [128, 64]) for d_ in 'fb'}
    LAm = sb("LAm", [64, 8, 64]); LAb = sb("LAb", [64, 8, 64])
    decT = {d_: sb(f"decT_{d_}", [64, 8, 64]) for d_ in 'fb'}; tY = {d_: sb(f"tY_{d_}", [64, 8, 64]) for d_ in 'fb'}
    Yb = {d_: [sb(f"Yb{d_}{i}", [64, 8, 64], BF16) for i in range(2)] for d_ in 'fb'}
    Zb = {d_: [sb(f"Zb{d_}{i}", [64, 8, 64], BF16) for i in range(2)] for d_ in 'fb'}
    Gm = {d_: [sb(f"Gm{d_}{i}", [64, 8, 64], BF16) for i in range(2)] for d_ in 'fb'}
    Sst = {d_: sb(f"S_{d_}", [128, 128]) for d_ in 'fb'}
    Sbf = {d_: sb(f"Sb_{d_}", [128, 128], BF16) for d_ in 'fb'}
    Rt = {d_: sb(f"R_{d_}", [64, 128], BF16) for d_ in 'fb'}
    vnew = {d_: sb(f"vnew_{d_}", [64, 128], BF16) for d_ in 'fb'}
    tq = {d_: sb(f"tq_{d_}", [64, 128]) for d_ in 'fb'}
    ktl = {d_: sb(f"ktl_{d_}", [64, 128], BF16) for d_ in 'fb'}
    otmp = sb("otmp", [64, 128])
    ssq = sb("ssq", [64, 64])
    wcnt = [0]; pcnt = [0]

    def nextbank():
        b_ = bank[pcnt[0] % 2]; pcnt[0] += 1
        return b_

    def inproj_chunk(col0, evac):
        wb = Wg[wcnt[0] % 2]; wcnt[0] += 1
        P.dma('pool', wb[:], w_in_v[:, :, col0:col0 + 128])
        for tt_ in range(8):
            hb = load_hT(tt_)
            pb = nextbank()
            for kc in range(8):
                P.mm(pb[:, :], wb[:, kc, :], hb[:, kc, :], start=(kc == 0), stop=(kc == 7))
            evac(tt_, pb)

    def evac_U(tt_, pb):
        P.copy('act', U[:, 2 + tt_ * 512:2 + (tt_ + 1) * 512], pb[:, :])

    def conv_silu(ci):
        for t in range(5):
            P.ts('dve', Dg[:, t, :], identb[:], cwq[:, t * 24 + ci:t * 24 + ci + 1], None, ALU.mult)
        for tt_ in range(8):
            pb = nextbank()
            for t in range(5):
                P.mm(pb[:, :], Dg[:, t, :], U[:, tt_ * 512 + t:tt_ * 512 + t + 512], start=(t == 0), stop=(t == 4))
            P.act(C[:, tt_ * 512:(tt_ + 1) * 512], pb[:, :], AF.Silu)

    def l2norm_to(dst, scale):
        for tt_ in range(8):
            sl = slice(tt_ * 512, (tt_ + 1) * 512)
            P.act(sqb[:], C[:, sl], AF.Square)
            pb = nextbank()
            P.mm(pb[:, :], onesb[:], sqb[:])
            P.act(lnb[:], pb[:, :], AF.Ln, bias=eps_l2[:, 0:1])
            P.act(lnb[:], lnb[:], AF.Exp, scale=-0.5)
            P.stt(dst[:, sl], C[:, sl], scale, lnb[:], ALU.mult, ALU.mult)

    def to_tokmajor(src, dst):
        for g in range(8):
            bi = 2 + g % 2
            pbf = bank_bf(bi)
            for cc in range(8):
                c = g * 8 + cc
                P.tr(pbf[0:64, cc * 128:(cc + 1) * 128], src[:, c * 64:(c + 1) * 64], identb[:])
            P.copy('dve' if g % 2 == 0 else 'act', dst[:, g * 8:(g + 1) * 8, :], pbf[0:64, :].rearrange("p (c d) -> p c d", d=128))

    def flat(t3):
        return t3[:].rearrange("p c i -> p (c i)")

    for h in range(nheads):
        inproj_chunk(COL_Q + h * 128, evac_U); conv_silu(h); l2norm_to(qT, 128.0 ** -0.5)
        inproj_chunk(COL_K + h * 128, evac_U); conv_silu(8 + h); l2norm_to(kT, 1.0)
        to_tokmajor(kT, Ktok)
        inproj_chunk(COL_V + h * 128, evac_U); conv_silu(16 + h)
        to_tokmajor(C, Vtok)
        if debug and h == 0:
            dbg_out("dbg_qT", qT[:], [128, 4096], BF16)
            dbg_out("dbg_kT", kT[:], [128, 4096], BF16)
            dbg_out("dbg_Ktok", Ktok[:], [64, 64, 128], BF16)
            dbg_out("dbg_Vtok", Vtok[:], [64, 64, 128], BF16)
        if stage <= 3:
            continue
        for d_ in 'fb':
            la_h = LA[d_][:, h, :]
            P.mm(bank[4][0:64, 0:64], tri[d_][:], la_h)
            P.copy('dve', Gd[d_][:], bank[4][0:64, 0:64])
            P.mm(bank[5][:, 0:64], onesf[0:64, :], la_h)
            P.act(CD[d_][:], bank[5][:, 0:64], AF.Exp)
            P.tt('dve', TAIL[d_][:], bank[5][0:64, 0:64], Gd[d_][:], ALU.subtract)
            P.act(TAIL[d_][:], TAIL[d_][:], AF.Exp)
            P.act(EG[d_][:], Gd[d_][:], AF.Exp)
            P.ts('dve', NEG[d_][:], EG[d_][:], -1.0, None, ALU.mult)
        for g in range(8):
            c0 = g * 8
            for cc in range(8):
                ch = slice((c0 + cc) * 64, (c0 + cc + 1) * 64)
                P.mm(bank[4][0:64, cc * 64:(cc + 1) * 64], kT[:, ch], kT[:, ch])
            for cc in range(8):
                ch = slice((c0 + cc) * 64, (c0 + cc + 1) * 64)
                P.mm(bank[5][0:64, cc * 64:(cc + 1) * 64], kT[:, ch], qT[:, ch])
            bk = {'f': (2, 3, 7), 'b': (0, 1, 6)}
            for d_ in 'fb':
                la_g = LA[d_][:, h, c0:c0 + 8]
                P.tt('pool', LAm[:], bc(la_g, [64, 8, 64], 2), bc(tri[d_][:], [64, 8, 64], 1), ALU.mult)
                P.copy('pool', LAb[:], bc(la_g, [64, 8, 64], 2))
                P.mm(bank[6][0:64, :], onesf[0:64, 0:64], flat(LAm), start=True, stop=False)
                P.mm(bank[6][0:64, :], ntri[d_][:], flat(LAb), start=False, stop=False)
                P.mm(bank[6][0:64, :], identf[0:64, 0:64], flat(negT[d_]), start=False, stop=True)
                P.act(flat(decT[d_]), bank[6][0:64, :], AF.Exp)
            for d_ in 'fb':
                P.tt('dve', flat(qkT[d_])[:, c0 * 64:(c0 + 8) * 64], bank[5][0:64, :], flat(decT[d_]), ALU.mult)
                P.tt('dve', flat(tY[d_]), bank[4][0:64, :], flat(decT[d_]), ALU.mult)
                P.tt('pool', tY[d_][:], tY[d_][:], bc(BETA[d_][:, h, c0:c0 + 8], [64, 8, 64], 2), ALU.mult)
                P.tt('dve', tY[d_][:], tY[d_][:], bc(nstrT[d_][:], [64, 8, 64], 1), ALU.mult)
                P.copy('act', Yb[d_][0][:], tY[d_][:])
                P.tt('pool', Gm[d_][0][:], tY[d_][:], bc(identf[0:64, 0:64], [64, 8, 64], 1), ALU.add)
            for d_ in 'fb':
                z0 = bank_bf(bk[d_][2])
                for cc in range(8):
                    P.tr(z0[0:64, cc * 64:(cc + 1) * 64], Yb[d_][0][:, cc, :], identb[0:64, 0:64])
                P.copy('dve' if d_ == 'f' else 'act', flat(Zb[d_][0]), z0[0:64, 0:512])
            for k in range(5):
                cur = k % 2; nxt = 1 - cur
                for d_ in 'fb':
                    by_, bz_, bg_ = bk[d_]
                    if k < 4:
                        for cc in range(8):
                            P.mm(bank[by_][0:64, cc * 64:(cc + 1) * 64], Zb[d_][cur][:, cc, :], Yb[d_][cur][:, cc, :])
                    for cc in range(8):
                        P.mm(bank[bz_][0:64, cc * 64:(cc + 1) * 64], Yb[d_][cur][:, cc, :], Zb[d_][cur][:, cc, :])
                for d_ in 'fb':
                    by_, bz_, bg_ = bk[d_]
                    if k < 4:
                        P.copy('act', flat(Yb[d_][nxt]), bank[by_][0:64, :])
                    P.copy('dve', flat(Zb[d_][nxt]), bank[bz_][0:64, :])
                for d_ in 'fb':
                    by_, bz_, bg_ = bk[d_]
                    for cc in range(8):
                        P.mm(bank[bg_][0:64, cc * 64:(cc + 1) * 64], Zb[d_][nxt][:, cc, :], Gm[d_][cur][:, cc, :])
                for d_ in 'fb':
                    by_, bz_, bg_ = bk[d_]
                    dstG = flat(Gm[d_][nxt]) if k < 4 else flat(TT[d_])[:, c0 * 64:(c0 + 8) * 64]
                    P.tt('dve' if d_ == 'f' else 'pool', dstG, bank[bg_][0:64, :], flat(Gm[d_][cur]), ALU.add) if d_ == 'f' else P.tt('dve', dstG, bank[bg_][0:64, :], flat(Gm[d_][cur]), ALU.add)
        if debug and h == 0:
            for d_ in 'fb':
                dbg_out(f"dbg_TT_{d_}", TT[d_][:], [64, 64, 64], BF16)
                dbg_out(f"dbg_qkT_{d_}", qkT[d_][:], [64, 64, 64], BF16)
        if stage <= 4:
            continue
        for d_ in 'fb':
            P.memset('pool', Sst[d_][:], 0.0)
            P.memset('pool', Sbf[d_][:], 0.0)
        nsteps = int(os.environ.get('MK_STEPS', '64')) if debug else 64
        for s_ in range(nsteps):
            cs = {'f': s_, 'b': 63 - s_}
            pv = {}
            for di, d_ in enumerate('fb'):
                bA = bank[di * 3]; bB_ = bank[di * 3 + 1]; bC = bank[di * 3 + 2]
                pv[d_] = (bA[0:64, 0:128], bB_[0:64, 0:128], bA[0:64, 128:256], bB_[0:64, 128:256], bC[:, 0:128])
            for d_ in 'fb':
                c = cs[d_]; ch = slice(c * 64, (c + 1) * 64); ps1, ps2, ps3, ps4, ps5 = pv[d_]
                P.mm(ps1, kT[:, ch], Sbf[d_][:])
                P.mm(ps2, qT[:, ch], Sbf[d_][:])
            for d_ in 'fb':
                c = cs[d_]; ps1, ps2, ps3, ps4, ps5 = pv[d_]
                P.stt(Rt[d_][:], ps1, NEG[d_][:, c:c + 1], Vtok[:, c, :], ALU.mult, ALU.add)
                P.act(ktl[d_][:], Ktok[:, c, :], AF.Identity, scale=TAIL[d_][:, c:c + 1])
                P.act(tq[d_][:], ps2, AF.Identity, scale=EG[d_][:, c:c + 1])
            for d_ in 'fb':
                c = cs[d_]; ps1, ps2, ps3, ps4, ps5 = pv[d_]
                P.mm(ps3, TT[d_][:, c, :], Rt[d_][:])
            for d_ in 'fb':
                c = cs[d_]; ps1, ps2, ps3, ps4, ps5 = pv[d_]
                P.act(vnew[d_][:], ps3, AF.Identity, scale=BETA[d_][:, h, c:c + 1])
            for d_ in 'fb':
                c = cs[d_]; ps1, ps2, ps3, ps4, ps5 = pv[d_]
                P.mm(ps5, ktl[d_][:], vnew[d_][:])
                P.mm(ps4, qkT[d_][:, c, :], vnew[d_][:])
            for d_ in 'fb':
                c = cs[d_]; ps1, ps2, ps3, ps4, ps5 = pv[d_]
                P.stt(Sbf[d_][:], Sst[d_][:], CD[d_][:, c:c + 1], ps5, ALU.mult, ALU.add)
            for d_ in 'fb':
                c = cs[d_]; ps1, ps2, ps3, ps4, ps5 = pv[d_]
                P.stt(Sst[d_][:], Sst[d_][:], CD[d_][:, c:c + 1], ps5, ALU.mult, ALU.add)
                first = (d_ == 'f' and c < 32) or (d_ == 'b' and c >= 32)
                if first:
                    P.tt('dve', O[:, c, :], tq[d_][:], ps4, ALU.add)
                else:
                    P.tt('dve', otmp[:], tq[d_][:], ps4, ALU.add)
                    P.tt('pool', O[:, c, :], O[:, c, :], otmp[:], ALU.add)
        if debug and h == 0:
            dbg_out("dbg_O", O[:], [64, 64, 128], F32)
            dbg_out("dbg_S_f", Sst['f'][:], [128, 128], F32)
            dbg_out("dbg_vnew_f", vnew['f'][:], [64, 128], BF16)
            dbg_out("dbg_R_f", Rt['f'][:], [64, 128], BF16)
            dbg_out("dbg_tq_f", tq['f'][:], [64, 128], F32)
            dbg_out("dbg_ktl_f", ktl['f'][:], [64, 128], BF16)
            dbg_out("dbg_CD_f", CD['f'][:], [128, 64], F32)
            dbg_out("dbg_TAIL_f", TAIL['f'][:], [64, 64], F32)
            dbg_out("dbg_EG_f", EG['f'][:], [64, 64], F32)
        if debug and os.environ.get('MK_NOPOST'):
            continue
        Zs = Ktok
        wz = Wg[wcnt[0] % 2]; wcnt[0] += 1
        P.dma('pool', wz[:], w_in_v[:, :, COL_Z + h * 128:COL_Z + (h + 1) * 128])
        for tt_ in range(8):
            hb = load_hT(tt_)
            for hf in range(2):
                pb = nextbank()
                for c4 in range(4):
                    cc = hf * 4 + c4
                    for kc in range(8):
                        P.mm(pb[0:64, c4 * 128:(c4 + 1) * 128], hb[:, kc, cc * 64:(cc + 1) * 64], wz[:, kc, :], start=(kc == 0), stop=(kc == 7))
                c_lo = tt_ * 8 + hf * 4
                P.act(Zs[:, c_lo:c_lo + 4, :], pb[0:64, :].rearrange("p (c d) -> p c d", d=128), AF.Silu)
        SQ = Vtok
        P.tt('dve', SQ[:], O[:], O[:], ALU.mult)
        P.op('dve', lambda e: e.tensor_reduce(out=ssq[:], in_=SQ[:], axis=AX.X, op=ALU.add), [SQ], [ssq])
        P.act(ssq[:], ssq[:], AF.Ln, bias=eps_rms[0:64, 0:1], scale=1.0 / 128.0)
        P.act(ssq[:], ssq[:], AF.Exp, scale=-0.5)
        P.tt('dve', O[:], O[:], bc(ssq[:], [64, 64, 128], 2), ALU.mult)
        P.tt('pool', O[:], O[:], bc(ngB[:], [64, 64, 128], 1), ALU.mult)
        OGb = Vtok
        P.tt('dve', OGb[:], O[:], Zs[:], ALU.mult)
        for g in range(8):
            bi = 2 + g % 2
            pbf = bank_bf(bi)
            for cc in range(8):
                P.tr(pbf[:, cc * 64:(cc + 1) * 64], OGb[:, g * 8 + cc, :], identb[0:64, 0:64])
            P.copy('act', C[:, g * 512:(g + 1) * 512], pbf[:, 0:512])
        P.dma('sp', ogT_d[h * 128:(h + 1) * 128, :], C[:], w=[('ogT_d', h)])
    if stage <= 5:
        P.finish()
        return nc, dbg

    A.release(head_mark)
    vst = sb("vst", [64, 128])
    for r0, src, n in ((0, b_glu, 16), (16, b_gate, 16), (32, b_dw, 8), (40, conv_ln_g, 8), (48, conv_ln_b, 8), (56, b_b_o, 8)):
        P.dma('sp', vst[r0:r0 + n, :], src.rearrange("(c p) -> c p", p=128))
    P.tr(bank[0][:, 0:64], vst[:, :], identf[0:64, 0:64])
    P.copy('dve', vp[:], bank[0][:, 0:64])
    VB_GLU, VB_GATE, VB_DW, VB_LNG, VB_LNB, VB_BBO = 0, 16, 32, 40, 48, 56
    cdst1 = sb("cdst1", [128, 128]); cdst2 = sb("cdst2", [120, 128]); cwd = sb("cwd", [128, 248])
    cdv = conv_dw.rearrange("t (c p) -> (t c) p", p=128)
    P.dma('sp', cdst1[:], cdv[0:128, :])
    P.dma('sp', cdst2[:], cdv[128:248, :])
    P.tr(bank[1][:, 0:128], cdst1[:, :], identf[:, :])
    P.copy('dve', cwd[:, 0:128], bank[1][:, 0:128])
    P.tr(bank[1][:, 128:248], cdst2[:, :], identf[0:120, 0:120])
    P.copy('dve', cwd[:, 128:248], bank[1][:, 128:248])
    Wg4 = [sb(f"Wg4_{i}", [128, 8, 512], BF16) for i in range(2)]
    GA = [sb(f"GA{i}", [128, 4096], BF16) for i in range(2)]
    Ug = [sb(f"Ug{i}", [128, 4096 + 30], BF16) for i in range(2)]
    for u_ in Ug:
        P.memset('pool', u_[:, 0:15], 0.0); P.memset('pool', u_[:, 4096 + 15:4096 + 30], 0.0)
    Ycb = [sb(f"Ycb{i}", [128, 4096], BF16) for i in range(4)]
    Dg31 = sb("Dg31", [128, 31, 128], BF16)
    sgt = sb("sgt", [128, 512]); sq2 = sb("sq2", [128, 512], BF16)
    SUM = sb("SUM", [1, 4096]); SSQ = sb("SSQ", [1, 4096])
    w4cnt = [0]

    def inproj_group(cols, evacs):
        wb = Wg4[w4cnt[0] % 2]; w4cnt[0] += 1
        for jj, c0_ in enumerate(cols):
            P.dma('pool', wb[:, :, jj * 128:(jj + 1) * 128], w_in_v[:, :, c0_:c0_ + 128], w=[f"{wb.name}_{jj}"])
        for tt_ in range(8):
            hb = load_hT(tt_)
            for jj in range(len(cols)):
                pb = nextbank()
                for kc in range(8):
                    P.mm(pb[:, :], wb[:, kc, jj * 128:(jj + 1) * 128], hb[:, kc, :], start=(kc == 0), stop=(kc == 7),
                         r=[f"{wb.name}_{jj}", hb])
                evacs[jj](tt_, pb)

    for g4 in range(4):
        evs = []
        for jj in range(4):
            cg = g4 * 4 + jj

            def evac_gate(tt_, pb, cg=cg, jj=jj):
                P.act(Ycb[jj][:, tt_ * 512:(tt_ + 1) * 512], pb[:, :], AF.Sigmoid, bias=vp[:, VB_GATE + cg:VB_GATE + cg + 1])
            evs.append(evac_gate)
        inproj_group([COL_GATE + (g4 * 4 + jj) * 128 for jj in range(4)], evs)
        for jj in range(4):
            cg = g4 * 4 + jj
            P.dma('sp', gate_d[cg * 128:(cg + 1) * 128, :], Ycb[jj][:], w=[('gate_d', cg)])
    for j2 in range(4):
        evs = []; cols = []
        for u in range(2):
            j = j2 * 2 + u

            def evac_a(tt_, pb, j=j, u=u):
                P.act(GA[u][:, tt_ * 512:(tt_ + 1) * 512], pb[:, :], AF.Identity, bias=vp[:, VB_GLU + j:VB_GLU + j + 1])

            def evac_g(tt_, pb, j=j, u=u):
                P.act(sgt[:], pb[:, :], AF.Sigmoid, bias=vp[:, VB_GLU + 8 + j:VB_GLU + 8 + j + 1])
                P.tt('dve', Ug[u][:, 15 + tt_ * 512:15 + (tt_ + 1) * 512], GA[u][:, tt_ * 512:(tt_ + 1) * 512], sgt[:], ALU.mult)
            cols += [COL_GLU + j * 128, COL_GLU + 1024 + j * 128]
            evs += [evac_a, evac_g]
        inproj_group(cols, evs)
        for u in range(2):
            j = j2 * 2 + u
            yc = Ycb[u]
            for t in range(31):
                P.ts('dve' if t % 2 == 0 else 'pool', Dg31[:, t, :], identb[:], cwd[:, t * 8 + j:t * 8 + j + 1], None, ALU.mult)
            for tt_ in range(8):
                pb = nextbank()
                for t in range(31):
                    P.mm(pb[:, :], Dg31[:, t, :], Ug[u][:, tt_ * 512 + t:tt_ * 512 + t + 512], start=(t == 0), stop=(t == 30))
                sl = slice(tt_ * 512, (tt_ + 1) * 512)
                P.act(yc[:, sl], pb[:, :], AF.Identity, bias=vp[:, VB_DW + j:VB_DW + j + 1])
                P.act(sq2[:], pb[:, :], AF.Square, bias=vp[:, VB_DW + j:VB_DW + j + 1])
                P.mm(bank[2][0:1, :], onesb[:, 0:1], yc[:, sl])
                P.mm(bank[3][0:1, :], onesb[:, 0:1], sq2[:])
                if j == 0:
                    P.copy('dve', SUM[:, sl], bank[2][0:1, :])
                    P.copy('dve', SSQ[:, sl], bank[3][0:1, :])
                else:
                    P.tt('dve', SUM[:, sl], SUM[:, sl], bank[2][0:1, :], ALU.add)
                    P.tt('dve', SSQ[:, sl], SSQ[:, sl], bank[3][0:1, :], ALU.add)
            P.dma('sp', ybT_d[j * 128:(j + 1) * 128, :], yc[:], w=[('ybT_d', j)])
    stt_ = sb("stt_", [1, 512])
    for tt_ in range(8):
        sl = slice(tt_ * 512, (tt_ + 1) * 512)
        P.ts('dve', SUM[:, sl], SUM[:, sl], 1.0 / 1024.0, None, ALU.mult)
        P.tt('dve', stt_[:], SUM[:, sl], SUM[:, sl], ALU.mult)
        P.stt(SSQ[:, sl], SSQ[:, sl], 1.0 / 1024.0, stt_[:], ALU.mult, ALU.subtract)
        P.act(SSQ[:, sl], SSQ[:, sl], AF.Ln, bias=eps_ln[0:1, 0:1])
        P.act(SSQ[:, sl], SSQ[:, sl], AF.Exp, scale=-0.5)
        P.stt(SUM[:, sl], SUM[:, sl], -1.0, SSQ[:, sl], ALU.mult, ALU.mult)
    RSTD = SSQ; NMR = SUM
    P.dma('sp', st_d[0:1, :], RSTD[:])
    P.dma('sp', st_d[1:2, :], NMR[:])
    s5_mark = A.mark()
    if stage <= 6:
        P.finish()
        return nc, dbg

    A.release(base_mark)
    LG = sb("LG", [128, 32, 32]); M8 = sb("M8", [128, 32, 8]); MSK = sb("MSK", [128, 32, 32], BF16); G4 = sb("G4", [128, 32, 4])
    moe_mark = A.mark()
    Wao = sb("Wao", [128, 8, 1024], BF16); Wbo = sb("Wbo", [128, 8, 1024], BF16); Wout = sb("Wout", [128, 8, 1024], BF16)
    P.dma('pool', Wao[:], w_a_o.rearrange("(kc p) c -> p kc c", p=128))
    P.dma('pool', Wbo[:], w_b_o.rearrange("(kc p) c -> p kc c", p=128))
    P.dma('pool', Wout[:], w_out.rearrange("(kc p) c -> p kc c", p=128))
    Wr = sb("Wr", [128, 8, 32]); brB = sb("brB", [128, 32])
    P.dma('sp', Wr[:], w_router.rearrange("(kc p) c -> p kc c", p=128))
    P.dma('sp', brB[:], b_router.partition_broadcast(128))
    lnp['g'] = sb("gB1", [128, D]); lnp['b'] = sb("bB1", [128, D])
    P.dma('sp', lnp['g'][:], ln1_g.partition_broadcast(128))
    P.dma('sp', lnp['b'][:], ln1_b.partition_broadcast(128))
    ogt2 = [sb(f"ogt{i}", [128, 8, 512], BF16) for i in range(2)]; ybt2 = [sb(f"ybt{i}", [128, 8, 512], BF16) for i in range(2)]
    gtt2 = [sb(f"gtt{i}", [128, 16, 512], BF16) for i in range(2)]
    ybaT = sb("ybaT", [128, 8, 512], BF16); mixT = sb("mixT", [128, 8, 512], BF16)
    t1s = [sb(f"t1_{i}", [128, 512]) for i in range(2)]; t2s = [sb(f"t2_{i}", [128, 512]) for i in range(2)]
    rs_t = sb("rs_t", [1, 512]); nm_t = sb("nm_t", [1, 512])
    h0t2 = [sb(f"h0t{i}", [128, D]) for i in range(2)]; rt_2 = [sb(f"rt{i}", [128, D]) for i in range(2)]
    h1t2 = [sb(f"h1t{i}", [128, D]) for i in range(2)]; h1Tf2 = [sb(f"h1Tf{i}", [128, 8, 128]) for i in range(2)]
    nmx2 = [sb(f"nmx{i}", [128, 1]) for i in range(2)]; esum2 = [sb(f"esum{i}", [128, 1]) for i in range(2)]
    h1tb2 = [sb(f"h1tb{i}", [128, D], BF16) for i in range(2)]
    ogv = ogT_d.rearrange("(j p) t -> p j t", p=128)
    ybv = ybT_d.rearrange("(j p) t -> p j t", p=128)
    gtv = gate_d.rearrange("(j p) t -> p j t", p=128)
    def s6_M(tt_):
        sl = slice(tt_ * 512, (tt_ + 1) * 512)
        ogt = ogt2[tt_ % 2]; ybt = ybt2[tt_ % 2]; gtt = gtt2[tt_ % 2]
        P.dma('sp', ogt[:], ogv[:, :, sl], r=[('ogT_d', j_) for j_ in range(8)])
        P.dma('sp', ybt[:], ybv[:, :, sl], r=[('ybT_d', j_) for j_ in range(8)])
        P.dma('sp', gtt[:], gtv[:, :, sl], r=[('gate_d', j_) for j_ in range(16)])
        P.dma('sp', rs_t[:], st_d[0:1, sl])
        P.dma('sp', nm_t[:], st_d[1:2, sl])
        P.mm(bank[6][:, :], onesf[0:1, :], rs_t[0:1, :])
        P.mm(bank[7][:, :], onesf[0:1, :], nm_t[0:1, :])
        for j in range(8):
            t1 = t1s[j % 2]
            P.tt('dve', t1[:], ybt[:, j, :], bank[6][:, :], ALU.mult)
            P.tt('dve', t1[:], t1[:], bank[7][:, :], ALU.add)
            P.act(ybaT[:, j, :], t1[:], AF.Silu, bias=vp[:, VB_LNB + j:VB_LNB + j + 1], scale=vp[:, VB_LNG + j:VB_LNG + j + 1])
        if debug and tt_ == 0:
            dbg_out("dbg_ybaT", ybaT[:], [128, 8, 512], BF16)
        for m in range(8):
            pa = bank[(m % 2) * 2]; pb_ = bank[(m % 2) * 2 + 1]
            t1 = t1s[m % 2]; t2 = t2s[m % 2]
            for kc in range(8):
                P.mm(pa[:, :], Wao[:, kc, m * 128:(m + 1) * 128], ogt[:, kc, :], start=(kc == 0), stop=(kc == 7))
            for kc in range(8):
                P.mm(pb_[:, :], Wbo[:, kc, m * 128:(m + 1) * 128], ybaT[:, kc, :], start=(kc == 0), stop=(kc == 7))
            P.tt('dve', t1[:], pa[:, :], gtt[:, m, :], ALU.mult)
            P.stt(t2[:], pb_[:, :], vp[:, VB_BBO + m:VB_BBO + m + 1], gtt[:, 8 + m, :], ALU.add, ALU.mult)
            P.tt('pool', mixT[:, m, :], t1[:], t2[:], ALU.add)
    def s6_A(tt_, sub):
        i = tt_ * 4 + sub
        h0t = h0t2[i % 2]; rt = rt_2[i % 2]; h1t = h1t2[i % 2]; h1Tf = h1Tf2[i % 2]
        nmx = nmx2[i % 2]; esum = esum2[i % 2]; h1tb = h1tb2[i % 2]
        P.dma('sp', h0t[:], h0_d[i * 128:(i + 1) * 128, :], r=[('h0_d', i)])
        for hf in range(2):
            pm = bank[4 + hf]
            for m in range(8):
                P.mm(pm[:, :], mixT[:, m, sub * 128:(sub + 1) * 128], Wout[:, m, hf * 512:(hf + 1) * 512], start=(m == 0), stop=(m == 7))
            P.stt(rt[:, hf * 512:(hf + 1) * 512], h0t[:, hf * 512:(hf + 1) * 512], DN_ALPHA, pm[:, :], ALU.mult, ALU.add)
        layer_norm_rows(rt, h1t)
        P.dma('sp', h1_d[i * 128:(i + 1) * 128, :], h1t[:], w=[('h1_d', i)])
    def s6_B(tt_, sub):
        i = tt_ * 4 + sub
        h0t = h0t2[i % 2]; rt = rt_2[i % 2]; h1t = h1t2[i % 2]; h1Tf = h1Tf2[i % 2]
        nmx = nmx2[i % 2]; esum = esum2[i % 2]; h1tb = h1tb2[i % 2]
        for hf in range(2):
            pt = bank[6 + hf]
            for q4 in range(4):
                kc = hf * 4 + q4
                P.tr(pt[:, q4 * 128:(q4 + 1) * 128], h1t[:, kc * 128:(kc + 1) * 128], identf[:])
            P.copy('dve', h1Tf[:, hf * 4:(hf + 1) * 4, :], pt[:].rearrange("p (k t) -> p k t", t=128))
        pr_ = bank[(sub % 2) * 2]
        for kc in range(8):
            P.mm(pr_[:, 0:32], h1Tf[:, kc, :], Wr[:, kc, :], start=(kc == 0), stop=(kc == 7))
        P.tt('dve', LG[:, i, :], pr_[:, 0:32], brB[:], ALU.add)
        P.op('dve', lambda e, i=i: e.max(out=M8[:, i, :], in_=LG[:, i, :]), [LG], [M8])
        P.ts('dve', MSK[:, i, :], LG[:, i, :], M8[:, i, 3:4], None, ALU.is_ge)
        P.ts('dve', nmx[:], M8[:, i, 0:1], -1.0, None, ALU.mult)
        P.act(G4[:, i, :], M8[:, i, 0:4], AF.Exp, bias=nmx[:, 0:1])
        P.op('dve', lambda e, i=i, esum=esum: e.tensor_reduce(out=esum[:], in_=G4[:, i, :], axis=AX.X, op=ALU.add), [G4], [esum])
        P.op('dve', lambda e, esum=esum: e.reciprocal(out=esum[:], in_=esum[:]), [esum], [esum])
        P.ts('dve', G4[:, i, :], G4[:, i, :], esum[:, 0:1], None, ALU.mult)
        P.copy('act', h1tb[:], h1t[:])
        P.dma('sp', h1b_d[i * 128:(i + 1) * 128, :], h1tb[:], w=[('h1b_d', i)])

    pend_B = []
    for tt_ in range(8):
        s6_M(tt_)
        for pb_item in pend_B:
            s6_B(*pb_item)
        pend_B = []
        for sub in range(4):
            s6_A(tt_, sub)
            if sub >= 1:
                s6_B(tt_, sub - 1)
        pend_B.append((tt_, 3))
    for pb_item in pend_B:
        s6_B(*pb_item)
    if debug:
        dbg_out("dbg_G4", G4[:], [128, 32, 4], F32)
        dbg_out("dbg_MSK", MSK[:], [128, 32, 32], BF16)
    if stage <= 7:
        P.finish()
        return nc, dbg

    A.release(moe_mark)
    I32 = mybir.dt.int32
    NBLK = 64
    lnp['g'] = sb("gB2", [128, D]); lnp['b'] = sb("bB2", [128, D])
    P.dma('sp', lnp['g'][:], ln2_g.partition_broadcast(128))
    P.dma('sp', lnp['b'][:], ln2_b.partition_broadcast(128))
    stri = sb("stri", [128, 128], BF16)
    strf = sb("strf", [128, 128])
    P.op('pool', lambda e: e.affine_select(out=strf[:], in_=onesf[:], pattern=[[1, 128]], compare_op=ALU.is_gt, fill=0.0,
                                           base=0, channel_multiplier=-1), [onesf], [strf])
    P.copy('dve', stri[:], strf[:])
    POS = sb("POS", [128, 32, 32]); run = sb("run", [128, 32], BF16)
    P.memset('pool', run[:], 0.0)
    for i in range(32):
        pp = bank[i % 2]
        P.mm(pp[:, 0:32], stri[:], MSK[:, i, :], start=True, stop=False)
        P.mm(pp[:, 0:32], onesb[:], run[:], start=False, stop=True)
        P.copy('act', POS[:, i, :], pp[:, 0:32])
        P.tt('dve', run[:], run[:], MSK[:, i, :], ALU.add)
    cnt = sb("cnt", [128, 32]); padded = sb("padded", [128, 32]); pend = sb("pend", [128, 32]); pstart = sb("pstart", [128, 32])
    tmp32 = sb("tmp32", [128, 32]); ones32 = sb("ones32", [128, 32])
    P.memset('pool', ones32[:], 1.0)
    P.mm(bank[2][:, 0:32], onesb[:], run[:])
    P.copy('dve', cnt[:], bank[2][:, 0:32])
    P.memset('pool', padded[:], 0.0)
    for j in range(8):
        P.ts('dve', tmp32[:], cnt[:], float(512 * j), 512.0, ALU.is_gt, ALU.mult)
        P.tt('dve', padded[:], padded[:], tmp32[:], ALU.add)
    P.op('dve', lambda e: e.tensor_tensor_scan(out=pend[:], data0=ones32[:], data1=padded[:], initial=0.0, op0=ALU.mult, op1=ALU.add),
         [ones32, padded], [pend])
    P.tt('dve', pstart[:], pend[:], padded[:], ALU.subtract)
    bthr = sb("bthr", [128, NBLK, 32]); BE = sb("BE", [128, NBLK])
    P.op('pool', lambda e: e.iota(out=bthr[:], pattern=[[512, NBLK], [0, 32]], base=0, channel_multiplier=0,
                                  allow_small_or_imprecise_dtypes=True), [], [bthr])
    P.tt('dve', bthr[:], bc(pend[:], [128, NBLK, 32], 1), bthr[:], ALU.is_le)
    P.op('dve', lambda e: e.tensor_reduce(out=BE[:], in_=bthr[:], axis=AX.X, op=ALU.add), [bthr], [BE])
    P.ts('dve', BE[:], BE[:], 31.0, None, ALU.min)
    basef = sb("basef", [128, 8]); IDXF = sb("IDXF", [128, NBLK, 8]); IDXW = sb("IDXW", [128, NBLK, 8], I32)
    P.op('pool', lambda e: e.iota(out=basef[:], pattern=[[128, 8]], base=0, channel_multiplier=1,
                                  allow_small_or_imprecise_dtypes=True), [], [basef])
    P.stt(IDXF[:], bc(BE[:], [128, NBLK, 8], 2), 1024.0, bc(basef[:], [128, NBLK, 8], 1), ALU.mult, ALU.add)
    P.copy('dve', IDXW[:], IDXF[:])
    pidx = sb("pidx", [32, 1]); OH = sb("OH", [32, NBLK], BF16)
    P.op('pool', lambda e: e.iota(out=pidx[:], pattern=[[0, 1]], base=0, channel_multiplier=1,
                                  allow_small_or_imprecise_dtypes=True), [], [pidx])
    P.ts('dve', OH[:], BE[0:32, :], pidx[0:32, 0:1], None, ALU.is_equal)
    thrb = sb("thrb", [1, NBLK]); ACTF = sb("ACTF", [1, NBLK], I32)
    P.op('pool', lambda e: e.iota(out=thrb[:], pattern=[[512, NBLK]], base=0, channel_multiplier=0,
                                  allow_small_or_imprecise_dtypes=True), [], [thrb])
    P.ts('dve', thrb[:], thrb[:], pend[0:1, 31:32], None, ALU.is_lt)
    P.copy('dve', ACTF[:], thrb[:])
    bgu_all = sb("bgu_all", [32, 2048], BF16); bd_all = sb("bd_all", [32, D], BF16)
    P.dma('pool', bgu_all[:], b_gu)
    P.dma('pool', bd_all[:], b_down)
    SLOTF = sb("SLOTF", [128, 32, 4]); SLOT = sb("SLOT", [128, 32, 4], I32)
    valt = sb("valt", [128, 32]); mk = sb("mk", [128, 4, 32])
    for i in range(32):
        P.tt('dve', valt[:], POS[:, i, :], pstart[:], ALU.add)
        P.tt('dve', mk[:], bc(LG[:, i, :], [128, 4, 32], 1), bc(M8[:, i, 0:4], [128, 4, 32], 2), ALU.is_equal)
        P.tt('dve', mk[:], mk[:], bc(valt[:], [128, 4, 32], 1), ALU.mult)
        P.op('dve', lambda e, i=i: e.tensor_reduce(out=SLOTF[:, i, :], in_=mk[:], axis=AX.X, op=ALU.add), [mk], [SLOTF])
    P.copy('dve', SLOT[:], SLOTF[:])
    if debug:
        dbg_out("dbg_SLOT", SLOT[:], [128, 32, 4], I32)
        dbg_out("dbg_BE", BE[:], [128, NBLK], F32)
        dbg_out("dbg_cnt", cnt[:], [128, 32], F32)
    disp_mark = A.mark()
    hb_t = [sb(f"hb_t{i}", [128, D], BF16) for i in range(2)]
    for i in range(32):
        hb_ = hb_t[i % 2]
        P.dma('sp', hb_[:], h1b_d[i * 128:(i + 1) * 128, :], r=[('h1b_d', i)])
        for k in range(4):
            P.idma(xs_d, hb_[:], out_off=SLOT[:, i, k:k + 1], w=[('xs', i, k)])
    A.release(disp_mark)
    if debug and os.environ.get("MK_MOE_STOP") == "1":
        P.finish()
        return nc, dbg
    wguB = [sb(f"wguB{i}", [128, 8, 2048], BF16) for i in range(2)]
    wdB = [sb(f"wdB{i}", [128, 8, D], BF16) for i in range(2)]
    xtm = [sb(f"xtm{i}", [128, 4, D], BF16) for i in range(2)]; xT = sb("xT", [128, 8, 512], BF16)
    actT = sb("actT", [128, 8, 512], BF16)
    OHs = [sb(f"OHs{i}", [32, 512], BF16) for i in range(2)]
    NROT = 2
    gl = [sb(f"gl{i}", [128, 512]) for i in range(NROT)]; sg = [sb(f"sg{i}", [128, 512]) for i in range(NROT)]
    li = [sb(f"li{i}", [128, 512]) for i in range(NROT)]; p1 = [sb(f"p1{i}", [128, 512]) for i in range(NROT)]
    ysb = [sb(f"ysb{i}", [128, D]) for i in range(2)]
    wgu_rows = w_gu.rearrange("e k c -> (e k) c")
    wd_rows = w_down.rearrange("e k c -> (e k) c")
    nblk_run = int(os.environ.get("MK_NBLK", str(NBLK))) if debug else NBLK
    SIG_MAX = float(1.0 / (1.0 + np.exp(-1.702 * 7.0)))

    def load_block_weights(b):
        for kc in range(8):
            P.idma(wguB[b % 2][:, kc, :], wgu_rows, in_off=IDXW[:, b, kc:kc + 1], r=[IDXW], w=[f"wguB{b % 2}_{kc}"])
        for kc in range(8):
            P.idma(wdB[b % 2][:, kc, :], wd_rows, in_off=IDXW[:, b, kc:kc + 1], r=[IDXW], w=[f"wdB{b % 2}_{kc}"])

    def load_block_x(b):
        P.dma('sp', xtm[b % 2][:], xs_d[b * 512:(b + 1) * 512, :].rearrange("(s p) d -> p s d", p=128), r=[])
        P.copy('act', OHs[b % 2][:], OH[:, b:b + 1].to_broadcast([32, 512]))

    l1b = [0]; l2b = [0]; ycnt = [0]; rot = [0]
    load_block_weights(0)
    load_block_x(0)
    use_skip = os.environ.get("MK_NOSKIP") is None
    for b in range(nblk_run):
        if use_skip:
            P.cond_begin(ACTF[0:1, b:b + 1])
        if b + 1 < nblk_run:
            load_block_weights(b + 1)
            load_block_x(b + 1)
        wg_ = wguB[b % 2]; wd_ = wdB[b % 2]; oh_ = OHs[b % 2]; xt_ = xtm[b % 2]
        for k2 in range(4):
            pbf = bank_bf(4 + k2 % 2)
            for kk in range(2):
                kc = k2 * 2 + kk
                for sub in range(4):
                    P.tr(pbf[:, kk * 512 + sub * 128:kk * 512 + (sub + 1) * 128], xt_[:, sub, kc * 128:(kc + 1) * 128], identb[:])
            P.copy('act' if k2 % 2 == 0 else 'dve', xT[:, k2 * 2:k2 * 2 + 2, :], pbf[:, :].rearrange("p (k t) -> p k t", t=512))
        for m in range(8):
            pbg = bank[l1b[0] % 4]; pbl = bank[(l1b[0] + 1) % 4]; l1b[0] += 2
            ri = rot[0] % NROT; rot[0] += 1
            for kc in range(8):
                P.mm(pbg[:, :], wg_[:, kc, m * 128:(m + 1) * 128], xT[:, kc, :], start=(kc == 0), stop=False, r=[f"wguB{b % 2}_{kc}", xT])
            P.mm(pbg[:, :], bgu_all[:, m * 128:(m + 1) * 128], oh_[:], start=False, stop=True)
            for kc in range(8):
                P.mm(pbl[:, :], wg_[:, kc, 1024 + m * 128:1024 + (m + 1) * 128], xT[:, kc, :], start=(kc == 0), stop=False, r=[f"wguB{b % 2}_{kc}", xT])
            P.mm(pbl[:, :], bgu_all[:, 1024 + m * 128:1024 + (m + 1) * 128], oh_[:], start=False, stop=True)
            P.act(sg[ri][:], pbg[:, :], AF.Sigmoid, scale=1.702)
            P.ts('dve', gl[ri][:], pbg[:, :], 7.0, None, ALU.min)
            P.ts('dve', li[ri][:], pbl[:, :], 7.0, -7.0, ALU.min, ALU.max)
            P.stt(p1[ri][:], sg[ri][:], SIG_MAX, gl[ri][:], ALU.min, ALU.mult)
            P.stt(actT[:, m, :], li[ri][:], 1.0, p1[ri][:], ALU.add, ALU.mult)
        for sub in range(4):
            yb_ = ysb[ycnt[0] % 2]; ycnt[0] += 1
            for hf in range(2):
                pb = bank[4 + l2b[0] % 4]; l2b[0] += 1
                for m in range(8):
                    P.mm(pb[:, :], actT[:, m, sub * 128:(sub + 1) * 128], wd_[:, m, hf * 512:(hf + 1) * 512], start=(m == 0), stop=False, r=[f"wdB{b % 2}_{m}", actT])
                P.mm(pb[:, :], oh_[:, 0:128], bd_all[:, hf * 512:(hf + 1) * 512], start=False, stop=True)
                P.copy('act' if hf == 0 else 'pool_never', yb_[:, hf * 512:(hf + 1) * 512], pb[:, :]) if hf == 0 else P.copy('dve', yb_[:, hf * 512:(hf + 1) * 512], pb[:, :])
            P.dma('sp', ys_d[b * 512 + sub * 128:b * 512 + (sub + 1) * 128, :], yb_[:], w=[('ys', b, sub)])
        if use_skip:
            P.cond_end()
    A.release(disp_mark)
    if debug and os.environ.get("MK_MOE_STOP") == "2":
        P.finish()
        return nc, dbg
    yk = [[sb(f"yk{j}_{k}", [128, D]) for k in range(4)] for j in range(2)]
    rt2 = [sb(f"rt2_{j}", [128, D]) for j in range(2)]; ot = [sb(f"ot{j}", [128, D]) for j in range(2)]
    for i in range(32):
        j = i % 2
        P.dma('sp', rt2[j][:], h1_d[i * 128:(i + 1) * 128, :], r=[('h1_d', i)])
        for k in range(4):
            P.idma(yk[j][k][:], ys_d, in_off=SLOT[:, i, k:k + 1], r=[SLOT])
        P.ts('dve', rt2[j][:], rt2[j][:], DN_ALPHA, None, ALU.mult)
        for k in range(4):
            P.stt(rt2[j][:], yk[j][k][:], G4[:, i, k:k + 1], rt2[j][:], ALU.mult, ALU.add)
        layer_norm_rows(rt2[j], ot[j])
        P.dma('sp', out[i * 128:(i + 1) * 128, :], ot[j][:])
    P.finish()
    return nc, dbg


_IN_NAMES = ['emb_ln_g', 'emb_ln_b', 'w_in', 'conv_qkv', 'a_log', 'dt_bias', 'dn_norm_g', 'w_a_o', 'b_glu', 'conv_dw',
             'b_dw', 'conv_ln_g', 'conv_ln_b', 'w_b_o', 'b_b_o', 'b_gate', 'w_out', 'ln1_g', 'ln1_b', 'w_router',
             'b_router', 'w_gu', 'b_gu', 'w_down', 'b_down', 'ln2_g', 'ln2_b']


def make_in_maps(inputs, n_cores=8):
    shared = {}
    for k in _IN_NAMES:
        a = np.ascontiguousarray(np.asarray(inputs[k], dtype=np.float32))
        a = a[0] if k not in ('emb_ln_g', 'emb_ln_b') else a
        if k in ('a_log', 'dt_bias'):
            a = a.reshape(16)
        shared[k] = np.ascontiguousarray(a)
    xs = np.asarray(inputs['x'], dtype=np.float32)
    maps = []
    for c in range(n_cores):
        m = dict(shared)
        m['x'] = np.ascontiguousarray(xs[c])
        maps.append(m)
    return maps


def kernel(**inputs):
    nc, _ = build_program()
    maps = make_in_maps(inputs, 8)
    res = run_bass_kernel_spmd(nc, maps, core_ids=list(range(8)))
    return np.stack([np.asarray(res.results[c]["out"]) for c in range(8)], axis=0).astype(np.float32)
```

```python
import os
import numpy as np
import concourse.bass as bass
import concourse.mybir as mybir
from concourse.bass_utils import run_bass_kernel_spmd

F32 = mybir.dt.float32
BF16 = mybir.dt.bfloat16
AF = mybir.ActivationFunctionType
ALU = mybir.AluOpType
AX = mybir.AxisListType

S = 4096
D = 1024
NCOL = 8224
NCH = 64
DN_ALPHA = 2.0 ** 0.25
LN_EPS = 1e-5
RMS_EPS = 1e-6
L2_EPS = 1e-6
NEG_BIG = -1.0e30
COL_Q, COL_K, COL_V, COL_Z, COL_SM, COL_GLU, COL_GATE = 0, 1024, 2048, 3072, 4096, 4128, 6176


class Prog:
    def __init__(self, nc, n_dma_sems=12, self_sync=True):
        self.nc = nc
        self.eng = {'pe': nc.tensor, 'act': nc.scalar, 'dve': nc.vector, 'pool': nc.gpsimd, 'sp': nc.sync}
        self.streams = {e: [] for e in self.eng}
        self.cnt = {e: 0 for e in self.eng}
        self.sem = {e: nc.alloc_semaphore(name=f"s_{e}") for e in self.eng}
        self.seen = {e: {} for e in self.eng}
        self.lastw = {}
        self.reads = {}
        self.semobj = {('e', e): self.sem[e] for e in self.eng}
        self.dma_sems = {}
        self.n_dma_sems = n_dma_sems
        self.self_sync = self_sync
        self.n_inst = 0

    def _dma_sem(self, q):
        if q not in self.dma_sems:
            lst = []
            for i in range(self.n_dma_sems):
                s = self.nc.alloc_semaphore(name=f"d_{q}_{i}")
                key = ('d', q, i)
                self.semobj[key] = s
                lst.append([key, 0])
            self.dma_sems[q] = [lst, 0]
        lst, idx = self.dma_sems[q]
        self.dma_sems[q][1] = (idx + 1) % len(lst)
        return lst[idx]

    def _deps(self, e, reads, writes):
        deps = {}

        def add(k, v):
            if deps.get(k, 0) < v:
                deps[k] = v
        for r in reads:
            ev = self.lastw.get(r)
            if ev is not None:
                add(*ev)
        for w in writes:
            ev = self.lastw.get(w)
            if ev is not None:
                add(*ev)
            for k, v in self.reads.get(w, {}).items():
                add(k, v)
        waits = []
        for k, v in deps.items():
            if k == ('e', e):
                if not self.self_sync or e in ('pe', 'sp'):
                    continue
            if self.seen[e].get(k, 0) >= v:
                continue
            self.seen[e][k] = v
            waits.append((self.semobj[k], v))
        return waits

    def _commit(self, ev, reads, writes):
        k, v = ev
        for w in writes:
            self.lastw[w] = ev
            self.reads[w] = {}
        for r in reads:
            d = self.reads.setdefault(r, {})
            if d.get(k, 0) < v:
                d[k] = v

    @staticmethod
    def _names(aps):
        out = []
        for a in aps:
            if a is None or isinstance(a, (int, float)):
                continue
            if isinstance(a, (str, tuple)):
                out.append(a)
            else:
                out.append(a.name)
        return out

    def op(self, e, fn, reads, writes):
        reads = self._names(reads)
        writes = self._names(writes)
        pr = [n for n in reads if isinstance(n, str) and n.startswith('bank')]
        if pr:
            reads = [n for n in reads if n not in pr]
            writes = writes + [n for n in pr if n not in writes]
        waits = self._deps(e, reads, writes)
        self.cnt[e] += 1
        seq = self.cnt[e]
        sem = self.sem[e]

        def emit(engobj, waits=waits, fn=fn, sem=sem):
            for s, v in waits:
                engobj.wait_ge(s, v)
            fn(engobj).then_inc(sem, 1)
        self.streams[e].append(emit)
        self._commit((('e', e), seq), reads, writes)
        self.n_inst += 1

    def dma(self, q, out, in_, r=None, w=None, **kw):
        reads = self._names(r if r is not None else [in_])
        writes = self._names(w if w is not None else [out])
        slot = self._dma_sem(q)
        key, prev = slot
        waits = self._deps(q, reads, writes)
        if prev > 0 and self.seen[q].get(key, 0) < prev:
            self.seen[q][key] = prev
            waits.append((self.semobj[key], prev))
        slot[1] = prev + 16
        tgt = slot[1]
        s = self.semobj[key]

        def emit(engobj, waits=waits, s=s, out=out, in_=in_, kw=kw):
            for ss, v in waits:
                engobj.wait_ge(ss, v)
            engobj.dma_start(out=out, in_=in_, **kw).then_inc(s, 16)
        self.streams[q].append(emit)
        self._commit((key, tgt), reads, writes)
        self.n_inst += 1

    def idma(self, out, in_, out_off=None, in_off=None, r=None, w=None):
        q = 'pool'
        reads = self._names(r if r is not None else [in_, out_off, in_off])
        writes = self._names(w if w is not None else [out])
        slot = self._dma_sem(q)
        key, prev = slot
        waits = self._deps(q, reads, writes)
        if prev > 0 and self.seen[q].get(key, 0) < prev:
            self.seen[q][key] = prev
            waits.append((self.semobj[key], prev))
        slot[1] = prev + 16
        tgt = slot[1]
        s = self.semobj[key]

        def emit(engobj, waits=waits, s=s):
            for ss, v in waits:
                engobj.wait_ge(ss, v)
            oo = bass.IndirectOffsetOnAxis(ap=out_off, axis=0) if out_off is not None else None
            io = bass.IndirectOffsetOnAxis(ap=in_off, axis=0) if in_off is not None else None
            engobj.indirect_dma_start(out=out, out_offset=oo, in_=in_, in_offset=io).then_inc(s, 16)
        self.streams[q].append(emit)
        self._commit((key, tgt), reads, writes)
        self.n_inst += 1

    def cond_begin(self, flag_ap):
        st = {'cnt0': dict(self.cnt), 'seen0': {e: dict(d) for e, d in self.seen.items()}, 'dma0': {}, 'ctx': {}, 'used': {}}
        for q, (lst, _) in self.dma_sems.items():
            for key, val in lst:
                st['dma0'][key] = val
        self._cond = st
        if not hasattr(self, '_cregs'):
            self._cregs = {}
        for e in self.eng:
            waits = self._deps(e, self._names([flag_ap]), [])

            def emit(engobj, waits=waits, e=e, st=st):
                if not st['used'].get(e):
                    return
                for s_, v in waits:
                    engobj.wait_ge(s_, v)
                if e not in self._cregs:
                    self._cregs[e] = engobj.alloc_register(f"cflag_{e}")
                r = self._cregs[e]
                engobj.reg_load(r, flag_ap)
                v = engobj.snap(r)
                ctx = engobj.If(v == 1)
                ctx.__enter__()
                st['ctx'][e] = (ctx, v)
            self.streams[e].append(emit)

    def cond_end(self):
        st = self._cond
        for e in self.eng:
            n_e = self.cnt[e] - st['cnt0'].get(e, 0)
            comp = []
            if e in self.dma_sems:
                for key, val in self.dma_sems[e][0]:
                    d = val - st['dma0'].get(key, 0)
                    if d:
                        comp.append((self.semobj[key], d, st['dma0'].get(key, 0)))
            st['used'][e] = bool(n_e or comp)

            def emit(engobj, e=e, n_e=n_e, comp=comp, st=st):
                if not st['used'].get(e):
                    return
                ctx, v = st['ctx'][e]
                ctx.__exit__(None, None, None)
                with engobj.Else():
                    engobj.drain()
                    for so, d, before in comp:
                        if before > 0:
                            engobj.wait_ge(so, before)
                        engobj.sem_inc(so, d)
                    if n_e:
                        engobj.sem_inc(self.sem[e], n_e)
                engobj.free_register(v.val)
            self.streams[e].append(emit)
        self.seen = st['seen0']
        self._cond = None

    def barrier(self):
        cur = {}
        for e in self.eng:
            if self.cnt[e] > 0:
                cur[('e', e)] = self.cnt[e]
        for q, (lst, _) in self.dma_sems.items():
            for key, val in lst:
                if val > 0:
                    cur[key] = val
        for e in self.eng:
            waits = []
            for k, v in cur.items():
                if k == ('e', e):
                    continue
                if self.seen[e].get(k, 0) >= v:
                    continue
                self.seen[e][k] = v
                waits.append((self.semobj[k], v))

            def emit(engobj, waits=waits):
                for s, v in waits:
                    engobj.wait_ge(s, v)
            self.streams[e].append(emit)

    def finish(self):
        self.barrier()
        nc = self.nc
        with nc.Block() as block:
            @block.tensor
            def _(e):
                for f in self.streams['pe']:
                    f(e)

            @block.scalar
            def _(e):
                for f in self.streams['act']:
                    f(e)

            @block.vector
            def _(e):
                for f in self.streams['dve']:
                    f(e)

            @block.gpsimd
            def _(e):
                for f in self.streams['pool']:
                    f(e)

            @block.sync
            def _(e):
                for f in self.streams['sp']:
                    f(e)

    def mm(self, out, lhsT, rhs, start=True, stop=True, r=None, w=None):
        self.op('pe', lambda e: e.matmul(out, lhsT, rhs, start=start, stop=stop),
                r if r is not None else [lhsT, rhs], w if w is not None else [out])

    def tr(self, out, in_, ident, r=None, w=None):
        self.op('pe', lambda e: e.transpose(out=out, in_=in_, identity=ident),
                r if r is not None else [in_, ident], w if w is not None else [out])

    def act(self, out, in_, func, bias=None, scale=None, accum=None, r=None, w=None):
        kw = {}
        if bias is not None:
            kw['bias'] = bias
        if scale is not None:
            kw['scale'] = scale
        if accum is not None:
            kw['accum_out'] = accum
        self.op('act', lambda e: e.activation(out=out, in_=in_, func=func, **kw),
                r if r is not None else [in_, bias, scale], w if w is not None else [out, accum])

    def tt(self, eng, out, in0, in1, op, r=None, w=None):
        self.op(eng, lambda e: e.tensor_tensor(out=out, in0=in0, in1=in1, op=op),
                r if r is not None else [in0, in1], w if w is not None else [out])

    def ts(self, eng, out, in0, s1, s2, op0, op1=None, r=None, w=None):
        if op1 is None:
            fn = lambda e: e.tensor_scalar(out=out, in0=in0, scalar1=s1, scalar2=None, op0=op0)
        else:
            fn = lambda e: e.tensor_scalar(out=out, in0=in0, scalar1=s1, scalar2=s2, op0=op0, op1=op1)
        self.op(eng, fn, r if r is not None else [in0, s1, s2], w if w is not None else [out])

    def stt(self, out, in0, scalar, in1, op0, op1, r=None, w=None):
        self.op('dve', lambda e: e.scalar_tensor_tensor(out=out, in0=in0, scalar=scalar, in1=in1, op0=op0, op1=op1),
                r if r is not None else [in0, scalar, in1], w if w is not None else [out])

    def copy(self, eng, out, in_, r=None, w=None):
        if eng == 'act':
            fn = lambda e: e.copy(out=out, in_=in_)
        else:
            fn = lambda e: e.tensor_copy(out=out, in_=in_)
        self.op(eng, fn, r if r is not None else [in_], w if w is not None else [out])

    def memset(self, eng, ap, val):
        self.op(eng, lambda e: e.memset(ap, val), [], [ap])


def bc(ap, shape, axis):
    return ap.unsqueeze(axis).to_broadcast(shape)


class Arena:
    def __init__(self, nc, P, base=16512, top=229344):
        self.nc = nc; self.P = P; self.ptr = base; self.top = top; self.n = 0; self.peak = base

    def alloc(self, name, shape, dt=F32):
        size = 2 if dt == BF16 else 4
        n = 1
        for d in shape[1:]:
            n *= d
        nbytes = (n * size + 63) // 64 * 64
        self.n += 1
        t = self.nc.alloc_sbuf_tensor_at(f"{name}_{self.n}", list(shape), dt, offset=self.ptr)
        self.ptr += nbytes
        self.peak = max(self.peak, self.ptr)
        assert self.ptr <= self.top, f"SBUF arena overflow at {name}: {self.ptr} > {self.top}"
        return t

    def mark(self):
        return self.ptr

    def release(self, m):
        self.P.barrier()
        self.ptr = m


def build_program(stage=99, debug=False):
    nc = bass.Bass("TRN2", target_bir_lowering=False)
    P = Prog(nc)
    A = Arena(nc, P)
    dbg = {}

    def dram_in(name, shape):
        return nc.dram_tensor(name, shape, F32, kind="ExternalInput").ap()

    def scratch(name, shape, dt, force_internal=False):
        kind = "ExternalOutput" if (debug and not force_internal) else "Internal"
        return nc.dram_tensor(name, shape, dt, kind=kind).ap()

    def dbg_out(name, src_ap, shape, dt, r=None):
        if not debug:
            return
        t = nc.dram_tensor(name, shape, dt, kind="ExternalOutput").ap()
        P.dma('sp', t, src_ap, r=r)

    sb = A.alloc

    x = dram_in("x", [S, D])
    emb_ln_g = dram_in("emb_ln_g", [D]); emb_ln_b = dram_in("emb_ln_b", [D])
    w_in = dram_in("w_in", [D, NCOL])
    conv_qkv = dram_in("conv_qkv", [5, 3072])
    a_log = dram_in("a_log", [16]); dt_bias = dram_in("dt_bias", [16])
    dn_norm_g = dram_in("dn_norm_g", [128])
    w_a_o = dram_in("w_a_o", [D, D])
    b_glu = dram_in("b_glu", [2048]); conv_dw = dram_in("conv_dw", [31, D]); b_dw = dram_in("b_dw", [D])
    conv_ln_g = dram_in("conv_ln_g", [D]); conv_ln_b = dram_in("conv_ln_b", [D])
    w_b_o = dram_in("w_b_o", [D, D]); b_b_o = dram_in("b_b_o", [D]); b_gate = dram_in("b_gate", [2048])
    w_out = dram_in("w_out", [D, D])
    ln1_g = dram_in("ln1_g", [D]); ln1_b = dram_in("ln1_b", [D])
    w_router = dram_in("w_router", [D, 32]); b_router = dram_in("b_router", [32])
    w_gu = dram_in("w_gu", [32, D, 2048]); b_gu = dram_in("b_gu", [32, 2048])
    w_down = dram_in("w_down", [32, D, D]); b_down = dram_in("b_down", [32, D])
    ln2_g = dram_in("ln2_g", [D]); ln2_b = dram_in("ln2_b", [D])
    out = nc.dram_tensor("out", [S, D], F32, kind="ExternalOutput").ap()

    h0_d = scratch("h0_d", [S, D], F32)
    hT_d = scratch("hT_d", [D, S], BF16)
    ogT_d = scratch("ogT_d", [D, S], BF16)
    ybT_d = scratch("ybT_d", [D, S], BF16)
    gate_d = scratch("gate_d", [2048, S], BF16)
    h1_d = scratch("h1_d", [S, D], F32)
    st_d = scratch("st_d", [2, S], F32)
    h1b_d = scratch("h1b_d", [S, D], BF16)
    xs_d = scratch("xs_d", [32768, D], BF16, force_internal=True)
    ys_d = scratch("ys_d", [32768, D], F32, force_internal=True)

    hT_dv = hT_d.rearrange("(kc p) t -> p kc t", p=128)
    w_in_v = w_in.rearrange("(kc p) c -> p kc c", p=128)

    identf = sb("identf", [128, 128]); identb = sb("identb", [128, 128], BF16)
    onesf = sb("onesf", [128, 128]); onesb = sb("onesb", [128, 128], BF16)
    zerof = sb("zerof", [128, 512])
    P.memset('pool', onesf[:], 1.0)
    P.memset('pool', zerof[:], 0.0)
    P.memset('pool', identf[:], 1.0)
    P.op('pool', lambda e: e.affine_select(out=identf[:], in_=identf[:], pattern=[[-1, 128]], compare_op=ALU.is_equal,
                                           fill=0.0, base=0, channel_multiplier=1), [identf], [identf])
    P.copy('dve', identb[:], identf[:])
    P.copy('dve', onesb[:], onesf[:])
    eps_ln = sb("eps_ln", [128, 1])
    P.memset('pool', eps_ln[:], LN_EPS)
    bank = [nc.alloc_psum_tensor(f"bank{i}", [128, 512], F32) for i in range(8)]

    def bank_bf(i):
        return bank[i][:].bitcast(BF16)

    ln_sets = [(sb(f"ln_st{i}", [128, 12]), sb(f"ln_mv{i}", [128, 2]), sb(f"ln_rs{i}", [128, 1])) for i in range(3)]
    ln_cnt = [0]
    lnp = {}

    def layer_norm_rows(t, o, np_=128, mul_eng='pool'):
        ln_st, ln_mv, ln_rs = ln_sets[ln_cnt[0] % 3]; ln_cnt[0] += 1
        P.op('dve', lambda e: e.bn_stats(out=ln_st[:np_, 0:6], in_=t[:np_, 0:512]), [t], [ln_st])
        P.op('dve', lambda e: e.bn_stats(out=ln_st[:np_, 6:12], in_=t[:np_, 512:1024]), [t], [ln_st])
        P.op('dve', lambda e: e.bn_aggr(out=ln_mv[:np_, :], in_=ln_st[:np_, :]), [ln_st], [ln_mv])
        P.act(ln_rs[:np_, :], ln_mv[:np_, 1:2], AF.Sqrt, bias=eps_ln[:np_, 0:1])
        P.op('dve', lambda e: e.reciprocal(out=ln_rs[:np_, :], in_=ln_rs[:np_, :]), [ln_rs], [ln_rs])
        P.ts('dve', t[:np_, :], t[:np_, :], ln_mv[:np_, 0:1], ln_rs[:np_, 0:1], ALU.subtract, ALU.mult)
        P.tt(mul_eng, t[:np_, :], t[:np_, :], lnp['g'][:np_, :], ALU.mult)
        P.tt('dve', o[:np_, :], t[:np_, :], lnp['b'][:np_, :], ALU.add)

    vp = sb("vp", [128, 64])
    base_mark = A.mark()
    lnp['g'] = sb("gB0", [128, D]); lnp['b'] = sb("bB0", [128, D])
    P.dma('sp', lnp['g'][:], emb_ln_g.partition_broadcast(128))
    P.dma('sp', lnp['b'][:], emb_ln_b.partition_broadcast(128))
    xt = [sb(f"xt{i}", [128, D]) for i in range(2)]
    ht = [sb(f"ht{i}", [128, D]) for i in range(2)]
    hTs = [sb(f"hTs{i}", [128, 8, 512], BF16) for i in range(2)]
    for i in range(32):
        xb = xt[i % 2]; hb = ht[i % 2]; hs = hTs[(i // 4) % 2]
        P.dma('sp', xb[:], x[i * 128:(i + 1) * 128, :])
        layer_norm_rows(xb, hb)
        P.dma('sp', h0_d[i * 128:(i + 1) * 128, :], hb[:], w=[('h0_d', i)])
        for half in range(2):
            pb = bank[half]
            for q4 in range(4):
                kc = half * 4 + q4
                P.tr(pb[:, q4 * 128:(q4 + 1) * 128], hb[:, kc * 128:(kc + 1) * 128], identf[:])
            P.copy('act', hs[:, half * 4:(half + 1) * 4, (i % 4) * 128:(i % 4 + 1) * 128],
                   pb[:].rearrange("p (k t) -> p k t", t=128))
        if i % 4 == 3:
            tt_ = i // 4
            P.dma('sp', hT_dv[:, :, tt_ * 512:(tt_ + 1) * 512], hs[:], w=[('hT_d', tt_)])
    A.release(base_mark)
    if stage <= 1:
        P.finish()
        return nc, dbg

    tri = {}; ntri = {}; negT = {}; nstrT = {}
    for d_, (cm, st_) in (('f', (-1, 1)), ('b', (1, -1))):
        t_ = sb(f"tri_{d_}", [64, 64]); nt_ = sb(f"ntri_{d_}", [64, 64])
        P.op('pool', lambda e, t_=t_, cm=cm, st_=st_: e.affine_select(out=t_[:], in_=onesf[0:64, 0:64], pattern=[[st_, 64]],
             compare_op=ALU.is_ge, fill=0.0, base=0, channel_multiplier=cm), [onesf], [t_])
        P.ts('dve', nt_[:], t_[:], -1.0, None, ALU.mult)
        tri[d_] = t_; ntri[d_] = nt_
        ng_ = sb(f"negT_{d_}", [64, 8, 64])
        P.op('pool', lambda e, ng_=ng_, cm=cm, st_=st_: e.affine_select(out=ng_[:], in_=zerof[0:64, 0:512].rearrange("p (c i) -> p c i", i=64),
             pattern=[[0, 8], [st_, 64]], compare_op=ALU.is_ge, fill=NEG_BIG, base=0, channel_multiplier=cm), [zerof], [ng_])
        negT[d_] = ng_
        ns_ = sb(f"nstrT_{d_}", [64, 64])
        P.op('pool', lambda e, ns_=ns_, cm=cm, st_=st_: e.affine_select(out=ns_[:], in_=onesf[0:64, 0:64], pattern=[[st_, 64]],
             compare_op=ALU.is_gt, fill=0.0, base=0, channel_multiplier=cm), [onesf], [ns_])
        P.ts('dve', ns_[:], ns_[:], -1.0, None, ALU.mult)
        nstrT[d_] = ns_
    BETA = {d_: sb(f"BETA_{d_}", [64, 8, 64]) for d_ in 'fb'}
    LA = {d_: sb(f"LA_{d_}", [64, 8, 64]) for d_ in 'fb'}
    hTt = [sb(f"hTt{i}", [128, 8, 512], BF16) for i in range(2)]
    hcnt = [0]

    def load_hT(tt_):
        hb = hTt[hcnt[0] % 2]; hcnt[0] += 1
        P.dma('sp', hb[:], hT_dv[:, :, tt_ * 512:(tt_ + 1) * 512], r=[('hT_d', tt_)])
        return hb

    s2_mark = A.mark()
    SMraw = sb("SMraw", [64, 64, 32])
    Wsm = sb("Wsm", [128, 8, 32], BF16)
    alB = sb("alB", [64, 16]); dtB = sb("dtB", [64, 16]); nea = sb("nea", [64, 16])
    X_ = sb("spX", [64, 8, 64]); A_ = sb("spA", [64, 8, 64])
    P.dma('pool', Wsm[:], w_in_v[:, :, COL_SM:COL_SM + 32])
    P.dma('sp', alB[:], a_log.partition_broadcast(64))
    P.dma('sp', dtB[:], dt_bias.partition_broadcast(64))
    P.act(nea[:], alB[:], AF.Exp)
    P.ts('dve', nea[:], nea[:], -1.0, None, ALU.mult)
    for tt_ in range(8):
        hb = load_hT(tt_)
        pb = bank[2 + tt_ % 2]
        for cc in range(8):
            for kc in range(8):
                P.mm(pb[0:64, cc * 32:(cc + 1) * 32], hb[:, kc, cc * 64:(cc + 1) * 64], Wsm[:, kc, :], start=(kc == 0), stop=(kc == 7))
        P.copy('dve', SMraw[:, tt_ * 8:(tt_ + 1) * 8, :], pb[0:64, 0:256].rearrange("p (c k) -> p c k", k=32))
    for di, d_ in enumerate('fb'):
        ub = SMraw[:, :, di * 8:(di + 1) * 8].rearrange("p c h -> p h c")
        ua = SMraw[:, :, 16 + di * 8:24 + di * 8].rearrange("p c h -> p h c")
        P.act(BETA[d_][:], ub, AF.Sigmoid)
        P.tt('dve', X_[:], ua, bc(dtB[:, di * 8:(di + 1) * 8], [64, 8, 64], 2), ALU.add)
        P.stt(A_[:], X_[:], -1.0, X_[:], ALU.mult, ALU.max)
        P.act(A_[:], A_[:], AF.Exp, scale=-1.0)
        P.act(A_[:], A_[:], AF.Ln, bias=1.0)
        P.stt(A_[:], X_[:], 0.0, A_[:], ALU.max, ALU.add)
        P.tt('dve', LA[d_][:], A_[:], bc(nea[:, di * 8:(di + 1) * 8], [64, 8, 64], 2), ALU.mult)
    if debug:
        for d_ in 'fb':
            dbg_out(f"dbg_beta_{d_}", BETA[d_][:], [64, 8, 64], F32)
            dbg_out(f"dbg_la_{d_}", LA[d_][:], [64, 8, 64], F32)
    A.release(s2_mark)
    if stage <= 2:
        P.finish()
        return nc, dbg


    nheads = int(os.environ.get("MK_NHEADS", "8")) if debug else 8
    head_mark = A.mark()
    cst = sb("cst", [120, 128]); cwq = sb("cwq", [128, 120])
    P.dma('sp', cst[:], conv_qkv.rearrange("t (c p) -> (t c) p", p=128))
    P.tr(bank[0][:, 0:120], cst[:, :], identf[0:120, 0:120])
    P.copy('dve', cwq[:], bank[0][:, 0:120])
    eps_l2 = sb("eps_l2", [128, 1]); P.memset('pool', eps_l2[:], L2_EPS)
    eps_rms = sb("eps_rms", [128, 1]); P.memset('pool', eps_rms[:], RMS_EPS)
    ngB = sb("ngB", [64, 128]); P.dma('sp', ngB[:], dn_norm_g.partition_broadcast(64))
    Wg = [sb(f"Wg{i}", [128, 8, 128], BF16) for i in range(1)]
    U3 = [sb(f"U{j}", [128, 4100], BF16) for j in range(3)]
    for U in U3:
        P.memset('pool', U[:, 0:2], 0.0); P.memset('pool', U[:, 4098:4100], 0.0)
    Wg3 = sb("Wg3", [128, 8, 384], BF16)
    C = sb("C", [128, 4096], BF16)
    Dg = sb("Dg", [128, 5, 128], BF16)
    sqb = sb("sqb", [128, 512], BF16); lnb = sb("lnb", [128, 512])
    qT = sb("qT", [128, 4096], BF16); kT = sb("kT", [128, 4096], BF16)
    Ktok = sb("Ktok", [64, 64, 128], BF16); Vtok = sb("Vtok", [64, 64, 128], BF16)
    O = sb("O", [64, 64, 128], BF16)
    qkT = {d_: sb(f"qkT_{d_}", [64, 64, 64], BF16) for d_ in 'fb'}
    TT = {d_: sb(f"TT_{d_}", [64, 64, 64], BF16) for d_ in 'fb'}
    Gd = {d_: sb(f"Gd_{d_}", [64, 64]) for d_ in 'fb'}
    EG = {d_: sb(f"EG_{d_}", [64, 64]) for d_ in 'fb'}
    NEG = {d_: sb(f"NEG_{d_}", [64, 64]) for d_ in 'fb'}
    TAIL = {d_: sb(f"TAIL_{d_}", [64, 64]) for d_ in 'fb'}
    CD = {d_: sb(f"CD_{d_}", [128, 64]) for d_ in 'fb'}
    LAm = sb("LAm", [64, 8, 64]); LAb = sb("LAb", [64, 8, 64])
    decT = {d_: sb(f"decT_{d_}", [64, 8, 64]) for d_ in 'fb'}; tY = {d_: sb(f"tY_{d_}", [64, 8, 64]) for d_ in 'fb'}
    Yb = {d_: [sb(f"Yb{d_}{i}", [64, 8, 64], BF16) for i in range(2)] for d_ in 'fb'}
    Zb = {d_: [sb(f"Zb{d_}{i}", [64, 8, 64], BF16) for i in range(2)] for d_ in 'fb'}
    Gm = {d_: [sb(f"Gm{d_}{i}", [64, 8, 64], BF16) for i in range(2)] for d_ in 'fb'}
    Sst = {d_: sb(f"S_{d_}", [128, 128]) for d_ in 'fb'}
    Sbf = {d_: sb(f"Sb_{d_}", [128, 128], BF16) for d_ in 'fb'}
    Rt = {d_: sb(f"R_{d_}", [64, 128], BF16) for d_ in 'fb'}
    vnew = {d_: sb(f"vnew_{d_}", [64, 128], BF16) for d_ in 'fb'}
    tq = {d_: sb(f"tq_{d_}", [64, 128]) for d_ in 'fb'}
    ktl = {d_: sb(f"ktl_{d_}", [64, 128], BF16) for d_ in 'fb'}
    otmp = sb("otmp", [64, 128])
    ssq = sb("ssq", [64, 64])
    wcnt = [0]; pcnt = [0]

    def nextbank():
        b_ = bank[pcnt[0] % 2]; pcnt[0] += 1
        return b_

    def inproj_chunk(col0, evac):
        wb = Wg[0]; wcnt[0] += 1
        P.dma('pool', wb[:], w_in_v[:, :, col0:col0 + 128])
        for tt_ in range(8):
            hb = load_hT(tt_)
            pb = nextbank()
            for kc in range(8):
                P.mm(pb[:, :], wb[:, kc, :], hb[:, kc, :], start=(kc == 0), stop=(kc == 7))
            evac(tt_, pb)

    def inproj_qkv(h_):
        for j, cb in enumerate((COL_Q, COL_K, COL_V)):
            P.dma('pool', Wg3[:, :, j * 128:(j + 1) * 128], w_in_v[:, :, cb + h_ * 128:cb + (h_ + 1) * 128], w=[f"Wg3_{j}"])
        for tt_ in range(8):
            hb = load_hT(tt_)
            for j in range(3):
                pb = nextbank()
                for kc in range(8):
                    P.mm(pb[:, :], Wg3[:, kc, j * 128:(j + 1) * 128], hb[:, kc, :], start=(kc == 0), stop=(kc == 7), r=[f"Wg3_{j}", hb])
                P.copy('act', U3[j][:, 2 + tt_ * 512:2 + (tt_ + 1) * 512], pb[:, :])

    def conv_silu(ci, U):
        for t in range(5):
            P.ts('dve', Dg[:, t, :], identb[:], cwq[:, t * 24 + ci:t * 24 + ci + 1], None, ALU.mult)
        for tt_ in range(8):
            pb = nextbank()
            for t in range(5):
                P.mm(pb[:, :], Dg[:, t, :], U[:, tt_ * 512 + t:tt_ * 512 + t + 512], start=(t == 0), stop=(t == 4))
            P.act(C[:, tt_ * 512:(tt_ + 1) * 512], pb[:, :], AF.Silu)

    def l2norm_to(dst, scale):
        for tt_ in range(8):
            sl = slice(tt_ * 512, (tt_ + 1) * 512)
            P.act(sqb[:], C[:, sl], AF.Square)
            pb = nextbank()
            P.mm(pb[:, :], onesb[:], sqb[:])
            P.act(lnb[:], pb[:, :], AF.Ln, bias=eps_l2[:, 0:1])
            P.act(lnb[:], lnb[:], AF.Exp, scale=-0.5)
            P.stt(dst[:, sl], C[:, sl], scale, lnb[:], ALU.mult, ALU.mult)

    def to_tokmajor(src, dst):
        for g in range(8):
            bi = 2 + g % 2
            pbf = bank_bf(bi)
            for cc in range(8):
                c = g * 8 + cc
                P.tr(pbf[0:64, cc * 128:(cc + 1) * 128], src[:, c * 64:(c + 1) * 64], identb[:])
            P.copy('dve' if g % 2 == 0 else 'act', dst[:, g * 8:(g + 1) * 8, :], pbf[0:64, :].rearrange("p (c d) -> p c d", d=128))

    def flat(t3):
        return t3[:].rearrange("p c i -> p (c i)")

    for h in range(nheads):
        inproj_qkv(h)
        conv_silu(h, U3[0]); l2norm_to(qT, 128.0 ** -0.5)
        conv_silu(8 + h, U3[1]); l2norm_to(kT, 1.0)
        to_tokmajor(kT, Ktok)
        conv_silu(16 + h, U3[2])
        to_tokmajor(C, Vtok)
        if debug and h == 0:
            dbg_out("dbg_qT", qT[:], [128, 4096], BF16)
            dbg_out("dbg_kT", kT[:], [128, 4096], BF16)
            dbg_out("dbg_Ktok", Ktok[:], [64, 64, 128], BF16)
            dbg_out("dbg_Vtok", Vtok[:], [64, 64, 128], BF16)
        if stage <= 3:
            continue
        for d_ in 'fb':
            la_h = LA[d_][:, h, :]
            P.mm(bank[4][0:64, 0:64], tri[d_][:], la_h)
            P.copy('dve', Gd[d_][:], bank[4][0:64, 0:64])
            P.mm(bank[5][:, 0:64], onesf[0:64, :], la_h)
            P.act(CD[d_][:], bank[5][:, 0:64], AF.Exp)
            P.tt('dve', TAIL[d_][:], bank[5][0:64, 0:64], Gd[d_][:], ALU.subtract)
            P.act(TAIL[d_][:], TAIL[d_][:], AF.Exp)
            P.act(EG[d_][:], Gd[d_][:], AF.Exp)
            P.ts('dve', NEG[d_][:], EG[d_][:], -1.0, None, ALU.mult)
        for g in range(8):
            c0 = g * 8
            for cc in range(8):
                ch = slice((c0 + cc) * 64, (c0 + cc + 1) * 64)
                P.mm(bank[4][0:64, cc * 64:(cc + 1) * 64], kT[:, ch], kT[:, ch])
            for cc in range(8):
                ch = slice((c0 + cc) * 64, (c0 + cc + 1) * 64)
                P.mm(bank[5][0:64, cc * 64:(cc + 1) * 64], kT[:, ch], qT[:, ch])
            bk = {'f': (2, 3, 7), 'b': (0, 1, 6)}
            for d_ in 'fb':
                la_g = LA[d_][:, h, c0:c0 + 8]
                P.tt('pool', LAm[:], bc(la_g, [64, 8, 64], 2), bc(tri[d_][:], [64, 8, 64], 1), ALU.mult)
                P.copy('pool', LAb[:], bc(la_g, [64, 8, 64], 2))
                P.mm(bank[6][0:64, :], onesf[0:64, 0:64], flat(LAm), start=True, stop=False)
                P.mm(bank[6][0:64, :], ntri[d_][:], flat(LAb), start=False, stop=False)
                P.mm(bank[6][0:64, :], identf[0:64, 0:64], flat(negT[d_]), start=False, stop=True)
                P.act(flat(decT[d_]), bank[6][0:64, :], AF.Exp)
            for d_ in 'fb':
                P.tt('dve', flat(qkT[d_])[:, c0 * 64:(c0 + 8) * 64], bank[5][0:64, :], flat(decT[d_]), ALU.mult)
                P.tt('dve', flat(tY[d_]), bank[4][0:64, :], flat(decT[d_]), ALU.mult)
                P.tt('pool', tY[d_][:], tY[d_][:], bc(BETA[d_][:, h, c0:c0 + 8], [64, 8, 64], 2), ALU.mult)
                P.tt('dve', tY[d_][:], tY[d_][:], bc(nstrT[d_][:], [64, 8, 64], 1), ALU.mult)
                P.copy('act', Yb[d_][0][:], tY[d_][:])
                P.tt('pool', Gm[d_][0][:], tY[d_][:], bc(identf[0:64, 0:64], [64, 8, 64], 1), ALU.add)
            for d_ in 'fb':
                z0 = bank_bf(bk[d_][2])
                for cc in range(8):
                    P.tr(z0[0:64, cc * 64:(cc + 1) * 64], Yb[d_][0][:, cc, :], identb[0:64, 0:64])
                P.copy('dve' if d_ == 'f' else 'act', flat(Zb[d_][0]), z0[0:64, 0:512])
            for k in range(5):
                cur = k % 2; nxt = 1 - cur
                for d_ in 'fb':
                    by_, bz_, bg_ = bk[d_]
                    if k < 4:
                        for cc in range(8):
                            P.mm(bank[by_][0:64, cc * 64:(cc + 1) * 64], Zb[d_][cur][:, cc, :], Yb[d_][cur][:, cc, :])
                    for cc in range(8):
                        P.mm(bank[bz_][0:64, cc * 64:(cc + 1) * 64], Yb[d_][cur][:, cc, :], Zb[d_][cur][:, cc, :])
                for d_ in 'fb':
                    by_, bz_, bg_ = bk[d_]
                    if k < 4:
                        P.copy('act', flat(Yb[d_][nxt]), bank[by_][0:64, :])
                    P.copy('dve', flat(Zb[d_][nxt]), bank[bz_][0:64, :])
                for d_ in 'fb':
                    by_, bz_, bg_ = bk[d_]
                    for cc in range(8):
                        P.mm(bank[bg_][0:64, cc * 64:(cc + 1) * 64], Zb[d_][nxt][:, cc, :], Gm[d_][cur][:, cc, :])
                for d_ in 'fb':
                    by_, bz_, bg_ = bk[d_]
                    dstG = flat(Gm[d_][nxt]) if k < 4 else flat(TT[d_])[:, c0 * 64:(c0 + 8) * 64]
                    P.tt('dve' if d_ == 'f' else 'pool', dstG, bank[bg_][0:64, :], flat(Gm[d_][cur]), ALU.add) if d_ == 'f' else P.tt('dve', dstG, bank[bg_][0:64, :], flat(Gm[d_][cur]), ALU.add)
        if debug and h == 0:
            for d_ in 'fb':
                dbg_out(f"dbg_TT_{d_}", TT[d_][:], [64, 64, 64], BF16)
                dbg_out(f"dbg_qkT_{d_}", qkT[d_][:], [64, 64, 64], BF16)
        if stage <= 4:
            continue
        for d_ in 'fb':
            P.memset('pool', Sst[d_][:], 0.0)
            P.memset('pool', Sbf[d_][:], 0.0)
        nsteps = int(os.environ.get('MK_STEPS', '64')) if debug else 64
        for s_ in range(nsteps):
            cs = {'f': s_, 'b': 63 - s_}
            pv = {}
            for di, d_ in enumerate('fb'):
                bA = bank[di * 3]; bB_ = bank[di * 3 + 1]; bC = bank[di * 3 + 2]
                pv[d_] = (bA[0:64, 0:128], bB_[0:64, 0:128], bA[0:64, 128:256], bB_[0:64, 128:256], bC[:, 0:128])
            for d_ in 'fb':
                c = cs[d_]; ch = slice(c * 64, (c + 1) * 64); ps1, ps2, ps3, ps4, ps5 = pv[d_]
                P.mm(ps1, kT[:, ch], Sbf[d_][:])
                P.mm(ps2, qT[:, ch], Sbf[d_][:])
            for d_ in 'fb':
                c = cs[d_]; ps1, ps2, ps3, ps4, ps5 = pv[d_]
                P.stt(Rt[d_][:], ps1, NEG[d_][:, c:c + 1], Vtok[:, c, :], ALU.mult, ALU.add)
                P.act(ktl[d_][:], Ktok[:, c, :], AF.Identity, scale=TAIL[d_][:, c:c + 1])
                P.act(tq[d_][:], ps2, AF.Identity, scale=EG[d_][:, c:c + 1])
            for d_ in 'fb':
                c = cs[d_]; ps1, ps2, ps3, ps4, ps5 = pv[d_]
                P.mm(ps3, TT[d_][:, c, :], Rt[d_][:])
            for d_ in 'fb':
                c = cs[d_]; ps1, ps2, ps3, ps4, ps5 = pv[d_]
                P.act(vnew[d_][:], ps3, AF.Identity, scale=BETA[d_][:, h, c:c + 1])
            for d_ in 'fb':
                c = cs[d_]; ps1, ps2, ps3, ps4, ps5 = pv[d_]
                P.mm(ps5, ktl[d_][:], vnew[d_][:])
                P.mm(ps4, qkT[d_][:, c, :], vnew[d_][:])
            for d_ in 'fb':
                c = cs[d_]; ps1, ps2, ps3, ps4, ps5 = pv[d_]
                P.stt(Sbf[d_][:], Sst[d_][:], CD[d_][:, c:c + 1], ps5, ALU.mult, ALU.add)
            for d_ in 'fb':
                c = cs[d_]; ps1, ps2, ps3, ps4, ps5 = pv[d_]
                P.stt(Sst[d_][:], Sst[d_][:], CD[d_][:, c:c + 1], ps5, ALU.mult, ALU.add)
                first = (d_ == 'f' and c < 32) or (d_ == 'b' and c >= 32)
                if first:
                    P.tt('dve', O[:, c, :], tq[d_][:], ps4, ALU.add)
                else:
                    P.tt('dve', otmp[:], tq[d_][:], ps4, ALU.add)
                    P.tt('pool', O[:, c, :], O[:, c, :], otmp[:], ALU.add)
        if debug and h == 0:
            dbg_out("dbg_O", O[:], [64, 64, 128], BF16)
            dbg_out("dbg_S_f", Sst['f'][:], [128, 128], F32)
            dbg_out("dbg_vnew_f", vnew['f'][:], [64, 128], BF16)
            dbg_out("dbg_R_f", Rt['f'][:], [64, 128], BF16)
            dbg_out("dbg_tq_f", tq['f'][:], [64, 128], F32)
            dbg_out("dbg_ktl_f", ktl['f'][:], [64, 128], BF16)
            dbg_out("dbg_CD_f", CD['f'][:], [128, 64], F32)
            dbg_out("dbg_TAIL_f", TAIL['f'][:], [64, 64], F32)
            dbg_out("dbg_EG_f", EG['f'][:], [64, 64], F32)
        if debug and os.environ.get('MK_NOPOST'):
            continue
        Zs = Ktok
        wz = Wg[0]; wcnt[0] += 1
        P.dma('pool', wz[:], w_in_v[:, :, COL_Z + h * 128:COL_Z + (h + 1) * 128])
        for tt_ in range(8):
            hb = load_hT(tt_)
            for hf in range(2):
                pb = nextbank()
                for c4 in range(4):
                    cc = hf * 4 + c4
                    for kc in range(8):
                        P.mm(pb[0:64, c4 * 128:(c4 + 1) * 128], hb[:, kc, cc * 64:(cc + 1) * 64], wz[:, kc, :], start=(kc == 0), stop=(kc == 7))
                c_lo = tt_ * 8 + hf * 4
                P.act(Zs[:, c_lo:c_lo + 4, :], pb[0:64, :].rearrange("p (c d) -> p c d", d=128), AF.Silu)
        SQ = Vtok
        P.tt('dve', SQ[:], O[:], O[:], ALU.mult)
        P.op('dve', lambda e: e.tensor_reduce(out=ssq[:], in_=SQ[:], axis=AX.X, op=ALU.add), [SQ], [ssq])
        P.act(ssq[:], ssq[:], AF.Ln, bias=eps_rms[0:64, 0:1], scale=1.0 / 128.0)
        P.act(ssq[:], ssq[:], AF.Exp, scale=-0.5)
        P.tt('dve', O[:], O[:], bc(ssq[:], [64, 64, 128], 2), ALU.mult)
        P.tt('pool', O[:], O[:], bc(ngB[:], [64, 64, 128], 1), ALU.mult)
        OGb = Vtok
        P.tt('dve', OGb[:], O[:], Zs[:], ALU.mult)
        for g in range(8):
            bi = 2 + g % 2
            pbf = bank_bf(bi)
            for cc in range(8):
                P.tr(pbf[:, cc * 64:(cc + 1) * 64], OGb[:, g * 8 + cc, :], identb[0:64, 0:64])
            P.copy('act', C[:, g * 512:(g + 1) * 512], pbf[:, 0:512])
        P.dma('sp', ogT_d[h * 128:(h + 1) * 128, :], C[:], w=[('ogT_d', h)])
    if stage <= 5:
        P.finish()
        return nc, dbg

    A.release(head_mark)
    vst = sb("vst", [64, 128])
    for r0, src, n in ((0, b_glu, 16), (16, b_gate, 16), (32, b_dw, 8), (40, conv_ln_g, 8), (48, conv_ln_b, 8), (56, b_b_o, 8)):
        P.dma('sp', vst[r0:r0 + n, :], src.rearrange("(c p) -> c p", p=128))
    P.tr(bank[0][:, 0:64], vst[:, :], identf[0:64, 0:64])
    P.copy('dve', vp[:], bank[0][:, 0:64])
    VB_GLU, VB_GATE, VB_DW, VB_LNG, VB_LNB, VB_BBO = 0, 16, 32, 40, 48, 56
    cdst1 = sb("cdst1", [128, 128]); cdst2 = sb("cdst2", [120, 128]); cwd = sb("cwd", [128, 248])
    cdv = conv_dw.rearrange("t (c p) -> (t c) p", p=128)
    P.dma('sp', cdst1[:], cdv[0:128, :])
    P.dma('sp', cdst2[:], cdv[128:248, :])
    P.tr(bank[1][:, 0:128], cdst1[:, :], identf[:, :])
    P.copy('dve', cwd[:, 0:128], bank[1][:, 0:128])
    P.tr(bank[1][:, 128:248], cdst2[:, :], identf[0:120, 0:120])
    P.copy('dve', cwd[:, 128:248], bank[1][:, 128:248])
    Wg4 = [sb(f"Wg4_{i}", [128, 8, 512], BF16) for i in range(2)]
    GA = [sb(f"GA{i}", [128, 4096], BF16) for i in range(2)]
    Ug = [sb(f"Ug{i}", [128, 4096 + 30], BF16) for i in range(2)]
    for u_ in Ug:
        P.memset('pool', u_[:, 0:15], 0.0); P.memset('pool', u_[:, 4096 + 15:4096 + 30], 0.0)
    Ycb = [sb(f"Ycb{i}", [128, 4096], BF16) for i in range(4)]
    Dg31 = sb("Dg31", [128, 31, 128], BF16)
    sgt = sb("sgt", [128, 512]); sq2 = sb("sq2", [128, 512], BF16)
    SUM = sb("SUM", [1, 4096]); SSQ = sb("SSQ", [1, 4096])
    w4cnt = [0]

    def inproj_group(cols, evacs):
        wb = Wg4[w4cnt[0] % 2]; w4cnt[0] += 1
        for jj, c0_ in enumerate(cols):
            P.dma('pool', wb[:, :, jj * 128:(jj + 1) * 128], w_in_v[:, :, c0_:c0_ + 128], w=[f"{wb.name}_{jj}"])
        for tt_ in range(8):
            hb = load_hT(tt_)
            for jj in range(len(cols)):
                pb = nextbank()
                for kc in range(8):
                    P.mm(pb[:, :], wb[:, kc, jj * 128:(jj + 1) * 128], hb[:, kc, :], start=(kc == 0), stop=(kc == 7),
                         r=[f"{wb.name}_{jj}", hb])
                evacs[jj](tt_, pb)

    for g4 in range(4):
        evs = []
        for jj in range(4):
            cg = g4 * 4 + jj

            def evac_gate(tt_, pb, cg=cg, jj=jj):
                P.act(Ycb[jj][:, tt_ * 512:(tt_ + 1) * 512], pb[:, :], AF.Sigmoid, bias=vp[:, VB_GATE + cg:VB_GATE + cg + 1])
            evs.append(evac_gate)
        inproj_group([COL_GATE + (g4 * 4 + jj) * 128 for jj in range(4)], evs)
        for jj in range(4):
            cg = g4 * 4 + jj
            P.dma('sp', gate_d[cg * 128:(cg + 1) * 128, :], Ycb[jj][:], w=[('gate_d', cg)])
    for j2 in range(4):
        evs = []; cols = []
        for u in range(2):
            j = j2 * 2 + u

            def evac_a(tt_, pb, j=j, u=u):
                P.act(GA[u][:, tt_ * 512:(tt_ + 1) * 512], pb[:, :], AF.Identity, bias=vp[:, VB_GLU + j:VB_GLU + j + 1])

            def evac_g(tt_, pb, j=j, u=u):
                P.act(sgt[:], pb[:, :], AF.Sigmoid, bias=vp[:, VB_GLU + 8 + j:VB_GLU + 8 + j + 1])
                P.tt('dve', Ug[u][:, 15 + tt_ * 512:15 + (tt_ + 1) * 512], GA[u][:, tt_ * 512:(tt_ + 1) * 512], sgt[:], ALU.mult)
            cols += [COL_GLU + j * 128, COL_GLU + 1024 + j * 128]
            evs += [evac_a, evac_g]
        inproj_group(cols, evs)
        for u in range(2):
            j = j2 * 2 + u
            yc = Ycb[u]
            for t in range(31):
                P.ts('dve' if t % 2 == 0 else 'pool', Dg31[:, t, :], identb[:], cwd[:, t * 8 + j:t * 8 + j + 1], None, ALU.mult)
            for tt_ in range(8):
                pb = nextbank()
                for t in range(31):
                    P.mm(pb[:, :], Dg31[:, t, :], Ug[u][:, tt_ * 512 + t:tt_ * 512 + t + 512], start=(t == 0), stop=(t == 30))
                sl = slice(tt_ * 512, (tt_ + 1) * 512)
                P.act(yc[:, sl], pb[:, :], AF.Identity, bias=vp[:, VB_DW + j:VB_DW + j + 1])
                P.act(sq2[:], pb[:, :], AF.Square, bias=vp[:, VB_DW + j:VB_DW + j + 1])
                P.mm(bank[2][0:1, :], onesb[:, 0:1], yc[:, sl])
                P.mm(bank[3][0:1, :], onesb[:, 0:1], sq2[:])
                if j == 0:
                    P.copy('dve', SUM[:, sl], bank[2][0:1, :])
                    P.copy('dve', SSQ[:, sl], bank[3][0:1, :])
                else:
                    P.tt('dve', SUM[:, sl], SUM[:, sl], bank[2][0:1, :], ALU.add)
                    P.tt('dve', SSQ[:, sl], SSQ[:, sl], bank[3][0:1, :], ALU.add)
            P.dma('sp', ybT_d[j * 128:(j + 1) * 128, :], yc[:], w=[('ybT_d', j)])
    stt_ = sb("stt_", [1, 512])
    for tt_ in range(8):
        sl = slice(tt_ * 512, (tt_ + 1) * 512)
        P.ts('dve', SUM[:, sl], SUM[:, sl], 1.0 / 1024.0, None, ALU.mult)
        P.tt('dve', stt_[:], SUM[:, sl], SUM[:, sl], ALU.mult)
        P.stt(SSQ[:, sl], SSQ[:, sl], 1.0 / 1024.0, stt_[:], ALU.mult, ALU.subtract)
        P.act(SSQ[:, sl], SSQ[:, sl], AF.Ln, bias=eps_ln[0:1, 0:1])
        P.act(SSQ[:, sl], SSQ[:, sl], AF.Exp, scale=-0.5)
        P.stt(SUM[:, sl], SUM[:, sl], -1.0, SSQ[:, sl], ALU.mult, ALU.mult)
    RSTD = SSQ; NMR = SUM
    P.dma('sp', st_d[0:1, :], RSTD[:])
    P.dma('sp', st_d[1:2, :], NMR[:])
    s5_mark = A.mark()
    if stage <= 6:
        P.finish()
        return nc, dbg

    A.release(base_mark)
    LG = sb("LG", [128, 32, 32]); M8 = sb("M8", [128, 32, 8]); MSK = sb("MSK", [128, 32, 32], BF16); G4 = sb("G4", [128, 32, 4])
    moe_mark = A.mark()
    zbf = zerof[:].bitcast(BF16)
    xs_v = xs_d.rearrange("(n p) d -> p n d", p=128)
    for n0 in range(0, 256, 32):
        P.dma('sp', xs_v[:, n0:n0 + 32, :], zbf.unsqueeze(1).to_broadcast([128, 32, D]), r=[zerof], w=[('xs_fill', n0)])
    Wao = sb("Wao", [128, 8, 1024], BF16); Wbo = sb("Wbo", [128, 8, 1024], BF16); Wout = sb("Wout", [128, 8, 1024], BF16)
    P.dma('pool', Wao[:], w_a_o.rearrange("(kc p) c -> p kc c", p=128))
    P.dma('pool', Wbo[:], w_b_o.rearrange("(kc p) c -> p kc c", p=128))
    P.dma('pool', Wout[:], w_out.rearrange("(kc p) c -> p kc c", p=128))
    Wr = sb("Wr", [128, 8, 32]); brB = sb("brB", [128, 32])
    P.dma('sp', Wr[:], w_router.rearrange("(kc p) c -> p kc c", p=128))
    P.dma('sp', brB[:], b_router.partition_broadcast(128))
    lnp['g'] = sb("gB1", [128, D]); lnp['b'] = sb("bB1", [128, D])
    P.dma('sp', lnp['g'][:], ln1_g.partition_broadcast(128))
    P.dma('sp', lnp['b'][:], ln1_b.partition_broadcast(128))
    ogt2 = [sb(f"ogt{i}", [128, 8, 512], BF16) for i in range(2)]; ybt2 = [sb(f"ybt{i}", [128, 8, 512], BF16) for i in range(2)]
    gtt2 = [sb(f"gtt{i}", [128, 16, 512], BF16) for i in range(2)]
    ybaT = sb("ybaT", [128, 8, 512], BF16); mixT = sb("mixT", [128, 8, 512], BF16)
    t1s = [sb(f"t1_{i}", [128, 512]) for i in range(2)]; t2s = [sb(f"t2_{i}", [128, 512]) for i in range(2)]
    rs_t = sb("rs_t", [1, 512]); nm_t = sb("nm_t", [1, 512])
    h0t2 = [sb(f"h0t{i}", [128, D]) for i in range(2)]; rt_2 = [sb(f"rt{i}", [128, D]) for i in range(2)]
    h1t2 = [sb(f"h1t{i}", [128, D]) for i in range(2)]; h1Tf2 = [sb(f"h1Tf{i}", [128, 8, 128]) for i in range(2)]
    nmx2 = [sb(f"nmx{i}", [128, 1]) for i in range(2)]; esum2 = [sb(f"esum{i}", [128, 1]) for i in range(2)]
    h1tb2 = [sb(f"h1tb{i}", [128, D], BF16) for i in range(2)]
    ogv = ogT_d.rearrange("(j p) t -> p j t", p=128)
    ybv = ybT_d.rearrange("(j p) t -> p j t", p=128)
    gtv = gate_d.rearrange("(j p) t -> p j t", p=128)
    def s6_M(tt_):
        sl = slice(tt_ * 512, (tt_ + 1) * 512)
        ogt = ogt2[tt_ % 2]; ybt = ybt2[tt_ % 2]; gtt = gtt2[tt_ % 2]
        P.dma('sp', ogt[:], ogv[:, :, sl], r=[('ogT_d', j_) for j_ in range(8)])
        P.dma('sp', ybt[:], ybv[:, :, sl], r=[('ybT_d', j_) for j_ in range(8)])
        P.dma('sp', gtt[:], gtv[:, :, sl], r=[('gate_d', j_) for j_ in range(16)])
        P.dma('sp', rs_t[:], st_d[0:1, sl])
        P.dma('sp', nm_t[:], st_d[1:2, sl])
        P.mm(bank[6][:, :], onesf[0:1, :], rs_t[0:1, :])
        P.mm(bank[7][:, :], onesf[0:1, :], nm_t[0:1, :])
        for j in range(8):
            t1 = t1s[j % 2]
            P.tt('dve', t1[:], ybt[:, j, :], bank[6][:, :], ALU.mult)
            P.tt('dve', t1[:], t1[:], bank[7][:, :], ALU.add)
            P.act(ybaT[:, j, :], t1[:], AF.Silu, bias=vp[:, VB_LNB + j:VB_LNB + j + 1], scale=vp[:, VB_LNG + j:VB_LNG + j + 1])
        if debug and tt_ == 0:
            dbg_out("dbg_ybaT", ybaT[:], [128, 8, 512], BF16)
        for m in range(8):
            pa = bank[(m % 2) * 2]; pb_ = bank[(m % 2) * 2 + 1]
            t1 = t1s[m % 2]; t2 = t2s[m % 2]
            for kc in range(8):
                P.mm(pa[:, :], Wao[:, kc, m * 128:(m + 1) * 128], ogt[:, kc, :], start=(kc == 0), stop=(kc == 7))
            for kc in range(8):
                P.mm(pb_[:, :], Wbo[:, kc, m * 128:(m + 1) * 128], ybaT[:, kc, :], start=(kc == 0), stop=(kc == 7))
            P.tt('dve', t1[:], pa[:, :], gtt[:, m, :], ALU.mult)
            P.stt(t2[:], pb_[:, :], vp[:, VB_BBO + m:VB_BBO + m + 1], gtt[:, 8 + m, :], ALU.add, ALU.mult)
            P.tt('pool', mixT[:, m, :], t1[:], t2[:], ALU.add)
    def s6_A(tt_, sub):
        i = tt_ * 4 + sub
        h0t = h0t2[i % 2]; rt = rt_2[i % 2]; h1t = h1t2[i % 2]; h1Tf = h1Tf2[i % 2]
        nmx = nmx2[i % 2]; esum = esum2[i % 2]; h1tb = h1tb2[i % 2]
        P.dma('sp', h0t[:], h0_d[i * 128:(i + 1) * 128, :], r=[('h0_d', i)])
        for hf in range(2):
            pm = bank[4 + hf]
            for m in range(8):
                P.mm(pm[:, :], mixT[:, m, sub * 128:(sub + 1) * 128], Wout[:, m, hf * 512:(hf + 1) * 512], start=(m == 0), stop=(m == 7))
            P.stt(rt[:, hf * 512:(hf + 1) * 512], h0t[:, hf * 512:(hf + 1) * 512], DN_ALPHA, pm[:, :], ALU.mult, ALU.add)
        layer_norm_rows(rt, h1t)
        P.dma('sp', h1_d[i * 128:(i + 1) * 128, :], h1t[:], w=[('h1_d', i)])
    def s6_B(tt_, sub):
        i = tt_ * 4 + sub
        h0t = h0t2[i % 2]; rt = rt_2[i % 2]; h1t = h1t2[i % 2]; h1Tf = h1Tf2[i % 2]
        nmx = nmx2[i % 2]; esum = esum2[i % 2]; h1tb = h1tb2[i % 2]
        for hf in range(2):
            pt = bank[6 + hf]
            for q4 in range(4):
                kc = hf * 4 + q4
                P.tr(pt[:, q4 * 128:(q4 + 1) * 128], h1t[:, kc * 128:(kc + 1) * 128], identf[:])
            P.copy('dve', h1Tf[:, hf * 4:(hf + 1) * 4, :], pt[:].rearrange("p (k t) -> p k t", t=128))
        pr_ = bank[(sub % 2) * 2]
        for kc in range(8):
            P.mm(pr_[:, 0:32], h1Tf[:, kc, :], Wr[:, kc, :], start=(kc == 0), stop=(kc == 7))
        P.tt('dve', LG[:, i, :], pr_[:, 0:32], brB[:], ALU.add)
        P.op('dve', lambda e, i=i: e.max(out=M8[:, i, :], in_=LG[:, i, :]), [LG], [M8])
        P.ts('dve', MSK[:, i, :], LG[:, i, :], M8[:, i, 3:4], None, ALU.is_ge)
        P.ts('dve', nmx[:], M8[:, i, 0:1], -1.0, None, ALU.mult)
        P.act(G4[:, i, :], M8[:, i, 0:4], AF.Exp, bias=nmx[:, 0:1])
        P.op('dve', lambda e, i=i, esum=esum: e.tensor_reduce(out=esum[:], in_=G4[:, i, :], axis=AX.X, op=ALU.add), [G4], [esum])
        P.op('dve', lambda e, esum=esum: e.reciprocal(out=esum[:], in_=esum[:]), [esum], [esum])
        P.ts('dve', G4[:, i, :], G4[:, i, :], esum[:, 0:1], None, ALU.mult)
        P.copy('act', h1tb[:], h1t[:])
        P.dma('sp', h1b_d[i * 128:(i + 1) * 128, :], h1tb[:], w=[('h1b_d', i)])

    pend_B = []
    for tt_ in range(8):
        s6_M(tt_)
        for pb_item in pend_B:
            s6_B(*pb_item)
        pend_B = []
        for sub in range(4):
            s6_A(tt_, sub)
            if sub >= 1:
                s6_B(tt_, sub - 1)
        pend_B.append((tt_, 3))
    for pb_item in pend_B:
        s6_B(*pb_item)
    if debug:
        dbg_out("dbg_G4", G4[:], [128, 32, 4], F32)
        dbg_out("dbg_MSK", MSK[:], [128, 32, 32], BF16)
    if stage <= 7:
        P.finish()
        return nc, dbg

    A.release(moe_mark)
    I32 = mybir.dt.int32
    NBLK = 64
    lnp['g'] = sb("gB2", [128, D]); lnp['b'] = sb("bB2", [128, D])
    P.dma('sp', lnp['g'][:], ln2_g.partition_broadcast(128))
    P.dma('sp', lnp['b'][:], ln2_b.partition_broadcast(128))
    stri = sb("stri", [128, 128], BF16)
    strf = sb("strf", [128, 128])
    P.op('pool', lambda e: e.affine_select(out=strf[:], in_=onesf[:], pattern=[[1, 128]], compare_op=ALU.is_gt, fill=0.0,
                                           base=0, channel_multiplier=-1), [onesf], [strf])
    P.copy('dve', stri[:], strf[:])
    POS = sb("POS", [128, 32, 32]); run = sb("run", [128, 32], BF16)
    P.memset('pool', run[:], 0.0)
    for i in range(32):
        pp = bank[i % 2]
        P.mm(pp[:, 0:32], stri[:], MSK[:, i, :], start=True, stop=False)
        P.mm(pp[:, 0:32], onesb[:], run[:], start=False, stop=True)
        P.copy('act', POS[:, i, :], pp[:, 0:32])
        P.tt('dve', run[:], run[:], MSK[:, i, :], ALU.add)
    cnt = sb("cnt", [128, 32]); padded = sb("padded", [128, 32]); pend = sb("pend", [128, 32]); pstart = sb("pstart", [128, 32])
    tmp32 = sb("tmp32", [128, 32]); ones32 = sb("ones32", [128, 32])
    P.memset('pool', ones32[:], 1.0)
    P.mm(bank[2][:, 0:32], onesb[:], run[:])
    P.copy('dve', cnt[:], bank[2][:, 0:32])
    P.memset('pool', padded[:], 0.0)
    for j in range(8):
        P.ts('dve', tmp32[:], cnt[:], float(512 * j), 512.0, ALU.is_gt, ALU.mult)
        P.tt('dve', padded[:], padded[:], tmp32[:], ALU.add)
    P.op('dve', lambda e: e.tensor_tensor_scan(out=pend[:], data0=ones32[:], data1=padded[:], initial=0.0, op0=ALU.mult, op1=ALU.add),
         [ones32, padded], [pend])
    P.tt('dve', pstart[:], pend[:], padded[:], ALU.subtract)
    bthr = sb("bthr", [128, NBLK, 32]); BE = sb("BE", [128, NBLK])
    P.op('pool', lambda e: e.iota(out=bthr[:], pattern=[[512, NBLK], [0, 32]], base=0, channel_multiplier=0,
                                  allow_small_or_imprecise_dtypes=True), [], [bthr])
    P.tt('dve', bthr[:], bc(pend[:], [128, NBLK, 32], 1), bthr[:], ALU.is_le)
    P.op('dve', lambda e: e.tensor_reduce(out=BE[:], in_=bthr[:], axis=AX.X, op=ALU.add), [bthr], [BE])
    P.ts('dve', BE[:], BE[:], 31.0, None, ALU.min)
    basef = sb("basef", [128, 8]); IDXF = sb("IDXF", [128, NBLK, 8]); IDXW = sb("IDXW", [128, NBLK, 8], I32)
    P.op('pool', lambda e: e.iota(out=basef[:], pattern=[[128, 8]], base=0, channel_multiplier=1,
                                  allow_small_or_imprecise_dtypes=True), [], [basef])
    P.stt(IDXF[:], bc(BE[:], [128, NBLK, 8], 2), 1024.0, bc(basef[:], [128, NBLK, 8], 1), ALU.mult, ALU.add)
    P.copy('dve', IDXW[:], IDXF[:])
    pidx = sb("pidx", [32, 1]); OH = sb("OH", [32, NBLK], BF16)
    P.op('pool', lambda e: e.iota(out=pidx[:], pattern=[[0, 1]], base=0, channel_multiplier=1,
                                  allow_small_or_imprecise_dtypes=True), [], [pidx])
    P.ts('dve', OH[:], BE[0:32, :], pidx[0:32, 0:1], None, ALU.is_equal)
    thrb = sb("thrb", [1, NBLK]); ACTF = sb("ACTF", [1, NBLK], I32)
    P.op('pool', lambda e: e.iota(out=thrb[:], pattern=[[512, NBLK]], base=0, channel_multiplier=0,
                                  allow_small_or_imprecise_dtypes=True), [], [thrb])
    P.ts('dve', thrb[:], thrb[:], pend[0:1, 31:32], None, ALU.is_lt)
    P.copy('dve', ACTF[:], thrb[:])
    bgu_all = sb("bgu_all", [32, 2048], BF16); bd_all = sb("bd_all", [32, D], BF16)
    P.dma('pool', bgu_all[:], b_gu)
    P.dma('pool', bd_all[:], b_down)
    SLOTF = sb("SLOTF", [128, 32, 4]); SLOT = sb("SLOT", [128, 32, 4], I32)
    valt = sb("valt", [128, 32]); mk = sb("mk", [128, 4, 32])
    for i in range(32):
        P.tt('dve', valt[:], POS[:, i, :], pstart[:], ALU.add)
        P.tt('dve', mk[:], bc(LG[:, i, :], [128, 4, 32], 1), bc(M8[:, i, 0:4], [128, 4, 32], 2), ALU.is_equal)
        P.tt('dve', mk[:], mk[:], bc(valt[:], [128, 4, 32], 1), ALU.mult)
        P.op('dve', lambda e, i=i: e.tensor_reduce(out=SLOTF[:, i, :], in_=mk[:], axis=AX.X, op=ALU.add), [mk], [SLOTF])
    P.copy('dve', SLOT[:], SLOTF[:])
    if debug:
        dbg_out("dbg_SLOT", SLOT[:], [128, 32, 4], I32)
        dbg_out("dbg_BE", BE[:], [128, NBLK], F32)
        dbg_out("dbg_cnt", cnt[:], [128, 32], F32)
    disp_mark = A.mark()
    hb_t = [sb(f"hb_t{i}", [128, D], BF16) for i in range(2)]
    fill_names = [('xs_fill', n0) for n0 in range(0, 256, 32)]
    for i in range(32):
        hb_ = hb_t[i % 2]
        P.dma('sp', hb_[:], h1b_d[i * 128:(i + 1) * 128, :], r=[('h1b_d', i)])
        for k in range(4):
            P.idma(xs_d, hb_[:], out_off=SLOT[:, i, k:k + 1], r=[hb_, SLOT] + fill_names, w=[('xs', i, k)])
    A.release(disp_mark)
    if debug and os.environ.get("MK_MOE_STOP") == "1":
        P.finish()
        return nc, dbg
    wguB = [sb(f"wguB{i}", [128, 8, 2048], BF16) for i in range(2)]
    wdB = [sb(f"wdB{i}", [128, 8, D], BF16) for i in range(2)]
    xtm = [sb(f"xtm{i}", [128, 4, D], BF16) for i in range(2)]; xT = sb("xT", [128, 8, 512], BF16)
    actT = sb("actT", [128, 8, 512], BF16)
    OHs = [sb(f"OHs{i}", [32, 512], BF16) for i in range(2)]
    NROT = 2
    gl = [sb(f"gl{i}", [128, 512]) for i in range(NROT)]; sg = [sb(f"sg{i}", [128, 512]) for i in range(NROT)]
    li = [sb(f"li{i}", [128, 512]) for i in range(NROT)]; p1 = [sb(f"p1{i}", [128, 512]) for i in range(NROT)]
    ysb = [sb(f"ysb{i}", [128, D]) for i in range(2)]
    wgu_rows = w_gu.rearrange("e k c -> (e k) c")
    wd_rows = w_down.rearrange("e k c -> (e k) c")
    nblk_run = int(os.environ.get("MK_NBLK", str(NBLK))) if debug else NBLK
    SIG_MAX = float(1.0 / (1.0 + np.exp(-1.702 * 7.0)))

    def load_block_weights(b):
        for kc in range(8):
            P.idma(wguB[b % 2][:, kc, :], wgu_rows, in_off=IDXW[:, b, kc:kc + 1], r=[IDXW], w=[f"wguB{b % 2}_{kc}"])
        for kc in range(8):
            P.idma(wdB[b % 2][:, kc, :], wd_rows, in_off=IDXW[:, b, kc:kc + 1], r=[IDXW], w=[f"wdB{b % 2}_{kc}"])

    def load_block_x(b):
        P.dma('sp', xtm[b % 2][:], xs_d[b * 512:(b + 1) * 512, :].rearrange("(s p) d -> p s d", p=128), r=[])
        P.copy('act', OHs[b % 2][:], OH[:, b:b + 1].to_broadcast([32, 512]))

    l1b = [0]; l2b = [0]; ycnt = [0]; rot = [0]
    load_block_weights(0)
    load_block_x(0)
    use_skip = os.environ.get("MK_NOSKIP") is None
    for b in range(nblk_run):
        if use_skip:
            P.cond_begin(ACTF[0:1, b:b + 1])
        if b + 1 < nblk_run:
            load_block_weights(b + 1)
            load_block_x(b + 1)
        wg_ = wguB[b % 2]; wd_ = wdB[b % 2]; oh_ = OHs[b % 2]; xt_ = xtm[b % 2]
        for k2 in range(4):
            pbf = bank_bf(4 + k2 % 2)
            for kk in range(2):
                kc = k2 * 2 + kk
                for sub in range(4):
                    P.tr(pbf[:, kk * 512 + sub * 128:kk * 512 + (sub + 1) * 128], xt_[:, sub, kc * 128:(kc + 1) * 128], identb[:])
            P.copy('act' if k2 % 2 == 0 else 'dve', xT[:, k2 * 2:k2 * 2 + 2, :], pbf[:, :].rearrange("p (k t) -> p k t", t=512))
        for m in range(8):
            pbg = bank[l1b[0] % 4]; pbl = bank[(l1b[0] + 1) % 4]; l1b[0] += 2
            ri = rot[0] % NROT; rot[0] += 1
            for kc in range(8):
                P.mm(pbg[:, :], wg_[:, kc, m * 128:(m + 1) * 128], xT[:, kc, :], start=(kc == 0), stop=False, r=[f"wguB{b % 2}_{kc}", xT])
            P.mm(pbg[:, :], bgu_all[:, m * 128:(m + 1) * 128], oh_[:], start=False, stop=True)
            for kc in range(8):
                P.mm(pbl[:, :], wg_[:, kc, 1024 + m * 128:1024 + (m + 1) * 128], xT[:, kc, :], start=(kc == 0), stop=False, r=[f"wguB{b % 2}_{kc}", xT])
            P.mm(pbl[:, :], bgu_all[:, 1024 + m * 128:1024 + (m + 1) * 128], oh_[:], start=False, stop=True)
            P.act(sg[ri][:], pbg[:, :], AF.Sigmoid, scale=1.702)
            P.ts('dve', gl[ri][:], pbg[:, :], 7.0, None, ALU.min)
            P.ts('dve', li[ri][:], pbl[:, :], 7.0, -7.0, ALU.min, ALU.max)
            P.stt(p1[ri][:], sg[ri][:], SIG_MAX, gl[ri][:], ALU.min, ALU.mult)
            P.stt(actT[:, m, :], li[ri][:], 1.0, p1[ri][:], ALU.add, ALU.mult)
        for sub in range(4):
            yb_ = ysb[ycnt[0] % 2]; ycnt[0] += 1
            for hf in range(2):
                pb = bank[4 + l2b[0] % 4]; l2b[0] += 1
                for m in range(8):
                    P.mm(pb[:, :], actT[:, m, sub * 128:(sub + 1) * 128], wd_[:, m, hf * 512:(hf + 1) * 512], start=(m == 0), stop=False, r=[f"wdB{b % 2}_{m}", actT])
                P.mm(pb[:, :], oh_[:, 0:128], bd_all[:, hf * 512:(hf + 1) * 512], start=False, stop=True)
                P.copy('act' if hf == 0 else 'pool_never', yb_[:, hf * 512:(hf + 1) * 512], pb[:, :]) if hf == 0 else P.copy('dve', yb_[:, hf * 512:(hf + 1) * 512], pb[:, :])
            P.dma('sp', ys_d[b * 512 + sub * 128:b * 512 + (sub + 1) * 128, :], yb_[:], w=[('ys', b, sub)])
        if use_skip:
            P.cond_end()
    A.release(disp_mark)
    if debug and os.environ.get("MK_MOE_STOP") == "2":
        P.finish()
        return nc, dbg
    yk = [[sb(f"yk{j}_{k}", [128, D]) for k in range(4)] for j in range(3)]
    rt2 = [sb(f"rt2_{j}", [128, D]) for j in range(3)]; ot = [sb(f"ot{j}", [128, D]) for j in range(3)]
    for i in range(32):
        j = i % 3
        P.dma('sp', rt2[j][:], h1_d[i * 128:(i + 1) * 128, :], r=[('h1_d', i)])
        for k in range(4):
            P.idma(yk[j][k][:], ys_d, in_off=SLOT[:, i, k:k + 1], r=[SLOT])
        P.act(yk[j][0][:], yk[j][0][:], AF.Identity, scale=G4[:, i, 0:1])
        P.stt(rt2[j][:], rt2[j][:], DN_ALPHA, yk[j][0][:], ALU.mult, ALU.add)
        for k in range(1, 4):
            P.stt(rt2[j][:], yk[j][k][:], G4[:, i, k:k + 1], rt2[j][:], ALU.mult, ALU.add)
        layer_norm_rows(rt2[j], ot[j], mul_eng='dve')
        P.dma('sp', out[i * 128:(i + 1) * 128, :], ot[j][:])
    P.finish()
    return nc, dbg


_IN_NAMES = ['emb_ln_g', 'emb_ln_b', 'w_in', 'conv_qkv', 'a_log', 'dt_bias', 'dn_norm_g', 'w_a_o', 'b_glu', 'conv_dw',
             'b_dw', 'conv_ln_g', 'conv_ln_b', 'w_b_o', 'b_b_o', 'b_gate', 'w_out', 'ln1_g', 'ln1_b', 'w_router',
             'b_router', 'w_gu', 'b_gu', 'w_down', 'b_down', 'ln2_g', 'ln2_b']


def make_in_maps(inputs, n_cores=8):
    shared = {}
    for k in _IN_NAMES:
        a = np.ascontiguousarray(np.asarray(inputs[k], dtype=np.float32))
        a = a[0] if k not in ('emb_ln_g', 'emb_ln_b') else a
        if k in ('a_log', 'dt_bias'):
            a = a.reshape(16)
        shared[k] = np.ascontiguousarray(a)
    xs = np.asarray(inputs['x'], dtype=np.float32)
    maps = []
    for c in range(n_cores):
        m = dict(shared)
        m['x'] = np.ascontiguousarray(xs[c])
        maps.append(m)
    return maps


def kernel(**inputs):
    nc, _ = build_program()
    maps = make_in_maps(inputs, 8)
    res = run_bass_kernel_spmd(nc, maps, core_ids=list(range(8)))
    return np.stack([np.asarray(res.results[c]["out"]) for c in range(8)], axis=0).astype(np.float32)
```

```python
import os
import numpy as np
import concourse.bass as bass
import concourse.mybir as mybir
from concourse.bass_utils import run_bass_kernel_spmd

F32 = mybir.dt.float32
BF16 = mybir.dt.bfloat16
AF = mybir.ActivationFunctionType
ALU = mybir.AluOpType
AX = mybir.AxisListType

S = 4096
D = 1024
NCOL = 8224
NCH = 64
DN_ALPHA = 2.0 ** 0.25
LN_EPS = 1e-5
RMS_EPS = 1e-6
L2_EPS = 1e-6
NEG_BIG = -1.0e30
COL_Q, COL_K, COL_V, COL_Z, COL_SM, COL_GLU, COL_GATE = 0, 1024, 2048, 3072, 4096, 4128, 6176


class Prog:
    def __init__(self, nc, n_dma_sems=12, self_sync=True):
        self.nc = nc
        self.eng = {'pe': nc.tensor, 'act': nc.scalar, 'dve': nc.vector, 'pool': nc.gpsimd, 'sp': nc.sync}
        self.streams = {e: [] for e in self.eng}
        self.cnt = {e: 0 for e in self.eng}
        self.sem = {e: nc.alloc_semaphore(name=f"s_{e}") for e in self.eng}
        self.seen = {e: {} for e in self.eng}
        self.lastw = {}
        self.reads = {}
        self.semobj = {('e', e): self.sem[e] for e in self.eng}
        self.dma_sems = {}
        self.n_dma_sems = n_dma_sems
        self.self_sync = self_sync
        self.n_inst = 0

    def _dma_sem(self, q):
        if q not in self.dma_sems:
            lst = []
            for i in range(self.n_dma_sems):
                s = self.nc.alloc_semaphore(name=f"d_{q}_{i}")
                key = ('d', q, i)
                self.semobj[key] = s
                lst.append([key, 0])
            self.dma_sems[q] = [lst, 0]
        lst, idx = self.dma_sems[q]
        self.dma_sems[q][1] = (idx + 1) % len(lst)
        return lst[idx]

    def _deps(self, e, reads, writes):
        deps = {}

        def add(k, v):
            if deps.get(k, 0) < v:
                deps[k] = v
        for r in reads:
            ev = self.lastw.get(r)
            if ev is not None:
                add(*ev)
        for w in writes:
            ev = self.lastw.get(w)
            if ev is not None:
                add(*ev)
            for k, v in self.reads.get(w, {}).items():
                add(k, v)
        waits = []
        for k, v in deps.items():
            if k == ('e', e):
                if not self.self_sync or e in ('pe', 'sp'):
                    continue
            if self.seen[e].get(k, 0) >= v:
                continue
            self.seen[e][k] = v
            waits.append((self.semobj[k], v))
        return waits

    def _commit(self, ev, reads, writes):
        k, v = ev
        for w in writes:
            self.lastw[w] = ev
            self.reads[w] = {}
        for r in reads:
            d = self.reads.setdefault(r, {})
            if d.get(k, 0) < v:
                d[k] = v

    @staticmethod
    def _names(aps):
        out = []
        for a in aps:
            if a is None or isinstance(a, (int, float)):
                continue
            if isinstance(a, (str, tuple)):
                out.append(a)
            else:
                out.append(a.name)
        return out

    def op(self, e, fn, reads, writes):
        reads = self._names(reads)
        writes = self._names(writes)
        pr = [n for n in reads if isinstance(n, str) and n.startswith('bank')]
        if pr:
            reads = [n for n in reads if n not in pr]
            writes = writes + [n for n in pr if n not in writes]
        waits = self._deps(e, reads, writes)
        self.cnt[e] += 1
        seq = self.cnt[e]
        sem = self.sem[e]

        def emit(engobj, waits=waits, fn=fn, sem=sem):
            for s, v in waits:
                engobj.wait_ge(s, v)
            fn(engobj).then_inc(sem, 1)
        self.streams[e].append(emit)
        self._commit((('e', e), seq), reads, writes)
        self.n_inst += 1

    def dma(self, q, out, in_, r=None, w=None, **kw):
        reads = self._names(r if r is not None else [in_])
        writes = self._names(w if w is not None else [out])
        slot = self._dma_sem(q)
        key, prev = slot
        waits = self._deps(q, reads, writes)
        if prev > 0 and self.seen[q].get(key, 0) < prev:
            self.seen[q][key] = prev
            waits.append((self.semobj[key], prev))
        slot[1] = prev + 16
        tgt = slot[1]
        s = self.semobj[key]

        def emit(engobj, waits=waits, s=s, out=out, in_=in_, kw=kw):
            for ss, v in waits:
                engobj.wait_ge(ss, v)
            engobj.dma_start(out=out, in_=in_, **kw).then_inc(s, 16)
        self.streams[q].append(emit)
        self._commit((key, tgt), reads, writes)
        self.n_inst += 1

    def idma(self, out, in_, out_off=None, in_off=None, r=None, w=None):
        q = 'pool'
        reads = self._names(r if r is not None else [in_, out_off, in_off])
        writes = self._names(w if w is not None else [out])
        slot = self._dma_sem(q)
        key, prev = slot
        waits = self._deps(q, reads, writes)
        if prev > 0 and self.seen[q].get(key, 0) < prev:
            self.seen[q][key] = prev
            waits.append((self.semobj[key], prev))
        slot[1] = prev + 16
        tgt = slot[1]
        s = self.semobj[key]

        def emit(engobj, waits=waits, s=s):
            for ss, v in waits:
                engobj.wait_ge(ss, v)
            oo = bass.IndirectOffsetOnAxis(ap=out_off, axis=0) if out_off is not None else None
            io = bass.IndirectOffsetOnAxis(ap=in_off, axis=0) if in_off is not None else None
            engobj.indirect_dma_start(out=out, out_offset=oo, in_=in_, in_offset=io).then_inc(s, 16)
        self.streams[q].append(emit)
        self._commit((key, tgt), reads, writes)
        self.n_inst += 1

    def cond_begin(self, flag_ap):
        st = {'cnt0': dict(self.cnt), 'seen0': {e: dict(d) for e, d in self.seen.items()}, 'dma0': {}, 'ctx': {}, 'used': {}}
        for q, (lst, _) in self.dma_sems.items():
            for key, val in lst:
                st['dma0'][key] = val
        self._cond = st
        if not hasattr(self, '_cregs'):
            self._cregs = {}
        for e in self.eng:
            waits = self._deps(e, self._names([flag_ap]), [])

            def emit(engobj, waits=waits, e=e, st=st):
                if not st['used'].get(e):
                    return
                for s_, v in waits:
                    engobj.wait_ge(s_, v)
                if e not in self._cregs:
                    self._cregs[e] = engobj.alloc_register(f"cflag_{e}")
                r = self._cregs[e]
                engobj.reg_load(r, flag_ap)
                v = engobj.snap(r)
                ctx = engobj.If(v == 1)
                ctx.__enter__()
                st['ctx'][e] = (ctx, v)
            self.streams[e].append(emit)

    def cond_end(self):
        st = self._cond
        for e in self.eng:
            n_e = self.cnt[e] - st['cnt0'].get(e, 0)
            comp = []
            if e in self.dma_sems:
                for key, val in self.dma_sems[e][0]:
                    d = val - st['dma0'].get(key, 0)
                    if d:
                        comp.append((self.semobj[key], d, st['dma0'].get(key, 0)))
            st['used'][e] = bool(n_e or comp)

            def emit(engobj, e=e, n_e=n_e, comp=comp, st=st):
                if not st['used'].get(e):
                    return
                ctx, v = st['ctx'][e]
                ctx.__exit__(None, None, None)
                with engobj.Else():
                    engobj.drain()
                    for so, d, before in comp:
                        if before > 0:
                            engobj.wait_ge(so, before)
                        engobj.sem_inc(so, d)
                    if n_e:
                        engobj.sem_inc(self.sem[e], n_e)
                engobj.free_register(v.val)
            self.streams[e].append(emit)
        self.seen = st['seen0']
        self._cond = None

    def barrier(self):
        cur = {}
        for e in self.eng:
            if self.cnt[e] > 0:
                cur[('e', e)] = self.cnt[e]
        for q, (lst, _) in self.dma_sems.items():
            for key, val in lst:
                if val > 0:
                    cur[key] = val
        for e in self.eng:
            waits = []
            for k, v in cur.items():
                if k == ('e', e):
                    continue
                if self.seen[e].get(k, 0) >= v:
                    continue
                self.seen[e][k] = v
                waits.append((self.semobj[k], v))

            def emit(engobj, waits=waits):
                for s, v in waits:
                    engobj.wait_ge(s, v)
            self.streams[e].append(emit)

    def finish(self):
        self.barrier()
        nc = self.nc
        with nc.Block() as block:
            @block.tensor
            def _(e):
                for f in self.streams['pe']:
                    f(e)

            @block.scalar
            def _(e):
                for f in self.streams['act']:
                    f(e)

            @block.vector
            def _(e):
                for f in self.streams['dve']:
                    f(e)

            @block.gpsimd
            def _(e):
                for f in self.streams['pool']:
                    f(e)

            @block.sync
            def _(e):
                for f in self.streams['sp']:
                    f(e)

    def mm(self, out, lhsT, rhs, start=True, stop=True, r=None, w=None):
        self.op('pe', lambda e: e.matmul(out, lhsT, rhs, start=start, stop=stop),
                r if r is not None else [lhsT, rhs], w if w is not None else [out])

    def tr(self, out, in_, ident, r=None, w=None):
        self.op('pe', lambda e: e.transpose(out=out, in_=in_, identity=ident),
                r if r is not None else [in_, ident], w if w is not None else [out])

    def act(self, out, in_, func, bias=None, scale=None, accum=None, r=None, w=None):
        kw = {}
        if bias is not None:
            kw['bias'] = bias
        if scale is not None:
            kw['scale'] = scale
        if accum is not None:
            kw['accum_out'] = accum
        self.op('act', lambda e: e.activation(out=out, in_=in_, func=func, **kw),
                r if r is not None else [in_, bias, scale], w if w is not None else [out, accum])

    def tt(self, eng, out, in0, in1, op, r=None, w=None):
        self.op(eng, lambda e: e.tensor_tensor(out=out, in0=in0, in1=in1, op=op),
                r if r is not None else [in0, in1], w if w is not None else [out])

    def ts(self, eng, out, in0, s1, s2, op0, op1=None, r=None, w=None):
        if op1 is None:
            fn = lambda e: e.tensor_scalar(out=out, in0=in0, scalar1=s1, scalar2=None, op0=op0)
        else:
            fn = lambda e: e.tensor_scalar(out=out, in0=in0, scalar1=s1, scalar2=s2, op0=op0, op1=op1)
        self.op(eng, fn, r if r is not None else [in0, s1, s2], w if w is not None else [out])

    def stt(self, out, in0, scalar, in1, op0, op1, r=None, w=None):
        self.op('dve', lambda e: e.scalar_tensor_tensor(out=out, in0=in0, scalar=scalar, in1=in1, op0=op0, op1=op1),
                r if r is not None else [in0, scalar, in1], w if w is not None else [out])

    def copy(self, eng, out, in_, r=None, w=None):
        if eng == 'act':
            fn = lambda e: e.copy(out=out, in_=in_)
        else:
            fn = lambda e: e.tensor_copy(out=out, in_=in_)
        self.op(eng, fn, r if r is not None else [in_], w if w is not None else [out])

    def memset(self, eng, ap, val):
        self.op(eng, lambda e: e.memset(ap, val), [], [ap])


def bc(ap, shape, axis):
    return ap.unsqueeze(axis).to_broadcast(shape)


class Arena:
    def __init__(self, nc, P, base=16512, top=229344):
        self.nc = nc; self.P = P; self.ptr = base; self.top = top; self.n = 0; self.peak = base

    def alloc(self, name, shape, dt=F32):
        size = 2 if dt == BF16 else 4
        n = 1
        for d in shape[1:]:
            n *= d
        nbytes = (n * size + 63) // 64 * 64
        self.n += 1
        t = self.nc.alloc_sbuf_tensor_at(f"{name}_{self.n}", list(shape), dt, offset=self.ptr)
        self.ptr += nbytes
        self.peak = max(self.peak, self.ptr)
        assert self.ptr <= self.top, f"SBUF arena overflow at {name}: {self.ptr} > {self.top}"
        return t

    def mark(self):
        return self.ptr

    def release(self, m):
        self.P.barrier()
        self.ptr = m


def build_program(stage=99, debug=False):
    nc = bass.Bass("TRN2", target_bir_lowering=False)
    P = Prog(nc)
    A = Arena(nc, P)
    dbg = {}

    def dram_in(name, shape):
        return nc.dram_tensor(name, shape, F32, kind="ExternalInput").ap()

    def scratch(name, shape, dt, force_internal=False):
        kind = "ExternalOutput" if (debug and not force_internal) else "Internal"
        return nc.dram_tensor(name, shape, dt, kind=kind).ap()

    def dbg_out(name, src_ap, shape, dt, r=None):
        if not debug:
            return
        t = nc.dram_tensor(name, shape, dt, kind="ExternalOutput").ap()
        P.dma('sp', t, src_ap, r=r)

    sb = A.alloc

    x = dram_in("x", [S, D])
    emb_ln_g = dram_in("emb_ln_g", [D]); emb_ln_b = dram_in("emb_ln_b", [D])
    w_in = dram_in("w_in", [D, NCOL])
    conv_qkv = dram_in("conv_qkv", [5, 3072])
    a_log = dram_in("a_log", [16]); dt_bias = dram_in("dt_bias", [16])
    dn_norm_g = dram_in("dn_norm_g", [128])
    w_a_o = dram_in("w_a_o", [D, D])
    b_glu = dram_in("b_glu", [2048]); conv_dw = dram_in("conv_dw", [31, D]); b_dw = dram_in("b_dw", [D])
    conv_ln_g = dram_in("conv_ln_g", [D]); conv_ln_b = dram_in("conv_ln_b", [D])
    w_b_o = dram_in("w_b_o", [D, D]); b_b_o = dram_in("b_b_o", [D]); b_gate = dram_in("b_gate", [2048])
    w_out = dram_in("w_out", [D, D])
    ln1_g = dram_in("ln1_g", [D]); ln1_b = dram_in("ln1_b", [D])
    w_router = dram_in("w_router", [D, 32]); b_router = dram_in("b_router", [32])
    w_gu = dram_in("w_gu", [32, D, 2048]); b_gu = dram_in("b_gu", [32, 2048])
    w_down = dram_in("w_down", [32, D, D]); b_down = dram_in("b_down", [32, D])
    ln2_g = dram_in("ln2_g", [D]); ln2_b = dram_in("ln2_b", [D])
    out = nc.dram_tensor("out", [S, D], F32, kind="ExternalOutput").ap()

    h0_d = scratch("h0_d", [S, D], F32)
    hT_d = scratch("hT_d", [D, S], BF16)
    ogT_d = scratch("ogT_d", [D, S], BF16)
    ybT_d = scratch("ybT_d", [D, S], BF16)
    gate_d = scratch("gate_d", [2048, S], BF16)
    h1_d = scratch("h1_d", [S, D], F32)
    st_d = scratch("st_d", [2, S], F32)
    h1b_d = scratch("h1b_d", [S, D], BF16)
    xs_d = scratch("xs_d", [32768, D], BF16, force_internal=True)
    ys_d = scratch("ys_d", [32768, D], F32, force_internal=True)

    hT_dv = hT_d.rearrange("(kc p) t -> p kc t", p=128)
    w_in_v = w_in.rearrange("(kc p) c -> p kc c", p=128)

    identf = sb("identf", [128, 128]); identb = sb("identb", [128, 128], BF16)
    onesf = sb("onesf", [128, 128]); onesb = sb("onesb", [128, 128], BF16)
    zerof = sb("zerof", [128, 512])
    P.memset('pool', onesf[:], 1.0)
    P.memset('pool', zerof[:], 0.0)
    P.memset('pool', identf[:], 1.0)
    P.op('pool', lambda e: e.affine_select(out=identf[:], in_=identf[:], pattern=[[-1, 128]], compare_op=ALU.is_equal,
                                           fill=0.0, base=0, channel_multiplier=1), [identf], [identf])
    P.copy('dve', identb[:], identf[:])
    P.copy('dve', onesb[:], onesf[:])
    eps_ln = sb("eps_ln", [128, 1])
    P.memset('pool', eps_ln[:], LN_EPS)
    bank = [nc.alloc_psum_tensor(f"bank{i}", [128, 512], F32) for i in range(8)]

    def bank_bf(i):
        return bank[i][:].bitcast(BF16)

    ln_sets = [(sb(f"ln_st{i}", [128, 12]), sb(f"ln_mv{i}", [128, 2]), sb(f"ln_rs{i}", [128, 1])) for i in range(3)]
    ln_cnt = [0]
    lnp = {}

    def layer_norm_rows(t, o, np_=128, mul_eng='pool'):
        ln_st, ln_mv, ln_rs = ln_sets[ln_cnt[0] % 3]; ln_cnt[0] += 1
        P.op('dve', lambda e: e.bn_stats(out=ln_st[:np_, 0:6], in_=t[:np_, 0:512]), [t], [ln_st])
        P.op('dve', lambda e: e.bn_stats(out=ln_st[:np_, 6:12], in_=t[:np_, 512:1024]), [t], [ln_st])
        P.op('dve', lambda e: e.bn_aggr(out=ln_mv[:np_, :], in_=ln_st[:np_, :]), [ln_st], [ln_mv])
        P.act(ln_rs[:np_, :], ln_mv[:np_, 1:2], AF.Sqrt, bias=eps_ln[:np_, 0:1])
        P.op('dve', lambda e: e.reciprocal(out=ln_rs[:np_, :], in_=ln_rs[:np_, :]), [ln_rs], [ln_rs])
        P.ts('dve', t[:np_, :], t[:np_, :], ln_mv[:np_, 0:1], ln_rs[:np_, 0:1], ALU.subtract, ALU.mult)
        P.tt(mul_eng, t[:np_, :], t[:np_, :], lnp['g'][:np_, :], ALU.mult)
        P.tt('dve', o[:np_, :], t[:np_, :], lnp['b'][:np_, :], ALU.add)

    vp = sb("vp", [128, 64])
    base_mark = A.mark()
    lnp['g'] = sb("gB0", [128, D]); lnp['b'] = sb("bB0", [128, D])
    P.dma('sp', lnp['g'][:], emb_ln_g.partition_broadcast(128))
    P.dma('sp', lnp['b'][:], emb_ln_b.partition_broadcast(128))
    xt = [sb(f"xt{i}", [128, D]) for i in range(2)]
    ht = [sb(f"ht{i}", [128, D]) for i in range(2)]
    hTs = [sb(f"hTs{i}", [128, 8, 512], BF16) for i in range(2)]
    for i in range(32):
        xb = xt[i % 2]; hb = ht[i % 2]; hs = hTs[(i // 4) % 2]
        P.dma('sp', xb[:], x[i * 128:(i + 1) * 128, :])
        layer_norm_rows(xb, hb)
        P.dma('sp', h0_d[i * 128:(i + 1) * 128, :], hb[:], w=[('h0_d', i)])
        for half in range(2):
            pb = bank[half]
            for q4 in range(4):
                kc = half * 4 + q4
                P.tr(pb[:, q4 * 128:(q4 + 1) * 128], hb[:, kc * 128:(kc + 1) * 128], identf[:])
            P.copy('act', hs[:, half * 4:(half + 1) * 4, (i % 4) * 128:(i % 4 + 1) * 128],
                   pb[:].rearrange("p (k t) -> p k t", t=128))
        if i % 4 == 3:
            tt_ = i // 4
            P.dma('sp', hT_dv[:, :, tt_ * 512:(tt_ + 1) * 512], hs[:], w=[('hT_d', tt_)])
    A.release(base_mark)
    if stage <= 1:
        P.finish()
        return nc, dbg

    tri = {}; ntri = {}; negT = {}; nstrT = {}
    for d_, (cm, st_) in (('f', (-1, 1)), ('b', (1, -1))):
        t_ = sb(f"tri_{d_}", [64, 64]); nt_ = sb(f"ntri_{d_}", [64, 64])
        P.op('pool', lambda e, t_=t_, cm=cm, st_=st_: e.affine_select(out=t_[:], in_=onesf[0:64, 0:64], pattern=[[st_, 64]],
             compare_op=ALU.is_ge, fill=0.0, base=0, channel_multiplier=cm), [onesf], [t_])
        P.ts('dve', nt_[:], t_[:], -1.0, None, ALU.mult)
        tri[d_] = t_; ntri[d_] = nt_
        ng_ = sb(f"negT_{d_}", [64, 8, 64])
        P.op('pool', lambda e, ng_=ng_, cm=cm, st_=st_: e.affine_select(out=ng_[:], in_=zerof[0:64, 0:512].rearrange("p (c i) -> p c i", i=64),
             pattern=[[0, 8], [st_, 64]], compare_op=ALU.is_ge, fill=NEG_BIG, base=0, channel_multiplier=cm), [zerof], [ng_])
        negT[d_] = ng_
        ns_ = sb(f"nstrT_{d_}", [64, 64])
        P.op('pool', lambda e, ns_=ns_, cm=cm, st_=st_: e.affine_select(out=ns_[:], in_=onesf[0:64, 0:64], pattern=[[st_, 64]],
             compare_op=ALU.is_gt, fill=0.0, base=0, channel_multiplier=cm), [onesf], [ns_])
        P.ts('dve', ns_[:], ns_[:], -1.0, None, ALU.mult)
        nstrT[d_] = ns_
    BETA = {d_: sb(f"BETA_{d_}", [64, 8, 64]) for d_ in 'fb'}
    LA = {d_: sb(f"LA_{d_}", [64, 8, 64]) for d_ in 'fb'}
    hTt = [sb(f"hTt{i}", [128, 8, 512], BF16) for i in range(2)]
    hcnt = [0]

    def load_hT(tt_):
        hb = hTt[hcnt[0] % 2]; hcnt[0] += 1
        P.dma('sp', hb[:], hT_dv[:, :, tt_ * 512:(tt_ + 1) * 512], r=[('hT_d', tt_)])
        return hb

    s2_mark = A.mark()
    SMraw = sb("SMraw", [64, 64, 32])
    Wsm = sb("Wsm", [128, 8, 32], BF16)
    alB = sb("alB", [64, 16]); dtB = sb("dtB", [64, 16]); nea = sb("nea", [64, 16])
    X_ = sb("spX", [64, 8, 64]); A_ = sb("spA", [64, 8, 64])
    P.dma('pool', Wsm[:], w_in_v[:, :, COL_SM:COL_SM + 32])
    P.dma('sp', alB[:], a_log.partition_broadcast(64))
    P.dma('sp', dtB[:], dt_bias.partition_broadcast(64))
    P.act(nea[:], alB[:], AF.Exp)
    P.ts('dve', nea[:], nea[:], -1.0, None, ALU.mult)
    for tt_ in range(8):
        hb = load_hT(tt_)
        pb = bank[2 + tt_ % 2]
        for cc in range(8):
            for kc in range(8):
                P.mm(pb[0:64, cc * 32:(cc + 1) * 32], hb[:, kc, cc * 64:(cc + 1) * 64], Wsm[:, kc, :], start=(kc == 0), stop=(kc == 7))
        P.copy('dve', SMraw[:, tt_ * 8:(tt_ + 1) * 8, :], pb[0:64, 0:256].rearrange("p (c k) -> p c k", k=32))
    for di, d_ in enumerate('fb'):
        ub = SMraw[:, :, di * 8:(di + 1) * 8].rearrange("p c h -> p h c")
        ua = SMraw[:, :, 16 + di * 8:24 + di * 8].rearrange("p c h -> p h c")
        P.act(BETA[d_][:], ub, AF.Sigmoid)
        P.tt('dve', X_[:], ua, bc(dtB[:, di * 8:(di + 1) * 8], [64, 8, 64], 2), ALU.add)
        P.stt(A_[:], X_[:], -1.0, X_[:], ALU.mult, ALU.max)
        P.act(A_[:], A_[:], AF.Exp, scale=-1.0)
        P.act(A_[:], A_[:], AF.Ln, bias=1.0)
        P.stt(A_[:], X_[:], 0.0, A_[:], ALU.max, ALU.add)
        P.tt('dve', LA[d_][:], A_[:], bc(nea[:, di * 8:(di + 1) * 8], [64, 8, 64], 2), ALU.mult)
    if debug:
        for d_ in 'fb':
            dbg_out(f"dbg_beta_{d_}", BETA[d_][:], [64, 8, 64], F32)
            dbg_out(f"dbg_la_{d_}", LA[d_][:], [64, 8, 64], F32)
    A.release(s2_mark)
    if stage <= 2:
        P.finish()
        return nc, dbg


    nheads = int(os.environ.get("MK_NHEADS", "8")) if debug else 8
    head_mark = A.mark()
    cst = sb("cst", [120, 128]); cwq = sb("cwq", [128, 120])
    P.dma('sp', cst[:], conv_qkv.rearrange("t (c p) -> (t c) p", p=128))
    P.tr(bank[0][:, 0:120], cst[:, :], identf[0:120, 0:120])
    P.copy('dve', cwq[:], bank[0][:, 0:120])
    eps_l2 = sb("eps_l2", [128, 1]); P.memset('pool', eps_l2[:], L2_EPS)
    eps_rms = sb("eps_rms", [128, 1]); P.memset('pool', eps_rms[:], RMS_EPS)
    ngcol = sb("ngcol", [128, 1]); P.dma('sp', ngcol[:], dn_norm_g.rearrange("(p o) -> p o", o=1))
    Wg = [sb(f"Wg{i}", [128, 8, 128], BF16) for i in range(1)]
    U3 = [sb(f"U{j}", [128, 4100], BF16) for j in range(3)]
    for U in U3:
        P.memset('pool', U[:, 0:2], 0.0); P.memset('pool', U[:, 4098:4100], 0.0)
    Wg3 = sb("Wg3", [128, 8, 384], BF16)
    C = sb("C", [128, 4096], BF16)
    Dg = sb("Dg", [128, 5, 128], BF16)
    sqb = sb("sqb", [128, 512], BF16); lnb = sb("lnb", [128, 512])
    qT = sb("qT", [128, 4096], BF16); kT = sb("kT", [128, 4096], BF16)
    Ktok = sb("Ktok", [64, 64, 128], BF16); Vtok = sb("Vtok", [64, 64, 128], BF16)
    O = sb("O", [64, 64, 128], BF16)
    qkT = {d_: sb(f"qkT_{d_}", [64, 64, 64], BF16) for d_ in 'fb'}
    TT = {d_: sb(f"TT_{d_}", [64, 64, 64], BF16) for d_ in 'fb'}
    Gd = {d_: sb(f"Gd_{d_}", [64, 64]) for d_ in 'fb'}
    EG = {d_: sb(f"EG_{d_}", [64, 64]) for d_ in 'fb'}
    NEG = {d_: sb(f"NEG_{d_}", [64, 64]) for d_ in 'fb'}
    TAIL = {d_: sb(f"TAIL_{d_}", [64, 64]) for d_ in 'fb'}
    CD = {d_: sb(f"CD_{d_}", [128, 64]) for d_ in 'fb'}
    LAm = sb("LAm", [64, 8, 64]); LAb = sb("LAb", [64, 8, 64])
    decT = {d_: sb(f"decT_{d_}", [64, 8, 64]) for d_ in 'fb'}; tY = {d_: sb(f"tY_{d_}", [64, 8, 64]) for d_ in 'fb'}
    Yb = {d_: [sb(f"Yb{d_}{i}", [64, 8, 64], BF16) for i in range(2)] for d_ in 'fb'}
    Zb = {d_: [sb(f"Zb{d_}{i}", [64, 8, 64], BF16) for i in range(2)] for d_ in 'fb'}
    Gm = {d_: [sb(f"Gm{d_}{i}", [64, 8, 64], BF16) for i in range(2)] for d_ in 'fb'}
    Sst = {d_: sb(f"S_{d_}", [128, 128]) for d_ in 'fb'}
    Sbf = {d_: sb(f"Sb_{d_}", [128, 128], BF16) for d_ in 'fb'}
    Rt = {d_: sb(f"R_{d_}", [64, 128], BF16) for d_ in 'fb'}
    vnew = {d_: sb(f"vnew_{d_}", [64, 128], BF16) for d_ in 'fb'}
    tq = {d_: sb(f"tq_{d_}", [64, 128]) for d_ in 'fb'}
    ktl = {d_: sb(f"ktl_{d_}", [64, 128], BF16) for d_ in 'fb'}
    otmp = sb("otmp", [64, 128])
    ssq = sb("ssq", [64, 64])
    wcnt = [0]; pcnt = [0]

    def nextbank():
        b_ = bank[pcnt[0] % 2]; pcnt[0] += 1
        return b_

    def inproj_chunk(col0, evac):
        wb = Wg[0]; wcnt[0] += 1
        P.dma('pool', wb[:], w_in_v[:, :, col0:col0 + 128])
        for tt_ in range(8):
            hb = load_hT(tt_)
            pb = nextbank()
            for kc in range(8):
                P.mm(pb[:, :], wb[:, kc, :], hb[:, kc, :], start=(kc == 0), stop=(kc == 7))
            evac(tt_, pb)

    def inproj_qkv(h_):
        for j, cb in enumerate((COL_Q, COL_K, COL_V)):
            P.dma('pool', Wg3[:, :, j * 128:(j + 1) * 128], w_in_v[:, :, cb + h_ * 128:cb + (h_ + 1) * 128], w=[f"Wg3_{j}"])
        for tt_ in range(8):
            hb = load_hT(tt_)
            for j in range(3):
                pb = nextbank()
                for kc in range(8):
                    P.mm(pb[:, :], Wg3[:, kc, j * 128:(j + 1) * 128], hb[:, kc, :], start=(kc == 0), stop=(kc == 7), r=[f"Wg3_{j}", hb])
                P.copy('act', U3[j][:, 2 + tt_ * 512:2 + (tt_ + 1) * 512], pb[:, :])

    def conv_silu(ci, U):
        for t in range(5):
            P.ts('dve', Dg[:, t, :], identb[:], cwq[:, t * 24 + ci:t * 24 + ci + 1], None, ALU.mult)
        for tt_ in range(8):
            pb = nextbank()
            for t in range(5):
                P.mm(pb[:, :], Dg[:, t, :], U[:, tt_ * 512 + t:tt_ * 512 + t + 512], start=(t == 0), stop=(t == 4))
            P.act(C[:, tt_ * 512:(tt_ + 1) * 512], pb[:, :], AF.Silu)

    def l2norm_to(dst, scale):
        for tt_ in range(8):
            sl = slice(tt_ * 512, (tt_ + 1) * 512)
            P.act(sqb[:], C[:, sl], AF.Square)
            pb = nextbank()
            P.mm(pb[:, :], onesb[:], sqb[:])
            P.act(lnb[:], pb[:, :], AF.Ln, bias=eps_l2[:, 0:1])
            P.act(lnb[:], lnb[:], AF.Exp, scale=-0.5)
            P.stt(dst[:, sl], C[:, sl], scale, lnb[:], ALU.mult, ALU.mult)

    def to_tokmajor(src, dst):
        for g in range(8):
            bi = 2 + g % 2
            pbf = bank_bf(bi)
            for cc in range(8):
                c = g * 8 + cc
                P.tr(pbf[0:64, cc * 128:(cc + 1) * 128], src[:, c * 64:(c + 1) * 64], identb[:])
            P.copy('dve' if g % 2 == 0 else 'act', dst[:, g * 8:(g + 1) * 8, :], pbf[0:64, :].rearrange("p (c d) -> p c d", d=128))

    def flat(t3):
        return t3[:].rearrange("p c i -> p (c i)")

    for h in range(nheads):
        inproj_qkv(h)
        conv_silu(h, U3[0]); l2norm_to(qT, 128.0 ** -0.5)
        conv_silu(8 + h, U3[1]); l2norm_to(kT, 1.0)
        to_tokmajor(kT, Ktok)
        conv_silu(16 + h, U3[2])
        to_tokmajor(C, Vtok)
        if debug and h == 0:
            dbg_out("dbg_qT", qT[:], [128, 4096], BF16)
            dbg_out("dbg_kT", kT[:], [128, 4096], BF16)
            dbg_out("dbg_Ktok", Ktok[:], [64, 64, 128], BF16)
            dbg_out("dbg_Vtok", Vtok[:], [64, 64, 128], BF16)
        if stage <= 3:
            continue
        for d_ in 'fb':
            la_h = LA[d_][:, h, :]
            P.mm(bank[4][0:64, 0:64], tri[d_][:], la_h)
            P.copy('dve', Gd[d_][:], bank[4][0:64, 0:64])
            P.mm(bank[5][:, 0:64], onesf[0:64, :], la_h)
            P.act(CD[d_][:], bank[5][:, 0:64], AF.Exp)
            P.tt('dve', TAIL[d_][:], bank[5][0:64, 0:64], Gd[d_][:], ALU.subtract)
            P.act(TAIL[d_][:], TAIL[d_][:], AF.Exp)
            P.act(EG[d_][:], Gd[d_][:], AF.Exp)
            P.ts('dve', NEG[d_][:], EG[d_][:], -1.0, None, ALU.mult)
        for g in range(8):
            c0 = g * 8
            for cc in range(8):
                ch = slice((c0 + cc) * 64, (c0 + cc + 1) * 64)
                P.mm(bank[4][0:64, cc * 64:(cc + 1) * 64], kT[:, ch], kT[:, ch])
            for cc in range(8):
                ch = slice((c0 + cc) * 64, (c0 + cc + 1) * 64)
                P.mm(bank[5][0:64, cc * 64:(cc + 1) * 64], kT[:, ch], qT[:, ch])
            bk = {'f': (2, 3, 7), 'b': (0, 1, 6)}
            for d_ in 'fb':
                la_g = LA[d_][:, h, c0:c0 + 8]
                P.tt('pool', LAm[:], bc(la_g, [64, 8, 64], 2), bc(tri[d_][:], [64, 8, 64], 1), ALU.mult)
                P.copy('pool', LAb[:], bc(la_g, [64, 8, 64], 2))
                P.mm(bank[6][0:64, :], onesf[0:64, 0:64], flat(LAm), start=True, stop=False)
                P.mm(bank[6][0:64, :], ntri[d_][:], flat(LAb), start=False, stop=False)
                P.mm(bank[6][0:64, :], identf[0:64, 0:64], flat(negT[d_]), start=False, stop=True)
                P.act(flat(decT[d_]), bank[6][0:64, :], AF.Exp)
            for d_ in 'fb':
                P.tt('dve', flat(qkT[d_])[:, c0 * 64:(c0 + 8) * 64], bank[5][0:64, :], flat(decT[d_]), ALU.mult)
                P.tt('dve', flat(tY[d_]), bank[4][0:64, :], flat(decT[d_]), ALU.mult)
                P.tt('pool', tY[d_][:], tY[d_][:], bc(BETA[d_][:, h, c0:c0 + 8], [64, 8, 64], 2), ALU.mult)
                P.tt('dve', tY[d_][:], tY[d_][:], bc(nstrT[d_][:], [64, 8, 64], 1), ALU.mult)
                P.copy('act', Yb[d_][0][:], tY[d_][:])
                P.tt('pool', Gm[d_][0][:], tY[d_][:], bc(identf[0:64, 0:64], [64, 8, 64], 1), ALU.add)
            for d_ in 'fb':
                z0 = bank_bf(bk[d_][2])
                for cc in range(8):
                    P.tr(z0[0:64, cc * 64:(cc + 1) * 64], Yb[d_][0][:, cc, :], identb[0:64, 0:64])
                P.copy('dve' if d_ == 'f' else 'act', flat(Zb[d_][0]), z0[0:64, 0:512])
            for k in range(5):
                cur = k % 2; nxt = 1 - cur
                for d_ in 'fb':
                    by_, bz_, bg_ = bk[d_]
                    if k < 4:
                        for cc in range(8):
                            P.mm(bank[by_][0:64, cc * 64:(cc + 1) * 64], Zb[d_][cur][:, cc, :], Yb[d_][cur][:, cc, :])
                    for cc in range(8):
                        P.mm(bank[bz_][0:64, cc * 64:(cc + 1) * 64], Yb[d_][cur][:, cc, :], Zb[d_][cur][:, cc, :])
                for d_ in 'fb':
                    by_, bz_, bg_ = bk[d_]
                    if k < 4:
                        P.copy('act', flat(Yb[d_][nxt]), bank[by_][0:64, :])
                    P.copy('dve', flat(Zb[d_][nxt]), bank[bz_][0:64, :])
                for d_ in 'fb':
                    by_, bz_, bg_ = bk[d_]
                    for cc in range(8):
                        P.mm(bank[bg_][0:64, cc * 64:(cc + 1) * 64], Zb[d_][nxt][:, cc, :], Gm[d_][cur][:, cc, :])
                for d_ in 'fb':
                    by_, bz_, bg_ = bk[d_]
                    dstG = flat(Gm[d_][nxt]) if k < 4 else flat(TT[d_])[:, c0 * 64:(c0 + 8) * 64]
                    P.tt('dve' if d_ == 'f' else 'pool', dstG, bank[bg_][0:64, :], flat(Gm[d_][cur]), ALU.add) if d_ == 'f' else P.tt('dve', dstG, bank[bg_][0:64, :], flat(Gm[d_][cur]), ALU.add)
        if debug and h == 0:
            for d_ in 'fb':
                dbg_out(f"dbg_TT_{d_}", TT[d_][:], [64, 64, 64], BF16)
                dbg_out(f"dbg_qkT_{d_}", qkT[d_][:], [64, 64, 64], BF16)
        if stage <= 4:
            continue
        for d_ in 'fb':
            P.memset('pool', Sst[d_][:], 0.0)
            P.memset('pool', Sbf[d_][:], 0.0)
        nsteps = int(os.environ.get('MK_STEPS', '64')) if debug else 64
        for s_ in range(nsteps):
            cs = {'f': s_, 'b': 63 - s_}
            pv = {}
            for di, d_ in enumerate('fb'):
                bA = bank[di * 3]; bB_ = bank[di * 3 + 1]; bC = bank[di * 3 + 2]
                pv[d_] = (bA[0:64, 0:128], bB_[0:64, 0:128], bA[0:64, 128:256], bB_[0:64, 128:256], bC[:, 0:128])
            for d_ in 'fb':
                c = cs[d_]; ch = slice(c * 64, (c + 1) * 64); ps1, ps2, ps3, ps4, ps5 = pv[d_]
                P.mm(ps1, kT[:, ch], Sbf[d_][:])
                P.mm(ps2, qT[:, ch], Sbf[d_][:])
            for d_ in 'fb':
                c = cs[d_]; ps1, ps2, ps3, ps4, ps5 = pv[d_]
                P.stt(Rt[d_][:], ps1, NEG[d_][:, c:c + 1], Vtok[:, c, :], ALU.mult, ALU.add)
                P.act(ktl[d_][:], Ktok[:, c, :], AF.Identity, scale=TAIL[d_][:, c:c + 1])
                P.act(tq[d_][:], ps2, AF.Identity, scale=EG[d_][:, c:c + 1])
            for d_ in 'fb':
                c = cs[d_]; ps1, ps2, ps3, ps4, ps5 = pv[d_]
                P.mm(ps3, TT[d_][:, c, :], Rt[d_][:])
            for d_ in 'fb':
                c = cs[d_]; ps1, ps2, ps3, ps4, ps5 = pv[d_]
                P.act(vnew[d_][:], ps3, AF.Identity, scale=BETA[d_][:, h, c:c + 1])
            for d_ in 'fb':
                c = cs[d_]; ps1, ps2, ps3, ps4, ps5 = pv[d_]
                P.mm(ps5, ktl[d_][:], vnew[d_][:])
                P.mm(ps4, qkT[d_][:, c, :], vnew[d_][:])
            for d_ in 'fb':
                c = cs[d_]; ps1, ps2, ps3, ps4, ps5 = pv[d_]
                P.stt(Sbf[d_][:], Sst[d_][:], CD[d_][:, c:c + 1], ps5, ALU.mult, ALU.add)
            for d_ in 'fb':
                c = cs[d_]; ps1, ps2, ps3, ps4, ps5 = pv[d_]
                P.stt(Sst[d_][:], Sst[d_][:], CD[d_][:, c:c + 1], ps5, ALU.mult, ALU.add)
                first = (d_ == 'f' and c < 32) or (d_ == 'b' and c >= 32)
                if first:
                    P.tt('dve', O[:, c, :], tq[d_][:], ps4, ALU.add)
                else:
                    P.tt('dve', otmp[:], tq[d_][:], ps4, ALU.add)
                    P.tt('pool', O[:, c, :], O[:, c, :], otmp[:], ALU.add)
        if debug and h == 0:
            dbg_out("dbg_O", O[:], [64, 64, 128], BF16)
            dbg_out("dbg_S_f", Sst['f'][:], [128, 128], F32)
            dbg_out("dbg_vnew_f", vnew['f'][:], [64, 128], BF16)
            dbg_out("dbg_R_f", Rt['f'][:], [64, 128], BF16)
            dbg_out("dbg_tq_f", tq['f'][:], [64, 128], F32)
            dbg_out("dbg_ktl_f", ktl['f'][:], [64, 128], BF16)
            dbg_out("dbg_CD_f", CD['f'][:], [128, 64], F32)
            dbg_out("dbg_TAIL_f", TAIL['f'][:], [64, 64], F32)
            dbg_out("dbg_EG_f", EG['f'][:], [64, 64], F32)
        if debug and os.environ.get('MK_NOPOST'):
            continue
        Zs = Ktok
        wz = Wg[0]; wcnt[0] += 1
        P.dma('pool', wz[:], w_in_v[:, :, COL_Z + h * 128:COL_Z + (h + 1) * 128])
        for tt_ in range(8):
            hb = load_hT(tt_)
            for hf in range(2):
                pb = nextbank()
                for c4 in range(4):
                    cc = hf * 4 + c4
                    for kc in range(8):
                        P.mm(pb[0:64, c4 * 128:(c4 + 1) * 128], hb[:, kc, cc * 64:(cc + 1) * 64], wz[:, kc, :], start=(kc == 0), stop=(kc == 7))
                c_lo = tt_ * 8 + hf * 4
                P.act(Zs[:, c_lo:c_lo + 4, :], pb[0:64, :].rearrange("p (c d) -> p c d", d=128), AF.Silu)
        SQ = Vtok
        P.act(SQ[:], O[:], AF.Square)
        P.op('dve', lambda e: e.tensor_reduce(out=ssq[:], in_=SQ[:], axis=AX.X, op=ALU.add), [SQ], [ssq])
        P.act(ssq[:], ssq[:], AF.Ln, bias=eps_rms[0:64, 0:1], scale=1.0 / 128.0)
        P.act(ssq[:], ssq[:], AF.Exp, scale=-0.5)
        P.tt('dve', O[:], O[:], bc(ssq[:], [64, 64, 128], 2), ALU.mult)
        OGb = Vtok
        P.tt('dve', OGb[:], O[:], Zs[:], ALU.mult)
        for g in range(8):
            bi = 2 + g % 2
            pbf = bank_bf(bi)
            for cc in range(8):
                P.tr(pbf[:, cc * 64:(cc + 1) * 64], OGb[:, g * 8 + cc, :], identb[0:64, 0:64])
            P.act(C[:, g * 512:(g + 1) * 512], pbf[:, 0:512], AF.Identity, scale=ngcol[:, 0:1])
        P.dma('sp', ogT_d[h * 128:(h + 1) * 128, :], C[:], w=[('ogT_d', h)])
    if stage <= 5:
        P.finish()
        return nc, dbg

    A.release(head_mark)
    vst = sb("vst", [64, 128])
    for r0, src, n in ((0, b_glu, 16), (16, b_gate, 16), (32, b_dw, 8), (40, conv_ln_g, 8), (48, conv_ln_b, 8), (56, b_b_o, 8)):
        P.dma('sp', vst[r0:r0 + n, :], src.rearrange("(c p) -> c p", p=128))
    P.tr(bank[0][:, 0:64], vst[:, :], identf[0:64, 0:64])
    P.copy('dve', vp[:], bank[0][:, 0:64])
    VB_GLU, VB_GATE, VB_DW, VB_LNG, VB_LNB, VB_BBO = 0, 16, 32, 40, 48, 56
    cdst1 = sb("cdst1", [128, 128]); cdst2 = sb("cdst2", [120, 128]); cwd = sb("cwd", [128, 248])
    cdv = conv_dw.rearrange("t (c p) -> (t c) p", p=128)
    P.dma('sp', cdst1[:], cdv[0:128, :])
    P.dma('sp', cdst2[:], cdv[128:248, :])
    P.tr(bank[1][:, 0:128], cdst1[:, :], identf[:, :])
    P.copy('dve', cwd[:, 0:128], bank[1][:, 0:128])
    P.tr(bank[1][:, 128:248], cdst2[:, :], identf[0:120, 0:120])
    P.copy('dve', cwd[:, 128:248], bank[1][:, 128:248])
    Wg4 = [sb(f"Wg4_{i}", [128, 8, 512], BF16) for i in range(2)]
    GA = [sb(f"GA{i}", [128, 4096], BF16) for i in range(2)]
    Ug = [sb(f"Ug{i}", [128, 4096 + 30], BF16) for i in range(2)]
    for u_ in Ug:
        P.memset('pool', u_[:, 0:15], 0.0); P.memset('pool', u_[:, 4096 + 15:4096 + 30], 0.0)
    Ycb = [sb(f"Ycb{i}", [128, 4096], BF16) for i in range(4)]
    Dg31 = sb("Dg31", [128, 31, 128], BF16)
    sgt = sb("sgt", [128, 512]); sq2 = sb("sq2", [128, 512], BF16)
    SUM = sb("SUM", [1, 4096]); SSQ = sb("SSQ", [1, 4096])
    w4cnt = [0]

    def inproj_group(cols, evacs):
        wb = Wg4[w4cnt[0] % 2]; w4cnt[0] += 1
        for jj, c0_ in enumerate(cols):
            P.dma('pool', wb[:, :, jj * 128:(jj + 1) * 128], w_in_v[:, :, c0_:c0_ + 128], w=[f"{wb.name}_{jj}"])
        for tt_ in range(8):
            hb = load_hT(tt_)
            for jj in range(len(cols)):
                pb = nextbank()
                for kc in range(8):
                    P.mm(pb[:, :], wb[:, kc, jj * 128:(jj + 1) * 128], hb[:, kc, :], start=(kc == 0), stop=(kc == 7),
                         r=[f"{wb.name}_{jj}", hb])
                evacs[jj](tt_, pb)

    for g4 in range(4):
        evs = []
        for jj in range(4):
            cg = g4 * 4 + jj

            def evac_gate(tt_, pb, cg=cg, jj=jj):
                P.act(Ycb[jj][:, tt_ * 512:(tt_ + 1) * 512], pb[:, :], AF.Sigmoid, bias=vp[:, VB_GATE + cg:VB_GATE + cg + 1])
            evs.append(evac_gate)
        inproj_group([COL_GATE + (g4 * 4 + jj) * 128 for jj in range(4)], evs)
        for jj in range(4):
            cg = g4 * 4 + jj
            P.dma('sp', gate_d[cg * 128:(cg + 1) * 128, :], Ycb[jj][:], w=[('gate_d', cg)])
    for j2 in range(4):
        evs = []; cols = []
        for u in range(2):
            j = j2 * 2 + u

            def evac_a(tt_, pb, j=j, u=u):
                P.act(GA[u][:, tt_ * 512:(tt_ + 1) * 512], pb[:, :], AF.Identity, bias=vp[:, VB_GLU + j:VB_GLU + j + 1])

            def evac_g(tt_, pb, j=j, u=u):
                P.act(sgt[:], pb[:, :], AF.Sigmoid, bias=vp[:, VB_GLU + 8 + j:VB_GLU + 8 + j + 1])
                P.tt('dve', Ug[u][:, 15 + tt_ * 512:15 + (tt_ + 1) * 512], GA[u][:, tt_ * 512:(tt_ + 1) * 512], sgt[:], ALU.mult)
            cols += [COL_GLU + j * 128, COL_GLU + 1024 + j * 128]
            evs += [evac_a, evac_g]
        inproj_group(cols, evs)
        for u in range(2):
            j = j2 * 2 + u
            yc = Ycb[u]
            for t in range(31):
                P.ts('dve' if t % 2 == 0 else 'pool', Dg31[:, t, :], identb[:], cwd[:, t * 8 + j:t * 8 + j + 1], None, ALU.mult)
            for tt_ in range(8):
                pb = nextbank()
                for t in range(31):
                    P.mm(pb[:, :], Dg31[:, t, :], Ug[u][:, tt_ * 512 + t:tt_ * 512 + t + 512], start=(t == 0), stop=(t == 30))
                sl = slice(tt_ * 512, (tt_ + 1) * 512)
                P.act(yc[:, sl], pb[:, :], AF.Identity, bias=vp[:, VB_DW + j:VB_DW + j + 1])
                P.act(sq2[:], pb[:, :], AF.Square, bias=vp[:, VB_DW + j:VB_DW + j + 1])
                P.mm(bank[2][0:1, :], onesb[:, 0:1], yc[:, sl])
                P.mm(bank[3][0:1, :], onesb[:, 0:1], sq2[:])
                if j == 0:
                    P.copy('dve', SUM[:, sl], bank[2][0:1, :])
                    P.copy('dve', SSQ[:, sl], bank[3][0:1, :])
                else:
                    P.tt('dve', SUM[:, sl], SUM[:, sl], bank[2][0:1, :], ALU.add)
                    P.tt('dve', SSQ[:, sl], SSQ[:, sl], bank[3][0:1, :], ALU.add)
            P.dma('sp', ybT_d[j * 128:(j + 1) * 128, :], yc[:], w=[('ybT_d', j)])
    stt_ = sb("stt_", [1, 512])
    for tt_ in range(8):
        sl = slice(tt_ * 512, (tt_ + 1) * 512)
        P.ts('dve', SUM[:, sl], SUM[:, sl], 1.0 / 1024.0, None, ALU.mult)
        P.tt('dve', stt_[:], SUM[:, sl], SUM[:, sl], ALU.mult)
        P.stt(SSQ[:, sl], SSQ[:, sl], 1.0 / 1024.0, stt_[:], ALU.mult, ALU.subtract)
        P.act(SSQ[:, sl], SSQ[:, sl], AF.Ln, bias=eps_ln[0:1, 0:1])
        P.act(SSQ[:, sl], SSQ[:, sl], AF.Exp, scale=-0.5)
        P.stt(SUM[:, sl], SUM[:, sl], -1.0, SSQ[:, sl], ALU.mult, ALU.mult)
    RSTD = SSQ; NMR = SUM
    P.dma('sp', st_d[0:1, :], RSTD[:])
    P.dma('sp', st_d[1:2, :], NMR[:])
    s5_mark = A.mark()
    if stage <= 6:
        P.finish()
        return nc, dbg

    A.release(base_mark)
    LG = sb("LG", [128, 32, 32]); M8 = sb("M8", [128, 32, 8]); MSK = sb("MSK", [128, 32, 32], BF16); G4 = sb("G4", [128, 32, 4])
    moe_mark = A.mark()
    zbf = zerof[:].bitcast(BF16)
    xs_v = xs_d.rearrange("(n p) d -> p n d", p=128)
    for n0 in range(0, 256, 32):
        P.dma('sp', xs_v[:, n0:n0 + 32, :], zbf.unsqueeze(1).to_broadcast([128, 32, D]), r=[zerof], w=[('xs_fill', n0)])
    Wao = sb("Wao", [128, 8, 1024], BF16); Wbo = sb("Wbo", [128, 8, 1024], BF16); Wout = sb("Wout", [128, 8, 1024], BF16)
    P.dma('pool', Wao[:], w_a_o.rearrange("(kc p) c -> p kc c", p=128))
    P.dma('pool', Wbo[:], w_b_o.rearrange("(kc p) c -> p kc c", p=128))
    P.dma('pool', Wout[:], w_out.rearrange("(kc p) c -> p kc c", p=128))
    Wr = sb("Wr", [128, 8, 32]); brB = sb("brB", [128, 32])
    P.dma('sp', Wr[:], w_router.rearrange("(kc p) c -> p kc c", p=128))
    P.dma('sp', brB[:], b_router.partition_broadcast(128))
    lnp['g'] = sb("gB1", [128, D]); lnp['b'] = sb("bB1", [128, D])
    P.dma('sp', lnp['g'][:], ln1_g.partition_broadcast(128))
    P.dma('sp', lnp['b'][:], ln1_b.partition_broadcast(128))
    ogt2 = [sb(f"ogt{i}", [128, 8, 512], BF16) for i in range(2)]; ybt2 = [sb(f"ybt{i}", [128, 8, 512], BF16) for i in range(2)]
    gtt2 = [sb(f"gtt{i}", [128, 16, 512], BF16) for i in range(2)]
    ybaT = sb("ybaT", [128, 8, 512], BF16); mixT = sb("mixT", [128, 8, 512], BF16)
    t1s = [sb(f"t1_{i}", [128, 512]) for i in range(2)]; t2s = [sb(f"t2_{i}", [128, 512]) for i in range(2)]
    rs_t = sb("rs_t", [1, 512]); nm_t = sb("nm_t", [1, 512])
    h0t2 = [sb(f"h0t{i}", [128, D]) for i in range(2)]; rt_2 = [sb(f"rt{i}", [128, D]) for i in range(2)]
    h1t2 = [sb(f"h1t{i}", [128, D]) for i in range(2)]; h1Tf2 = [sb(f"h1Tf{i}", [128, 8, 128]) for i in range(2)]
    nmx2 = [sb(f"nmx{i}", [128, 1]) for i in range(2)]; esum2 = [sb(f"esum{i}", [128, 1]) for i in range(2)]
    h1tb2 = [sb(f"h1tb{i}", [128, D], BF16) for i in range(2)]
    ogv = ogT_d.rearrange("(j p) t -> p j t", p=128)
    ybv = ybT_d.rearrange("(j p) t -> p j t", p=128)
    gtv = gate_d.rearrange("(j p) t -> p j t", p=128)
    def s6_M(tt_):
        sl = slice(tt_ * 512, (tt_ + 1) * 512)
        ogt = ogt2[tt_ % 2]; ybt = ybt2[tt_ % 2]; gtt = gtt2[tt_ % 2]
        P.dma('sp', ogt[:], ogv[:, :, sl], r=[('ogT_d', j_) for j_ in range(8)])
        P.dma('sp', ybt[:], ybv[:, :, sl], r=[('ybT_d', j_) for j_ in range(8)])
        P.dma('sp', gtt[:], gtv[:, :, sl], r=[('gate_d', j_) for j_ in range(16)])
        P.dma('sp', rs_t[:], st_d[0:1, sl])
        P.dma('sp', nm_t[:], st_d[1:2, sl])
        P.mm(bank[6][:, :], onesf[0:1, :], rs_t[0:1, :])
        P.mm(bank[7][:, :], onesf[0:1, :], nm_t[0:1, :])
        for j in range(8):
            t1 = t1s[j % 2]
            P.tt('dve', t1[:], ybt[:, j, :], bank[6][:, :], ALU.mult)
            P.tt('dve', t1[:], t1[:], bank[7][:, :], ALU.add)
            P.act(ybaT[:, j, :], t1[:], AF.Silu, bias=vp[:, VB_LNB + j:VB_LNB + j + 1], scale=vp[:, VB_LNG + j:VB_LNG + j + 1])
        if debug and tt_ == 0:
            dbg_out("dbg_ybaT", ybaT[:], [128, 8, 512], BF16)
        for m in range(8):
            pa = bank[(m % 2) * 2]; pb_ = bank[(m % 2) * 2 + 1]
            t1 = t1s[m % 2]; t2 = t2s[m % 2]
            for kc in range(8):
                P.mm(pa[:, :], Wao[:, kc, m * 128:(m + 1) * 128], ogt[:, kc, :], start=(kc == 0), stop=(kc == 7))
            for kc in range(8):
                P.mm(pb_[:, :], Wbo[:, kc, m * 128:(m + 1) * 128], ybaT[:, kc, :], start=(kc == 0), stop=(kc == 7))
            P.tt('dve', t1[:], pa[:, :], gtt[:, m, :], ALU.mult)
            P.stt(t2[:], pb_[:, :], vp[:, VB_BBO + m:VB_BBO + m + 1], gtt[:, 8 + m, :], ALU.add, ALU.mult)
            P.tt('pool', mixT[:, m, :], t1[:], t2[:], ALU.add)
    def s6_A(tt_, sub):
        i = tt_ * 4 + sub
        h0t = h0t2[i % 2]; rt = rt_2[i % 2]; h1t = h1t2[i % 2]; h1Tf = h1Tf2[i % 2]
        nmx = nmx2[i % 2]; esum = esum2[i % 2]; h1tb = h1tb2[i % 2]
        P.dma('sp', h0t[:], h0_d[i * 128:(i + 1) * 128, :], r=[('h0_d', i)])
        for hf in range(2):
            pm = bank[4 + hf]
            for m in range(8):
                P.mm(pm[:, :], mixT[:, m, sub * 128:(sub + 1) * 128], Wout[:, m, hf * 512:(hf + 1) * 512], start=(m == 0), stop=(m == 7))
            P.stt(rt[:, hf * 512:(hf + 1) * 512], h0t[:, hf * 512:(hf + 1) * 512], DN_ALPHA, pm[:, :], ALU.mult, ALU.add)
        layer_norm_rows(rt, h1t)
        P.dma('sp', h1_d[i * 128:(i + 1) * 128, :], h1t[:], w=[('h1_d', i)])
    def s6_B(tt_, sub):
        i = tt_ * 4 + sub
        h0t = h0t2[i % 2]; rt = rt_2[i % 2]; h1t = h1t2[i % 2]; h1Tf = h1Tf2[i % 2]
        nmx = nmx2[i % 2]; esum = esum2[i % 2]; h1tb = h1tb2[i % 2]
        for hf in range(2):
            pt = bank[6 + hf]
            for q4 in range(4):
                kc = hf * 4 + q4
                P.tr(pt[:, q4 * 128:(q4 + 1) * 128], h1t[:, kc * 128:(kc + 1) * 128], identf[:])
            P.copy('dve', h1Tf[:, hf * 4:(hf + 1) * 4, :], pt[:].rearrange("p (k t) -> p k t", t=128))
        pr_ = bank[(sub % 2) * 2]
        for kc in range(8):
            P.mm(pr_[:, 0:32], h1Tf[:, kc, :], Wr[:, kc, :], start=(kc == 0), stop=(kc == 7))
        P.tt('dve', LG[:, i, :], pr_[:, 0:32], brB[:], ALU.add)
        P.op('dve', lambda e, i=i: e.max(out=M8[:, i, :], in_=LG[:, i, :]), [LG], [M8])
        P.ts('dve', MSK[:, i, :], LG[:, i, :], M8[:, i, 3:4], None, ALU.is_ge)
        P.ts('dve', nmx[:], M8[:, i, 0:1], -1.0, None, ALU.mult)
        P.act(G4[:, i, :], M8[:, i, 0:4], AF.Exp, bias=nmx[:, 0:1])
        P.op('dve', lambda e, i=i, esum=esum: e.tensor_reduce(out=esum[:], in_=G4[:, i, :], axis=AX.X, op=ALU.add), [G4], [esum])
        P.op('dve', lambda e, esum=esum: e.reciprocal(out=esum[:], in_=esum[:]), [esum], [esum])
        P.ts('dve', G4[:, i, :], G4[:, i, :], esum[:, 0:1], None, ALU.mult)
        P.copy('act', h1tb[:], h1t[:])
        P.dma('sp', h1b_d[i * 128:(i + 1) * 128, :], h1tb[:], w=[('h1b_d', i)])

    pend_B = []
    for tt_ in range(8):
        s6_M(tt_)
        for pb_item in pend_B:
            s6_B(*pb_item)
        pend_B = []
        for sub in range(4):
            s6_A(tt_, sub)
            if sub >= 1:
                s6_B(tt_, sub - 1)
        pend_B.append((tt_, 3))
    for pb_item in pend_B:
        s6_B(*pb_item)
    if debug:
        dbg_out("dbg_G4", G4[:], [128, 32, 4], F32)
        dbg_out("dbg_MSK", MSK[:], [128, 32, 32], BF16)
    if stage <= 7:
        P.finish()
        return nc, dbg

    A.release(moe_mark)
    I32 = mybir.dt.int32
    NBLK = 64
    lnp['g'] = sb("gB2", [128, D]); lnp['b'] = sb("bB2", [128, D])
    P.dma('sp', lnp['g'][:], ln2_g.partition_broadcast(128))
    P.dma('sp', lnp['b'][:], ln2_b.partition_broadcast(128))
    stri = sb("stri", [128, 128], BF16)
    strf = sb("strf", [128, 128])
    P.op('pool', lambda e: e.affine_select(out=strf[:], in_=onesf[:], pattern=[[1, 128]], compare_op=ALU.is_gt, fill=0.0,
                                           base=0, channel_multiplier=-1), [onesf], [strf])
    P.copy('dve', stri[:], strf[:])
    POS = sb("POS", [128, 32, 32]); run = sb("run", [128, 32], BF16)
    P.memset('pool', run[:], 0.0)
    for i in range(32):
        pp = bank[i % 2]
        P.mm(pp[:, 0:32], stri[:], MSK[:, i, :], start=True, stop=False)
        P.mm(pp[:, 0:32], onesb[:], run[:], start=False, stop=True)
        P.copy('act', POS[:, i, :], pp[:, 0:32])
        P.tt('dve', run[:], run[:], MSK[:, i, :], ALU.add)
    cnt = sb("cnt", [128, 32]); padded = sb("padded", [128, 32]); pend = sb("pend", [128, 32]); pstart = sb("pstart", [128, 32])
    tmp32 = sb("tmp32", [128, 32]); ones32 = sb("ones32", [128, 32])
    P.memset('pool', ones32[:], 1.0)
    P.mm(bank[2][:, 0:32], onesb[:], run[:])
    P.copy('dve', cnt[:], bank[2][:, 0:32])
    P.memset('pool', padded[:], 0.0)
    for j in range(8):
        P.ts('dve', tmp32[:], cnt[:], float(512 * j), 512.0, ALU.is_gt, ALU.mult)
        P.tt('dve', padded[:], padded[:], tmp32[:], ALU.add)
    P.op('dve', lambda e: e.tensor_tensor_scan(out=pend[:], data0=ones32[:], data1=padded[:], initial=0.0, op0=ALU.mult, op1=ALU.add),
         [ones32, padded], [pend])
    P.tt('dve', pstart[:], pend[:], padded[:], ALU.subtract)
    bthr = sb("bthr", [128, NBLK, 32]); BE = sb("BE", [128, NBLK])
    P.op('pool', lambda e: e.iota(out=bthr[:], pattern=[[512, NBLK], [0, 32]], base=0, channel_multiplier=0,
                                  allow_small_or_imprecise_dtypes=True), [], [bthr])
    P.tt('dve', bthr[:], bc(pend[:], [128, NBLK, 32], 1), bthr[:], ALU.is_le)
    P.op('dve', lambda e: e.tensor_reduce(out=BE[:], in_=bthr[:], axis=AX.X, op=ALU.add), [bthr], [BE])
    P.ts('dve', BE[:], BE[:], 31.0, None, ALU.min)
    basef = sb("basef", [128, 8]); IDXF = sb("IDXF", [128, NBLK, 8]); IDXW = sb("IDXW", [128, NBLK, 8], I32)
    P.op('pool', lambda e: e.iota(out=basef[:], pattern=[[128, 8]], base=0, channel_multiplier=1,
                                  allow_small_or_imprecise_dtypes=True), [], [basef])
    P.stt(IDXF[:], bc(BE[:], [128, NBLK, 8], 2), 1024.0, bc(basef[:], [128, NBLK, 8], 1), ALU.mult, ALU.add)
    P.copy('dve', IDXW[:], IDXF[:])
    pidx = sb("pidx", [32, 1]); OH = sb("OH", [32, NBLK], BF16)
    P.op('pool', lambda e: e.iota(out=pidx[:], pattern=[[0, 1]], base=0, channel_multiplier=1,
                                  allow_small_or_imprecise_dtypes=True), [], [pidx])
    P.ts('dve', OH[:], BE[0:32, :], pidx[0:32, 0:1], None, ALU.is_equal)
    thrb = sb("thrb", [1, NBLK]); ACTF = sb("ACTF", [1, NBLK], I32)
    P.op('pool', lambda e: e.iota(out=thrb[:], pattern=[[512, NBLK]], base=0, channel_multiplier=0,
                                  allow_small_or_imprecise_dtypes=True), [], [thrb])
    P.ts('dve', thrb[:], thrb[:], pend[0:1, 31:32], None, ALU.is_lt)
    P.copy('dve', ACTF[:], thrb[:])
    bgu_all = sb("bgu_all", [32, 2048], BF16); bd_all = sb("bd_all", [32, D], BF16)
    P.dma('pool', bgu_all[:], b_gu)
    P.dma('pool', bd_all[:], b_down)
    SLOTF = sb("SLOTF", [128, 32, 4]); SLOT = sb("SLOT", [128, 32, 4], I32)
    valt = sb("valt", [128, 32]); mk = sb("mk", [128, 4, 32])
    for i in range(32):
        P.tt('dve', valt[:], POS[:, i, :], pstart[:], ALU.add)
        P.tt('dve', mk[:], bc(LG[:, i, :], [128, 4, 32], 1), bc(M8[:, i, 0:4], [128, 4, 32], 2), ALU.is_equal)
        P.tt('dve', mk[:], mk[:], bc(valt[:], [128, 4, 32], 1), ALU.mult)
        P.op('dve', lambda e, i=i: e.tensor_reduce(out=SLOTF[:, i, :], in_=mk[:], axis=AX.X, op=ALU.add), [mk], [SLOTF])
    P.copy('dve', SLOT[:], SLOTF[:])
    if debug:
        dbg_out("dbg_SLOT", SLOT[:], [128, 32, 4], I32)
        dbg_out("dbg_BE", BE[:], [128, NBLK], F32)
        dbg_out("dbg_cnt", cnt[:], [128, 32], F32)
    disp_mark = A.mark()
    hb_t = [sb(f"hb_t{i}", [128, D], BF16) for i in range(2)]
    fill_names = [('xs_fill', n0) for n0 in range(0, 256, 32)]
    for i in range(32):
        hb_ = hb_t[i % 2]
        P.dma('sp', hb_[:], h1b_d[i * 128:(i + 1) * 128, :], r=[('h1b_d', i)])
        for k in range(4):
            P.idma(xs_d, hb_[:], out_off=SLOT[:, i, k:k + 1], r=[hb_, SLOT] + fill_names, w=[('xs', i, k)])
    A.release(disp_mark)
    if debug and os.environ.get("MK_MOE_STOP") == "1":
        P.finish()
        return nc, dbg
    wguB = [sb(f"wguB{i}", [128, 8, 2048], BF16) for i in range(2)]
    wdB = [sb(f"wdB{i}", [128, 8, D], BF16) for i in range(2)]
    xtm = [sb(f"xtm{i}", [128, 4, D], BF16) for i in range(2)]; xT = sb("xT", [128, 8, 512], BF16)
    actT = sb("actT", [128, 8, 512], BF16)
    OHs = [sb(f"OHs{i}", [32, 512], BF16) for i in range(2)]
    NROT = 2
    gl = [sb(f"gl{i}", [128, 512]) for i in range(NROT)]; sg = [sb(f"sg{i}", [128, 512]) for i in range(NROT)]
    li = [sb(f"li{i}", [128, 512]) for i in range(NROT)]; p1 = [sb(f"p1{i}", [128, 512]) for i in range(NROT)]
    ysb = [sb(f"ysb{i}", [128, D]) for i in range(2)]
    wgu_rows = w_gu.rearrange("e k c -> (e k) c")
    wd_rows = w_down.rearrange("e k c -> (e k) c")
    nblk_run = int(os.environ.get("MK_NBLK", str(NBLK))) if debug else NBLK
    SIG_MAX = float(1.0 / (1.0 + np.exp(-1.702 * 7.0)))

    def load_block_weights(b):
        for kc in range(8):
            P.idma(wguB[b % 2][:, kc, :], wgu_rows, in_off=IDXW[:, b, kc:kc + 1], r=[IDXW], w=[f"wguB{b % 2}_{kc}"])
        for kc in range(8):
            P.idma(wdB[b % 2][:, kc, :], wd_rows, in_off=IDXW[:, b, kc:kc + 1], r=[IDXW], w=[f"wdB{b % 2}_{kc}"])

    def load_block_x(b):
        P.dma('sp', xtm[b % 2][:], xs_d[b * 512:(b + 1) * 512, :].rearrange("(s p) d -> p s d", p=128), r=[])
        P.copy('act', OHs[b % 2][:], OH[:, b:b + 1].to_broadcast([32, 512]))

    l1b = [0]; l2b = [0]; ycnt = [0]; rot = [0]
    load_block_weights(0)
    load_block_x(0)
    use_skip = os.environ.get("MK_NOSKIP") is None
    for b in range(nblk_run):
        if use_skip:
            P.cond_begin(ACTF[0:1, b:b + 1])
        if b + 1 < nblk_run:
            load_block_weights(b + 1)
            load_block_x(b + 1)
        wg_ = wguB[b % 2]; wd_ = wdB[b % 2]; oh_ = OHs[b % 2]; xt_ = xtm[b % 2]
        for k2 in range(4):
            pbf = bank_bf(4 + k2 % 2)
            for kk in range(2):
                kc = k2 * 2 + kk
                for sub in range(4):
                    P.tr(pbf[:, kk * 512 + sub * 128:kk * 512 + (sub + 1) * 128], xt_[:, sub, kc * 128:(kc + 1) * 128], identb[:])
            P.copy('act' if k2 % 2 == 0 else 'dve', xT[:, k2 * 2:k2 * 2 + 2, :], pbf[:, :].rearrange("p (k t) -> p k t", t=512))
        for m in range(8):
            pbg = bank[l1b[0] % 4]; pbl = bank[(l1b[0] + 1) % 4]; l1b[0] += 2
            ri = rot[0] % NROT; rot[0] += 1
            for kc in range(8):
                P.mm(pbg[:, :], wg_[:, kc, m * 128:(m + 1) * 128], xT[:, kc, :], start=(kc == 0), stop=False, r=[f"wguB{b % 2}_{kc}", xT])
            P.mm(pbg[:, :], bgu_all[:, m * 128:(m + 1) * 128], oh_[:], start=False, stop=True)
            for kc in range(8):
                P.mm(pbl[:, :], wg_[:, kc, 1024 + m * 128:1024 + (m + 1) * 128], xT[:, kc, :], start=(kc == 0), stop=False, r=[f"wguB{b % 2}_{kc}", xT])
            P.mm(pbl[:, :], bgu_all[:, 1024 + m * 128:1024 + (m + 1) * 128], oh_[:], start=False, stop=True)
            P.act(sg[ri][:], pbg[:, :], AF.Sigmoid, scale=1.702)
            P.ts('dve', gl[ri][:], pbg[:, :], 7.0, None, ALU.min)
            P.ts('dve', li[ri][:], pbl[:, :], 7.0, -7.0, ALU.min, ALU.max)
            P.stt(p1[ri][:], sg[ri][:], SIG_MAX, gl[ri][:], ALU.min, ALU.mult)
            P.stt(actT[:, m, :], li[ri][:], 1.0, p1[ri][:], ALU.add, ALU.mult)
        for sub in range(4):
            yb_ = ysb[ycnt[0] % 2]; ycnt[0] += 1
            for hf in range(2):
                pb = bank[4 + l2b[0] % 4]; l2b[0] += 1
                for m in range(8):
                    P.mm(pb[:, :], actT[:, m, sub * 128:(sub + 1) * 128], wd_[:, m, hf * 512:(hf + 1) * 512], start=(m == 0), stop=False, r=[f"wdB{b % 2}_{m}", actT])
                P.mm(pb[:, :], oh_[:, 0:128], bd_all[:, hf * 512:(hf + 1) * 512], start=False, stop=True)
                P.copy('act' if hf == 0 else 'pool_never', yb_[:, hf * 512:(hf + 1) * 512], pb[:, :]) if hf == 0 else P.copy('dve', yb_[:, hf * 512:(hf + 1) * 512], pb[:, :])
            P.dma('sp', ys_d[b * 512 + sub * 128:b * 512 + (sub + 1) * 128, :], yb_[:], w=[('ys', b, sub)])
        if use_skip:
            P.cond_end()
    A.release(disp_mark)
    if debug and os.environ.get("MK_MOE_STOP") == "2":
        P.finish()
        return nc, dbg
    yk = [[sb(f"yk{j}_{k}", [128, D]) for k in range(4)] for j in range(3)]
    rt2 = [sb(f"rt2_{j}", [128, D]) for j in range(3)]; ot = [sb(f"ot{j}", [128, D]) for j in range(3)]
    for i in range(32):
        j = i % 3
        P.dma('sp', rt2[j][:], h1_d[i * 128:(i + 1) * 128, :], r=[('h1_d', i)])
        for k in range(4):
            P.idma(yk[j][k][:], ys_d, in_off=SLOT[:, i, k:k + 1], r=[SLOT])
        P.act(yk[j][0][:], yk[j][0][:], AF.Identity, scale=G4[:, i, 0:1])
        P.stt(rt2[j][:], rt2[j][:], DN_ALPHA, yk[j][0][:], ALU.mult, ALU.add)
        for k in range(1, 4):
            P.stt(rt2[j][:], yk[j][k][:], G4[:, i, k:k + 1], rt2[j][:], ALU.mult, ALU.add)
        layer_norm_rows(rt2[j], ot[j], mul_eng='dve')
        P.dma('sp', out[i * 128:(i + 1) * 128, :], ot[j][:])
    P.finish()
    return nc, dbg


_IN_NAMES = ['emb_ln_g', 'emb_ln_b', 'w_in', 'conv_qkv', 'a_log', 'dt_bias', 'dn_norm_g', 'w_a_o', 'b_glu', 'conv_dw',
             'b_dw', 'conv_ln_g', 'conv_ln_b', 'w_b_o', 'b_b_o', 'b_gate', 'w_out', 'ln1_g', 'ln1_b', 'w_router',
             'b_router', 'w_gu', 'b_gu', 'w_down', 'b_down', 'ln2_g', 'ln2_b']


def make_in_maps(inputs, n_cores=8):
    shared = {}
    for k in _IN_NAMES:
        a = np.ascontiguousarray(np.asarray(inputs[k], dtype=np.float32))
        a = a[0] if k not in ('emb_ln_g', 'emb_ln_b') else a
        if k in ('a_log', 'dt_bias'):
            a = a.reshape(16)
        shared[k] = np.ascontiguousarray(a)
    xs = np.asarray(inputs['x'], dtype=np.float32)
    maps = []
    for c in range(n_cores):
        m = dict(shared)
        m['x'] = np.ascontiguousarray(xs[c])
        maps.append(m)
    return maps


def kernel(**inputs):
    nc, _ = build_program()
    maps = make_in_maps(inputs, 8)
    res = run_bass_kernel_spmd(nc, maps, core_ids=list(range(8)))
    return np.stack([np.asarray(res.results[c]["out"]) for c in range(8)], axis=0).astype(np.float32)
```

```python
import os
import numpy as np
import concourse.bass as bass
import concourse.mybir as mybir
from concourse.bass_utils import run_bass_kernel_spmd

F32 = mybir.dt.float32
BF16 = mybir.dt.bfloat16
AF = mybir.ActivationFunctionType
ALU = mybir.AluOpType
AX = mybir.AxisListType

S = 4096
D = 1024
NCOL = 8224
NCH = 64
DN_ALPHA = 2.0 ** 0.25
LN_EPS = 1e-5
RMS_EPS = 1e-6
L2_EPS = 1e-6
NEG_BIG = -1.0e30
COL_Q, COL_K, COL_V, COL_Z, COL_SM, COL_GLU, COL_GATE = 0, 1024, 2048, 3072, 4096, 4128, 6176


class Prog:
    def __init__(self, nc, n_dma_sems=12, self_sync=True):
        self.nc = nc
        self.eng = {'pe': nc.tensor, 'act': nc.scalar, 'dve': nc.vector, 'pool': nc.gpsimd, 'sp': nc.sync}
        self.streams = {e: [] for e in self.eng}
        self.cnt = {e: 0 for e in self.eng}
        self.sem = {e: nc.alloc_semaphore(name=f"s_{e}") for e in self.eng}
        self.seen = {e: {} for e in self.eng}
        self.lastw = {}
        self.reads = {}
        self.semobj = {('e', e): self.sem[e] for e in self.eng}
        self.dma_sems = {}
        self.n_dma_sems = n_dma_sems
        self.self_sync = self_sync
        self.n_inst = 0

    def _dma_sem(self, q):
        if q not in self.dma_sems:
            lst = []
            for i in range(self.n_dma_sems):
                s = self.nc.alloc_semaphore(name=f"d_{q}_{i}")
                key = ('d', q, i)
                self.semobj[key] = s
                lst.append([key, 0])
            self.dma_sems[q] = [lst, 0]
        lst, idx = self.dma_sems[q]
        self.dma_sems[q][1] = (idx + 1) % len(lst)
        return lst[idx]

    def _deps(self, e, reads, writes):
        deps = {}

        def add(k, v):
            if deps.get(k, 0) < v:
                deps[k] = v
        for r in reads:
            ev = self.lastw.get(r)
            if ev is not None:
                add(*ev)
        for w in writes:
            ev = self.lastw.get(w)
            if ev is not None:
                add(*ev)
            for k, v in self.reads.get(w, {}).items():
                add(k, v)
        waits = []
        for k, v in deps.items():
            if k == ('e', e):
                if not self.self_sync or e in ('pe', 'sp'):
                    continue
            if self.seen[e].get(k, 0) >= v:
                continue
            self.seen[e][k] = v
            waits.append((self.semobj[k], v))
        return waits

    def _commit(self, ev, reads, writes):
        k, v = ev
        for w in writes:
            self.lastw[w] = ev
            self.reads[w] = {}
        for r in reads:
            d = self.reads.setdefault(r, {})
            if d.get(k, 0) < v:
                d[k] = v

    @staticmethod
    def _names(aps):
        out = []
        for a in aps:
            if a is None or isinstance(a, (int, float)):
                continue
            if isinstance(a, (str, tuple)):
                out.append(a)
            else:
                out.append(a.name)
        return out

    def op(self, e, fn, reads, writes):
        reads = self._names(reads)
        writes = self._names(writes)
        pr = [n for n in reads if isinstance(n, str) and n.startswith('bank')]
        if pr:
            reads = [n for n in reads if n not in pr]
            writes = writes + [n for n in pr if n not in writes]
        waits = self._deps(e, reads, writes)
        self.cnt[e] += 1
        seq = self.cnt[e]
        sem = self.sem[e]

        def emit(engobj, waits=waits, fn=fn, sem=sem):
            for s, v in waits:
                engobj.wait_ge(s, v)
            fn(engobj).then_inc(sem, 1)
        self.streams[e].append(emit)
        self._commit((('e', e), seq), reads, writes)
        self.n_inst += 1

    def dma(self, q, out, in_, r=None, w=None, **kw):
        reads = self._names(r if r is not None else [in_])
        writes = self._names(w if w is not None else [out])
        slot = self._dma_sem(q)
        key, prev = slot
        waits = self._deps(q, reads, writes)
        if prev > 0 and self.seen[q].get(key, 0) < prev:
            self.seen[q][key] = prev
            waits.append((self.semobj[key], prev))
        slot[1] = prev + 16
        tgt = slot[1]
        s = self.semobj[key]

        def emit(engobj, waits=waits, s=s, out=out, in_=in_, kw=kw):
            for ss, v in waits:
                engobj.wait_ge(ss, v)
            engobj.dma_start(out=out, in_=in_, **kw).then_inc(s, 16)
        self.streams[q].append(emit)
        self._commit((key, tgt), reads, writes)
        self.n_inst += 1

    def idma(self, out, in_, out_off=None, in_off=None, r=None, w=None):
        q = 'pool'
        reads = self._names(r if r is not None else [in_, out_off, in_off])
        writes = self._names(w if w is not None else [out])
        slot = self._dma_sem(q)
        key, prev = slot
        waits = self._deps(q, reads, writes)
        if prev > 0 and self.seen[q].get(key, 0) < prev:
            self.seen[q][key] = prev
            waits.append((self.semobj[key], prev))
        slot[1] = prev + 16
        tgt = slot[1]
        s = self.semobj[key]

        def emit(engobj, waits=waits, s=s):
            for ss, v in waits:
                engobj.wait_ge(ss, v)
            oo = bass.IndirectOffsetOnAxis(ap=out_off, axis=0) if out_off is not None else None
            io = bass.IndirectOffsetOnAxis(ap=in_off, axis=0) if in_off is not None else None
            engobj.indirect_dma_start(out=out, out_offset=oo, in_=in_, in_offset=io).then_inc(s, 16)
        self.streams[q].append(emit)
        self._commit((key, tgt), reads, writes)
        self.n_inst += 1

    def cond_begin(self, flag_ap):
        st = {'cnt0': dict(self.cnt), 'seen0': {e: dict(d) for e, d in self.seen.items()}, 'dma0': {}, 'ctx': {}, 'used': {}}
        for q, (lst, _) in self.dma_sems.items():
            for key, val in lst:
                st['dma0'][key] = val
        self._cond = st
        if not hasattr(self, '_cregs'):
            self._cregs = {}
        for e in self.eng:
            waits = self._deps(e, self._names([flag_ap]), [])

            def emit(engobj, waits=waits, e=e, st=st):
                if not st['used'].get(e):
                    return
                for s_, v in waits:
                    engobj.wait_ge(s_, v)
                if e not in self._cregs:
                    self._cregs[e] = engobj.alloc_register(f"cflag_{e}")
                r = self._cregs[e]
                engobj.reg_load(r, flag_ap)
                v = engobj.snap(r)
                ctx = engobj.If(v == 1)
                ctx.__enter__()
                st['ctx'][e] = (ctx, v)
            self.streams[e].append(emit)

    def cond_end(self, else_dma=None):
        st = self._cond
        for e in self.eng:
            n_e = self.cnt[e] - st['cnt0'].get(e, 0)
            comp = []
            if e in self.dma_sems:
                for key, val in self.dma_sems[e][0]:
                    d = val - st['dma0'].get(key, 0)
                    if d:
                        comp.append((self.semobj[key], d, st['dma0'].get(key, 0)))
            st['used'][e] = bool(n_e or comp)

            def emit(engobj, e=e, n_e=n_e, comp=comp, st=st):
                if not st['used'].get(e):
                    return
                ctx, v = st['ctx'][e]
                ctx.__exit__(None, None, None)
                with engobj.Else():
                    engobj.drain()
                    for so, d, before in comp:
                        if before > 0:
                            engobj.wait_ge(so, before)
                        for _ in range(d // 16):
                            if else_dma is not None and else_dma[0] == e and st.get('else_done', 0) < len(else_dma[1]):
                                o_, i_ = else_dma[1][st.get('else_done', 0)]
                                st['else_done'] = st.get('else_done', 0) + 1
                                engobj.dma_start(out=o_, in_=i_).then_inc(so, 16)
                                continue
                            row = self.dummy_n; self.dummy_n += 1
                            engobj.dma_start(out=self.dummy_out[row:row + 1, :], in_=self.dummy_dram).then_inc(so, 16)
                    if n_e:
                        engobj.sem_inc(self.sem[e], n_e)
                engobj.free_register(v.val)
            self.streams[e].append(emit)
        self.seen = st['seen0']
        self._cond = None

    def barrier(self):
        cur = {}
        for e in self.eng:
            if self.cnt[e] > 0:
                cur[('e', e)] = self.cnt[e]
        for q, (lst, _) in self.dma_sems.items():
            for key, val in lst:
                if val > 0:
                    cur[key] = val
        for e in self.eng:
            waits = []
            for k, v in cur.items():
                if k == ('e', e):
                    continue
                if self.seen[e].get(k, 0) >= v:
                    continue
                self.seen[e][k] = v
                waits.append((self.semobj[k], v))

            def emit(engobj, waits=waits):
                for s, v in waits:
                    engobj.wait_ge(s, v)
            self.streams[e].append(emit)

    def finish(self):
        self.barrier()
        nc = self.nc
        with nc.Block() as block:
            @block.tensor
            def _(e):
                for f in self.streams['pe']:
                    f(e)

            @block.scalar
            def _(e):
                for f in self.streams['act']:
                    f(e)

            @block.vector
            def _(e):
                for f in self.streams['dve']:
                    f(e)

            @block.gpsimd
            def _(e):
                for f in self.streams['pool']:
                    f(e)

            @block.sync
            def _(e):
                for f in self.streams['sp']:
                    f(e)

    def mm(self, out, lhsT, rhs, start=True, stop=True, r=None, w=None):
        self.op('pe', lambda e: e.matmul(out, lhsT, rhs, start=start, stop=stop),
                r if r is not None else [lhsT, rhs], w if w is not None else [out])

    def tr(self, out, in_, ident, r=None, w=None):
        self.op('pe', lambda e: e.transpose(out=out, in_=in_, identity=ident),
                r if r is not None else [in_, ident], w if w is not None else [out])

    def act(self, out, in_, func, bias=None, scale=None, accum=None, r=None, w=None):
        kw = {}
        if bias is not None:
            kw['bias'] = bias
        if scale is not None:
            kw['scale'] = scale
        if accum is not None:
            kw['accum_out'] = accum
        self.op('act', lambda e: e.activation(out=out, in_=in_, func=func, **kw),
                r if r is not None else [in_, bias, scale], w if w is not None else [out, accum])

    def tt(self, eng, out, in0, in1, op, r=None, w=None):
        self.op(eng, lambda e: e.tensor_tensor(out=out, in0=in0, in1=in1, op=op),
                r if r is not None else [in0, in1], w if w is not None else [out])

    def ts(self, eng, out, in0, s1, s2, op0, op1=None, r=None, w=None):
        if op1 is None:
            fn = lambda e: e.tensor_scalar(out=out, in0=in0, scalar1=s1, scalar2=None, op0=op0)
        else:
            fn = lambda e: e.tensor_scalar(out=out, in0=in0, scalar1=s1, scalar2=s2, op0=op0, op1=op1)
        self.op(eng, fn, r if r is not None else [in0, s1, s2], w if w is not None else [out])

    def stt(self, out, in0, scalar, in1, op0, op1, r=None, w=None):
        self.op('dve', lambda e: e.scalar_tensor_tensor(out=out, in0=in0, scalar=scalar, in1=in1, op0=op0, op1=op1),
                r if r is not None else [in0, scalar, in1], w if w is not None else [out])

    def copy(self, eng, out, in_, r=None, w=None):
        if eng == 'act':
            fn = lambda e: e.copy(out=out, in_=in_)
        else:
            fn = lambda e: e.tensor_copy(out=out, in_=in_)
        self.op(eng, fn, r if r is not None else [in_], w if w is not None else [out])

    def memset(self, eng, ap, val):
        self.op(eng, lambda e: e.memset(ap, val), [], [ap])


def bc(ap, shape, axis):
    return ap.unsqueeze(axis).to_broadcast(shape)


class Arena:
    def __init__(self, nc, P, base=16512, top=229344):
        self.nc = nc; self.P = P; self.ptr = base; self.top = top; self.n = 0; self.peak = base

    def alloc(self, name, shape, dt=F32):
        size = 2 if dt == BF16 else 4
        n = 1
        for d in shape[1:]:
            n *= d
        nbytes = (n * size + 63) // 64 * 64
        self.n += 1
        t = self.nc.alloc_sbuf_tensor_at(f"{name}_{self.n}", list(shape), dt, offset=self.ptr)
        self.ptr += nbytes
        self.peak = max(self.peak, self.ptr)
        assert self.ptr <= self.top, f"SBUF arena overflow at {name}: {self.ptr} > {self.top}"
        return t

    def mark(self):
        return self.ptr

    def release(self, m):
        self.P.barrier()
        self.ptr = m


def build_program(stage=99, debug=False):
    nc = bass.Bass("TRN2", target_bir_lowering=False)
    P = Prog(nc)
    A = Arena(nc, P)
    dbg = {}

    def dram_in(name, shape):
        return nc.dram_tensor(name, shape, F32, kind="ExternalInput").ap()

    def scratch(name, shape, dt, force_internal=False):
        kind = "ExternalOutput" if (debug and not force_internal) else "Internal"
        return nc.dram_tensor(name, shape, dt, kind=kind).ap()

    def dbg_out(name, src_ap, shape, dt, r=None):
        if not debug:
            return
        t = nc.dram_tensor(name, shape, dt, kind="ExternalOutput").ap()
        P.dma('sp', t, src_ap, r=r)

    sb = A.alloc
    _dmy_d = nc.dram_tensor("dmy_d", [1, 64], F32).ap()
    _dmy_z = sb("dmy_z", [1, 64])
    P.memset('pool', _dmy_z[:], 0.0)
    P.dma('sp', _dmy_d, _dmy_z[:])
    P.dummy_out = nc.dram_tensor("dmy_o", [4096, 64], F32).ap()
    P.dummy_dram = _dmy_d; P.dummy_n = 0

    x = dram_in("x", [S, D])
    emb_ln_g = dram_in("emb_ln_g", [D]); emb_ln_b = dram_in("emb_ln_b", [D])
    w_in = dram_in("w_in", [D, NCOL])
    conv_qkv = dram_in("conv_qkv", [5, 3072])
    a_log = dram_in("a_log", [16]); dt_bias = dram_in("dt_bias", [16])
    dn_norm_g = dram_in("dn_norm_g", [128])
    w_a_o = dram_in("w_a_o", [D, D])
    b_glu = dram_in("b_glu", [2048]); conv_dw = dram_in("conv_dw", [31, D]); b_dw = dram_in("b_dw", [D])
    conv_ln_g = dram_in("conv_ln_g", [D]); conv_ln_b = dram_in("conv_ln_b", [D])
    w_b_o = dram_in("w_b_o", [D, D]); b_b_o = dram_in("b_b_o", [D]); b_gate = dram_in("b_gate", [2048])
    w_out = dram_in("w_out", [D, D])
    ln1_g = dram_in("ln1_g", [D]); ln1_b = dram_in("ln1_b", [D])
    w_router = dram_in("w_router", [D, 32]); b_router = dram_in("b_router", [32])
    w_gu = dram_in("w_gu", [32, D, 2048]); b_gu = dram_in("b_gu", [32, 2048])
    w_down = dram_in("w_down", [32, D, D]); b_down = dram_in("b_down", [32, D])
    ln2_g = dram_in("ln2_g", [D]); ln2_b = dram_in("ln2_b", [D])
    out = nc.dram_tensor("out", [S, D], F32, kind="ExternalOutput").ap()

    h0_d = scratch("h0_d", [S, D], F32)
    hT_d = scratch("hT_d", [D, S], BF16)
    ogT_d = scratch("ogT_d", [D, S], BF16)
    ybT_d = scratch("ybT_d", [D, S], BF16)
    gate_d = scratch("gate_d", [2048, S], BF16)
    h1_d = scratch("h1_d", [S, D], F32)
    st_d = scratch("st_d", [2, S], F32)
    h1b_d = scratch("h1b_d", [S, D], BF16)
    xs_d = scratch("xs_d", [32768, D], BF16, force_internal=True)
    ys_d = scratch("ys_d", [32768, D], F32, force_internal=True)

    hT_dv = hT_d.rearrange("(kc p) t -> p kc t", p=128)
    w_in_v = w_in.rearrange("(kc p) c -> p kc c", p=128)

    identf = sb("identf", [128, 128]); identb = sb("identb", [128, 128], BF16)
    onesf = sb("onesf", [128, 128]); onesb = sb("onesb", [128, 128], BF16)
    zerof = sb("zerof", [128, 512])
    P.memset('pool', onesf[:], 1.0)
    P.memset('pool', zerof[:], 0.0)
    P.memset('pool', identf[:], 1.0)
    P.op('pool', lambda e: e.affine_select(out=identf[:], in_=identf[:], pattern=[[-1, 128]], compare_op=ALU.is_equal,
                                           fill=0.0, base=0, channel_multiplier=1), [identf], [identf])
    P.copy('dve', identb[:], identf[:])
    P.copy('dve', onesb[:], onesf[:])
    eps_ln = sb("eps_ln", [128, 1])
    P.memset('pool', eps_ln[:], LN_EPS)
    bank = [nc.alloc_psum_tensor(f"bank{i}", [128, 512], F32) for i in range(8)]

    def bank_bf(i):
        return bank[i][:].bitcast(BF16)

    ln_sets = [(sb(f"ln_st{i}", [128, 12]), sb(f"ln_mv{i}", [128, 2]), sb(f"ln_rs{i}", [128, 1])) for i in range(3)]
    ln_cnt = [0]
    lnp = {}

    def layer_norm_rows(t, o, np_=128, mul_eng='pool'):
        ln_st, ln_mv, ln_rs = ln_sets[ln_cnt[0] % 3]; ln_cnt[0] += 1
        P.op('dve', lambda e: e.bn_stats(out=ln_st[:np_, 0:6], in_=t[:np_, 0:512]), [t], [ln_st])
        P.op('dve', lambda e: e.bn_stats(out=ln_st[:np_, 6:12], in_=t[:np_, 512:1024]), [t], [ln_st])
        P.op('dve', lambda e: e.bn_aggr(out=ln_mv[:np_, :], in_=ln_st[:np_, :]), [ln_st], [ln_mv])
        P.act(ln_rs[:np_, :], ln_mv[:np_, 1:2], AF.Sqrt, bias=eps_ln[:np_, 0:1])
        P.op('dve', lambda e: e.reciprocal(out=ln_rs[:np_, :], in_=ln_rs[:np_, :]), [ln_rs], [ln_rs])
        P.ts('dve', t[:np_, :], t[:np_, :], ln_mv[:np_, 0:1], ln_rs[:np_, 0:1], ALU.subtract, ALU.mult)
        P.tt(mul_eng, t[:np_, :], t[:np_, :], lnp['g'][:np_, :], ALU.mult)
        P.tt('dve', o[:np_, :], t[:np_, :], lnp['b'][:np_, :], ALU.add)

    vp = sb("vp", [128, 64])
    base_mark = A.mark()
    lnp['g'] = sb("gB0", [128, D]); lnp['b'] = sb("bB0", [128, D])
    P.dma('sp', lnp['g'][:], emb_ln_g.partition_broadcast(128))
    P.dma('sp', lnp['b'][:], emb_ln_b.partition_broadcast(128))
    xt = [sb(f"xt{i}", [128, D]) for i in range(2)]
    ht = [sb(f"ht{i}", [128, D]) for i in range(2)]
    hTs = [sb(f"hTs{i}", [128, 8, 512], BF16) for i in range(2)]
    for i in range(32):
        xb = xt[i % 2]; hb = ht[i % 2]; hs = hTs[(i // 4) % 2]
        P.dma('sp', xb[:], x[i * 128:(i + 1) * 128, :])
        layer_norm_rows(xb, hb)
        P.dma('sp', h0_d[i * 128:(i + 1) * 128, :], hb[:], w=[('h0_d', i)])
        for half in range(2):
            pb = bank[half]
            for q4 in range(4):
                kc = half * 4 + q4
                P.tr(pb[:, q4 * 128:(q4 + 1) * 128], hb[:, kc * 128:(kc + 1) * 128], identf[:])
            P.copy('act', hs[:, half * 4:(half + 1) * 4, (i % 4) * 128:(i % 4 + 1) * 128],
                   pb[:].rearrange("p (k t) -> p k t", t=128))
        if i % 4 == 3:
            tt_ = i // 4
            P.dma('sp', hT_dv[:, :, tt_ * 512:(tt_ + 1) * 512], hs[:], w=[('hT_d', tt_)])
    A.release(base_mark)
    if stage <= 1:
        P.finish()
        return nc, dbg

    tri = {}; ntri = {}; negT = {}; nstrT = {}
    for d_, (cm, st_) in (('f', (-1, 1)), ('b', (1, -1))):
        t_ = sb(f"tri_{d_}", [64, 64]); nt_ = sb(f"ntri_{d_}", [64, 64])
        P.op('pool', lambda e, t_=t_, cm=cm, st_=st_: e.affine_select(out=t_[:], in_=onesf[0:64, 0:64], pattern=[[st_, 64]],
             compare_op=ALU.is_ge, fill=0.0, base=0, channel_multiplier=cm), [onesf], [t_])
        P.ts('dve', nt_[:], t_[:], -1.0, None, ALU.mult)
        tri[d_] = t_; ntri[d_] = nt_
        ng_ = sb(f"negT_{d_}", [64, 8, 64])
        P.op('pool', lambda e, ng_=ng_, cm=cm, st_=st_: e.affine_select(out=ng_[:], in_=zerof[0:64, 0:512].rearrange("p (c i) -> p c i", i=64),
             pattern=[[0, 8], [st_, 64]], compare_op=ALU.is_ge, fill=NEG_BIG, base=0, channel_multiplier=cm), [zerof], [ng_])
        negT[d_] = ng_
        ns_ = sb(f"nstrT_{d_}", [64, 64])
        P.op('pool', lambda e, ns_=ns_, cm=cm, st_=st_: e.affine_select(out=ns_[:], in_=onesf[0:64, 0:64], pattern=[[st_, 64]],
             compare_op=ALU.is_gt, fill=0.0, base=0, channel_multiplier=cm), [onesf], [ns_])
        P.ts('dve', ns_[:], ns_[:], -1.0, None, ALU.mult)
        nstrT[d_] = ns_
    BETA = {d_: sb(f"BETA_{d_}", [64, 8, 64]) for d_ in 'fb'}
    LA = {d_: sb(f"LA_{d_}", [64, 8, 64]) for d_ in 'fb'}
    hTt = [sb(f"hTt{i}", [128, 8, 512], BF16) for i in range(2)]
    hcnt = [0]

    def load_hT(tt_):
        hb = hTt[hcnt[0] % 2]; hcnt[0] += 1
        P.dma('sp', hb[:], hT_dv[:, :, tt_ * 512:(tt_ + 1) * 512], r=[('hT_d', tt_)])
        return hb

    s2_mark = A.mark()
    SMraw = sb("SMraw", [64, 64, 32])
    Wsm = sb("Wsm", [128, 8, 32], BF16)
    alB = sb("alB", [64, 16]); dtB = sb("dtB", [64, 16]); nea = sb("nea", [64, 16])
    X_ = sb("spX", [64, 8, 64]); A_ = sb("spA", [64, 8, 64])
    P.dma('pool', Wsm[:], w_in_v[:, :, COL_SM:COL_SM + 32])
    P.dma('sp', alB[:], a_log.partition_broadcast(64))
    P.dma('sp', dtB[:], dt_bias.partition_broadcast(64))
    P.act(nea[:], alB[:], AF.Exp)
    P.ts('dve', nea[:], nea[:], -1.0, None, ALU.mult)
    for tt_ in range(8):
        hb = load_hT(tt_)
        pb = bank[2 + tt_ % 2]
        for cc in range(8):
            for kc in range(8):
                P.mm(pb[0:64, cc * 32:(cc + 1) * 32], hb[:, kc, cc * 64:(cc + 1) * 64], Wsm[:, kc, :], start=(kc == 0), stop=(kc == 7))
        P.copy('dve', SMraw[:, tt_ * 8:(tt_ + 1) * 8, :], pb[0:64, 0:256].rearrange("p (c k) -> p c k", k=32))
    for di, d_ in enumerate('fb'):
        ub = SMraw[:, :, di * 8:(di + 1) * 8].rearrange("p c h -> p h c")
        ua = SMraw[:, :, 16 + di * 8:24 + di * 8].rearrange("p c h -> p h c")
        P.act(BETA[d_][:], ub, AF.Sigmoid)
        P.tt('dve', X_[:], ua, bc(dtB[:, di * 8:(di + 1) * 8], [64, 8, 64], 2), ALU.add)
        P.stt(A_[:], X_[:], -1.0, X_[:], ALU.mult, ALU.max)
        P.act(A_[:], A_[:], AF.Exp, scale=-1.0)
        P.act(A_[:], A_[:], AF.Ln, bias=1.0)
        P.stt(A_[:], X_[:], 0.0, A_[:], ALU.max, ALU.add)
        P.tt('dve', LA[d_][:], A_[:], bc(nea[:, di * 8:(di + 1) * 8], [64, 8, 64], 2), ALU.mult)
    if debug:
        for d_ in 'fb':
            dbg_out(f"dbg_beta_{d_}", BETA[d_][:], [64, 8, 64], F32)
            dbg_out(f"dbg_la_{d_}", LA[d_][:], [64, 8, 64], F32)
    A.release(s2_mark)
    if stage <= 2:
        P.finish()
        return nc, dbg


    nheads = int(os.environ.get("MK_NHEADS", "8")) if debug else 8
    head_mark = A.mark()
    cst = sb("cst", [120, 128]); cwq = sb("cwq", [128, 120])
    P.dma('sp', cst[:], conv_qkv.rearrange("t (c p) -> (t c) p", p=128))
    P.tr(bank[0][:, 0:120], cst[:, :], identf[0:120, 0:120])
    P.copy('dve', cwq[:], bank[0][:, 0:120])
    eps_l2 = sb("eps_l2", [128, 1]); P.memset('pool', eps_l2[:], L2_EPS)
    eps_rms = sb("eps_rms", [128, 1]); P.memset('pool', eps_rms[:], RMS_EPS)
    ngcol = sb("ngcol", [128, 1]); P.dma('sp', ngcol[:], dn_norm_g.rearrange("(p o) -> p o", o=1))
    Wg = [sb(f"Wg{i}", [128, 8, 128], BF16) for i in range(1)]
    U3 = [sb(f"U{j}", [128, 4100], BF16) for j in range(3)]
    for U in U3:
        P.memset('pool', U[:, 0:2], 0.0); P.memset('pool', U[:, 4098:4100], 0.0)
    Wg3 = sb("Wg3", [128, 8, 384], BF16)
    C = sb("C", [128, 4096], BF16)
    Dg = sb("Dg", [128, 5, 128], BF16)
    sqb = sb("sqb", [128, 512], BF16); lnb = sb("lnb", [128, 512])
    qT = sb("qT", [128, 4096], BF16); kT = sb("kT", [128, 4096], BF16)
    Ktok = sb("Ktok", [64, 64, 128], BF16); Vtok = sb("Vtok", [64, 64, 128], BF16)
    O = sb("O", [64, 64, 128], BF16)
    qkT = {d_: sb(f"qkT_{d_}", [64, 64, 64], BF16) for d_ in 'fb'}
    TT = {d_: sb(f"TT_{d_}", [64, 64, 64], BF16) for d_ in 'fb'}
    Gd = {d_: sb(f"Gd_{d_}", [64, 64]) for d_ in 'fb'}
    EG = {d_: sb(f"EG_{d_}", [64, 64]) for d_ in 'fb'}
    NEG = {d_: sb(f"NEG_{d_}", [64, 64]) for d_ in 'fb'}
    TAIL = {d_: sb(f"TAIL_{d_}", [64, 64]) for d_ in 'fb'}
    CD = {d_: sb(f"CD_{d_}", [128, 64]) for d_ in 'fb'}
    LAm = sb("LAm", [64, 8, 64]); LAb = sb("LAb", [64, 8, 64])
    decT = {d_: sb(f"decT_{d_}", [64, 8, 64]) for d_ in 'fb'}; tY = {d_: sb(f"tY_{d_}", [64, 8, 64]) for d_ in 'fb'}
    Yb = {d_: [sb(f"Yb{d_}{i}", [64, 8, 64], BF16) for i in range(2)] for d_ in 'fb'}
    Zb = {d_: [sb(f"Zb{d_}{i}", [64, 8, 64], BF16) for i in range(2)] for d_ in 'fb'}
    Gm = {d_: [sb(f"Gm{d_}{i}", [64, 8, 64], BF16) for i in range(2)] for d_ in 'fb'}
    Sst = {d_: sb(f"S_{d_}", [128, 128]) for d_ in 'fb'}
    Sbf = {d_: sb(f"Sb_{d_}", [128, 128], BF16) for d_ in 'fb'}
    Rt = {d_: sb(f"R_{d_}", [64, 128], BF16) for d_ in 'fb'}
    vnew = {d_: sb(f"vnew_{d_}", [64, 128], BF16) for d_ in 'fb'}
    tq = {d_: sb(f"tq_{d_}", [64, 128]) for d_ in 'fb'}
    ktl = {d_: sb(f"ktl_{d_}", [64, 128], BF16) for d_ in 'fb'}
    otmp = sb("otmp", [64, 128])
    ssq = sb("ssq", [64, 64])
    wcnt = [0]; pcnt = [0]

    def nextbank():
        b_ = bank[pcnt[0] % 2]; pcnt[0] += 1
        return b_

    def inproj_chunk(col0, evac):
        wb = Wg[0]; wcnt[0] += 1
        P.dma('pool', wb[:], w_in_v[:, :, col0:col0 + 128])
        for tt_ in range(8):
            hb = load_hT(tt_)
            pb = nextbank()
            for kc in range(8):
                P.mm(pb[:, :], wb[:, kc, :], hb[:, kc, :], start=(kc == 0), stop=(kc == 7))
            evac(tt_, pb)

    def inproj_qkv(h_):
        for j, cb in enumerate((COL_Q, COL_K, COL_V)):
            P.dma('pool', Wg3[:, :, j * 128:(j + 1) * 128], w_in_v[:, :, cb + h_ * 128:cb + (h_ + 1) * 128], w=[f"Wg3_{j}"])
        for tt_ in range(8):
            hb = load_hT(tt_)
            for j in range(3):
                pb = nextbank()
                for kc in range(8):
                    P.mm(pb[:, :], Wg3[:, kc, j * 128:(j + 1) * 128], hb[:, kc, :], start=(kc == 0), stop=(kc == 7), r=[f"Wg3_{j}", hb])
                P.copy('act', U3[j][:, 2 + tt_ * 512:2 + (tt_ + 1) * 512], pb[:, :])

    def conv_silu(ci, U):
        for t in range(5):
            P.ts('dve', Dg[:, t, :], identb[:], cwq[:, t * 24 + ci:t * 24 + ci + 1], None, ALU.mult)
        for tt_ in range(8):
            pb = nextbank()
            for t in range(5):
                P.mm(pb[:, :], Dg[:, t, :], U[:, tt_ * 512 + t:tt_ * 512 + t + 512], start=(t == 0), stop=(t == 4))
            P.act(C[:, tt_ * 512:(tt_ + 1) * 512], pb[:, :], AF.Silu)

    def l2norm_to(dst, scale):
        for tt_ in range(8):
            sl = slice(tt_ * 512, (tt_ + 1) * 512)
            P.act(sqb[:], C[:, sl], AF.Square)
            pb = nextbank()
            P.mm(pb[:, :], onesb[:], sqb[:])
            P.act(lnb[:], pb[:, :], AF.Ln, bias=eps_l2[:, 0:1])
            P.act(lnb[:], lnb[:], AF.Exp, scale=-0.5)
            P.stt(dst[:, sl], C[:, sl], scale, lnb[:], ALU.mult, ALU.mult)

    def to_tokmajor(src, dst):
        for g in range(8):
            bi = 2 + g % 2
            pbf = bank_bf(bi)
            for cc in range(8):
                c = g * 8 + cc
                P.tr(pbf[0:64, cc * 128:(cc + 1) * 128], src[:, c * 64:(c + 1) * 64], identb[:])
            P.copy('dve' if g % 2 == 0 else 'act', dst[:, g * 8:(g + 1) * 8, :], pbf[0:64, :].rearrange("p (c d) -> p c d", d=128))

    def flat(t3):
        return t3[:].rearrange("p c i -> p (c i)")

    for h in range(nheads):
        inproj_qkv(h)
        conv_silu(h, U3[0]); l2norm_to(qT, 128.0 ** -0.5)
        conv_silu(8 + h, U3[1]); l2norm_to(kT, 1.0)
        to_tokmajor(kT, Ktok)
        conv_silu(16 + h, U3[2])
        to_tokmajor(C, Vtok)
        if debug and h == 0:
            dbg_out("dbg_qT", qT[:], [128, 4096], BF16)
            dbg_out("dbg_kT", kT[:], [128, 4096], BF16)
            dbg_out("dbg_Ktok", Ktok[:], [64, 64, 128], BF16)
            dbg_out("dbg_Vtok", Vtok[:], [64, 64, 128], BF16)
        if stage <= 3:
            continue
        for d_ in 'fb':
            la_h = LA[d_][:, h, :]
            P.mm(bank[4][0:64, 0:64], tri[d_][:], la_h)
            P.copy('dve', Gd[d_][:], bank[4][0:64, 0:64])
            P.mm(bank[5][:, 0:64], onesf[0:64, :], la_h)
            P.act(CD[d_][:], bank[5][:, 0:64], AF.Exp)
            P.tt('dve', TAIL[d_][:], bank[5][0:64, 0:64], Gd[d_][:], ALU.subtract)
            P.act(TAIL[d_][:], TAIL[d_][:], AF.Exp)
            P.act(EG[d_][:], Gd[d_][:], AF.Exp)
            P.ts('dve', NEG[d_][:], EG[d_][:], -1.0, None, ALU.mult)
        for g in range(8):
            c0 = g * 8
            for cc in range(8):
                ch = slice((c0 + cc) * 64, (c0 + cc + 1) * 64)
                P.mm(bank[4][0:64, cc * 64:(cc + 1) * 64], kT[:, ch], kT[:, ch])
            for cc in range(8):
                ch = slice((c0 + cc) * 64, (c0 + cc + 1) * 64)
                P.mm(bank[5][0:64, cc * 64:(cc + 1) * 64], kT[:, ch], qT[:, ch])
            bk = {'f': (2, 3, 7), 'b': (0, 1, 6)}
            for d_ in 'fb':
                la_g = LA[d_][:, h, c0:c0 + 8]
                P.tt('pool', LAm[:], bc(la_g, [64, 8, 64], 2), bc(tri[d_][:], [64, 8, 64], 1), ALU.mult)
                P.copy('pool', LAb[:], bc(la_g, [64, 8, 64], 2))
                P.mm(bank[6][0:64, :], onesf[0:64, 0:64], flat(LAm), start=True, stop=False)
                P.mm(bank[6][0:64, :], ntri[d_][:], flat(LAb), start=False, stop=False)
                P.mm(bank[6][0:64, :], identf[0:64, 0:64], flat(negT[d_]), start=False, stop=True)
                P.act(flat(decT[d_]), bank[6][0:64, :], AF.Exp)
            for d_ in 'fb':
                P.tt('dve', flat(qkT[d_])[:, c0 * 64:(c0 + 8) * 64], bank[5][0:64, :], flat(decT[d_]), ALU.mult)
                P.tt('dve', flat(tY[d_]), bank[4][0:64, :], flat(decT[d_]), ALU.mult)
                P.tt('pool', tY[d_][:], tY[d_][:], bc(BETA[d_][:, h, c0:c0 + 8], [64, 8, 64], 2), ALU.mult)
                P.tt('dve', tY[d_][:], tY[d_][:], bc(nstrT[d_][:], [64, 8, 64], 1), ALU.mult)
                P.copy('act', Yb[d_][0][:], tY[d_][:])
                P.tt('pool', Gm[d_][0][:], tY[d_][:], bc(identf[0:64, 0:64], [64, 8, 64], 1), ALU.add)
            for d_ in 'fb':
                z0 = bank_bf(bk[d_][2])
                for cc in range(8):
                    P.tr(z0[0:64, cc * 64:(cc + 1) * 64], Yb[d_][0][:, cc, :], identb[0:64, 0:64])
                P.copy('dve' if d_ == 'f' else 'act', flat(Zb[d_][0]), z0[0:64, 0:512])
            for k in range(5):
                cur = k % 2; nxt = 1 - cur
                for d_ in 'fb':
                    by_, bz_, bg_ = bk[d_]
                    if k < 4:
                        for cc in range(8):
                            P.mm(bank[by_][0:64, cc * 64:(cc + 1) * 64], Zb[d_][cur][:, cc, :], Yb[d_][cur][:, cc, :])
                    for cc in range(8):
                        P.mm(bank[bz_][0:64, cc * 64:(cc + 1) * 64], Yb[d_][cur][:, cc, :], Zb[d_][cur][:, cc, :])
                for d_ in 'fb':
                    by_, bz_, bg_ = bk[d_]
                    if k < 4:
                        P.copy('act', flat(Yb[d_][nxt]), bank[by_][0:64, :])
                    P.copy('dve', flat(Zb[d_][nxt]), bank[bz_][0:64, :])
                for d_ in 'fb':
                    by_, bz_, bg_ = bk[d_]
                    for cc in range(8):
                        P.mm(bank[bg_][0:64, cc * 64:(cc + 1) * 64], Zb[d_][nxt][:, cc, :], Gm[d_][cur][:, cc, :])
                for d_ in 'fb':
                    by_, bz_, bg_ = bk[d_]
                    dstG = flat(Gm[d_][nxt]) if k < 4 else flat(TT[d_])[:, c0 * 64:(c0 + 8) * 64]
                    P.tt('dve' if d_ == 'f' else 'pool', dstG, bank[bg_][0:64, :], flat(Gm[d_][cur]), ALU.add) if d_ == 'f' else P.tt('dve', dstG, bank[bg_][0:64, :], flat(Gm[d_][cur]), ALU.add)
        if debug and h == 0:
            for d_ in 'fb':
                dbg_out(f"dbg_TT_{d_}", TT[d_][:], [64, 64, 64], BF16)
                dbg_out(f"dbg_qkT_{d_}", qkT[d_][:], [64, 64, 64], BF16)
        if stage <= 4:
            continue
        for d_ in 'fb':
            P.memset('pool', Sst[d_][:], 0.0)
            P.memset('pool', Sbf[d_][:], 0.0)
        nsteps = int(os.environ.get('MK_STEPS', '64')) if debug else 64
        for s_ in range(nsteps):
            cs = {'f': s_, 'b': 63 - s_}
            pv = {}
            for di, d_ in enumerate('fb'):
                bA = bank[di * 3]; bB_ = bank[di * 3 + 1]; bC = bank[di * 3 + 2]
                pv[d_] = (bA[0:64, 0:128], bB_[0:64, 0:128], bA[0:64, 128:256], bB_[0:64, 128:256], bC[:, 0:128])
            for d_ in 'fb':
                c = cs[d_]; ch = slice(c * 64, (c + 1) * 64); ps1, ps2, ps3, ps4, ps5 = pv[d_]
                P.mm(ps1, kT[:, ch], Sbf[d_][:])
                P.mm(ps2, qT[:, ch], Sbf[d_][:])
            for d_ in 'fb':
                c = cs[d_]; ps1, ps2, ps3, ps4, ps5 = pv[d_]
                P.stt(Rt[d_][:], ps1, NEG[d_][:, c:c + 1], Vtok[:, c, :], ALU.mult, ALU.add)
                P.act(ktl[d_][:], Ktok[:, c, :], AF.Identity, scale=TAIL[d_][:, c:c + 1])
                P.act(tq[d_][:], ps2, AF.Identity, scale=EG[d_][:, c:c + 1])
            for d_ in 'fb':
                c = cs[d_]; ps1, ps2, ps3, ps4, ps5 = pv[d_]
                P.mm(ps3, TT[d_][:, c, :], Rt[d_][:])
            for d_ in 'fb':
                c = cs[d_]; ps1, ps2, ps3, ps4, ps5 = pv[d_]
                P.act(vnew[d_][:], ps3, AF.Identity, scale=BETA[d_][:, h, c:c + 1])
            for d_ in 'fb':
                c = cs[d_]; ps1, ps2, ps3, ps4, ps5 = pv[d_]
                P.mm(ps5, ktl[d_][:], vnew[d_][:])
                P.mm(ps4, qkT[d_][:, c, :], vnew[d_][:])
            for d_ in 'fb':
                c = cs[d_]; ps1, ps2, ps3, ps4, ps5 = pv[d_]
                P.stt(Sbf[d_][:], Sst[d_][:], CD[d_][:, c:c + 1], ps5, ALU.mult, ALU.add)
            for d_ in 'fb':
                c = cs[d_]; ps1, ps2, ps3, ps4, ps5 = pv[d_]
                P.stt(Sst[d_][:], Sst[d_][:], CD[d_][:, c:c + 1], ps5, ALU.mult, ALU.add)
                first = (d_ == 'f' and c < 32) or (d_ == 'b' and c >= 32)
                if first:
                    P.tt('dve', O[:, c, :], tq[d_][:], ps4, ALU.add)
                else:
                    P.tt('dve', otmp[:], tq[d_][:], ps4, ALU.add)
                    P.tt('pool', O[:, c, :], O[:, c, :], otmp[:], ALU.add)
        if debug and h == 0:
            dbg_out("dbg_O", O[:], [64, 64, 128], BF16)
            dbg_out("dbg_S_f", Sst['f'][:], [128, 128], F32)
            dbg_out("dbg_vnew_f", vnew['f'][:], [64, 128], BF16)
            dbg_out("dbg_R_f", Rt['f'][:], [64, 128], BF16)
            dbg_out("dbg_tq_f", tq['f'][:], [64, 128], F32)
            dbg_out("dbg_ktl_f", ktl['f'][:], [64, 128], BF16)
            dbg_out("dbg_CD_f", CD['f'][:], [128, 64], F32)
            dbg_out("dbg_TAIL_f", TAIL['f'][:], [64, 64], F32)
            dbg_out("dbg_EG_f", EG['f'][:], [64, 64], F32)
        if debug and os.environ.get('MK_NOPOST'):
            continue
        Zs = Ktok
        wz = Wg[0]; wcnt[0] += 1
        P.dma('pool', wz[:], w_in_v[:, :, COL_Z + h * 128:COL_Z + (h + 1) * 128])
        for tt_ in range(8):
            hb = load_hT(tt_)
            for hf in range(2):
                pb = nextbank()
                for c4 in range(4):
                    cc = hf * 4 + c4
                    for kc in range(8):
                        P.mm(pb[0:64, c4 * 128:(c4 + 1) * 128], hb[:, kc, cc * 64:(cc + 1) * 64], wz[:, kc, :], start=(kc == 0), stop=(kc == 7))
                c_lo = tt_ * 8 + hf * 4
                P.act(Zs[:, c_lo:c_lo + 4, :], pb[0:64, :].rearrange("p (c d) -> p c d", d=128), AF.Silu)
        SQ = Vtok
        P.act(SQ[:], O[:], AF.Square)
        P.op('dve', lambda e: e.tensor_reduce(out=ssq[:], in_=SQ[:], axis=AX.X, op=ALU.add), [SQ], [ssq])
        P.act(ssq[:], ssq[:], AF.Ln, bias=eps_rms[0:64, 0:1], scale=1.0 / 128.0)
        P.act(ssq[:], ssq[:], AF.Exp, scale=-0.5)
        P.tt('dve', O[:], O[:], bc(ssq[:], [64, 64, 128], 2), ALU.mult)
        OGb = Vtok
        P.tt('dve', OGb[:], O[:], Zs[:], ALU.mult)
        for g in range(8):
            bi = 2 + g % 2
            pbf = bank_bf(bi)
            for cc in range(8):
                P.tr(pbf[:, cc * 64:(cc + 1) * 64], OGb[:, g * 8 + cc, :], identb[0:64, 0:64])
            P.act(C[:, g * 512:(g + 1) * 512], pbf[:, 0:512], AF.Identity, scale=ngcol[:, 0:1])
        P.dma('sp', ogT_d[h * 128:(h + 1) * 128, :], C[:], w=[('ogT_d', h)])
    if stage <= 5:
        P.finish()
        return nc, dbg

    A.release(head_mark)
    vst = sb("vst", [64, 128])
    for r0, src, n in ((0, b_glu, 16), (16, b_gate, 16), (32, b_dw, 8), (40, conv_ln_g, 8), (48, conv_ln_b, 8), (56, b_b_o, 8)):
        P.dma('sp', vst[r0:r0 + n, :], src.rearrange("(c p) -> c p", p=128))
    P.tr(bank[0][:, 0:64], vst[:, :], identf[0:64, 0:64])
    P.copy('dve', vp[:], bank[0][:, 0:64])
    VB_GLU, VB_GATE, VB_DW, VB_LNG, VB_LNB, VB_BBO = 0, 16, 32, 40, 48, 56
    cdst1 = sb("cdst1", [128, 128]); cdst2 = sb("cdst2", [120, 128]); cwd = sb("cwd", [128, 248])
    cdv = conv_dw.rearrange("t (c p) -> (t c) p", p=128)
    P.dma('sp', cdst1[:], cdv[0:128, :])
    P.dma('sp', cdst2[:], cdv[128:248, :])
    P.tr(bank[1][:, 0:128], cdst1[:, :], identf[:, :])
    P.copy('dve', cwd[:, 0:128], bank[1][:, 0:128])
    P.tr(bank[1][:, 128:248], cdst2[:, :], identf[0:120, 0:120])
    P.copy('dve', cwd[:, 128:248], bank[1][:, 128:248])
    Wg4 = [sb(f"Wg4_{i}", [128, 8, 512], BF16) for i in range(2)]
    GA = [sb(f"GA{i}", [128, 4096], BF16) for i in range(2)]
    Ug = [sb(f"Ug{i}", [128, 4096 + 30], BF16) for i in range(2)]
    for u_ in Ug:
        P.memset('pool', u_[:, 0:15], 0.0); P.memset('pool', u_[:, 4096 + 15:4096 + 30], 0.0)
    Ycb = [sb(f"Ycb{i}", [128, 4096], BF16) for i in range(4)]
    Dg31 = sb("Dg31", [128, 31, 128], BF16)
    sgt = sb("sgt", [128, 512]); sq2 = sb("sq2", [128, 512], BF16)
    SUM = sb("SUM", [1, 4096]); SSQ = sb("SSQ", [1, 4096])
    w4cnt = [0]

    def inproj_group(cols, evacs):
        wb = Wg4[w4cnt[0] % 2]; w4cnt[0] += 1
        for jj, c0_ in enumerate(cols):
            P.dma('pool', wb[:, :, jj * 128:(jj + 1) * 128], w_in_v[:, :, c0_:c0_ + 128], w=[f"{wb.name}_{jj}"])
        for tt_ in range(8):
            hb = load_hT(tt_)
            for jj in range(len(cols)):
                pb = nextbank()
                for kc in range(8):
                    P.mm(pb[:, :], wb[:, kc, jj * 128:(jj + 1) * 128], hb[:, kc, :], start=(kc == 0), stop=(kc == 7),
                         r=[f"{wb.name}_{jj}", hb])
                evacs[jj](tt_, pb)

    for g4 in range(4):
        evs = []
        for jj in range(4):
            cg = g4 * 4 + jj

            def evac_gate(tt_, pb, cg=cg, jj=jj):
                P.act(Ycb[jj][:, tt_ * 512:(tt_ + 1) * 512], pb[:, :], AF.Sigmoid, bias=vp[:, VB_GATE + cg:VB_GATE + cg + 1])
            evs.append(evac_gate)
        inproj_group([COL_GATE + (g4 * 4 + jj) * 128 for jj in range(4)], evs)
        for jj in range(4):
            cg = g4 * 4 + jj
            P.dma('sp', gate_d[cg * 128:(cg + 1) * 128, :], Ycb[jj][:], w=[('gate_d', cg)])
    for j2 in range(4):
        evs = []; cols = []
        for u in range(2):
            j = j2 * 2 + u

            def evac_a(tt_, pb, j=j, u=u):
                P.act(GA[u][:, tt_ * 512:(tt_ + 1) * 512], pb[:, :], AF.Identity, bias=vp[:, VB_GLU + j:VB_GLU + j + 1])

            def evac_g(tt_, pb, j=j, u=u):
                P.act(sgt[:], pb[:, :], AF.Sigmoid, bias=vp[:, VB_GLU + 8 + j:VB_GLU + 8 + j + 1])
                P.tt('dve', Ug[u][:, 15 + tt_ * 512:15 + (tt_ + 1) * 512], GA[u][:, tt_ * 512:(tt_ + 1) * 512], sgt[:], ALU.mult)
            cols += [COL_GLU + j * 128, COL_GLU + 1024 + j * 128]
            evs += [evac_a, evac_g]
        inproj_group(cols, evs)
        for u in range(2):
            j = j2 * 2 + u
            yc = Ycb[u]
            for t in range(31):
                P.ts('dve' if t % 2 == 0 else 'pool', Dg31[:, t, :], identb[:], cwd[:, t * 8 + j:t * 8 + j + 1], None, ALU.mult)
            for tt_ in range(8):
                pb = nextbank()
                for t in range(31):
                    P.mm(pb[:, :], Dg31[:, t, :], Ug[u][:, tt_ * 512 + t:tt_ * 512 + t + 512], start=(t == 0), stop=(t == 30))
                sl = slice(tt_ * 512, (tt_ + 1) * 512)
                P.act(yc[:, sl], pb[:, :], AF.Identity, bias=vp[:, VB_DW + j:VB_DW + j + 1])
                P.act(sq2[:], pb[:, :], AF.Square, bias=vp[:, VB_DW + j:VB_DW + j + 1])
                P.mm(bank[2][0:1, :], onesb[:, 0:1], yc[:, sl])
                P.mm(bank[3][0:1, :], onesb[:, 0:1], sq2[:])
                if j == 0:
                    P.copy('dve', SUM[:, sl], bank[2][0:1, :])
                    P.copy('dve', SSQ[:, sl], bank[3][0:1, :])
                else:
                    P.tt('dve', SUM[:, sl], SUM[:, sl], bank[2][0:1, :], ALU.add)
                    P.tt('dve', SSQ[:, sl], SSQ[:, sl], bank[3][0:1, :], ALU.add)
            P.dma('sp', ybT_d[j * 128:(j + 1) * 128, :], yc[:], w=[('ybT_d', j)])
    stt_ = sb("stt_", [1, 512])
    for tt_ in range(8):
        sl = slice(tt_ * 512, (tt_ + 1) * 512)
        P.ts('dve', SUM[:, sl], SUM[:, sl], 1.0 / 1024.0, None, ALU.mult)
        P.tt('dve', stt_[:], SUM[:, sl], SUM[:, sl], ALU.mult)
        P.stt(SSQ[:, sl], SSQ[:, sl], 1.0 / 1024.0, stt_[:], ALU.mult, ALU.subtract)
        P.act(SSQ[:, sl], SSQ[:, sl], AF.Ln, bias=eps_ln[0:1, 0:1])
        P.act(SSQ[:, sl], SSQ[:, sl], AF.Exp, scale=-0.5)
        P.stt(SUM[:, sl], SUM[:, sl], -1.0, SSQ[:, sl], ALU.mult, ALU.mult)
    RSTD = SSQ; NMR = SUM
    P.dma('sp', st_d[0:1, :], RSTD[:])
    P.dma('sp', st_d[1:2, :], NMR[:])
    s5_mark = A.mark()
    if stage <= 6:
        P.finish()
        return nc, dbg

    A.release(base_mark)
    LG = sb("LG", [128, 32, 32]); M8 = sb("M8", [128, 32, 8]); MSK = sb("MSK", [128, 32, 32], BF16); G4 = sb("G4", [128, 32, 4])
    moe_mark = A.mark()
    zbf = zerof[:].bitcast(BF16)
    xs_v = xs_d.rearrange("(n p) d -> p n d", p=128)
    for n0 in range(0, 256, 32):
        P.dma('sp', xs_v[:, n0:n0 + 32, :], zbf.unsqueeze(1).to_broadcast([128, 32, D]), r=[zerof], w=[('xs_fill', n0)])
    Wao = sb("Wao", [128, 8, 1024], BF16); Wbo = sb("Wbo", [128, 8, 1024], BF16); Wout = sb("Wout", [128, 8, 1024], BF16)
    P.dma('pool', Wao[:], w_a_o.rearrange("(kc p) c -> p kc c", p=128))
    P.dma('pool', Wbo[:], w_b_o.rearrange("(kc p) c -> p kc c", p=128))
    P.dma('pool', Wout[:], w_out.rearrange("(kc p) c -> p kc c", p=128))
    Wr = sb("Wr", [128, 8, 32]); brB = sb("brB", [128, 32])
    P.dma('sp', Wr[:], w_router.rearrange("(kc p) c -> p kc c", p=128))
    P.dma('sp', brB[:], b_router.partition_broadcast(128))
    lnp['g'] = sb("gB1", [128, D]); lnp['b'] = sb("bB1", [128, D])
    P.dma('sp', lnp['g'][:], ln1_g.partition_broadcast(128))
    P.dma('sp', lnp['b'][:], ln1_b.partition_broadcast(128))
    ogt2 = [sb(f"ogt{i}", [128, 8, 512], BF16) for i in range(2)]; ybt2 = [sb(f"ybt{i}", [128, 8, 512], BF16) for i in range(2)]
    gtt2 = [sb(f"gtt{i}", [128, 16, 512], BF16) for i in range(2)]
    ybaT = sb("ybaT", [128, 8, 512], BF16); mixT = sb("mixT", [128, 8, 512], BF16)
    t1s = [sb(f"t1_{i}", [128, 512]) for i in range(2)]; t2s = [sb(f"t2_{i}", [128, 512]) for i in range(2)]
    rs_t2 = [sb(f"rs_t{i}", [1, 512]) for i in range(2)]; nm_t2 = [sb(f"nm_t{i}", [1, 512]) for i in range(2)]
    h0t2 = [sb(f"h0t{i}", [128, D]) for i in range(2)]; rt_2 = [sb(f"rt{i}", [128, D]) for i in range(2)]
    h1t2 = [sb(f"h1t{i}", [128, D]) for i in range(2)]; h1Tf2 = [sb(f"h1Tf{i}", [128, 8, 128]) for i in range(2)]
    nmx2 = [sb(f"nmx{i}", [128, 1]) for i in range(2)]; esum2 = [sb(f"esum{i}", [128, 1]) for i in range(2)]
    h1tb2 = [sb(f"h1tb{i}", [128, D], BF16) for i in range(2)]
    ogv = ogT_d.rearrange("(j p) t -> p j t", p=128)
    ybv = ybT_d.rearrange("(j p) t -> p j t", p=128)
    gtv = gate_d.rearrange("(j p) t -> p j t", p=128)
    def s6_loads(tt_):
        sl = slice(tt_ * 512, (tt_ + 1) * 512)
        P.dma('sp', ogt2[tt_ % 2][:], ogv[:, :, sl], r=[('ogT_d', j_) for j_ in range(8)])
        P.dma('sp', ybt2[tt_ % 2][:], ybv[:, :, sl], r=[('ybT_d', j_) for j_ in range(8)])
        P.dma('sp', gtt2[tt_ % 2][:], gtv[:, :, sl], r=[('gate_d', j_) for j_ in range(16)])
        P.dma('sp', rs_t2[tt_ % 2][:], st_d[0:1, sl])
        P.dma('sp', nm_t2[tt_ % 2][:], st_d[1:2, sl])

    def s6_M(tt_):
        sl = slice(tt_ * 512, (tt_ + 1) * 512)
        ogt = ogt2[tt_ % 2]; ybt = ybt2[tt_ % 2]; gtt = gtt2[tt_ % 2]
        rs_t = rs_t2[tt_ % 2]; nm_t = nm_t2[tt_ % 2]
        if tt_ == 0:
            s6_loads(0)
        if tt_ + 1 < 8:
            s6_loads(tt_ + 1)
        P.mm(bank[6][:, :], onesf[0:1, :], rs_t[0:1, :])
        P.mm(bank[7][:, :], onesf[0:1, :], nm_t[0:1, :])
        for j in range(8):
            t1 = t1s[j % 2]
            P.tt('dve', t1[:], ybt[:, j, :], bank[6][:, :], ALU.mult)
            P.tt('dve', t1[:], t1[:], bank[7][:, :], ALU.add)
            P.act(ybaT[:, j, :], t1[:], AF.Silu, bias=vp[:, VB_LNB + j:VB_LNB + j + 1], scale=vp[:, VB_LNG + j:VB_LNG + j + 1])
        if debug and tt_ == 0:
            dbg_out("dbg_ybaT", ybaT[:], [128, 8, 512], BF16)
        for m in range(8):
            pa = bank[(m % 2) * 2]; pb_ = bank[(m % 2) * 2 + 1]
            t1 = t1s[m % 2]; t2 = t2s[m % 2]
            for kc in range(8):
                P.mm(pa[:, :], Wao[:, kc, m * 128:(m + 1) * 128], ogt[:, kc, :], start=(kc == 0), stop=(kc == 7))
            for kc in range(8):
                P.mm(pb_[:, :], Wbo[:, kc, m * 128:(m + 1) * 128], ybaT[:, kc, :], start=(kc == 0), stop=(kc == 7))
            P.tt('dve', t1[:], pa[:, :], gtt[:, m, :], ALU.mult)
            P.stt(t2[:], pb_[:, :], vp[:, VB_BBO + m:VB_BBO + m + 1], gtt[:, 8 + m, :], ALU.add, ALU.mult)
            P.tt('pool', mixT[:, m, :], t1[:], t2[:], ALU.add)
    def s6_A(tt_, sub):
        i = tt_ * 4 + sub
        h0t = h0t2[i % 2]; rt = rt_2[i % 2]; h1t = h1t2[i % 2]; h1Tf = h1Tf2[i % 2]
        nmx = nmx2[i % 2]; esum = esum2[i % 2]; h1tb = h1tb2[i % 2]
        P.dma('sp', h0t[:], h0_d[i * 128:(i + 1) * 128, :], r=[('h0_d', i)])
        for hf in range(2):
            pm = bank[4 + hf]
            for m in range(8):
                P.mm(pm[:, :], mixT[:, m, sub * 128:(sub + 1) * 128], Wout[:, m, hf * 512:(hf + 1) * 512], start=(m == 0), stop=(m == 7))
            P.stt(rt[:, hf * 512:(hf + 1) * 512], h0t[:, hf * 512:(hf + 1) * 512], DN_ALPHA, pm[:, :], ALU.mult, ALU.add)
        layer_norm_rows(rt, h1t)
        P.dma('sp', h1_d[i * 128:(i + 1) * 128, :], h1t[:], w=[('h1_d', i)])
    def s6_B(tt_, sub):
        i = tt_ * 4 + sub
        h0t = h0t2[i % 2]; rt = rt_2[i % 2]; h1t = h1t2[i % 2]; h1Tf = h1Tf2[i % 2]
        nmx = nmx2[i % 2]; esum = esum2[i % 2]; h1tb = h1tb2[i % 2]
        for hf in range(2):
            pt = bank[6 + hf]
            for q4 in range(4):
                kc = hf * 4 + q4
                P.tr(pt[:, q4 * 128:(q4 + 1) * 128], h1t[:, kc * 128:(kc + 1) * 128], identf[:])
            P.copy('dve', h1Tf[:, hf * 4:(hf + 1) * 4, :], pt[:].rearrange("p (k t) -> p k t", t=128))
        pr_ = bank[(sub % 2) * 2]
        for kc in range(8):
            P.mm(pr_[:, 0:32], h1Tf[:, kc, :], Wr[:, kc, :], start=(kc == 0), stop=(kc == 7))
        P.tt('dve', LG[:, i, :], pr_[:, 0:32], brB[:], ALU.add)
        P.op('dve', lambda e, i=i: e.max(out=M8[:, i, :], in_=LG[:, i, :]), [LG], [M8])
        P.ts('dve', MSK[:, i, :], LG[:, i, :], M8[:, i, 3:4], None, ALU.is_ge)
        P.ts('dve', nmx[:], M8[:, i, 0:1], -1.0, None, ALU.mult)
        P.act(G4[:, i, :], M8[:, i, 0:4], AF.Exp, bias=nmx[:, 0:1])
        P.op('dve', lambda e, i=i, esum=esum: e.tensor_reduce(out=esum[:], in_=G4[:, i, :], axis=AX.X, op=ALU.add), [G4], [esum])
        P.op('dve', lambda e, esum=esum: e.reciprocal(out=esum[:], in_=esum[:]), [esum], [esum])
        P.ts('dve', G4[:, i, :], G4[:, i, :], esum[:, 0:1], None, ALU.mult)
        P.copy('act', h1tb[:], h1t[:])
        P.dma('sp', h1b_d[i * 128:(i + 1) * 128, :], h1tb[:], w=[('h1b_d', i)])

    pend_B = []
    for tt_ in range(8):
        s6_M(tt_)
        for pb_item in pend_B:
            s6_B(*pb_item)
        pend_B = []
        for sub in range(4):
            s6_A(tt_, sub)
            if sub >= 1:
                s6_B(tt_, sub - 1)
        pend_B.append((tt_, 3))
    for pb_item in pend_B:
        s6_B(*pb_item)
    if debug:
        dbg_out("dbg_G4", G4[:], [128, 32, 4], F32)
        dbg_out("dbg_MSK", MSK[:], [128, 32, 32], BF16)
    if stage <= 7:
        P.finish()
        return nc, dbg

    A.release(moe_mark)
    I32 = mybir.dt.int32
    NBLK = 64
    lnp['g'] = sb("gB2", [128, D]); lnp['b'] = sb("bB2", [128, D])
    P.dma('sp', lnp['g'][:], ln2_g.partition_broadcast(128))
    P.dma('sp', lnp['b'][:], ln2_b.partition_broadcast(128))
    stri = sb("stri", [128, 128], BF16)
    strf = sb("strf", [128, 128])
    P.op('pool', lambda e: e.affine_select(out=strf[:], in_=onesf[:], pattern=[[1, 128]], compare_op=ALU.is_gt, fill=0.0,
                                           base=0, channel_multiplier=-1), [onesf], [strf])
    P.copy('dve', stri[:], strf[:])
    POS = sb("POS", [128, 32, 32]); run = sb("run", [128, 32], BF16)
    P.memset('pool', run[:], 0.0)
    for i in range(32):
        pp = bank[i % 2]
        P.mm(pp[:, 0:32], stri[:], MSK[:, i, :], start=True, stop=False)
        P.mm(pp[:, 0:32], onesb[:], run[:], start=False, stop=True)
        P.copy('act', POS[:, i, :], pp[:, 0:32])
        P.tt('dve', run[:], run[:], MSK[:, i, :], ALU.add)
    cnt = sb("cnt", [128, 32]); padded = sb("padded", [128, 32]); pend = sb("pend", [128, 32]); pstart = sb("pstart", [128, 32])
    tmp32 = sb("tmp32", [128, 32]); ones32 = sb("ones32", [128, 32])
    P.memset('pool', ones32[:], 1.0)
    P.mm(bank[2][:, 0:32], onesb[:], run[:])
    P.copy('dve', cnt[:], bank[2][:, 0:32])
    P.memset('pool', padded[:], 0.0)
    for j in range(8):
        P.ts('dve', tmp32[:], cnt[:], float(512 * j), 512.0, ALU.is_gt, ALU.mult)
        P.tt('dve', padded[:], padded[:], tmp32[:], ALU.add)
    P.op('dve', lambda e: e.tensor_tensor_scan(out=pend[:], data0=ones32[:], data1=padded[:], initial=0.0, op0=ALU.mult, op1=ALU.add),
         [ones32, padded], [pend])
    P.tt('dve', pstart[:], pend[:], padded[:], ALU.subtract)
    bthr = sb("bthr", [128, NBLK, 32]); BE = sb("BE", [128, NBLK])
    P.op('pool', lambda e: e.iota(out=bthr[:], pattern=[[512, NBLK], [0, 32]], base=0, channel_multiplier=0,
                                  allow_small_or_imprecise_dtypes=True), [], [bthr])
    P.tt('dve', bthr[:], bc(pend[:], [128, NBLK, 32], 1), bthr[:], ALU.is_le)
    P.op('dve', lambda e: e.tensor_reduce(out=BE[:], in_=bthr[:], axis=AX.X, op=ALU.add), [bthr], [BE])
    P.ts('dve', BE[:], BE[:], 31.0, None, ALU.min)
    basef = sb("basef", [128, 8]); IDXF = sb("IDXF", [128, NBLK, 8]); IDXW = sb("IDXW", [128, NBLK, 8], I32)
    P.op('pool', lambda e: e.iota(out=basef[:], pattern=[[128, 8]], base=0, channel_multiplier=1,
                                  allow_small_or_imprecise_dtypes=True), [], [basef])
    P.stt(IDXF[:], bc(BE[:], [128, NBLK, 8], 2), 1024.0, bc(basef[:], [128, NBLK, 8], 1), ALU.mult, ALU.add)
    P.copy('dve', IDXW[:], IDXF[:])
    pidx = sb("pidx", [32, 1]); OH = sb("OH", [32, NBLK], BF16)
    P.op('pool', lambda e: e.iota(out=pidx[:], pattern=[[0, 1]], base=0, channel_multiplier=1,
                                  allow_small_or_imprecise_dtypes=True), [], [pidx])
    P.ts('dve', OH[:], BE[0:32, :], pidx[0:32, 0:1], None, ALU.is_equal)
    thrb = sb("thrb", [1, NBLK]); ACTF = sb("ACTF", [1, NBLK], I32)
    P.op('pool', lambda e: e.iota(out=thrb[:], pattern=[[512, NBLK]], base=0, channel_multiplier=0,
                                  allow_small_or_imprecise_dtypes=True), [], [thrb])
    P.ts('dve', thrb[:], thrb[:], pend[0:1, 31:32], None, ALU.is_lt)
    P.copy('dve', ACTF[:], thrb[:])
    bgu_all = sb("bgu_all", [32, 2048], BF16); bd_all = sb("bd_all", [32, D], BF16)
    P.dma('pool', bgu_all[:], b_gu)
    P.dma('pool', bd_all[:], b_down)
    SLOTF = sb("SLOTF", [128, 32, 4]); SLOT = sb("SLOT", [128, 32, 4], I32)
    valt = sb("valt", [128, 32]); mk = sb("mk", [128, 4, 32])
    for i in range(32):
        P.tt('dve', valt[:], POS[:, i, :], pstart[:], ALU.add)
        P.tt('dve', mk[:], bc(LG[:, i, :], [128, 4, 32], 1), bc(M8[:, i, 0:4], [128, 4, 32], 2), ALU.is_equal)
        P.tt('dve', mk[:], mk[:], bc(valt[:], [128, 4, 32], 1), ALU.mult)
        P.op('dve', lambda e, i=i: e.tensor_reduce(out=SLOTF[:, i, :], in_=mk[:], axis=AX.X, op=ALU.add), [mk], [SLOTF])
    P.copy('dve', SLOT[:], SLOTF[:])
    if debug:
        dbg_out("dbg_SLOT", SLOT[:], [128, 32, 4], I32)
        dbg_out("dbg_BE", BE[:], [128, NBLK], F32)
        dbg_out("dbg_cnt", cnt[:], [128, 32], F32)
    disp_mark = A.mark()
    hb_t = [sb(f"hb_t{i}", [128, D], BF16) for i in range(2)]
    fill_names = [('xs_fill', n0) for n0 in range(0, 256, 32)]
    for i in range(32):
        hb_ = hb_t[i % 2]
        P.dma('sp', hb_[:], h1b_d[i * 128:(i + 1) * 128, :], r=[('h1b_d', i)])
        for k in range(4):
            P.idma(xs_d, hb_[:], out_off=SLOT[:, i, k:k + 1], r=[hb_, SLOT] + fill_names, w=[('xs', i, k)])
    A.release(disp_mark)
    if debug and os.environ.get("MK_MOE_STOP") == "1":
        P.finish()
        return nc, dbg
    wguB = [sb(f"wguB{i}", [128, 8, 2048], BF16) for i in range(2)]
    wdB = [sb(f"wdB{i}", [128, 8, D], BF16) for i in range(2)]
    xtm = [sb(f"xtm{i}", [128, 4, D], BF16) for i in range(2)]; xT = sb("xT", [128, 8, 512], BF16)
    actT = sb("actT", [128, 8, 512], BF16)
    OHs = [sb(f"OHs{i}", [32, 512], BF16) for i in range(2)]
    NROT = 2
    gl = [sb(f"gl{i}", [128, 512]) for i in range(NROT)]; sg = [sb(f"sg{i}", [128, 512]) for i in range(NROT)]
    li = [sb(f"li{i}", [128, 512]) for i in range(NROT)]; p1 = [sb(f"p1{i}", [128, 512]) for i in range(NROT)]
    ysb = [sb(f"ysb{i}", [128, D]) for i in range(2)]
    wgu_rows = w_gu.rearrange("e k c -> (e k) c")
    wd_rows = w_down.rearrange("e k c -> (e k) c")
    nblk_run = int(os.environ.get("MK_NBLK", str(NBLK))) if debug else NBLK
    SIG_MAX = float(1.0 / (1.0 + np.exp(-1.702 * 7.0)))

    def load_block_weights(b):
        for kc in range(8):
            P.idma(wguB[b % 2][:, kc, :], wgu_rows, in_off=IDXW[:, b, kc:kc + 1], r=[IDXW], w=[f"wguB{b % 2}_{kc}"])
        for kc in range(8):
            P.idma(wdB[b % 2][:, kc, :], wd_rows, in_off=IDXW[:, b, kc:kc + 1], r=[IDXW], w=[f"wdB{b % 2}_{kc}"])

    def load_block_x(b):
        P.dma('sp', xtm[b % 2][:], xs_d[b * 512:(b + 1) * 512, :].rearrange("(s p) d -> p s d", p=128), r=[])
        P.copy('act', OHs[b % 2][:], OH[:, b:b + 1].to_broadcast([32, 512]))

    l1b = [0]; l2b = [0]; ycnt = [0]; rot = [0]
    load_block_weights(0)
    load_block_x(0)
    use_skip = os.environ.get("MK_NOSKIP") is None
    for b in range(nblk_run):
        if use_skip:
            P.cond_begin(ACTF[0:1, b:b + 1])
        if b + 1 < nblk_run:
            load_block_weights(b + 1)
            load_block_x(b + 1)
        wg_ = wguB[b % 2]; wd_ = wdB[b % 2]; oh_ = OHs[b % 2]; xt_ = xtm[b % 2]
        for k2 in range(4):
            pbf = bank_bf(4 + k2 % 2)
            for kk in range(2):
                kc = k2 * 2 + kk
                for sub in range(4):
                    P.tr(pbf[:, kk * 512 + sub * 128:kk * 512 + (sub + 1) * 128], xt_[:, sub, kc * 128:(kc + 1) * 128], identb[:])
            P.copy('act' if k2 % 2 == 0 else 'dve', xT[:, k2 * 2:k2 * 2 + 2, :], pbf[:, :].rearrange("p (k t) -> p k t", t=512))
        for m in range(8):
            pbg = bank[l1b[0] % 4]; pbl = bank[(l1b[0] + 1) % 4]; l1b[0] += 2
            ri = rot[0] % NROT; rot[0] += 1
            for kc in range(8):
                P.mm(pbg[:, :], wg_[:, kc, m * 128:(m + 1) * 128], xT[:, kc, :], start=(kc == 0), stop=False, r=[f"wguB{b % 2}_{kc}", xT])
            P.mm(pbg[:, :], bgu_all[:, m * 128:(m + 1) * 128], oh_[:], start=False, stop=True)
            for kc in range(8):
                P.mm(pbl[:, :], wg_[:, kc, 1024 + m * 128:1024 + (m + 1) * 128], xT[:, kc, :], start=(kc == 0), stop=False, r=[f"wguB{b % 2}_{kc}", xT])
            P.mm(pbl[:, :], bgu_all[:, 1024 + m * 128:1024 + (m + 1) * 128], oh_[:], start=False, stop=True)
            P.act(sg[ri][:], pbg[:, :], AF.Sigmoid, scale=1.702)
            P.ts('dve', gl[ri][:], pbg[:, :], 7.0, None, ALU.min)
            P.ts('dve', li[ri][:], pbl[:, :], 7.0, -7.0, ALU.min, ALU.max)
            P.stt(p1[ri][:], sg[ri][:], SIG_MAX, gl[ri][:], ALU.min, ALU.mult)
            P.stt(actT[:, m, :], li[ri][:], 1.0, p1[ri][:], ALU.add, ALU.mult)
        for sub in range(4):
            yb_ = ysb[ycnt[0] % 2]; ycnt[0] += 1
            for hf in range(2):
                pb = bank[4 + l2b[0] % 4]; l2b[0] += 1
                for m in range(8):
                    P.mm(pb[:, :], actT[:, m, sub * 128:(sub + 1) * 128], wd_[:, m, hf * 512:(hf + 1) * 512], start=(m == 0), stop=False, r=[f"wdB{b % 2}_{m}", actT])
                P.mm(pb[:, :], oh_[:, 0:128], bd_all[:, hf * 512:(hf + 1) * 512], start=False, stop=True)
                P.copy('act' if hf == 0 else 'pool_never', yb_[:, hf * 512:(hf + 1) * 512], pb[:, :]) if hf == 0 else P.copy('dve', yb_[:, hf * 512:(hf + 1) * 512], pb[:, :])
            P.dma('sp', ys_d[b * 512 + sub * 128:b * 512 + (sub + 1) * 128, :], yb_[:], w=[('ys', b, sub)])
        if use_skip:
            zsrc = zerof[:].unsqueeze(1).to_broadcast([128, 4, 512])
            fills = [(ys_d[b * 512:(b + 1) * 512, hh * 512:(hh + 1) * 512].rearrange("(s p) f -> p s f", p=128), zsrc) for hh in range(2)]
            P.cond_end(else_dma=('sp', fills))
    A.release(disp_mark)
    if debug and os.environ.get("MK_MOE_STOP") == "2":
        P.finish()
        return nc, dbg
    yk = [[sb(f"yk{j}_{k}", [128, D]) for k in range(4)] for j in range(3)]
    rt2 = [sb(f"rt2_{j}", [128, D]) for j in range(3)]; ot = [sb(f"ot{j}", [128, D]) for j in range(3)]
    for i in range(32):
        j = i % 3
        P.dma('sp', rt2[j][:], h1_d[i * 128:(i + 1) * 128, :], r=[('h1_d', i)])
        for k in range(4):
            P.idma(yk[j][k][:], ys_d, in_off=SLOT[:, i, k:k + 1], r=[SLOT])
        P.act(yk[j][0][:], yk[j][0][:], AF.Identity, scale=G4[:, i, 0:1])
        P.stt(rt2[j][:], rt2[j][:], DN_ALPHA, yk[j][0][:], ALU.mult, ALU.add)
        for k in range(1, 4):
            P.stt(rt2[j][:], yk[j][k][:], G4[:, i, k:k + 1], rt2[j][:], ALU.mult, ALU.add)
        layer_norm_rows(rt2[j], ot[j], mul_eng='dve')
        P.dma('sp', out[i * 128:(i + 1) * 128, :], ot[j][:])
    P.finish()
    return nc, dbg


_IN_NAMES = ['emb_ln_g', 'emb_ln_b', 'w_in', 'conv_qkv', 'a_log', 'dt_bias', 'dn_norm_g', 'w_a_o', 'b_glu', 'conv_dw',
             'b_dw', 'conv_ln_g', 'conv_ln_b', 'w_b_o', 'b_b_o', 'b_gate', 'w_out', 'ln1_g', 'ln1_b', 'w_router',
             'b_router', 'w_gu', 'b_gu', 'w_down', 'b_down', 'ln2_g', 'ln2_b']


def make_in_maps(inputs, n_cores=8):
    shared = {}
    for k in _IN_NAMES:
        a = np.ascontiguousarray(np.asarray(inputs[k], dtype=np.float32))
        a = a[0] if k not in ('emb_ln_g', 'emb_ln_b') else a
        if k in ('a_log', 'dt_bias'):
            a = a.reshape(16)
        shared[k] = np.ascontiguousarray(a)
    xs = np.asarray(inputs['x'], dtype=np.float32)
    maps = []
    for c in range(n_cores):
        m = dict(shared)
        m['x'] = np.ascontiguousarray(xs[c])
        maps.append(m)
    return maps


def kernel(**inputs):
    nc, _ = build_program()
    maps = make_in_maps(inputs, 8)
    res = run_bass_kernel_spmd(nc, maps, core_ids=list(range(8)))
    return np.stack([np.asarray(res.results[c]["out"]) for c in range(8)], axis=0).astype(np.float32)
```
